# Optimizing a Trainium2 kernel written in Bass

```python
import math
import jax, jax.numpy as jnp
from jax import lax
import numpy as np


D_MODEL = 2048
BATCH = 2
SEQ = 8192
DEPTH = 2

HEAD_DIM = 128
N_HEADS_SB = 8
N_HEADS_DW = 8
DW_PATTERNS = ((128, 1), (512, 4), (2048, 16))
Q_BLOCK = 128
SSM_WIDTH = 1024
SSM_GROUP = 16
SSM_GROUPS = SSM_WIDTH // SSM_GROUP
SSM_STATE = 64
N_HEADS_GDN = 8
GDN_DK = 128
GDN_DV = 128
GDN_CONV = 4
GDN_CHUNK = 64
D_FF = 5632
N_EXPERTS = 8
TOP_K = 2
D_FF_EXPERT = 7168
DEEPNORM_ALPHA = (2 * DEPTH) ** 0.25
DEEPNORM_BETA = (8 * DEPTH) ** -0.25
LN_EPS = 1e-5
RMS_EPS = 1e-6

W_SB = N_HEADS_SB * HEAD_DIM
W_DW = N_HEADS_DW * HEAD_DIM
IN0 = 3 * W_SB + 3 * W_DW
MIX0 = W_SB + W_DW
GDN_QKV = N_HEADS_GDN * (2 * GDN_DK + GDN_DV)
IN1 = SSM_WIDTH + GDN_QKV + N_HEADS_GDN * GDN_DV + 2 * N_HEADS_GDN
MIX1 = SSM_WIDTH + N_HEADS_GDN * GDN_DV
N_EVEN = (DEPTH + 1) // 2
N_ODD = DEPTH // 2

kernel_name = 'hybrid_sb_dilated_s5_gdn_moe_deepnorm'

F32 = jnp.float32


def layer_norm(x, g, b):
    xf = x.astype(F32)
    mu = jnp.mean(xf, -1, keepdims=True)
    var = jnp.mean(jnp.square(xf - mu), -1, keepdims=True)
    return ((xf - mu) * lax.rsqrt(var + LN_EPS) * g + b).astype(x.dtype)


def split_heads(t, head_dim):
    b, s, _ = t.shape
    return t.reshape(b, s, -1, head_dim).transpose(0, 2, 1, 3)


def stick_breaking_attention(q, k, v):
    b, h, s, hd = q.shape
    nb = s // Q_BLOCK
    scale = hd ** -0.5
    kf = k.astype(F32)
    vf = v.astype(F32)
    qb = q.astype(F32).reshape(b, h, nb, Q_BLOCK, hd).transpose(2, 0, 1, 3, 4)
    starts = jnp.arange(nb, dtype=jnp.int32) * Q_BLOCK
    kpos = jnp.arange(s, dtype=jnp.int32)

    def block(args):
        qblk, q0 = args
        z = jnp.einsum('bhqd,bhkd->bhqk', qblk, kf) * scale
        qpos = q0 + jnp.arange(Q_BLOCK, dtype=jnp.int32)
        past = kpos[None, :] < qpos[:, None]
        log_keep = jnp.where(past, jax.nn.log_sigmoid(-z), 0.0)
        later = lax.cumsum(log_keep, axis=3, reverse=True) - log_keep
        w = jnp.where(past, jnp.exp(jax.nn.log_sigmoid(z) + later), 0.0)
        return jnp.einsum('bhqk,bhkd->bhqd', w, vf)

    out = lax.map(block, (qb, starts))
    return out.transpose(1, 2, 0, 3, 4).reshape(b, h, s, hd)


def dilated_window_attention(q, k, v, window, dilation):
    b, h, s, hd = q.shape
    n_keys = window // dilation
    sub_len = s // dilation
    nb = -(-sub_len // n_keys)
    padded = nb * n_keys
    scale = hd ** -0.5

    def to_sub(t):
        return t.astype(F32).reshape(b, h, sub_len, dilation, hd).transpose(0, 1, 3, 2, 4)

    qs = jnp.pad(to_sub(q), ((0, 0),) * 3 + ((0, padded - sub_len), (0, 0)))
    qs = qs.reshape(b, h, dilation, nb, n_keys, hd)

    def key_blocks(t):
        tp = jnp.pad(to_sub(t), ((0, 0),) * 3 + ((n_keys, padded - sub_len), (0, 0)))
        tp = tp.reshape(b, h, dilation, nb + 1, n_keys, hd)
        return jnp.concatenate([tp[:, :, :, :-1], tp[:, :, :, 1:]], axis=4)

    ks = key_blocks(k)
    vs = key_blocks(v)
    i = jnp.arange(n_keys)[:, None]
    j = jnp.arange(2 * n_keys)[None, :]
    dist = n_keys + i - j
    band = (dist >= 0) & (dist <= n_keys)
    blk = jnp.arange(nb)[:, None, None]
    mask = band[None] & ~((blk == 0) & (j[None] < n_keys))
    sc = jnp.einsum('bhrnqd,bhrnkd->bhrnqk', qs, ks) * scale
    sc = jnp.where(mask, sc, -jnp.inf)
    m = jnp.max(sc, -1, keepdims=True)
    p = jnp.exp(sc - m)
    den = jnp.sum(p, -1, keepdims=True)
    o = jnp.einsum('bhrnqk,bhrnkd->bhrnqd', p, vs) / den
    lse = (m + jnp.log(den))[..., 0]
    o = o.reshape(b, h, dilation, padded, hd)[:, :, :, :sub_len]
    o = o.transpose(0, 1, 3, 2, 4).reshape(b, h, s, hd)
    lse = lse.reshape(b, h, dilation, padded)[..., :sub_len].transpose(0, 1, 3, 2).reshape(b, h, s)
    return o, lse


def dilated_mixture(q, k, v):
    outs, lses = [], []
    for window, dilation in DW_PATTERNS:
        o, l = dilated_window_attention(q, k, v, window, dilation)
        outs.append(o)
        lses.append(l)
    wts = jax.nn.softmax(jnp.stack(lses), axis=0)
    return jnp.einsum('pbhs,pbhsd->bhsd', wts, jnp.stack(outs))


def s5_mixer(u, lam_re, lam_im, log_dt, b_re, b_im, c_re, c_im, d_skip, glu_w, glu_b):
    bsz, s, _ = u.shape
    uf = u.astype(F32)
    lam = lax.complex(lam_re.astype(F32), lam_im.astype(F32))
    dt = jnp.exp(log_dt.astype(F32))[:, None]
    lam_bar = jnp.exp(lam * dt)
    b_mat = lax.complex(b_re.astype(F32), b_im.astype(F32))
    b_bar = ((lam_bar - 1.0) / lam)[..., None] * b_mat
    c_mat = lax.complex(c_re.astype(F32), c_im.astype(F32))
    ug = uf.reshape(bsz, s, SSM_GROUPS, SSM_GROUP).astype(jnp.complex64)
    bu = jnp.einsum('gph,bsgh->bsgp', b_bar, ug)
    a = jnp.broadcast_to(lam_bar, bu.shape)

    def combine(e1, e2):
        a1, x1 = e1
        a2, x2 = e2
        return a1 * a2, a2 * x1 + x2

    _, states = lax.associative_scan(combine, (a, bu), axis=1)
    y = jnp.einsum('ghp,bsgp->bsgh', c_mat, states).real.reshape(bsz, s, SSM_WIDTH)
    y = y + d_skip.astype(F32) * uf
    z = jax.nn.gelu(y)
    return z * jax.nn.sigmoid(z @ glu_w.astype(F32) + glu_b.astype(F32))


def causal_depthwise_conv(x, w):
    return lax.conv_general_dilated(
        x, w[:, None, :].astype(x.dtype), window_strides=(1,),
        padding=((GDN_CONV - 1, 0),), dimension_numbers=('NWC', 'WIO', 'NWC'),
        feature_group_count=x.shape[-1])


def l2norm(t):
    return t * lax.rsqrt(jnp.sum(t * t, -1, keepdims=True) + RMS_EPS)


def gated_delta_rule(q, k, v, g, beta):
    b, s, h, dk = q.shape
    dv = v.shape[-1]
    c = GDN_CHUNK
    nc = s // c

    def chunk(t):
        return jnp.moveaxis(t.reshape((b, nc, c) + t.shape[2:]), 3, 2)

    qc = chunk(q) * dk ** -0.5
    kc = chunk(k)
    vc = chunk(v)
    bc = chunk(beta)
    gc = lax.cumsum(chunk(g), axis=3)
    tri = jnp.tril(jnp.ones((c, c), bool))
    strict = jnp.tril(jnp.ones((c, c), bool), -1)
    decay = jnp.exp(jnp.where(tri, gc[..., :, None] - gc[..., None, :], -jnp.inf))
    kb = kc * bc[..., None]
    lower = jnp.where(strict, jnp.einsum('bnhid,bnhjd->bnhij', kb, kc) * decay, 0.0)
    mat = jnp.eye(c, dtype=F32) + lower

    def solve(rhs):
        return lax.linalg.triangular_solve(mat, rhs, left_side=True, lower=True, unit_diagonal=True)

    u_val = solve(vc * bc[..., None])
    w_key = solve(kb * jnp.exp(gc)[..., None])
    intra = jnp.einsum('bnhid,bnhjd->bnhij', qc, kc) * decay
    q_dec = qc * jnp.exp(gc)[..., None]
    k_dec = kc * jnp.exp(gc[..., -1:] - gc)[..., None]
    g_last = jnp.exp(gc[..., -1])

    def step(state, xs):
        u_c, w_c, qd, kd, att, gl = xs
        v_new = u_c - jnp.einsum('bhck,bhkv->bhcv', w_c, state)
        o = jnp.einsum('bhck,bhkv->bhcv', qd, state) + jnp.einsum('bhij,bhjv->bhiv', att, v_new)
        state = state * gl[..., None, None] + jnp.einsum('bhck,bhcv->bhkv', kd, v_new)
        return state, o

    xs = tuple(jnp.moveaxis(t, 1, 0) for t in (u_val, w_key, q_dec, k_dec, intra, g_last))
    state0 = jnp.zeros((b, h, dk, dv), F32)
    _, o = lax.scan(step, state0, xs)
    return jnp.moveaxis(o, 0, 1).transpose(0, 1, 3, 2, 4).reshape(b, s, h, dv)


def gated_deltanet(qkv, gate, beta_logit, a_logit, conv_w, a_log, dt_bias, norm_w):
    b, s, _ = qkv.shape
    qkv = jax.nn.silu(causal_depthwise_conv(qkv.astype(F32), conv_w.astype(F32)))
    q, k, v = jnp.split(qkv, [N_HEADS_GDN * GDN_DK, 2 * N_HEADS_GDN * GDN_DK], axis=-1)
    q = l2norm(q.reshape(b, s, N_HEADS_GDN, GDN_DK))
    k = l2norm(k.reshape(b, s, N_HEADS_GDN, GDN_DK))
    v = v.reshape(b, s, N_HEADS_GDN, GDN_DV)
    beta = jax.nn.sigmoid(beta_logit.astype(F32))
    g = -jnp.exp(a_log.astype(F32)) * jax.nn.softplus(a_logit.astype(F32) + dt_bias.astype(F32))
    o = gated_delta_rule(q, k, v, g, beta)
    o = o * lax.rsqrt(jnp.mean(o * o, -1, keepdims=True) + RMS_EPS) * norm_w.astype(F32)
    o = o * jax.nn.silu(gate.astype(F32).reshape(b, s, N_HEADS_GDN, GDN_DV))
    return o.reshape(b, s, N_HEADS_GDN * GDN_DV)


def even_mixer(x, w_in, w_out):
    b, s, _ = x.shape
    h = x @ w_in
    cuts = [W_SB, 2 * W_SB, 3 * W_SB, 3 * W_SB + W_DW, 3 * W_SB + 2 * W_DW]
    qa, ka, va, qb, kb, vb = jnp.split(h, cuts, axis=-1)
    oa = stick_breaking_attention(split_heads(qa, HEAD_DIM), split_heads(ka, HEAD_DIM), split_heads(va, HEAD_DIM))
    ob = dilated_mixture(split_heads(qb, HEAD_DIM), split_heads(kb, HEAD_DIM), split_heads(vb, HEAD_DIM))
    o = jnp.concatenate([oa, ob], axis=1).transpose(0, 2, 1, 3).reshape(b, s, MIX0)
    return o.astype(x.dtype) @ w_out


def odd_mixer(x, w_in, lam_re, lam_im, log_dt, b_re, b_im, c_re, c_im, d_skip, glu_w, glu_b,
              conv_w, a_log, dt_bias, norm_w, w_out):
    h = x @ w_in
    c1 = SSM_WIDTH
    c2 = c1 + GDN_QKV
    c3 = c2 + N_HEADS_GDN * GDN_DV
    c4 = c3 + N_HEADS_GDN
    u, qkv, gate, beta_logit, a_logit = jnp.split(h, [c1, c2, c3, c4], axis=-1)
    oc = s5_mixer(u, lam_re, lam_im, log_dt, b_re, b_im, c_re, c_im, d_skip, glu_w, glu_b)
    od = gated_deltanet(qkv, gate, beta_logit, a_logit, conv_w, a_log, dt_bias, norm_w)
    o = jnp.concatenate([oc, od], axis=-1)
    return o.astype(x.dtype) @ w_out


def swiglu(x, w1, w3, w2):
    return (jax.nn.silu(x @ w1) * (x @ w3)) @ w2


def moe_swiglu(x, router_w, w1, w3, w2):
    logits = jnp.einsum('bsd,de->bse', x, router_w).astype(F32)
    top_val, top_idx = lax.top_k(logits, TOP_K)
    top_w = jax.nn.softmax(top_val, axis=-1)
    gates = jnp.sum(jax.nn.one_hot(top_idx, N_EXPERTS, dtype=F32) * top_w[..., None], axis=-2)
    y = jnp.zeros_like(x)
    for e in range(N_EXPERTS):
        y = y + gates[..., e:e + 1].astype(x.dtype) * swiglu(x, w1[e], w3[e], w2[e])
    return y


def setup_inputs(seed: int = 0) -> dict:
    key = jax.random.key(seed)
    ks = list(jax.random.split(key, 40))

    def nrm(shape, std):
        return jax.random.normal(ks.pop(), shape, F32) * std

    def unif(shape, lo, hi):
        return jax.random.uniform(ks.pop(), shape, F32, lo, hi)

    ne, no = N_EVEN, N_ODD
    d = D_MODEL
    inp = {}
    inp['x'] = nrm((BATCH, SEQ, d), 1.0)
    inp['even_w_in'] = nrm((ne, d, IN0), d ** -0.5)
    inp['even_w_out'] = nrm((ne, MIX0, d), MIX0 ** -0.5 * DEEPNORM_BETA)
    inp['even_ln_mix_g'] = 1.0 + nrm((ne, d), 0.02)
    inp['even_ln_mix_b'] = nrm((ne, d), 0.02)
    inp['even_ffn_w1'] = nrm((ne, d, D_FF), d ** -0.5)
    inp['even_ffn_w3'] = nrm((ne, d, D_FF), d ** -0.5)
    inp['even_ffn_w2'] = nrm((ne, D_FF, d), D_FF ** -0.5 * DEEPNORM_BETA)
    inp['even_ln_ffn_g'] = 1.0 + nrm((ne, d), 0.02)
    inp['even_ln_ffn_b'] = nrm((ne, d), 0.02)
    inp['odd_w_in'] = nrm((no, d, IN1), d ** -0.5)
    inp['odd_ssm_lam_re'] = -0.5 + nrm((no, SSM_GROUPS, SSM_STATE), 0.01)
    inp['odd_ssm_lam_im'] = math.pi * jnp.arange(SSM_STATE, dtype=F32) + nrm((no, SSM_GROUPS, SSM_STATE), 0.01)
    inp['odd_ssm_log_dt'] = unif((no, SSM_GROUPS), math.log(1e-3), math.log(1e-1))
    inp['odd_ssm_b_re'] = nrm((no, SSM_GROUPS, SSM_STATE, SSM_GROUP), (2 * SSM_GROUP) ** -0.5)
    inp['odd_ssm_b_im'] = nrm((no, SSM_GROUPS, SSM_STATE, SSM_GROUP), (2 * SSM_GROUP) ** -0.5)
    inp['odd_ssm_c_re'] = nrm((no, SSM_GROUPS, SSM_GROUP, SSM_STATE), SSM_STATE ** -0.5)
    inp['odd_ssm_c_im'] = nrm((no, SSM_GROUPS, SSM_GROUP, SSM_STATE), SSM_STATE ** -0.5)
    inp['odd_ssm_d'] = nrm((no, SSM_WIDTH), 1.0)
    inp['odd_glu_w'] = nrm((no, SSM_WIDTH, SSM_WIDTH), SSM_WIDTH ** -0.5)
    inp['odd_glu_b'] = nrm((no, SSM_WIDTH), 0.02)
    inp['odd_gdn_conv_w'] = nrm((no, GDN_CONV, GDN_QKV), GDN_CONV ** -0.5)
    inp['odd_gdn_a_log'] = jnp.log(unif((no, N_HEADS_GDN), 1.0, 16.0))
    dt0 = jnp.exp(unif((no, N_HEADS_GDN), math.log(1e-3), math.log(1e-1)))
    inp['odd_gdn_dt_bias'] = dt0 + jnp.log(-jnp.expm1(-dt0))
    inp['odd_gdn_norm_w'] = 1.0 + nrm((no, GDN_DV), 0.02)
    inp['odd_w_out'] = nrm((no, MIX1, d), MIX1 ** -0.5 * DEEPNORM_BETA)
    inp['odd_ln_mix_g'] = 1.0 + nrm((no, d), 0.02)
    inp['odd_ln_mix_b'] = nrm((no, d), 0.02)
    inp['odd_router_w'] = nrm((no, d, N_EXPERTS), d ** -0.5)
    inp['odd_moe_w1'] = nrm((no, N_EXPERTS, d, D_FF_EXPERT), d ** -0.5)
    inp['odd_moe_w3'] = nrm((no, N_EXPERTS, d, D_FF_EXPERT), d ** -0.5)
    inp['odd_moe_w2'] = nrm((no, N_EXPERTS, D_FF_EXPERT, d), D_FF_EXPERT ** -0.5 * DEEPNORM_BETA)
    inp['odd_ln_ffn_g'] = 1.0 + nrm((no, d), 0.02)
    inp['odd_ln_ffn_b'] = nrm((no, d), 0.02)
    return inp


def reference(x, even_w_in, even_w_out, even_ln_mix_g, even_ln_mix_b, even_ffn_w1, even_ffn_w3,
              even_ffn_w2, even_ln_ffn_g, even_ln_ffn_b, odd_w_in, odd_ssm_lam_re, odd_ssm_lam_im,
              odd_ssm_log_dt, odd_ssm_b_re, odd_ssm_b_im, odd_ssm_c_re, odd_ssm_c_im, odd_ssm_d,
              odd_glu_w, odd_glu_b, odd_gdn_conv_w, odd_gdn_a_log, odd_gdn_dt_bias, odd_gdn_norm_w,
              odd_w_out, odd_ln_mix_g, odd_ln_mix_b, odd_router_w, odd_moe_w1, odd_moe_w3,
              odd_moe_w2, odd_ln_ffn_g, odd_ln_ffn_b):
    for layer in range(DEPTH):
        i = layer // 2
        if layer % 2 == 0:
            mix = even_mixer(x, even_w_in[i], even_w_out[i])
            x = layer_norm(DEEPNORM_ALPHA * x + mix, even_ln_mix_g[i], even_ln_mix_b[i])
            ffn = swiglu(x, even_ffn_w1[i], even_ffn_w3[i], even_ffn_w2[i])
            x = layer_norm(DEEPNORM_ALPHA * x + ffn, even_ln_ffn_g[i], even_ln_ffn_b[i])
        else:
            mix = odd_mixer(x, odd_w_in[i], odd_ssm_lam_re[i], odd_ssm_lam_im[i], odd_ssm_log_dt[i],
                            odd_ssm_b_re[i], odd_ssm_b_im[i], odd_ssm_c_re[i], odd_ssm_c_im[i],
                            odd_ssm_d[i], odd_glu_w[i], odd_glu_b[i], odd_gdn_conv_w[i],
                            odd_gdn_a_log[i], odd_gdn_dt_bias[i], odd_gdn_norm_w[i], odd_w_out[i])
            x = layer_norm(DEEPNORM_ALPHA * x + mix, odd_ln_mix_g[i], odd_ln_mix_b[i])
            ffn = moe_swiglu(x, odd_router_w[i], odd_moe_w1[i], odd_moe_w3[i], odd_moe_w2[i])
            x = layer_norm(DEEPNORM_ALPHA * x + ffn, odd_ln_ffn_g[i], odd_ln_ffn_b[i])
    return x
```

```python
import os
import numpy as np
import ml_dtypes
import concourse.bass as bass
import concourse.mybir as mybir
from concourse.bass_utils import run_bass_kernel_spmd

F32 = mybir.dt.float32
BF16 = mybir.dt.bfloat16
I32 = mybir.dt.int32
AF = mybir.ActivationFunctionType
ALU = mybir.AluOpType
AX = mybir.AxisListType

NCORES = 8
D = 2048
B = 2
S = 8192
NTOK = B * S
TPC = NTOK // NCORES
ALPHA = (2 * 2) ** 0.25
LN_EPS = 1e-5
RMS_EPS = 1e-6
SEM_ROLL = 30000


class Buf:
    __slots__ = ("name", "w", "r", "dsem", "dcnt")

    def __init__(self, name):
        self.name = name
        self.w = None
        self.r = {}
        self.dsem = None
        self.dcnt = 0


class Prog:
    def __init__(self, nc, stack):
        self.nc = nc
        self.stack = stack
        self.eng = {"pe": nc.tensor, "act": nc.scalar, "dve": nc.vector, "pool": nc.gpsimd, "sp": nc.sync}
        self.cur = {}
        self.cnt = {}
        self.waited = {e: {} for e in self.eng}
        self.nsem = 0
        self.done_sems = []
        self.dma_events = {}
        self.dbufs = []
        self.dsem_pool = []
        for e in self.eng:
            self._roll(e)
        self.ninstr = 0

    def _newsem(self, name):
        self.nsem += 1
        return self.stack.enter_context(self.nc.semaphore(f"{name}_{self.nsem}"))

    def _roll(self, e):
        if e in self.cur:
            self.done_sems.append((self.cur[e], self.cnt[e]))
        self.cur[e] = self._newsem("e" + e)
        self.cnt[e] = 0

    def _dsem(self, buf):
        if buf.dsem is None:
            if self.dsem_pool:
                buf.dsem, buf.dcnt = self.dsem_pool.pop()
            else:
                buf.dsem, buf.dcnt = self._newsem("d"), 0
            self.dbufs.append(buf)

    def full_barrier(self, recycle=True):
        deps = list(self.done_sems) + [(self.cur[e], self.cnt[e]) for e in self.eng if self.cnt[e] > 0]
        deps += list(self.dma_events.items())
        for e in self.eng:
            self._wait(e, deps)
        if recycle:
            for b in self.dbufs:
                self.dsem_pool.append((b.dsem, b.dcnt))
                b.dsem = None
            self.dbufs = []

    def _wait(self, e, deps):
        w = self.waited[e]
        best = {}
        for sem, val in deps:
            if e == "pe" and sem is self.cur["pe"]:
                continue
            if w.get(sem, 0) >= val:
                continue
            if best.get(sem, (None, 0))[1] < val:
                best[sem] = (sem, val)
        for sem, val in best.values():
            self.eng[e].wait_ge(sem, val)
            w[sem] = val
            self.ninstr += 1

    @staticmethod
    def _deps(reads, writes):
        deps = []
        for b in reads:
            if b.w is not None:
                deps.append(b.w)
        for b in writes:
            if b.w is not None:
                deps.append(b.w)
            for s, v in b.r.items():
                deps.append((s, v))
        return deps

    @staticmethod
    def _record(ev, reads, writes):
        for b in reads:
            if b.r.get(ev[0], 0) < ev[1]:
                b.r[ev[0]] = ev[1]
        for b in writes:
            b.w = ev
            b.r = {}

    def op(self, e, fn, reads=(), writes=()):
        self._wait(e, self._deps(reads, writes))
        if self.cnt[e] >= SEM_ROLL:
            self._roll(e)
        ins = fn(self.eng[e])
        self.cnt[e] += 1
        ev = (self.cur[e], self.cnt[e])
        ins.then_inc(ev[0], 1)
        self.ninstr += 1
        self._record(ev, reads, writes)
        return ev

    def dma(self, q, out, in_, reads=(), writes=(), **kw):
        prim = writes[0] if writes else reads[0]
        deps = []
        for b in reads:
            if b.w is not None:
                deps.append(b.w)
        for b in writes:
            if b.w is not None and not (b.dsem is not None and b.w[0] is b.dsem):
                deps.append(b.w)
            for s, v in b.r.items():
                deps.append((s, v))
        self._wait(q, deps)
        self._dsem(prim)
        ins = self.eng[q].dma_start(out=out, in_=in_, **kw)
        prim.dcnt += 16
        ev = (prim.dsem, prim.dcnt)
        ins.then_inc(ev[0], 16)
        self.dma_events[ev[0]] = ev[1]
        self.ninstr += 1
        self._record(ev, reads, writes)
        return ev

    def collective(self, kind, groups, in_ap, out_ap, reads, writes):
        self._wait("pool", self._deps(reads, writes))
        sem = self._newsem("cc")
        ins = self.eng["pool"].collective_compute(kind, ALU.bypass, replica_groups=groups, ins=[in_ap], outs=[out_ap])
        ins.then_inc(sem)
        ev = (sem, 1)
        self.dma_events[sem] = 1
        self._record(ev, reads, writes)
        return ev

    def wait_all(self, e, bufs):
        deps = []
        for b in bufs:
            if b.w is not None:
                deps.append(b.w)
            deps.extend(b.r.items())
        self._wait(e, deps)


class Ctx:
    def __init__(self):
        import contextlib

        self.nc = bass.Bass("TRN2", target_bir_lowering=False)
        self.stack = contextlib.ExitStack()
        self.P = Prog(self.nc, self.stack)
        self.n = 0
        self.root = self.stack
        self.override = {}
        self.fused = False
        self._banks = None

    def sb(self, shape, dt, name=None):
        self.n += 1
        t = self.stack.enter_context(self.nc.sbuf_tensor(f"{name or 't'}_{self.n}", list(shape), dt))
        return t

    def psum_banks(self):
        if self._banks is None:
            banks = []
            for i in range(8):
                t = self.root.enter_context(self.nc.psum_tensor(f"ps{i}", [128, 512], F32))
                banks.append((t, Buf(f"ps{i}")))
            self._banks = banks
        return self._banks

    def din(self, name, shape, dt=F32):
        if name in self.override:
            ap = self.override[name]
            assert list(ap.shape) == list(shape), (name, ap.shape, shape)
            return ap
        return self.nc.dram_tensor(name, list(shape), dt, kind="ExternalInput").ap()

    def dout(self, name, shape, dt=F32):
        if name in self.override:
            ap = self.override[name]
            assert list(ap.shape) == list(shape), (name, ap.shape, shape)
            return ap
        return self.nc.dram_tensor(name, list(shape), dt, kind="ExternalOutput").ap()

    def scratch(self, name, shape, dt):
        return self.nc.dram_tensor(name, list(shape), dt, kind="Internal").ap()

    def begin_phase(self, override):
        import contextlib

        self.fused = True
        self.override = override
        self.stack = contextlib.ExitStack()

    def finish(self, bufs):
        if self.fused:
            self.P.full_barrier()
            self.stack.close()
            self.stack = self.root
            self.override = {}
        else:
            self.P.wait_all("sp", bufs)
            self.stack.close()

    def close(self):
        self.stack.close()


def bf16_view(a):
    return np.ascontiguousarray(a).view(ml_dtypes.bfloat16) if a.dtype == np.uint16 else a


def build_k1(F_out, ntok=TPC, env=None):
    c = env or Ctx()
    nc, P = c.nc, c.P
    xT = c.din("xT", [D, ntok])
    W = c.din("W", [D, F_out])
    hT = c.dout("hT", [F_out, ntok], BF16)
    KT = D // 128
    xb = c.sb([128, KT, ntok], BF16, "xb")
    xb_buf = [Buf(f"xb{k}") for k in range(KT)]
    for k in range(KT):
        P.dma("pool", xb[:, k, :], xT[k * 128:(k + 1) * 128, :], writes=[xb_buf[k]], max_dma_last_dim=8192)
    banks = c.psum_banks()
    FB = 512
    nfb = (F_out + FB - 1) // FB
    wt = [(c.sb([128, KT, FB], BF16, "wt"), Buf(f"wt{i}")) for i in range(2)]
    NT = ntok // 512
    ot = [(c.sb([128, ntok], BF16, "ot"), [Buf(f"ot{i}_{t}") for t in range(NT)]) for i in range(2)]
    Wv = W.rearrange("(kt p) f -> p kt f", p=128)
    bi = 0
    oi = 0
    for fb in range(nfb):
        f0 = fb * FB
        fw = min(FB, F_out - f0)
        wtile, wbuf = wt[fb % 2]
        for k in range(KT):
            P.dma("pool", wtile[:, k, :fw], Wv[:, k, f0:f0 + fw], writes=[wbuf], max_dma_last_dim=8192)
        for fc in range((fw + 127) // 128):
            m = min(128, fw - fc * 128)
            otile, obufs = ot[oi % 2]
            oi += 1
            for t in range(NT):
                ps, pbuf = banks[bi % 8]
                bi += 1
                for k in range(KT):
                    P.op("pe", lambda e, k=k, ps=ps, t=t: e.matmul(
                        ps[:m, :], wtile[:, k, fc * 128:fc * 128 + m], xb[:, k, t * 512:(t + 1) * 512],
                        start=(k == 0), stop=(k == KT - 1)),
                        reads=[wbuf, xb_buf[k]], writes=[pbuf])
                if t % 2 == 0:
                    P.op("act", lambda e, ps=ps, t=t: e.copy(otile[:m, t * 512:(t + 1) * 512], ps[:m, :]),
                         reads=[pbuf], writes=[obufs[t]])
                else:
                    P.op("dve", lambda e, ps=ps, t=t: e.tensor_copy(otile[:m, t * 512:(t + 1) * 512], ps[:m, :]),
                         reads=[pbuf], writes=[obufs[t]])
            r0 = f0 + fc * 128
            P.dma("sp", hT[r0:r0 + m, :], otile[:m, :], reads=obufs)
    c.finish([b for _, bs in ot for b in bs])
    return nc


def _barrier(P, bufs):
    for e in ("pe", "act", "dve", "pool", "sp"):
        P.wait_all(e, bufs)


def _layer_norm(c, P, y, ybuf, g_t, b_t, gbuf, small, sbuf_small):
    stats, mv, sd = small
    nchunk = D // 512
    for k in range(nchunk):
        P.op("dve", lambda e: e.bn_stats(stats[:, k, :], y[:, k * 512:(k + 1) * 512]), reads=[ybuf], writes=[sbuf_small])
    P.op("dve", lambda e: e.bn_aggr(mv[:, :], stats[:, :, :]), reads=[sbuf_small], writes=[sbuf_small])
    P.op("dve", lambda e: e.tensor_scalar(out=sd[:, :], in0=mv[:, 1:2], scalar1=LN_EPS, scalar2=None, op0=ALU.add),
         reads=[sbuf_small], writes=[sbuf_small])
    P.op("act", lambda e: e.activation(out=sd[:, :], in_=sd[:, :], func=AF.Sqrt), reads=[sbuf_small], writes=[sbuf_small])
    P.op("dve", lambda e: e.reciprocal(sd[:, :], sd[:, :]), reads=[sbuf_small], writes=[sbuf_small])
    P.op("dve", lambda e: e.tensor_scalar(out=y, in0=y, scalar1=mv[:, 0:1], scalar2=sd[:, 0:1],
                                          op0=ALU.subtract, op1=ALU.mult), reads=[ybuf, sbuf_small], writes=[ybuf])
    P.op("dve", lambda e: e.tensor_tensor(out=y, in0=y, in1=g_t[:, :], op=ALU.mult), reads=[ybuf, gbuf], writes=[ybuf])
    P.op("dve", lambda e: e.tensor_tensor(out=y, in0=y, in1=b_t[:, :], op=ALU.add), reads=[ybuf, gbuf], writes=[ybuf])


def build_tail(moe, ntok=TPC, TG=1024, n_exp=None, FF=None, glu=False, env=None, tok_off=None, src_ntok=None, xT_out=False):
    c = env or Ctx()
    nc, P = c.nc, c.P
    E = (8 if moe else 1) if n_exp is None else n_exp
    if FF is None:
        FF = 7168 if moe else 5632
    KT = D // 128
    src_ntok = src_ntok or ntok
    import concourse.bass as _b

    def tsl(t0_):
        return slice(t0_, t0_ + TG) if tok_off is None else _b.ds(tok_off + t0_, TG)

    if xT_out:
        xTo = c.dout("xTo", [D, ntok], BF16)
        xts = c.sb([128, KT, 128], BF16, "xts")
        xtsb = Buf("xts")
    if glu:
        yT_d = c.din("yT", [1024, src_ntok])
        odT_d = c.din("odT", [1024, src_ntok], BF16)
        gluw = c.din("gluw", [1024 // 256, 128, 8, 256])
        glub_d = c.din("glub", [128, 8])
    else:
        oT = c.din("oT", [D, src_ntok], BF16)
    x = c.din("x", [ntok, D])
    Wout = c.din("Wout", [D // 256, 128, KT, 256])
    lng = [c.din(f"ln{i}g", [D]) for i in (1, 2)]
    lnb = [c.din(f"ln{i}b", [D]) for i in (1, 2)]
    W1 = c.din("W1", [E, FF // 256, 128, KT, 256])
    W3 = c.din("W3", [E, FF // 256, 128, KT, 256])
    W2 = c.din("W2", [E, FF, D])
    if moe:
        Wr = c.din("Wr", [128, KT * 8])
    xo = c.dout("xo", [ntok, D])
    NTT = TG // 128
    NH = TG // 512
    banks = c.psum_banks()
    bi = [0]

    def bank():
        b = banks[bi[0] % 8]
        bi[0] += 1
        return b

    yacc = c.sb([128, NTT, D], F32, "yacc")
    ybuf = [Buf(f"y{t}") for t in range(NTT)]
    x1Traw = c.sb([128, KT * TG], BF16, "x1T")
    x1T = x1Traw[:, :].rearrange("p (k t) -> p k t", k=KT)
    x1Tbuf = [Buf(f"x1T{t}") for t in range(NTT)]
    if glu:
        ystage = x1Traw[:, :].bitcast(F32).rearrange("p (k t) -> p k t", k=8)
        ysbuf = Buf("ystage")
        glub = c.sb([128, 8], F32, "glub")
        glubuf = Buf("glub")
        P.dma("sp", glub[:, :], glub_d, writes=[glubuf])
        obz, obg = Buf("obz"), Buf("obg")
        gsc1 = c.sb([128, TG], F32, "gsc1")
        gsc2 = c.sb([128, TG], F32, "gsc2")
        gscb, gscb2 = Buf("gsc1"), Buf("gsc2")
    R = c.sb([128, KT * TG], BF16, "R")
    ob = R[:, :].rearrange("p (k t) -> p k t", k=KT)
    obuf = Buf("ob")
    FBK = 256
    NFC = FBK // 128
    w2blk = [R[:, i * NFC * D:(i + 1) * NFC * D].rearrange("p (f d) -> p f d", f=NFC) for i in range(2)]
    w2buf = [Buf(f"w2b{i}") for i in range(2)]
    off = 2 * NFC * D
    gT = [R[:, off + i * NFC * TG: off + (i + 1) * NFC * TG].rearrange("p (f t) -> p f t", f=NFC) for i in range(2)]
    gTbuf = [[Buf(f"gT{i}_{j}") for j in range(NFC * NH)] for i in range(2)]
    off += 2 * NFC * TG
    stmp = [R[:, off + i * 512: off + (i + 1) * 512] for i in range(2)]
    stbuf = [Buf(f"st{i}") for i in range(2)]
    wb = [c.sb([128, KT, FBK], BF16, "wb") for _ in range(4)]
    wbuf = [Buf(f"wb{i}") for i in range(4)]
    lnt = [c.sb([128, D], F32, "lnt") for _ in range(2)]
    lnbuf = Buf("lnt")
    stats = c.sb([128, D // 512, 6], F32, "stats")
    mv = c.sb([128, 2], F32, "mv")
    sd = c.sb([128, 1], F32, "sd")
    smallbuf = Buf("small")
    ident = c.sb([128, 128], F32, "ident")
    identbuf = Buf("ident")
    pslock = Buf("pslock")
    P.op("pool", lambda e: e.memset(ident[:, :], 0.0), writes=[identbuf])
    P.op("pool", lambda e: e.affine_select(out=ident[:, :], in_=ident[:, :], pattern=[[-1, 128]], compare_op=ALU.not_equal,
                                           fill=1.0, base=0, channel_multiplier=1), reads=[identbuf], writes=[identbuf])
    if moe:
        xT32 = c.sb([128, KT, 128], F32, "xT32")
        xT32buf = Buf("xT32")
        wr = c.sb([128, KT, 8], F32, "wr")
        wrbuf = Buf("wr")
        P.dma("sp", wr[:, :, :].rearrange("p k e -> p (k e)"), Wr, writes=[wrbuf])
        gates = c.sb([128, NTT, 8], F32, "gates")
        gatebuf = [Buf(f"gate{t}") for t in range(NTT)]
        rt = c.sb([128, 64], F32, "rt")
        rtbuf = Buf("rt")
    allbufs = (ybuf + x1Tbuf + [obuf] + w2buf + [b for g in gTbuf for b in g] + stbuf + wbuf + [lnbuf, smallbuf]
               + [b for _, b in banks])
    if not glu:
        oTv = oT.rearrange("(k p) t -> p k t", p=128)
    wi = [0]

    for tg in range(ntok // TG):
        t0 = tg * TG
        _barrier(P, allbufs)
        if glu:
            P.dma("sp", ystage, yT_d.rearrange("(k p) t -> p k t", p=128)[:, :, tsl(t0)], writes=[ysbuf])
            for k in range(8):
                yk = ystage[:, k, :]
                P.op("act", lambda e: e.activation(out=gsc1[:, :], in_=yk, func=AF.Square), reads=[ysbuf], writes=[gscb])
                P.op("dve", lambda e: e.tensor_scalar(out=gsc1[:, :], in0=gsc1[:, :], scalar1=0.044715, scalar2=1.0, op0=ALU.mult, op1=ALU.add),
                     reads=[gscb], writes=[gscb])
                P.op("dve", lambda e: e.tensor_tensor(out=gsc1[:, :], in0=gsc1[:, :], in1=yk, op=ALU.mult), reads=[gscb, ysbuf], writes=[gscb])
                P.op("act", lambda e: e.activation(out=gsc1[:, :], in_=gsc1[:, :], func=AF.Tanh, scale=float(np.sqrt(2.0 / np.pi))),
                     reads=[gscb], writes=[gscb])
                P.op("act", lambda e: e.mul(gsc2[:, :], yk, 0.5), reads=[ysbuf], writes=[gscb2])
                P.op("dve", lambda e: e.scalar_tensor_tensor(out=ob[:, k, :], in0=gsc1[:, :], scalar=1.0, in1=gsc2[:, :], op0=ALU.add, op1=ALU.mult),
                     reads=[gscb, gscb2], writes=[obz])
            for fcb in range(1024 // FBK):
                wt_, wb_ = wb[wi[0] % 4], wbuf[wi[0] % 4]
                wi[0] += 1
                P.dma("pool", wt_[:, 0:8, :], gluw[fcb], writes=[wb_], max_dma_last_dim=8192)
                for fc in range(NFC):
                    f = fcb * NFC + fc
                    for h in range(NH):
                        ps, pb = bank()
                        for k in range(8):
                            P.op("pe", lambda e: e.matmul(ps[:, :], wt_[:, k, fc * 128:(fc + 1) * 128], ob[:, k, h * 512:(h + 1) * 512],
                                                          start=(k == 0), stop=(k == 7)), reads=[wb_, obz], writes=[pb])
                        P.op("act", lambda e: e.activation(out=ob[:, 8 + f, h * 512:(h + 1) * 512], in_=ps[:, :], func=AF.Sigmoid,
                                                           bias=glub[:, f:f + 1]), reads=[pb, glubuf], writes=[obg])
            P.op("dve", lambda e: e.tensor_tensor(out=ob[:, 0:8, :], in0=ob[:, 0:8, :], in1=ob[:, 8:16, :], op=ALU.mult),
                 reads=[obz, obg], writes=[obz, obuf])
            P.dma("sp", ob[:, 8:16, :], odT_d.rearrange("(k p) t -> p k t", p=128)[:, :, tsl(t0)], reads=[obz], writes=[obg, obuf])
            _barrier(P, allbufs + [ysbuf, obz, obg])
        else:
            P.dma("sp", ob, oTv[:, :, tsl(t0)], writes=[obuf])
        P.dma("sp", lnt[0][:, :], lng[0].partition_broadcast(128), writes=[lnbuf])
        P.dma("sp", lnt[1][:, :], lnb[0].partition_broadcast(128), writes=[lnbuf])
        for tt in range(NTT):
            P.dma("sp", yacc[:, tt, :], x[t0 + tt * 128:t0 + (tt + 1) * 128, :], writes=[ybuf[tt]])
        for cb in range(D // FBK):
            wt_, wb_ = wb[wi[0] % 4], wbuf[wi[0] % 4]
            wi[0] += 1
            P.dma("pool", wt_[:, :, :], Wout[cb], writes=[wb_], max_dma_last_dim=8192)
            for tt in range(NTT):
                ps, pb = bank()
                for k in range(KT):
                    P.op("pe", lambda e: e.matmul(ps[:, :FBK], ob[:, k, tt * 128:(tt + 1) * 128], wt_[:, k, :],
                                                  start=(k == 0), stop=(k == KT - 1)), reads=[obuf, wb_], writes=[pb])
                ysl = yacc[:, tt, cb * FBK:(cb + 1) * FBK]
                P.op("dve", lambda e: e.scalar_tensor_tensor(out=ysl, in0=ysl, scalar=ALPHA, in1=ps[:, :FBK],
                                                             op0=ALU.mult, op1=ALU.add), reads=[pb, ybuf[tt]], writes=[ybuf[tt]])
        for tt in range(NTT):
            y = yacc[:, tt, :]
            _layer_norm(c, P, y, ybuf[tt], lnt[0], lnt[1], lnbuf, (stats, mv, sd), smallbuf)
            for q in range(KT // 4):
                ps, pb = bank()
                for j in range(4):
                    k = q * 4 + j
                    P.op("pe", lambda e: e.transpose(ps[:, j * 128:(j + 1) * 128], y[:, k * 128:(k + 1) * 128], ident[:, :]),
                         reads=[ybuf[tt], identbuf], writes=[pb])
                P.op("act", lambda e: e.copy(x1T[:, q * 4:(q + 1) * 4, tt * 128:(tt + 1) * 128],
                                             ps[:, :].rearrange("p (j t) -> p j t", j=4)), reads=[pb], writes=[x1Tbuf[tt], pslock])
                if moe and not os.environ.get("DBG_NOXT32"):
                    P.op("dve", lambda e: e.tensor_copy(xT32[:, q * 4:(q + 1) * 4, :],
                                                        ps[:, :].rearrange("p (j t) -> p j t", j=4)), reads=[pb], writes=[xT32buf, pslock])
            if moe:
                ps, pb = bank()
                if os.environ.get("DBG_NOROUTER"):
                    P.op("dve", lambda e: e.tensor_copy(ps[:, :8], xT32[:, 0, 0:8]), reads=[xT32buf], writes=[pb])
                else:
                  for k in range(KT):
                    P.op("pe", lambda e: e.matmul(ps[:, :8], xT32[:, k, :], wr[:, k, :], start=(k == 0), stop=(k == KT - 1)),
                         reads=[xT32buf, wrbuf], writes=[pb])
                lg, m1, eq1, l2, m2, eq2, dd, w1, w2 = (rt[:, 0:8], rt[:, 8:9], rt[:, 16:24], rt[:, 24:32], rt[:, 9:10],
                                                        rt[:, 32:40], rt[:, 10:11], rt[:, 11:12], rt[:, 12:13])
                V = lambda fn, rd=(), wr_=(): P.op("dve", fn, reads=[rtbuf] + list(rd), writes=[rtbuf] + list(wr_))
                V(lambda e: e.tensor_copy(lg, ps[:, :8]), rd=[pb])
                V(lambda e: e.reduce_max(m1, lg, axis=AX.X))
                V(lambda e: e.tensor_scalar(out=eq1, in0=lg, scalar1=m1, scalar2=None, op0=ALU.is_equal))
                V(lambda e: e.scalar_tensor_tensor(out=l2, in0=eq1, scalar=-1e30, in1=lg, op0=ALU.mult, op1=ALU.add))
                V(lambda e: e.reduce_max(m2, l2, axis=AX.X))
                V(lambda e: e.tensor_scalar(out=eq2, in0=l2, scalar1=m2, scalar2=None, op0=ALU.is_equal))
                V(lambda e: e.tensor_tensor(out=dd, in0=m2, in1=m1, op=ALU.subtract))
                P.op("act", lambda e: e.activation(out=dd, in_=dd, func=AF.Exp), reads=[rtbuf], writes=[rtbuf])
                V(lambda e: e.tensor_scalar(out=w1, in0=dd, scalar1=1.0, scalar2=None, op0=ALU.add))
                V(lambda e: e.reciprocal(w1, w1))
                V(lambda e: e.tensor_tensor(out=w2, in0=dd, in1=w1, op=ALU.mult))
                V(lambda e: e.tensor_scalar(out=eq1, in0=eq1, scalar1=w1, scalar2=None, op0=ALU.mult))
                V(lambda e: e.scalar_tensor_tensor(out=gates[:, tt, :], in0=eq2, scalar=w2, in1=eq1, op0=ALU.mult, op1=ALU.add),
                  wr_=[gatebuf[tt]])
            P.op("act", lambda e: e.mul(y, y, ALPHA), reads=[ybuf[tt]], writes=[ybuf[tt]])
        _barrier(P, allbufs)
        blk = 0
        for ex in range(E):
            W2v = W2[ex].rearrange("(f p) d -> p f d", p=128)
            for fb in range(FF // FBK):
                w1t, w1b = wb[wi[0] % 4], wbuf[wi[0] % 4]
                w3t, w3b = wb[(wi[0] + 1) % 4], wbuf[(wi[0] + 1) % 4]
                wi[0] += 2
                P.dma("pool", w1t[:, :, :], W1[ex, fb], writes=[w1b], max_dma_last_dim=8192)
                P.dma("pool", w3t[:, :, :], W3[ex, fb], writes=[w3b], max_dma_last_dim=8192)
                w2t, w2b = w2blk[blk % 2], w2buf[blk % 2]
                gt, gb = gT[blk % 2], gTbuf[blk % 2]
                blk += 1
                P.dma("pool", w2t, W2v[:, fb * NFC:(fb + 1) * NFC, :], writes=[w2b], max_dma_last_dim=8192)
                for fc in range(NFC):
                    for h in range(NH):
                        ps1, pb1 = bank()
                        ps3, pb3 = bank()
                        rds = [x1Tbuf[h * 4 + j] for j in range(4)]
                        for k in range(KT):
                            P.op("pe", lambda e: e.matmul(ps1[:, :], w1t[:, k, fc * 128:(fc + 1) * 128], x1T[:, k, h * 512:(h + 1) * 512],
                                                          start=(k == 0), stop=(k == KT - 1)), reads=[w1b] + rds, writes=[pb1])
                        for k in range(KT):
                            P.op("pe", lambda e: e.matmul(ps3[:, :], w3t[:, k, fc * 128:(fc + 1) * 128], x1T[:, k, h * 512:(h + 1) * 512],
                                                          start=(k == 0), stop=(k == KT - 1)), reads=[w3b] + rds, writes=[pb3])
                        si = (fc * NH + h) % 2
                        P.op("act", lambda e: e.activation(out=stmp[si], in_=ps1[:, :], func=AF.Silu), reads=[pb1], writes=[stbuf[si]])
                        P.op("dve", lambda e: e.tensor_tensor(out=gt[:, fc, h * 512:(h + 1) * 512], in0=stmp[si], in1=ps3[:, :], op=ALU.mult),
                             reads=[stbuf[si], pb3], writes=[gb[fc * NH + h]])
                for tt in range(NTT):
                    for dg in range(D // 512):
                        ps, pb = bank()
                        for fc in range(NFC):
                            P.op("pe", lambda e: e.matmul(ps[:, :], gt[:, fc, tt * 128:(tt + 1) * 128], w2t[:, fc, dg * 512:(dg + 1) * 512],
                                                          start=(fc == 0), stop=(fc == NFC - 1)),
                                 reads=[gb[fc * NH + tt // 4], w2b], writes=[pb])
                        ysl = yacc[:, tt, dg * 512:(dg + 1) * 512]
                        if moe and not os.environ.get("DBG_NOGATE"):
                            P.op("dve", lambda e: e.scalar_tensor_tensor(out=ysl, in0=ps[:, :], scalar=gates[:, tt, ex:ex + 1], in1=ysl,
                                                                         op0=ALU.mult, op1=ALU.add),
                                 reads=[pb, ybuf[tt], gatebuf[tt]], writes=[ybuf[tt]])
                        else:
                            P.op("dve", lambda e: e.tensor_tensor(out=ysl, in0=ysl, in1=ps[:, :], op=ALU.add),
                                 reads=[pb, ybuf[tt]], writes=[ybuf[tt]])
        P.dma("sp", lnt[0][:, :], lng[1].partition_broadcast(128), writes=[lnbuf])
        P.dma("sp", lnt[1][:, :], lnb[1].partition_broadcast(128), writes=[lnbuf])
        for tt in range(NTT):
            y = yacc[:, tt, :]
            _layer_norm(c, P, y, ybuf[tt], lnt[0], lnt[1], lnbuf, (stats, mv, sd), smallbuf)
            P.dma("sp", xo[t0 + tt * 128:t0 + (tt + 1) * 128, :], y, reads=[ybuf[tt]])
            if xT_out:
                for q in range(KT // 4):
                    ps, pb = bank()
                    for j in range(4):
                        k = q * 4 + j
                        P.op("pe", lambda e: e.transpose(ps[:, j * 128:(j + 1) * 128], y[:, k * 128:(k + 1) * 128], ident[:, :]),
                             reads=[ybuf[tt], identbuf], writes=[pb])
                    P.op("act", lambda e: e.copy(xts[:, q * 4:(q + 1) * 4, :], ps[:, :].rearrange("p (j t) -> p j t", j=4)),
                         reads=[pb], writes=[xtsb])
                P.dma("sp", xTo.rearrange("(k p) t -> p k t", p=128)[:, :, t0 + tt * 128:t0 + (tt + 1) * 128], xts[:, :, :], reads=[xtsb])
    c.finish(ybuf + ([xtsb] if xT_out else []))
    return nc


def dw_mult(delta):
    d = np.asarray(delta)
    m = ((d >= 0) & (d <= 128)).astype(np.float32)
    m += ((d >= 0) & (d <= 512) & (d % 4 == 0))
    m += ((d >= 0) & (d <= 2048) & (d % 16 == 0))
    return m


def k2_consts():
    s = np.arange(128)[:, None]
    t = np.arange(512)[None, :]
    sbm = np.stack([((128 * r + s) < t) for r in range(4)]).astype(np.float32)
    dwm = np.stack([dw_mult(t - s - 128 * r) for r in range(-16, 4)])
    tri = (np.arange(128)[:, None] >= np.arange(128)[None, :]).astype(np.float32)
    bf = ml_dtypes.bfloat16
    return {"sbm": np.ascontiguousarray(sbm.transpose(1, 0, 2)).astype(bf),
            "dwm": np.ascontiguousarray(dwm.transpose(1, 0, 2)).astype(bf),
            "tri": tri.astype(bf)}


def build_k2(seq=S, units=(0, 0, 1, 1), env=None):
    c = env or Ctx()
    nc, P = c.nc, c.P
    NU = len(units)
    NB = seq // 128
    NG = seq // 512
    scale = 128 ** -0.5
    qT = c.din("q", [NU, 128, seq], BF16)
    kT = c.din("k", [NU, 128, seq], BF16)
    vv = c.din("v", [NU, 128, NB, 128], BF16)
    sbm_d = c.din("sbm", [128, 4, 512], BF16)
    dwm_d = c.din("dwm", [128, 20, 512], BF16)
    tri_d = c.din("tri", [128, 128], BF16)
    oT = c.dout("oT", [NU, 128, seq], BF16)
    banks = c.psum_banks()
    zb, cb, sb_, ob = banks[0:2], banks[2:4], banks[4:6], banks[6:8]
    sbm = c.sb([128, 4, 512], BF16, "sbm")
    dwm = c.sb([128, 20, 512], BF16, "dwm")
    tri = c.sb([128, 128], BF16, "tri")
    ones = c.sb([128, 128], BF16, "ones")
    cbuf = Buf("consts")
    P.dma("sp", sbm[:, :, :], sbm_d, writes=[cbuf])
    P.dma("sp", dwm[:, :, :], dwm_d, writes=[cbuf])
    P.dma("sp", tri[:, :], tri_d, writes=[cbuf])
    P.op("pool", lambda e: e.memset(ones[:, :], 1.0), writes=[cbuf])
    qs = c.sb([128, seq], BF16, "qs")
    kraw = c.sb([128, seq], BF16, "kraw")
    ks = c.sb([128, seq], BF16, "ks")
    nks = c.sb([128, seq], BF16, "nks")
    vs = c.sb([128, NB, 128], BF16, "vs")
    os_ = c.sb([128, seq], BF16, "os")
    qbuf, krbuf, kbuf, vbuf = Buf("q"), Buf("kraw"), Buf("k"), Buf("v")
    obufs = [Buf(f"o{g}") for g in range(NG)]
    et = [(c.sb([128, 512], F32, "et"), Buf(f"et{i}")) for i in range(2)]
    spt = [(c.sb([128, 512], BF16, "spt"), Buf(f"spt{i}")) for i in range(2)]
    spm = [(c.sb([128, 512], BF16, "spm"), Buf(f"spm{i}")) for i in range(2)]
    tmp = [(c.sb([128, 512], F32, "tmp"), Buf(f"tmp{i}")) for i in range(2)]
    wt = [(c.sb([128, 512], BF16, "wt"), Buf(f"w{i}")) for i in range(2)]
    wm = [(c.sb([128, 512], BF16, "wm"), Buf(f"wm{i}")) for i in range(2)]
    Csb = c.sb([128, 512], F32, "Csb")
    Cbuf = Buf("C")
    rec = c.sb([128, 512], F32, "rec")
    recbuf = Buf("rec")
    it = 0
    for u, kind in enumerate(units):
        P.dma("sp", qs[:, :], qT[u], writes=[qbuf])
        P.dma("sp", kraw[:, :], kT[u], writes=[krbuf])
        P.dma("sp", vs[:, :, :], vv[u], writes=[vbuf])
        P.op("act", lambda e: e.mul(ks[:, :], kraw[:, :], scale), reads=[krbuf], writes=[kbuf])
        if kind == 0:
            P.op("pool", lambda e: e.tensor_scalar(out=nks[:, :], in0=ks[:, :], scalar1=-1.0, scalar2=None, op0=ALU.mult),
                 reads=[kbuf], writes=[kbuf])
        for g in range(NG):
            q0 = g * 512
            qsl = qs[:, q0:q0 + 512]
            o_ps, o_pb = ob[g % 2]
            if kind == 0:
                jlist = list(range(4 * g + 3, -1, -1))
                for n, j in enumerate(jlist):
                    it += 1
                    r = j - 4 * g
                    z_ps, z_pb = zb[it % 2]
                    c_ps, c_pb = cb[it % 2]
                    s_ps, s_pb = sb_[it % 2]
                    e_t, e_b = et[it % 2]
                    sp_t, sp_b = spt[it % 2]
                    ksl = ks[:, j * 128:(j + 1) * 128]
                    P.op("pe", lambda e: e.matmul(z_ps[:, :], ksl, qsl, start=True, stop=True), reads=[kbuf, qbuf], writes=[z_pb])
                    P.op("act", lambda e: e.activation(out=e_t[:, :], in_=z_ps[:, :], func=AF.Exp), reads=[z_pb], writes=[e_b])
                    P.op("act", lambda e: e.activation(out=sp_t[:, :], in_=e_t[:, :], func=AF.Ln, bias=1.0), reads=[e_b], writes=[sp_b])
                    if r >= 0:
                        sm_t, sm_b = spm[it % 2]
                        P.op("pool", lambda e: e.tensor_tensor(out=sm_t[:, :], in0=sp_t[:, :], in1=sbm[:, r, :], op=ALU.mult),
                             reads=[sp_b, cbuf], writes=[sm_b])
                    else:
                        sm_t, sm_b = sp_t, sp_b
                    P.op("pe", lambda e: e.matmul(c_ps[:, :], tri[:, :], sm_t[:, :], start=True, stop=False), reads=[cbuf, sm_b], writes=[c_pb])
                    P.op("pe", lambda e: e.matmul(c_ps[:, :], nks[:, j * 128:(j + 1) * 128], qsl, start=False, stop=True),
                         reads=[kbuf, qbuf], writes=[c_pb])
                    P.op("pe", lambda e: e.matmul(s_ps[:, :], ones[:, :], sm_t[:, :], start=True, stop=True), reads=[cbuf, sm_b], writes=[s_pb])
                    w_t, w_b = wt[it % 2]
                    if n == 0:
                        P.op("act", lambda e: e.activation(out=w_t[:, :], in_=c_ps[:, :], func=AF.Exp, scale=-1.0), reads=[c_pb], writes=[w_b])
                        P.op("dve", lambda e: e.tensor_copy(Csb[:, :], s_ps[:, :]), reads=[s_pb], writes=[Cbuf])
                    else:
                        t_t, t_b = tmp[it % 2]
                        P.op("dve", lambda e: e.tensor_tensor(out=t_t[:, :], in0=c_ps[:, :], in1=Csb[:, :], op=ALU.add),
                             reads=[c_pb, Cbuf], writes=[t_b])
                        P.op("act", lambda e: e.activation(out=w_t[:, :], in_=t_t[:, :], func=AF.Exp, scale=-1.0), reads=[t_b], writes=[w_b])
                        if n < len(jlist) - 1:
                            P.op("dve", lambda e: e.tensor_tensor(out=Csb[:, :], in0=s_ps[:, :], in1=Csb[:, :], op=ALU.add),
                                 reads=[s_pb, Cbuf], writes=[Cbuf])
                    if r >= 0:
                        wm_t, wm_b = wm[it % 2]
                        P.op("pool", lambda e: e.tensor_tensor(out=wm_t[:, :], in0=w_t[:, :], in1=sbm[:, r, :], op=ALU.mult),
                             reads=[w_b, cbuf], writes=[wm_b])
                    else:
                        wm_t, wm_b = w_t, w_b
                    P.op("pe", lambda e: e.matmul(o_ps[:, :], vs[:, j, :], wm_t[:, :], start=(n == 0), stop=(n == len(jlist) - 1)),
                         reads=[vbuf, wm_b], writes=[o_pb])
                P.op("act", lambda e: e.copy(os_[:, q0:q0 + 512], o_ps[:, :]), reads=[o_pb], writes=[obufs[g]])
            else:
                jlist = list(range(max(0, 4 * g - 16), 4 * g + 4))
                d_ps, d_pb = cb[g % 2]
                for n, j in enumerate(jlist):
                    it += 1
                    r = j - 4 * g
                    z_ps, z_pb = zb[it % 2]
                    P.op("pe", lambda e: e.matmul(z_ps[:, :], ks[:, j * 128:(j + 1) * 128], qsl, start=True, stop=True),
                         reads=[kbuf, qbuf], writes=[z_pb])
                    w_t, w_b = wt[it % 2]
                    P.op("act", lambda e: e.activation(out=w_t[:, :], in_=z_ps[:, :], func=AF.Exp), reads=[z_pb], writes=[w_b])
                    wm_t, wm_b = wm[it % 2]
                    P.op("dve", lambda e: e.tensor_tensor(out=wm_t[:, :], in0=w_t[:, :], in1=dwm[:, r + 16, :], op=ALU.mult),
                         reads=[w_b, cbuf], writes=[wm_b])
                    first, last = (n == 0), (n == len(jlist) - 1)
                    P.op("pe", lambda e: e.matmul(o_ps[:, :], vs[:, j, :], wm_t[:, :], start=first, stop=last), reads=[vbuf, wm_b], writes=[o_pb])
                    P.op("pe", lambda e: e.matmul(d_ps[:, :], ones[:, :], wm_t[:, :], start=first, stop=last), reads=[cbuf, wm_b], writes=[d_pb])
                P.op("dve", lambda e: e.reciprocal(rec[:, :], d_ps[:, :]), reads=[d_pb], writes=[recbuf])
                P.op("dve", lambda e: e.tensor_tensor(out=os_[:, q0:q0 + 512], in0=o_ps[:, :], in1=rec[:, :], op=ALU.mult),
                     reads=[o_pb, recbuf], writes=[obufs[g]])
        P.dma("sp", oT[u], os_[:, :], reads=obufs)
    c.finish(obufs)
    return nc


TWO_PI = 2.0 * np.pi


def s5_consts(L=64):
    return {"iota": np.tile(np.arange(L, dtype=np.float32), (128, 1))}


def build_s5(seq=S, npairs=8, L=64, env=None):
    c = env or Ctx()
    nc, P = c.nc, c.P
    NCH = seq // L
    NH = max(1, seq // 4096)
    HL = seq // NH
    NCHh = HL // L
    TPB = max(1, min(L, 512 // NCHh))
    uT = c.din("uT", [npairs, 32, seq], BF16)
    lam = c.din("lam", [npairs, 128, 3])
    Bm = c.din("Bm", [npairs, 128, 32])
    Cm = c.din("Cm", [npairs, 128, 32])
    Dk = c.din("Dk", [npairs, 32, 1])
    iota_d = c.din("iota", [128, L])
    yT = c.dout("yT", [npairs, 32, seq])
    banks = c.psum_banks()
    bi = [0]

    def bank():
        b = banks[bi[0] % 8]
        bi[0] += 1
        return b

    iota = c.sb([128, L], F32, "iota")
    iota1 = c.sb([128, L], F32, "iota1")
    ident = c.sb([128, 128], F32, "ident")
    cb = Buf("const")
    P.dma("sp", iota[:, :], iota_d, writes=[cb])
    P.op("pool", lambda e: e.memset(ident[:, :], 0.0), writes=[cb])
    P.op("pool", lambda e: e.affine_select(out=ident[:, :], in_=ident[:, :], pattern=[[-1, 128]], compare_op=ALU.not_equal,
                                           fill=1.0, base=0, channel_multiplier=1), reads=[cb], writes=[cb])
    P.op("dve", lambda e: e.tensor_scalar(out=iota1[:, :], in0=iota[:, :], scalar1=1.0, scalar2=None, op0=ALU.add), reads=[cb], writes=[cb])
    sm = c.sb([128, 64], F32, "sm")
    bm = c.sb([128, 32], F32, "bm")
    cm = c.sb([128, 32], F32, "cm")
    kbm = c.sb([128, 32], F32, "kbm")
    dk = c.sb([32, 1], F32, "dk")
    pre = Buf("pre")
    rr_f = c.sb([128, max(L, NCH)], F32, "rr_f")
    rr_i = c.sb([128, max(L, NCH)], I32, "rr_i")
    rr_g = c.sb([128, max(L, NCH)], F32, "rr_g")
    sint = c.sb([128, L], F32, "sint")
    cost = c.sb([128, L], F32, "cost")
    rpt = c.sb([128, L], F32, "rpt")
    G = [c.sb([128, L, 32], F32, "G") for _ in range(2)]
    Gb = Buf("G")
    Bt = [c.sb([32, L, 128], BF16, "Bt") for _ in range(2)]
    Btb = [Buf("Bt0"), Buf("Bt1")]
    Ct = [c.sb([128, L, 32], BF16, "Ct") for _ in range(4)]
    Ctb = Buf("Ct")
    pt = [c.sb([128, L, 16], F32, "pt") for _ in range(3)]
    for t_ in G:
        P.op("pool", lambda e: e.memset(t_[:, :, :], 0.0), writes=[Gb])
    for t_ in Ct:
        P.op("pool", lambda e: e.memset(t_[:, :, :], 0.0), writes=[Ctb])
    ub = c.sb([32, seq], BF16, "ub")
    ubuf = Buf("u")
    cc = [c.sb([128, HL], F32, "cc") for _ in range(2)]
    ccb = [Buf("cre"), Buf("cim")]
    at = c.sb([128, HL], F32, "at")
    atb = Buf("a")
    wb = [c.sb([128, seq], BF16, "wb") for _ in range(2)]
    wbb = [Buf("wre"), Buf("wim")]
    ysb = c.sb([32, HL], F32, "ysb")
    ybuf = Buf("y")
    ff = [c.sb([128, NCH], F32, "ff") for _ in range(2)]
    EE = [c.sb([128, NCH], F32, "EE") for _ in range(2)]
    gg = [c.sb([128, NCH], F32, "gg") for _ in range(2)]
    Rb = c.sb([128, NCH], F32, "Rb")
    Zb = [c.sb([128, NCH], BF16, "Zb") for _ in range(2)]
    chb = Buf("chunk")
    Zbb = Buf("Z")

    def V(fn, rd=(), wr=()):
        return P.op("dve", fn, reads=[pre] + list(rd), writes=[pre] + list(wr))

    def A(fn, rd=(), wr=()):
        return P.op("act", fn, reads=[pre] + list(rd), writes=[pre] + list(wr))

    def col(i):
        return sm[:, i:i + 1]

    def sincos(arg, n, s_out, c_out):
        for shift, out in ((0.0, s_out), (0.5 * np.pi, c_out)):
            f, i_, g = rr_f[:, :n], rr_i[:, :n], rr_g[:, :n]
            V(lambda e: e.tensor_scalar(out=g, in0=arg, scalar1=shift, scalar2=None, op0=ALU.add))
            V(lambda e: e.tensor_scalar(out=i_, in0=g, scalar1=1.0 / TWO_PI, scalar2=None, op0=ALU.mult))
            V(lambda e: e.tensor_copy(f, i_))
            V(lambda e: e.scalar_tensor_tensor(out=g, in0=f, scalar=-TWO_PI, in1=g, op0=ALU.mult, op1=ALU.add))
            V(lambda e: e.tensor_scalar(out=f, in0=g, scalar1=float(np.pi), scalar2=None, op0=ALU.is_gt))
            V(lambda e: e.scalar_tensor_tensor(out=g, in0=f, scalar=-TWO_PI, in1=g, op0=ALU.mult, op1=ALU.add))
            V(lambda e: e.tensor_scalar(out=f, in0=g, scalar1=-float(np.pi), scalar2=None, op0=ALU.is_lt))
            V(lambda e: e.scalar_tensor_tensor(out=g, in0=f, scalar=TWO_PI, in1=g, op0=ALU.mult, op1=ALU.add))
            A(lambda e: e.activation(out=out, in_=g, func=AF.Sin))

    for pr in range(npairs):
        P.dma("sp", sm[:, 0:3], lam[pr], writes=[pre])
        P.dma("sp", bm[:, :], Bm[pr], writes=[pre])
        P.dma("sp", cm[:, :], Cm[pr], writes=[pre])
        P.dma("sp", dk[:, :], Dk[pr], writes=[pre])
        P.dma("sp", ub[:, :], uT[pr], writes=[ubuf])
        lre, lim, ldt, dt_, rho, th, r_, sth, cth, nre, nim, l2, inv, kre, kim, t1, t2, phi, sph, cph, RL, nkim, sre, sim_, nsim = [
            col(i) for i in range(25)]
        A(lambda e: e.activation(out=dt_, in_=ldt, func=AF.Exp))
        V(lambda e: e.tensor_tensor(out=rho, in0=lre, in1=dt_, op=ALU.mult))
        V(lambda e: e.tensor_tensor(out=th, in0=lim, in1=dt_, op=ALU.mult))
        A(lambda e: e.activation(out=r_, in_=rho, func=AF.Exp))
        sincos(th, 1, sth, cth)
        V(lambda e: e.tensor_tensor(out=nre, in0=r_, in1=cth, op=ALU.mult))
        V(lambda e: e.tensor_scalar(out=nre, in0=nre, scalar1=-1.0, scalar2=None, op0=ALU.add))
        V(lambda e: e.tensor_tensor(out=nim, in0=r_, in1=sth, op=ALU.mult))
        V(lambda e: e.tensor_tensor(out=l2, in0=lre, in1=lre, op=ALU.mult))
        V(lambda e: e.scalar_tensor_tensor(out=l2, in0=lim, scalar=lim, in1=l2, op0=ALU.mult, op1=ALU.add))
        V(lambda e: e.reciprocal(inv, l2))
        V(lambda e: e.tensor_tensor(out=t1, in0=nre, in1=lre, op=ALU.mult))
        V(lambda e: e.scalar_tensor_tensor(out=t1, in0=nim, scalar=lim, in1=t1, op0=ALU.mult, op1=ALU.add))
        V(lambda e: e.tensor_tensor(out=kre, in0=t1, in1=inv, op=ALU.mult))
        V(lambda e: e.tensor_tensor(out=t1, in0=nim, in1=lre, op=ALU.mult))
        V(lambda e: e.tensor_tensor(out=t2, in0=nre, in1=lim, op=ALU.mult))
        V(lambda e: e.tensor_tensor(out=t1, in0=t1, in1=t2, op=ALU.subtract))
        V(lambda e: e.tensor_tensor(out=kim, in0=t1, in1=inv, op=ALU.mult))
        V(lambda e: e.tensor_scalar(out=nkim, in0=kim, scalar1=-1.0, scalar2=None, op0=ALU.mult))
        V(lambda e: e.tensor_scalar(out=kbm[:, 0:16], in0=bm[:, 0:16], scalar1=kre, scalar2=None, op0=ALU.mult))
        V(lambda e: e.scalar_tensor_tensor(out=kbm[:, 0:16], in0=bm[:, 16:32], scalar=nkim, in1=kbm[:, 0:16], op0=ALU.mult, op1=ALU.add))
        V(lambda e: e.tensor_scalar(out=kbm[:, 16:32], in0=bm[:, 16:32], scalar1=kre, scalar2=None, op0=ALU.mult))
        V(lambda e: e.scalar_tensor_tensor(out=kbm[:, 16:32], in0=bm[:, 0:16], scalar=kim, in1=kbm[:, 16:32], op0=ALU.mult, op1=ALU.add))
        V(lambda e: e.tensor_scalar(out=rpt[:, :], in0=iota[:, :], scalar1=th, scalar2=None, op0=ALU.mult), rd=[cb])
        sincos(rpt[:, :], L, sint[:, :], cost[:, :])
        A(lambda e: e.activation(out=rpt[:, :], in_=iota1[:, :], func=AF.Exp, scale=rho), rd=[cb])
        V(lambda e: e.tensor_scalar(out=phi, in0=th, scalar1=float(L), scalar2=None, op0=ALU.mult))
        sincos(phi, 1, sph, cph)
        V(lambda e: e.tensor_scalar(out=t1, in0=rho, scalar1=float(L), scalar2=None, op0=ALU.mult))
        A(lambda e: e.activation(out=RL, in_=t1, func=AF.Exp))
        for g2 in range(2):
            rows = slice(g2 * 64, g2 * 64 + 64)
            cols = slice(g2 * 16, g2 * 16 + 16)
            cs3 = cost[rows, :].unsqueeze(2).broadcast_to([64, L, 16])
            sn3 = sint[rows, :].unsqueeze(2).broadcast_to([64, L, 16])
            rp3 = rpt[rows, :].unsqueeze(2).broadcast_to([64, L, 16])
            kbre = kbm[rows, 0:16].unsqueeze(1).broadcast_to([64, L, 16])
            kbim = kbm[rows, 16:32].unsqueeze(1).broadcast_to([64, L, 16])
            cre3 = cm[rows, 0:16].unsqueeze(1).broadcast_to([64, L, 16])
            cim3 = cm[rows, 16:32].unsqueeze(1).broadcast_to([64, L, 16])
            p0, p1, p2 = [t_[rows, :, :] for t_ in pt]
            TT = lambda out, a, b, op, wr=(): V(lambda e: e.tensor_tensor(out=out, in0=a, in1=b, op=op), wr=wr)
            TT(p0, cs3, kbre, ALU.mult)
            TT(p1, sn3, kbim, ALU.mult)
            TT(G[0][rows, :, cols], p0, p1, ALU.add, wr=[Gb])
            TT(p0, cs3, kbim, ALU.mult)
            TT(p1, sn3, kbre, ALU.mult)
            TT(G[1][rows, :, cols], p0, p1, ALU.subtract, wr=[Gb])
            TT(p0, cs3, cre3, ALU.mult)
            TT(p1, sn3, cim3, ALU.mult)
            TT(p2, p0, p1, ALU.subtract)
            V(lambda e: e.tensor_copy(Ct[0][rows, :, cols], p2), wr=[Ctb])
            TT(Ct[2][rows, :, cols], p2, rp3, ALU.mult, wr=[Ctb])
            TT(p0, sn3, cre3, ALU.mult)
            TT(p1, cs3, cim3, ALU.mult)
            V(lambda e: e.scalar_tensor_tensor(out=p2, in0=p0, scalar=-1.0, in1=p1, op0=ALU.mult, op1=ALU.subtract))
            V(lambda e: e.tensor_copy(Ct[1][rows, :, cols], p2), wr=[Ctb])
            TT(Ct[3][rows, :, cols], p2, rp3, ALU.mult, wr=[Ctb])
        for ri in range(2):
            for q in range(L // 4):
                ps, pb = bank()
                for j in range(4):
                    P.op("pe", lambda e: e.transpose(ps[0:32, j * 128:(j + 1) * 128], G[ri][:, q * 4 + j, :], ident[:, :]),
                         reads=[Gb, cb, pre], writes=[pb])
                eng = "act" if (q % 2 == 0) else "dve"
                fn = (lambda e: e.copy(Bt[ri][:, q * 4:(q + 1) * 4, :], ps[0:32, :].rearrange("p (j m) -> p j m", j=4))) if eng == "act" else \
                     (lambda e: e.tensor_copy(Bt[ri][:, q * 4:(q + 1) * 4, :], ps[0:32, :].rearrange("p (j m) -> p j m", j=4)))
                P.op(eng, fn, reads=[pb], writes=[Btb[ri]])
        V(lambda e: e.memset(EE[0][:, 0:1], 1.0), wr=[chb])
        V(lambda e: e.memset(EE[1][:, 0:1], 0.0), wr=[chb])
        V(lambda e: e.tensor_copy(sre, cph))
        V(lambda e: e.tensor_copy(sim_, sph))
        n_ = 1
        while n_ < NCH:
            m_ = min(n_, NCH - n_)
            V(lambda e: e.tensor_scalar(out=nsim, in0=sim_, scalar1=-1.0, scalar2=None, op0=ALU.mult))
            V(lambda e: e.tensor_scalar(out=EE[0][:, n_:n_ + m_], in0=EE[0][:, 0:m_], scalar1=sre, scalar2=None, op0=ALU.mult), rd=[chb], wr=[chb])
            V(lambda e: e.scalar_tensor_tensor(out=EE[0][:, n_:n_ + m_], in0=EE[1][:, 0:m_], scalar=nsim, in1=EE[0][:, n_:n_ + m_],
                                               op0=ALU.mult, op1=ALU.add), rd=[chb], wr=[chb])
            V(lambda e: e.tensor_scalar(out=EE[1][:, n_:n_ + m_], in0=EE[0][:, 0:m_], scalar1=sim_, scalar2=None, op0=ALU.mult), rd=[chb], wr=[chb])
            V(lambda e: e.scalar_tensor_tensor(out=EE[1][:, n_:n_ + m_], in0=EE[1][:, 0:m_], scalar=sre, in1=EE[1][:, n_:n_ + m_],
                                               op0=ALU.mult, op1=ALU.add), rd=[chb], wr=[chb])
            n_ *= 2
            if n_ < NCH:
                V(lambda e: e.tensor_tensor(out=t1, in0=sre, in1=sre, op=ALU.mult))
                V(lambda e: e.scalar_tensor_tensor(out=t1, in0=sim_, scalar=nsim, in1=t1, op0=ALU.mult, op1=ALU.add))
                V(lambda e: e.tensor_tensor(out=t2, in0=sre, in1=sim_, op=ALU.mult))
                V(lambda e: e.tensor_scalar(out=sim_, in0=t2, scalar1=2.0, scalar2=None, op0=ALU.mult))
                V(lambda e: e.tensor_copy(sre, t1))
        a3 = at[:, :].rearrange("p (n t) -> p n t", t=L)
        V(lambda e: e.tensor_scalar(out=a3, in0=iota[:, :].unsqueeze(1).broadcast_to([128, NCHh, L]), scalar1=0.0, scalar2=r_,
                                    op0=ALU.mult, op1=ALU.add), rd=[cb], wr=[atb])
        V(lambda e: e.memset(at[:, 0::L], 0.0), rd=[atb], wr=[atb])
        V(lambda e: e.tensor_scalar(out=Rb[:, :], in0=EE[0][:, :], scalar1=0.0, scalar2=RL, op0=ALU.mult, op1=ALU.add), rd=[chb], wr=[chb])
        for hf in range(NH):
            off = hf * HL
            for tb in range(L // TPB):
                for ri in range(2):
                    ps, pb = bank()
                    for j in range(TPB):
                        tau = tb * TPB + j
                        P.op("pe", lambda e: e.matmul(ps[:, j * NCHh:(j + 1) * NCHh], Bt[ri][:, tau, :], ub[:, off + tau:off + HL:L],
                                                      start=True, stop=True), reads=[Btb[ri], ubuf], writes=[pb])
                    outv = cc[ri][:, :].rearrange("p (n t) -> p t n", t=L)[:, tb * TPB:(tb + 1) * TPB, :]
                    inv_ = ps[:, :TPB * NCHh].rearrange("p (t n) -> p t n", n=NCHh)
                    if ri == 0:
                        P.op("act", lambda e: e.copy(outv, inv_), reads=[pb], writes=[ccb[ri]])
                    else:
                        P.op("dve", lambda e: e.tensor_copy(outv, inv_), reads=[pb], writes=[ccb[ri]])
            for ri in range(2):
                P.op("dve", lambda e: e.tensor_tensor_scan(out=cc[ri][:, :], data0=at[:, :], data1=cc[ri][:, :], initial=0.0,
                                                           op0=ALU.mult, op1=ALU.add), reads=[atb, ccb[ri]], writes=[ccb[ri]])
                P.op("act", lambda e: e.copy(wb[ri][:, off:off + HL], cc[ri][:, :]), reads=[ccb[ri]], writes=[wbb[ri]])
                P.op("dve", lambda e: e.tensor_copy(ff[ri][:, hf * NCHh:(hf + 1) * NCHh], cc[ri][:, L - 1::L]),
                     reads=[ccb[ri]], writes=[chb])
        C2 = lambda fn: P.op("dve", fn, reads=[chb, pre], writes=[chb])
        C2(lambda e: e.tensor_tensor(out=gg[0][:, :], in0=EE[0][:, :], in1=ff[0][:, :], op=ALU.mult))
        C2(lambda e: e.tensor_tensor(out=rr_f[:, :NCH], in0=EE[1][:, :], in1=ff[1][:, :], op=ALU.mult))
        C2(lambda e: e.tensor_tensor(out=gg[0][:, :], in0=gg[0][:, :], in1=rr_f[:, :NCH], op=ALU.add))
        C2(lambda e: e.tensor_tensor(out=gg[1][:, :], in0=EE[0][:, :], in1=ff[1][:, :], op=ALU.mult))
        C2(lambda e: e.tensor_tensor(out=rr_f[:, :NCH], in0=EE[1][:, :], in1=ff[0][:, :], op=ALU.mult))
        C2(lambda e: e.tensor_tensor(out=gg[1][:, :], in0=gg[1][:, :], in1=rr_f[:, :NCH], op=ALU.subtract))
        for ri in range(2):
            C2(lambda e: e.tensor_tensor_scan(out=gg[ri][:, :], data0=Rb[:, :], data1=gg[ri][:, :], initial=0.0, op0=ALU.mult, op1=ALU.add))
        C2(lambda e: e.tensor_tensor(out=ff[0][:, :], in0=EE[0][:, :], in1=gg[0][:, :], op=ALU.mult))
        C2(lambda e: e.tensor_tensor(out=rr_f[:, :NCH], in0=EE[1][:, :], in1=gg[1][:, :], op=ALU.mult))
        C2(lambda e: e.tensor_tensor(out=ff[0][:, :], in0=ff[0][:, :], in1=rr_f[:, :NCH], op=ALU.subtract))
        C2(lambda e: e.tensor_tensor(out=ff[1][:, :], in0=EE[0][:, :], in1=gg[1][:, :], op=ALU.mult))
        C2(lambda e: e.tensor_tensor(out=rr_f[:, :NCH], in0=EE[1][:, :], in1=gg[0][:, :], op=ALU.mult))
        C2(lambda e: e.tensor_tensor(out=ff[1][:, :], in0=ff[1][:, :], in1=rr_f[:, :NCH], op=ALU.add))
        V(lambda e: e.tensor_scalar(out=t1, in0=sph, scalar1=-1.0, scalar2=None, op0=ALU.mult))
        ZW = lambda fn: P.op("dve", fn, reads=[chb, pre], writes=[Zbb, chb])
        ZW(lambda e: e.memset(Zb[0][:, 0:1], 0.0))
        ZW(lambda e: e.memset(Zb[1][:, 0:1], 0.0))
        if NCH > 1:
            ZW(lambda e: e.tensor_scalar(out=rr_f[:, :NCH - 1], in0=ff[0][:, :NCH - 1], scalar1=cph, scalar2=None, op0=ALU.mult))
            ZW(lambda e: e.scalar_tensor_tensor(out=Zb[0][:, 1:], in0=ff[1][:, :NCH - 1], scalar=t1, in1=rr_f[:, :NCH - 1], op0=ALU.mult, op1=ALU.add))
            ZW(lambda e: e.tensor_scalar(out=rr_f[:, :NCH - 1], in0=ff[0][:, :NCH - 1], scalar1=sph, scalar2=None, op0=ALU.mult))
            ZW(lambda e: e.scalar_tensor_tensor(out=Zb[1][:, 1:], in0=ff[1][:, :NCH - 1], scalar=cph, in1=rr_f[:, :NCH - 1], op0=ALU.mult, op1=ALU.add))
        for hf in range(NH):
            off = hf * HL
            for tb in range(L // TPB):
                ps, pb = bank()
                for j in range(TPB):
                    tau = tb * TPB + j
                    o_ = ps[0:32, j * NCHh:(j + 1) * NCHh]
                    P.op("pe", lambda e: e.matmul(o_, Ct[0][:, tau, :], wb[0][:, off + tau:off + HL:L], start=True, stop=False),
                         reads=[Ctb, wbb[0]], writes=[pb])
                    P.op("pe", lambda e: e.matmul(o_, Ct[1][:, tau, :], wb[1][:, off + tau:off + HL:L], start=False, stop=False),
                         reads=[Ctb, wbb[1]], writes=[pb])
                    P.op("pe", lambda e: e.matmul(o_, Ct[2][:, tau, :], Zb[0][:, hf * NCHh:(hf + 1) * NCHh], start=False, stop=False),
                         reads=[Ctb, Zbb], writes=[pb])
                    P.op("pe", lambda e: e.matmul(o_, Ct[3][:, tau, :], Zb[1][:, hf * NCHh:(hf + 1) * NCHh], start=False, stop=True),
                         reads=[Ctb, Zbb], writes=[pb])
                yv = ysb[:, :].rearrange("p (n t) -> p t n", t=L)[:, tb * TPB:(tb + 1) * TPB, :]
                uv = ub[:, off:off + HL].rearrange("p (n t) -> p t n", t=L)[:, tb * TPB:(tb + 1) * TPB, :]
                pv = ps[0:32, :TPB * NCHh].rearrange("p (t n) -> p t n", n=NCHh)
                P.op("dve", lambda e: e.scalar_tensor_tensor(out=yv, in0=uv, scalar=dk[:, 0:1], in1=pv, op0=ALU.mult, op1=ALU.add),
                     reads=[pb, ubuf, pre], writes=[ybuf])
            P.dma("sp", yT[pr, :, off:off + HL], ysb[:, :], reads=[ybuf])
    c.finish([ybuf])
    return nc


def gdn_consts():
    i = np.arange(64)[:, None]
    j = np.arange(64)[None, :]
    NEG = -30000.0
    mrow = np.ones((1, 512), np.float32)
    mrow[0, 0::64] = 0.0
    return {
        "gmask": np.stack([np.where(j < i, 0.0, NEG), np.where(j >= i, 0.0, NEG), (i <= j).astype(np.float32)], 1).astype(np.float32),
        "mrow": mrow,
    }


def build_gdn(seq=S, nheads=2, env=None, fm_out=False):
    c = env or Ctx()
    nc, P = c.nc, c.P
    NB = 8
    BT = NB * 64
    nbatch = seq // BT
    NCk = seq // 64
    qkv = c.din("qkv", [nheads, 3, 128, seq], BF16)
    gate = c.din("gate", [nheads, 64, NCk, 128], BF16)
    rows = c.din("rows", [nheads, 2, seq], BF16)
    cols = c.din("cols", [nheads, 64, 2, NCk], BF16)
    convw = c.din("convw", [nheads, 128, 12])
    hsc = c.din("hsc", [nheads, 2])
    normw = c.din("normw", [128])
    gmask_d = c.din("gmask", [64, 3, 64])
    mrow_d = c.din("mrow", [1, 512])
    if fm_out:
        oT = c.dout("oT", [nheads * 128, seq], BF16)
    else:
        oT = c.dout("o", [nheads, 64, NCk, 128], BF16)
    banks = c.psum_banks()
    bi = [0]

    def bank():
        b = banks[bi[0] % 8]
        bi[0] += 1
        return b

    cb = Buf("const")
    ident = c.sb([128, 128], F32, "ident")
    P.op("pool", lambda e: e.memset(ident[:, :], 0.0), writes=[cb])
    P.op("pool", lambda e: e.affine_select(out=ident[:, :], in_=ident[:, :], pattern=[[-1, 128]], compare_op=ALU.not_equal,
                                           fill=1.0, base=0, channel_multiplier=1), reads=[cb], writes=[cb])
    ones = c.sb([128, 128], F32, "ones")
    P.op("pool", lambda e: e.memset(ones[:, :], 1.0), writes=[cb])
    gmask = c.sb([64, 3, 64], F32, "gmask")
    mrow = c.sb([1, 512], F32, "mrow")
    nw = c.sb([64, 128], F32, "nw")
    P.dma("sp", gmask[:, :, :], gmask_d, writes=[cb])
    P.dma("sp", mrow[:, :], mrow_d, writes=[cb])
    P.dma("sp", nw[:, :], normw.partition_broadcast(64), writes=[cb])
    maskS = gmask[:, 0, :].unsqueeze(1).broadcast_to([64, NB, 64])
    maskU = gmask[:, 1, :].unsqueeze(1).broadcast_to([64, NB, 64])
    triI = gmask[:, 2, :]
    I3 = ident[0:64, 0:64].unsqueeze(1).broadcast_to([64, NB, 64])

    class H:
        pass

    hs = []
    for h in range(nheads):
        o = H()
        o.b = Buf(f"h{h}")
        o.cw = c.sb([128, 12], F32, "cw")
        o.sc = c.sb([128, 8], F32, "sc")
        o.xin = [c.sb([128, 3 + BT], BF16, "xin") for _ in range(3)]
        o.xb = [Buf(f"xin{h}_{i}") for i in range(3)]
        o.acc = [c.sb([128, BT], F32, "acc") for _ in range(3)]
        o.ab = [Buf(f"acc{h}_{i}") for i in range(3)]
        o.sq = c.sb([128, BT], F32, "sq")
        o.rs = c.sb([128, BT], F32, "rs")
        o.qT = c.sb([128, BT], BF16, "qT")
        o.kT = c.sb([128, BT], BF16, "kT")
        o.qdT = c.sb([128, BT], BF16, "qdT")
        o.qkb = Buf(f"qk{h}")
        o.kd_tm = c.sb([64, NB, 128], BF16, "kd_tm")
        o.kbg_tm = c.sb([64, NB, 128], BF16, "kbg_tm")
        o.vb_tm = c.sb([64, NB, 128], BF16, "vb_tm")
        o.tmb = Buf(f"tm{h}")
        o.gt = c.sb([64, NB, 128], BF16, "gt")
        o.sg = c.sb([64, NB, 128], F32, "sg")
        o.gtb = Buf(f"gt{h}")
        o.rowi = c.sb([1, BT], BF16, "rowi")
        o.rowf = c.sb([1, BT], F32, "rowf")
        o.gcrow = c.sb([1, BT], F32, "gcrow")
        o.rowb = Buf(f"row{h}")
        o.coli = c.sb([64, 2, NB], BF16, "coli")
        o.colf = c.sb([64, 16, NB], F32, "colf")
        o.colb = Buf(f"col{h}")
        o.gcbc = c.sb([128, BT], F32, "gcbc")
        o.egbc = c.sb([128, BT], F32, "egbc")
        o.gcb = Buf(f"gcbc{h}")
        o.dec = c.sb([64, NB, 64], F32, "dec")
        o.N = c.sb([64, NB, 64], F32, "N")
        o.NT = c.sb([64, NB, 64], F32, "NT")
        o.T = c.sb([64, NB, 64], F32, "T")
        o.TT = c.sb([64, NB, 64], F32, "TT")
        o.TTb = c.sb([64, NB, 64], BF16, "TTb")
        o.nb_ = Buf(f"N{h}")
        o.attT = c.sb([64, NB, 64], BF16, "attT")
        o.attb = Buf(f"att{h}")
        o.uval = c.sb([64, NB, 128], F32, "uval")
        o.ub = Buf(f"uval{h}")
        o.wkT = c.sb([128, NB, 64], BF16, "wkT")
        o.wkb = Buf(f"wk{h}")
        o.S = c.sb([128, 128], F32, "S")
        o.Sb = c.sb([128, 128], BF16, "Sb")
        o.Sbuf = Buf(f"S{h}")
        o.vnew = [c.sb([64, 128], BF16, "vnew") for _ in range(2)]
        o.vnb = [Buf(f"vn{h}_{i}") for i in range(2)]
        o.ss = c.sb([64, 4], F32, "ss")
        o.ssb = Buf(f"ss{h}")
        o.junk = c.sb([64, 128], F32, "junk")
        o.oout = c.sb([64, NB, 128], F32 if fm_out else BF16, "oout")
        o.ofm = c.sb([128, BT], BF16, "ofm")
        o.ofmb = Buf(f"ofm{h}")
        o.oob = Buf(f"oo{h}")
        hs.append(o)
        P.dma("sp", o.cw[:, :], convw[h], writes=[o.b])
        P.dma("sp", o.sc[:, 0:2], hsc[h].partition_broadcast(128), writes=[o.b])
        P.op("act", lambda e: e.activation(out=o.sc[:, 2:3], in_=o.sc[:, 0:1], func=AF.Exp), reads=[o.b], writes=[o.b])
        P.op("dve", lambda e: e.tensor_scalar(out=o.sc[:, 2:3], in0=o.sc[:, 2:3], scalar1=-1.0, scalar2=None, op0=ALU.mult), reads=[o.b], writes=[o.b])
        P.op("dve", lambda e: e.memset(o.S[:, :], 0.0), writes=[o.Sbuf])
        P.op("dve", lambda e: e.memset(o.Sb[:, :], 0.0), writes=[o.Sbuf])
    LNQ = -0.5 * float(np.log(128.0))

    def precompute(h, o, b):
        t0 = b * BT
        n0 = b * NB
        for i in range(3):
            if b == 0:
                P.op("dve", lambda e: e.memset(o.xin[i][:, 0:3], 0.0), writes=[o.xb[i]])
                P.dma("sp", o.xin[i][:, 3:], qkv[h, i, :, 0:BT], writes=[o.xb[i]])
            else:
                P.dma("sp", o.xin[i][:, :], qkv[h, i, :, t0 - 3:t0 + BT], writes=[o.xb[i]])
        P.dma("sp", o.gt[:, :, :], gate[h, :, n0:n0 + NB, :], writes=[o.gtb])
        P.dma("sp", o.rowi[:, :], rows[h, 0:1, t0:t0 + BT], writes=[o.rowb])
        for ci in range(2):
            P.dma("sp", o.coli[:, ci, :], cols[h, :, ci, n0:n0 + NB], writes=[o.colb], allow_slow_non_contiguous=True)
        for i in range(3):
            x, a = o.xin[i], o.acc[i]
            P.op("dve", lambda e: e.tensor_scalar(out=a[:, :], in0=x[:, 3:3 + BT], scalar1=o.cw[:, 4 * i + 3:4 * i + 4], scalar2=None, op0=ALU.mult),
                 reads=[o.xb[i], o.b], writes=[o.ab[i]])
            for tap in range(3):
                P.op("dve", lambda e: e.scalar_tensor_tensor(out=a[:, :], in0=x[:, tap:tap + BT], scalar=o.cw[:, 4 * i + tap:4 * i + tap + 1],
                                                             in1=a[:, :], op0=ALU.mult, op1=ALU.add), reads=[o.xb[i], o.ab[i], o.b], writes=[o.ab[i]])
            P.op("act", lambda e: e.activation(out=a[:, :], in_=a[:, :], func=AF.Silu), reads=[o.ab[i]], writes=[o.ab[i]])
        for i, dst, lnb in ((0, o.qT, LNQ), (1, o.kT, 0.0)):
            a = o.acc[i]
            P.op("act", lambda e: e.activation(out=o.sq[:, :], in_=a[:, :], func=AF.Square), reads=[o.ab[i]], writes=[o.b])
            ps, pb = bank()
            P.op("pe", lambda e: e.matmul(ps[:, :], ones[:, :], o.sq[:, :], start=True, stop=True), reads=[cb, o.b], writes=[pb])
            P.op("act", lambda e: e.activation(out=o.rs[:, :], in_=ps[:, :], func=AF.Ln, bias=RMS_EPS), reads=[pb], writes=[o.b])
            P.op("act", lambda e: e.activation(out=o.rs[:, :], in_=o.rs[:, :], func=AF.Exp, scale=-0.5, bias=lnb), reads=[o.b], writes=[o.b])
            P.op("dve", lambda e: e.tensor_tensor(out=a[:, :], in0=a[:, :], in1=o.rs[:, :], op=ALU.mult), reads=[o.ab[i], o.b], writes=[o.ab[i]])
            P.op("act", lambda e: e.copy(dst[:, :], a[:, :]), reads=[o.ab[i]], writes=[o.qkb])
        P.op("act", lambda e: e.activation(out=o.sg[:, :, :], in_=o.gt[:, :, :], func=AF.Silu), reads=[o.gtb], writes=[o.gtb])
        P.op("pool", lambda e: e.tensor_tensor(out=o.sg[:, :, :], in0=o.sg[:, :, :], in1=nw[:, :].unsqueeze(1).broadcast_to([64, NB, 128]), op=ALU.mult),
             reads=[o.gtb, cb], writes=[o.gtb])
        R_ = lambda eng, fn: P.op(eng, fn, reads=[o.rowb, o.b, cb], writes=[o.rowb])
        R_("act", lambda e: e.activation(out=o.rowf[:, :], in_=o.rowi[:, :], func=AF.Exp, bias=o.sc[0:1, 1:2]))
        R_("act", lambda e: e.activation(out=o.rowf[:, :], in_=o.rowf[:, :], func=AF.Ln, bias=1.0))
        R_("dve", lambda e: e.tensor_scalar(out=o.rowf[:, :], in0=o.rowf[:, :], scalar1=o.sc[0:1, 2:3], scalar2=None, op0=ALU.mult))
        R_("dve", lambda e: e.tensor_tensor_scan(out=o.gcrow[:, :], data0=mrow[:, :], data1=o.rowf[:, :], initial=0.0, op0=ALU.mult, op1=ALU.add))
        ps, pb = bank()
        P.op("pe", lambda e: e.matmul(ps[:, :], ones[0:1, :], o.gcrow[:, :], start=True, stop=True), reads=[cb, o.rowb], writes=[pb])
        P.op("dve", lambda e: e.tensor_copy(o.gcbc[:, :], ps[:, :]), reads=[pb], writes=[o.gcb])
        P.op("act", lambda e: e.activation(out=o.egbc[:, :], in_=o.gcbc[:, :], func=AF.Exp), reads=[o.gcb], writes=[o.gcb])
        C_ = lambda eng, fn, rd=(): P.op(eng, fn, reads=[o.colb, o.b, cb] + list(rd), writes=[o.colb])
        cf = lambda k: o.colf[:, k, :]
        C_("act", lambda e: e.activation(out=cf(0), in_=o.coli[:, 0, :], func=AF.Exp, bias=o.sc[0:64, 1:2]))
        C_("act", lambda e: e.activation(out=cf(0), in_=cf(0), func=AF.Ln, bias=1.0))
        C_("dve", lambda e: e.tensor_scalar(out=cf(0), in0=cf(0), scalar1=o.sc[0:64, 2:3], scalar2=None, op0=ALU.mult))
        C_("act", lambda e: e.activation(out=cf(1), in_=o.coli[:, 1, :], func=AF.Sigmoid))
        ps, pb = bank()
        P.op("pe", lambda e: e.matmul(ps[0:64, 0:NB], triI, cf(0), start=True, stop=True), reads=[cb, o.colb], writes=[pb])
        C_("dve", lambda e: e.tensor_copy(cf(2), ps[0:64, 0:NB]), rd=[pb])
        C_("act", lambda e: e.activation(out=cf(3), in_=cf(2), func=AF.Exp))
        C_("dve", lambda e: e.tensor_tensor(out=cf(4), in0=cf(3), in1=cf(1), op=ALU.mult))
        C_("dve", lambda e: e.tensor_copy(cf(5), o.gcbc[0:64, 63::64]), rd=[o.gcb])
        C_("dve", lambda e: e.tensor_tensor(out=cf(6), in0=cf(5), in1=cf(2), op=ALU.subtract))
        C_("act", lambda e: e.activation(out=cf(6), in_=cf(6), func=AF.Exp))
        C_("dve", lambda e: e.tensor_scalar(out=cf(7), in0=cf(1), scalar1=-1.0, scalar2=None, op0=ALU.mult))
        P.op("dve", lambda e: e.tensor_tensor(out=o.qdT[:, :], in0=o.acc[0][:, :], in1=o.egbc[:, :], op=ALU.mult),
             reads=[o.ab[0], o.gcb], writes=[o.qkb])
        for src, outs in ((1, ((o.kd_tm, 6), (o.kbg_tm, 4))), (2, ((o.vb_tm, 1),))):
            for q in range(NB // 4):
                ps, pb = bank()
                for j in range(4):
                    ch = q * 4 + j
                    P.op("pe", lambda e: e.transpose(ps[0:64, j * 128:(j + 1) * 128], o.acc[src][:, ch * 64:(ch + 1) * 64], ident[:, :]),
                         reads=[o.ab[src], cb], writes=[pb])
                pv = ps[0:64, :].rearrange("p (j d) -> p j d", j=4)
                for dst, k in outs:
                    P.op("dve", lambda e: e.tensor_tensor(out=dst[:, q * 4:(q + 1) * 4, :], in0=pv,
                                                          in1=o.colf[:, k, q * 4:(q + 1) * 4].unsqueeze(2).broadcast_to([64, 4, 128]), op=ALU.mult),
                         reads=[pb, o.colb], writes=[o.tmb])
        gcj = o.gcbc[0:64, :].rearrange("p (n j) -> p n j", j=64)
        gci = o.colf[:, 2, :].unsqueeze(2).broadcast_to([64, NB, 64])
        nbe = o.colf[:, 7, :].unsqueeze(2).broadcast_to([64, NB, 64])
        D_ = lambda eng, fn, rd=(), wr=(): P.op(eng, fn, reads=[o.nb_, o.colb, o.gcb, cb] + list(rd), writes=[o.nb_] + list(wr))
        D_("dve", lambda e: e.tensor_tensor(out=o.dec[:, :, :], in0=gci, in1=gcj, op=ALU.subtract))
        D_("dve", lambda e: e.tensor_tensor(out=o.dec[:, :, :], in0=o.dec[:, :, :], in1=maskS, op=ALU.add))
        D_("act", lambda e: e.activation(out=o.dec[:, :, :], in_=o.dec[:, :, :], func=AF.Exp))
        D_("dve", lambda e: e.tensor_tensor(out=o.dec[:, :, :], in0=o.dec[:, :, :], in1=nbe, op=ALU.mult))
        ps, pb = bank()
        for ch in range(NB):
            ksl = o.kT[:, ch * 64:(ch + 1) * 64]
            P.op("pe", lambda e: e.matmul(ps[0:64, ch * 64:(ch + 1) * 64], ksl, ksl, start=True, stop=True), reads=[o.qkb], writes=[pb])
        pv = ps[0:64, :].rearrange("p (n j) -> p n j", j=64)
        D_("dve", lambda e: e.tensor_tensor(out=o.N[:, :, :], in0=o.dec[:, :, :], in1=pv, op=ALU.mult), rd=[pb])
        ps, pb = bank()
        for ch in range(NB):
            P.op("pe", lambda e: e.transpose(ps[0:64, ch * 64:(ch + 1) * 64], o.N[:, ch, :], ident[0:64, 0:64]), reads=[o.nb_, cb], writes=[pb])
        pv = ps[0:64, :].rearrange("p (n j) -> p n j", j=64)
        D_("act", lambda e: e.copy(o.NT[:, :, :], pv), rd=[pb])
        D_("dve", lambda e: e.tensor_tensor(out=o.T[:, :, :], in0=o.N[:, :, :], in1=I3, op=ALU.add))
        D_("dve", lambda e: e.tensor_tensor(out=o.TT[:, :, :], in0=o.NT[:, :, :], in1=I3, op=ALU.add))
        for lvl in range(5):
            psa, pba = bank()
            psb, pbb = bank()
            for ch in range(NB):
                sl = slice(ch * 64, (ch + 1) * 64)
                P.op("pe", lambda e: e.matmul(psa[0:64, sl], o.NT[:, ch, :], o.N[:, ch, :], start=True, stop=True), reads=[o.nb_], writes=[pba])
                P.op("pe", lambda e: e.matmul(psb[0:64, sl], o.N[:, ch, :], o.NT[:, ch, :], start=True, stop=True), reads=[o.nb_], writes=[pbb])
            D_("act", lambda e: e.copy(o.N[:, :, :], psa[0:64, :].rearrange("p (n j) -> p n j", j=64)), rd=[pba])
            D_("dve", lambda e: e.tensor_copy(o.NT[:, :, :], psb[0:64, :].rearrange("p (n j) -> p n j", j=64)), rd=[pbb])
            psa, pba = bank()
            psb, pbb = bank()
            for ch in range(NB):
                sl = slice(ch * 64, (ch + 1) * 64)
                P.op("pe", lambda e: e.matmul(psa[0:64, sl], o.TT[:, ch, :], o.N[:, ch, :], start=True, stop=True), reads=[o.nb_], writes=[pba])
                P.op("pe", lambda e: e.matmul(psb[0:64, sl], o.N[:, ch, :], o.TT[:, ch, :], start=True, stop=True), reads=[o.nb_], writes=[pbb])
            D_("dve", lambda e: e.tensor_tensor(out=o.T[:, :, :], in0=o.T[:, :, :], in1=psa[0:64, :].rearrange("p (n j) -> p n j", j=64), op=ALU.add), rd=[pba])
            D_("dve", lambda e: e.tensor_tensor(out=o.TT[:, :, :], in0=o.TT[:, :, :], in1=psb[0:64, :].rearrange("p (n j) -> p n j", j=64), op=ALU.add), rd=[pbb])
        D_("act", lambda e: e.copy(o.TTb[:, :, :], o.TT[:, :, :]))
        for q in range(NB // 4):
            ps, pb = bank()
            for j in range(4):
                ch = q * 4 + j
                P.op("pe", lambda e: e.matmul(ps[0:64, j * 128:(j + 1) * 128], o.TTb[:, ch, :], o.vb_tm[:, ch, :], start=True, stop=True),
                     reads=[o.nb_, o.tmb], writes=[pb])
            P.op("act", lambda e: e.copy(o.uval[:, q * 4:(q + 1) * 4, :], ps[0:64, :].rearrange("p (j d) -> p j d", j=4)), reads=[pb], writes=[o.ub])
        ps, pb = bank()
        for ch in range(NB):
            P.op("pe", lambda e: e.matmul(ps[:, ch * 64:(ch + 1) * 64], o.kbg_tm[:, ch, :], o.TTb[:, ch, :], start=True, stop=True),
                 reads=[o.nb_, o.tmb], writes=[pb])
        P.op("act", lambda e: e.copy(o.wkT[:, :, :], ps[:, :].rearrange("p (n i) -> p n i", i=64)), reads=[pb], writes=[o.wkb])
        gcjc = o.colf[:, 2, :].unsqueeze(2).broadcast_to([64, NB, 64])
        D_("dve", lambda e: e.tensor_tensor(out=o.dec[:, :, :], in0=gcj, in1=gcjc, op=ALU.subtract))
        D_("dve", lambda e: e.tensor_tensor(out=o.dec[:, :, :], in0=o.dec[:, :, :], in1=maskU, op=ALU.add))
        D_("act", lambda e: e.activation(out=o.dec[:, :, :], in_=o.dec[:, :, :], func=AF.Exp))
        ps, pb = bank()
        for ch in range(NB):
            sl = slice(ch * 64, (ch + 1) * 64)
            P.op("pe", lambda e: e.matmul(ps[0:64, sl], o.kT[:, sl], o.qT[:, sl], start=True, stop=True), reads=[o.qkb], writes=[pb])
        D_("dve", lambda e: e.tensor_tensor(out=o.attT[:, :, :], in0=o.dec[:, :, :], in1=ps[0:64, :].rearrange("p (n i) -> p n i", i=64), op=ALU.mult),
           rd=[pb], wr=[o.attb])

    def chunk_step(h, o, b, ch):
        n = b * NB + ch
        sl = slice(ch * 64, (ch + 1) * 64)
        vn, vnb = o.vnew[n % 2], o.vnb[n % 2]
        ps1, pb1 = bank()
        P.op("pe", lambda e: e.matmul(ps1[0:64, 0:128], o.wkT[:, ch, :], o.Sb[:, :], start=True, stop=True), reads=[o.wkb, o.Sbuf], writes=[pb1])
        P.op("dve", lambda e: e.tensor_tensor(out=vn[:, :], in0=o.uval[:, ch, :], in1=ps1[0:64, 0:128], op=ALU.subtract), reads=[o.ub, pb1], writes=[vnb])
        ps2, pb2 = bank()
        P.op("pe", lambda e: e.matmul(ps2[0:64, 0:128], o.qdT[:, sl], o.Sb[:, :], start=True, stop=False), reads=[o.qkb, o.Sbuf], writes=[pb2])
        P.op("pe", lambda e: e.matmul(ps2[0:64, 0:128], o.attT[:, ch, :], vn[:, :], start=False, stop=True), reads=[o.attb, vnb], writes=[pb2])
        ps3, pb3 = bank()
        P.op("pe", lambda e: e.matmul(ps3[:, 0:128], o.kd_tm[:, ch, :], vn[:, :], start=True, stop=True), reads=[o.tmb, vnb], writes=[pb3])
        P.op("dve", lambda e: e.scalar_tensor_tensor(out=o.S[:, :], in0=o.S[:, :], scalar=o.egbc[:, ch * 64 + 63:ch * 64 + 64], in1=ps3[:, 0:128],
                                                     op0=ALU.mult, op1=ALU.add), reads=[pb3, o.gcb, o.Sbuf], writes=[o.Sbuf])
        P.op("act", lambda e: e.copy(o.Sb[:, :], o.S[:, :]), reads=[o.Sbuf], writes=[o.Sbuf])
        P.op("act", lambda e: e.activation(out=o.junk[:, :], in_=ps2[0:64, 0:128], func=AF.Square, accum_out=o.ss[:, 0:1]), reads=[pb2], writes=[o.ssb])
        P.op("act", lambda e: e.activation(out=o.ss[:, 1:2], in_=o.ss[:, 0:1], func=AF.Ln, scale=1.0 / 128.0, bias=RMS_EPS), reads=[o.ssb], writes=[o.ssb])
        P.op("act", lambda e: e.activation(out=o.ss[:, 2:3], in_=o.ss[:, 1:2], func=AF.Exp, scale=-0.5), reads=[o.ssb], writes=[o.ssb])
        P.op("dve", lambda e: e.scalar_tensor_tensor(out=o.oout[:, ch, :], in0=ps2[0:64, 0:128], scalar=o.ss[:, 2:3], in1=o.sg[:, ch, :],
                                                     op0=ALU.mult, op1=ALU.mult), reads=[pb2, o.ssb, o.gtb], writes=[o.oob])

    for b in range(nbatch):
        for h, o in enumerate(hs):
            precompute(h, o, b)
        for ch in range(NB):
            for h, o in enumerate(hs):
                chunk_step(h, o, b, ch)
        for h, o in enumerate(hs):
            if fm_out:
                ps, pb = bank()
                for ch in range(NB):
                    P.op("pe", lambda e: e.transpose(ps[:, ch * 64:(ch + 1) * 64], o.oout[:, ch, :], ident[0:64, 0:64]),
                         reads=[o.oob, cb], writes=[pb])
                P.op("act", lambda e: e.copy(o.ofm[:, :], ps[:, :]), reads=[pb], writes=[o.ofmb])
                P.dma("sp", oT[h * 128:(h + 1) * 128, b * BT:(b + 1) * BT], o.ofm[:, :], reads=[o.ofmb])
            else:
                P.dma("sp", oT[h, :, b * NB:(b + 1) * NB, :], o.oout[:, :, :], reads=[o.oob])
    c.finish([o.oob for o in hs] + [o.ofmb for o in hs])
    return nc


def phase_inproj(c, ntok, xsrc, src_bf16, W, segs, TB=2048):
    nc, P = c.nc, c.P
    KT = D // 128
    banks = c.psum_banks()
    bi = [0]

    def bank():
        b = banks[bi[0] % 8]
        bi[0] += 1
        return b

    xb = c.sb([128, KT, TB], BF16, "xb")
    xbuf = Buf("xb")
    wt = [(c.sb([128, KT, 512], BF16, "wt"), Buf(f"wt{i}")) for i in range(2)]
    NT = TB // 512
    ot = [(c.sb([128, TB], BF16, "ot"), [Buf(f"ot{i}_{t}") for t in range(NT)]) for i in range(2)]
    tt_ = [(c.sb([128, 512], BF16, "tt"), Buf(f"tt{i}")) for i in range(2)]
    wi = oi = ti = 0
    outbufs = []
    for blk in range(ntok // TB):
        woff = 0
        for k in range(KT):
            if src_bf16:
                P.dma("sp", xb[:, k, :], xsrc(blk, k), writes=[xbuf])
            else:
                P.dma("pool", xb[:, k, :], xsrc(blk, k), writes=[xbuf], max_dma_last_dim=8192)
        for kind, col0, ncols, out in segs:
            for f0 in range(col0, col0 + ncols, 512):
                fw = min(512, col0 + ncols - f0)
                wtile, wbuf = wt[wi % 2]
                wi += 1
                P.dma("pool", wtile[:, :, :fw], W[woff:woff + 128 * KT * fw].rearrange("(p k f) -> p k f", p=128, k=KT), writes=[wbuf],
                      max_dma_last_dim=8192)
                woff += 128 * KT * fw
                if kind == "fm":
                    for fc in range((fw + 127) // 128):
                        m = min(128, fw - fc * 128)
                        otile, obufs = ot[oi % 2]
                        oi += 1
                        for t in range(NT):
                            ps, pbuf = bank()
                            for k in range(KT):
                                P.op("pe", lambda e: e.matmul(ps[:m, :], wtile[:, k, fc * 128:fc * 128 + m], xb[:, k, t * 512:(t + 1) * 512],
                                                              start=(k == 0), stop=(k == KT - 1)), reads=[wbuf, xbuf], writes=[pbuf])
                            if t % 2 == 0:
                                P.op("act", lambda e: e.copy(otile[:m, t * 512:(t + 1) * 512], ps[:m, :]), reads=[pbuf], writes=[obufs[t]])
                            else:
                                P.op("dve", lambda e: e.tensor_copy(otile[:m, t * 512:(t + 1) * 512], ps[:m, :]), reads=[pbuf], writes=[obufs[t]])
                        r0 = f0 - col0 + fc * 128
                        P.dma("sp", out[r0:r0 + m, blk * TB:(blk + 1) * TB], otile[:m, :], reads=obufs)
                        outbufs.extend(obufs)
                else:
                    for t in range(TB // 128):
                        ps, pbuf = bank()
                        for k in range(KT):
                            P.op("pe", lambda e: e.matmul(ps[:, :fw], xb[:, k, t * 128:(t + 1) * 128], wtile[:, k, :fw],
                                                          start=(k == 0), stop=(k == KT - 1)), reads=[wbuf, xbuf], writes=[pbuf])
                        ttile, tbuf = tt_[ti % 2]
                        ti += 1
                        if t % 2 == 0:
                            P.op("act", lambda e: e.copy(ttile[:, :fw], ps[:, :fw]), reads=[pbuf], writes=[tbuf])
                        else:
                            P.op("dve", lambda e: e.tensor_copy(ttile[:, :fw], ps[:, :fw]), reads=[pbuf], writes=[tbuf])
                        c0 = f0 - col0
                        P.dma("sp", out[blk * TB + t * 128: blk * TB + (t + 1) * 128, c0:c0 + fw], ttile[:, :fw], reads=[tbuf])
                        outbufs.append(tbuf)
    c.finish(outbufs)


GROUPS = [[0, 1, 2, 3], [4, 5, 6, 7]]
L1COLS = 1028


def build_fused(stop_after=7):
    c = Ctx()
    nc, P = c.nc, c.P
    i_ = lambda n, sh, dt=F32: nc.dram_tensor(n, list(sh), dt, kind="ExternalInput").ap()

    def done(k):
        if stop_after == k:
            if k >= 3:
                dbg = Buf("dbg")
                P.dma("sp", out, x2tok, writes=[dbg])
                P.full_barrier()
            c.root.close()
            return True
        return False

    xTb = i_("xTb", [D, S])
    xtok = i_("xtok", [TPC, D])
    Win0 = i_("Win0", [D * 1536])
    sbm, dwm, tri = i_("sbm", [128, 4, 512], BF16), i_("dwm", [128, 20, 512], BF16), i_("tri", [128, 128], BF16)
    t0 = s5 = gd = t1 = Win1 = None
    if stop_after >= 3:
      t0 = {k: i_("t0_" + k, sh) for k, sh in (("Wout", [8, 128, 16, 256]), ("ln1g", [D]), ("ln1b", [D]), ("ln2g", [D]), ("ln2b", [D]),
                                             ("W1", [1, 22, 128, 16, 256]), ("W3", [1, 22, 128, 16, 256]), ("W2", [1, 5632, D]))}
    if stop_after >= 4:
      Win1 = i_("Win1", [D * (L1COLS + 256)])
      s5 = {k: i_("s5_" + k, sh) for k, sh in (("lam", [8, 128, 3]), ("Bm", [8, 128, 32]), ("Cm", [8, 128, 32]), ("Dk", [8, 32, 1]), ("iota", [128, 64]))}
    if stop_after >= 4:
      gd = {k: i_("gd_" + k, sh) for k, sh in (("convw", [2, 128, 12]), ("hsc", [2, 2]), ("normw", [128]), ("gmask", [64, 3, 64]), ("mrow", [1, 512]))}
    if stop_after >= 7:
      t1 = {k: i_("t1_" + k, sh) for k, sh in (("gluw", [4, 128, 8, 256]), ("glub", [128, 8]), ("Wout", [8, 128, 16, 256]), ("ln1g", [D]), ("ln1b", [D]),
                                             ("ln2g", [D]), ("ln2b", [D]), ("W1", [8, 28, 128, 16, 256]), ("W3", [8, 28, 128, 16, 256]), ("W2", [8, 7168, D]),
                                             ("Wr", [128, 128]))}
    out = nc.dram_tensor("out", [TPC, D], F32, kind="ExternalOutput").ap()
    hfm = c.scratch("hfm", [1024, S], BF16)
    vtok = c.scratch("vtok", [S, 512], BF16)
    ag1_in, ag1_out = c.scratch("ag1_in", [512, S], BF16), c.scratch("ag1_out", [2048, S], BF16)
    x2tok = c.scratch("x2tok", [TPC, D], F32)
    ag2_in, ag2_out = c.scratch("ag2_in", [D, TPC], BF16), c.scratch("ag2_out", [4 * D, TPC], BF16)
    h1fm = c.scratch("h1fm", [L1COLS, S], BF16)
    gate_tm = c.scratch("gate_tm", [S, 256], BF16)
    ag3y_in, ag3y_out = c.scratch("ag3y_in", [256, S], F32), c.scratch("ag3y_out", [1024, S], F32)
    ag3o_in, ag3o_out = c.scratch("ag3o_in", [256, S], BF16), c.scratch("ag3o_out", [1024, S], BF16)
    rank_off = (nc.sync.partition_id() % 4) * TPC
    dummy = Buf("cc")

    def gather(a_in, a_out, r0):
        for i in range(a_in.shape[0] // r0):
            P.collective("AllGather", GROUPS, a_in[i * r0:(i + 1) * r0, :], a_out[i * 4 * r0:(i + 1) * 4 * r0, :], reads=[dummy], writes=[dummy])

    def exchange(a_in, a_out, r0):
        gather(a_in, a_out, r0)
        P.full_barrier()

    c.begin_phase({})
    phase_inproj(c, S, lambda blk, k: xTb[k * 128:(k + 1) * 128, blk * 2048:(blk + 1) * 2048], False, Win0,
                 [("fm", 0, 1024, hfm), ("tm", 1024, 512, vtok)])
    hv = hfm.rearrange("(u two p) t -> two u p t", two=2, p=128)
    c.begin_phase({"q": hv[0], "k": hv[1], "v": vtok.rearrange("(n p) (u d) -> u p n d", p=128, u=4),
                   "sbm": sbm, "dwm": dwm, "tri": tri, "oT": ag1_in.rearrange("(u p) t -> u p t", p=128)})
    build_k2(env=c)
    if done(1):
        return nc
    exchange(ag1_in, ag1_out, 64)
    if done(2):
        return nc
    ov = dict(t0)
    ov.update({"oT": ag1_out, "x": xtok, "xo": x2tok, "xTo": ag2_in})
    c.begin_phase(ov)
    build_tail(False, env=c, tok_off=rank_off, src_ntok=S, xT_out=True)
    if done(3):
        return nc
    exchange(ag2_in, ag2_out, 256)
    c.begin_phase({})
    ag2v = ag2_out.rearrange("(i s j) t -> s i j t", i=8, s=4)
    phase_inproj(c, S, lambda blk, k: ag2v[blk, k // 2, (k % 2) * 128:(k % 2) * 128 + 128, :], True, Win1,
                 [("fm", 0, L1COLS, h1fm), ("tm", L1COLS, 256, gate_tm)])
    ov = dict(s5)
    ov.update({"uT": h1fm[0:256, :].rearrange("(r c) t -> r c t", c=32), "yT": ag3y_in.rearrange("(r c) t -> r c t", c=32)})
    c.begin_phase(ov)
    build_s5(env=c)
    if done(5):
        return nc
    gather(ag3y_in, ag3y_out, 32)
    ov = dict(gd)
    ov.update({"qkv": h1fm[256:1024, :].rearrange("(h i p) t -> h i p t", h=2, i=3),
               "gate": gate_tm.rearrange("(n p) (h d) -> h p n d", p=64, h=2),
               "rows": h1fm[1024:1028, :].rearrange("(h c) t -> h c t", c=2),
               "cols": h1fm[1024:1028, :].rearrange("(h c) (n p) -> h p c n", c=2, p=64),
               "oT": ag3o_in})
    c.begin_phase(ov)
    build_gdn(env=c, fm_out=True)
    if done(6):
        return nc
    exchange(ag3o_in, ag3o_out, 64)
    ov = dict(t1)
    ov.update({"yT": ag3y_out, "odT": ag3o_out, "x": x2tok, "xo": out})
    c.begin_phase(ov)
    build_tail(True, glu=True, env=c, tok_off=rank_off, src_ntok=S)
    c.root.close()
    return nc


_PROGS = {}


def _c(a):
    return np.ascontiguousarray(a)


def _tile_w(W, fb=256):
    K_, F_ = W.shape
    return _c(W.reshape(K_ // 128, 128, F_ // fb, fb).transpose(2, 1, 0, 3))


def _tile_flat(W, segs):
    parts = []
    for col0, ncols in segs:
        for f0 in range(col0, col0 + ncols, 512):
            fw = min(512, col0 + ncols - f0)
            parts.append(W[:, f0:f0 + fw].reshape(16, 128, fw).transpose(1, 0, 2).reshape(-1))
    return _c(np.concatenate(parts))


def kernel(**inp):
    f32 = np.float32
    g = lambda k: np.asarray(inp[k], dtype=f32)
    x0 = g("x").reshape(B, S, D)
    if "fused" not in _PROGS:
        _PROGS["fused"] = build_fused()
    nc = _PROGS["fused"]
    kc, sc, gc = k2_consts(), s5_consts(), gdn_consts()
    w_in0, w_in1 = g("even_w_in")[0], g("odd_w_in")[0]
    wout0, wout1 = g("even_w_out")[0], g("odd_w_out")[0]
    rank_rows = [np.concatenate([np.arange(kind * 1024 + h * 128, kind * 1024 + (h + 1) * 128)
                                 for kind, h in ((0, 2 * s_), (0, 2 * s_ + 1), (1, 2 * s_), (1, 2 * s_ + 1))]) for s_ in range(4)]
    perm0 = np.concatenate([rank_rows[s_][i * 64:(i + 1) * 64] for i in range(8) for s_ in range(4)])
    permy = np.concatenate([np.arange(s_ * 256 + i * 32, s_ * 256 + (i + 1) * 32) for i in range(8) for s_ in range(4)])
    permo = np.concatenate([np.arange(s_ * 256 + i * 64, s_ * 256 + (i + 1) * 64) for i in range(4) for s_ in range(4)])
    perm1 = np.concatenate([permy, 1024 + permo])
    shared = {"sbm": kc["sbm"], "dwm": kc["dwm"], "tri": kc["tri"],
              "t0_Wout": _tile_w(wout0[perm0]), "t0_ln1g": g("even_ln_mix_g")[0], "t0_ln1b": g("even_ln_mix_b")[0],
              "t0_ln2g": g("even_ln_ffn_g")[0], "t0_ln2b": g("even_ln_ffn_b")[0],
              "t0_W1": _tile_w(g("even_ffn_w1")[0])[None], "t0_W3": _tile_w(g("even_ffn_w3")[0])[None], "t0_W2": g("even_ffn_w2"),
              "s5_iota": sc["iota"], "gd_normw": g("odd_gdn_norm_w")[0], "gd_gmask": gc["gmask"], "gd_mrow": gc["mrow"],
              "t1_gluw": _tile_w(g("odd_glu_w")[0][permy][:, permy]), "t1_glub": _c(g("odd_glu_b")[0][permy].reshape(8, 128).T),
              "t1_Wout": _tile_w(wout1[perm1]), "t1_ln1g": g("odd_ln_mix_g")[0], "t1_ln1b": g("odd_ln_mix_b")[0],
              "t1_ln2g": g("odd_ln_ffn_g")[0], "t1_ln2b": g("odd_ln_ffn_b")[0],
              "t1_W1": np.stack([_tile_w(w) for w in g("odd_moe_w1")[0]]), "t1_W3": np.stack([_tile_w(w) for w in g("odd_moe_w3")[0]]),
              "t1_W2": g("odd_moe_w2")[0],
              "t1_Wr": _c(g("odd_router_w")[0].reshape(16, 128, 8).transpose(1, 0, 2).reshape(128, 128))}
    lre, lim, ldt = g("odd_ssm_lam_re")[0], g("odd_ssm_lam_im")[0], g("odd_ssm_log_dt")[0]
    bre, bim, cre, cim, dsk = g("odd_ssm_b_re")[0], g("odd_ssm_b_im")[0], g("odd_ssm_c_re")[0], g("odd_ssm_c_im")[0], g("odd_ssm_d")[0]
    cw = g("odd_gdn_conv_w")[0].reshape(4, 3, 8, 128)
    alog, dtb = g("odd_gdn_a_log")[0], g("odd_gdn_dt_bias")[0]
    xT = [_c(x0[b].T) for b in range(B)]
    in_maps = []
    for c in range(NCORES):
        b, r = c // 4, c % 4
        m = dict(shared)
        m["xTb"] = xT[b]
        m["xtok"] = _c(x0[b, r * TPC:(r + 1) * TPC])
        units = ((0, 2 * r), (0, 2 * r + 1), (1, 2 * r), (1, 2 * r + 1))
        cols = []
        for kind, h in units:
            cols += [np.arange(kind * 3072 + h * 128, kind * 3072 + (h + 1) * 128),
                     np.arange(kind * 3072 + 1024 + h * 128, kind * 3072 + 1024 + (h + 1) * 128)]
        for kind, h in units:
            cols.append(np.arange(kind * 3072 + 2048 + h * 128, kind * 3072 + 2048 + (h + 1) * 128))
        m["Win0"] = _tile_flat(w_in0[:, np.concatenate(cols)], [(0, 1024), (1024, 512)])
        hh = (2 * r, 2 * r + 1)
        cols = [np.arange(r * 256, (r + 1) * 256)]
        for h in hh:
            for i in range(3):
                cols.append(np.arange(1024 + i * 1024 + h * 128, 1024 + i * 1024 + (h + 1) * 128))
        for h in hh:
            cols.append(np.array([5128 + h, 5120 + h]))
        for h in hh:
            cols.append(np.arange(4096 + h * 128, 4096 + (h + 1) * 128))
        m["Win1"] = _tile_flat(w_in1[:, np.concatenate(cols)], [(0, L1COLS), (L1COLS, 256)])
        grp = np.arange(16 * r, 16 * r + 16)
        m["s5_lam"] = _c(np.stack([lre[grp], lim[grp], np.broadcast_to(ldt[grp][:, None], (16, 64))], -1).reshape(8, 128, 3))
        m["s5_Bm"] = _c(np.concatenate([bre[grp], bim[grp]], -1).reshape(8, 128, 32))
        m["s5_Cm"] = _c(np.concatenate([cre[grp].transpose(0, 2, 1), cim[grp].transpose(0, 2, 1)], -1).reshape(8, 128, 32))
        m["s5_Dk"] = _c(dsk[r * 256:(r + 1) * 256].reshape(8, 32, 1))
        m["gd_convw"] = _c(np.stack([cw[:, :, h, :].transpose(2, 1, 0).reshape(128, 12) for h in hh]))
        m["gd_hsc"] = _c(np.stack([alog[list(hh)], dtb[list(hh)]], 1))
        in_maps.append(m)
    res = run_bass_kernel_spmd(nc, in_maps, core_ids=list(range(NCORES)))
    outs = [np.asarray(res.results[c]["out"]) for c in range(NCORES)]
    return _c(np.concatenate(outs, axis=0).reshape(B, S, D).astype(f32, copy=False))
```

```python
import os
import numpy as np
import ml_dtypes
import concourse.bass as bass
import concourse.mybir as mybir
from concourse.bass_utils import run_bass_kernel_spmd

F32 = mybir.dt.float32
BF16 = mybir.dt.bfloat16
I32 = mybir.dt.int32
AF = mybir.ActivationFunctionType
ALU = mybir.AluOpType
AX = mybir.AxisListType

NCORES = 8
D = 2048
B = 2
S = 8192
NTOK = B * S
TPC = NTOK // NCORES
ALPHA = (2 * 2) ** 0.25
LN_EPS = 1e-5
RMS_EPS = 1e-6
SEM_ROLL = 30000


class Buf:
    __slots__ = ("name", "w", "r", "dsem", "dcnt")

    def __init__(self, name):
        self.name = name
        self.w = None
        self.r = {}
        self.dsem = None
        self.dcnt = 0


class Prog:
    def __init__(self, nc, stack):
        self.nc = nc
        self.stack = stack
        self.eng = {"pe": nc.tensor, "act": nc.scalar, "dve": nc.vector, "pool": nc.gpsimd, "sp": nc.sync}
        self.cur = {}
        self.cnt = {}
        self.waited = {e: {} for e in self.eng}
        self.nsem = 0
        self.done_sems = []
        self.dma_events = {}
        self.dbufs = []
        self.dsem_pool = []
        for e in self.eng:
            self._roll(e)
        self.ninstr = 0

    def _newsem(self, name):
        self.nsem += 1
        return self.stack.enter_context(self.nc.semaphore(f"{name}_{self.nsem}"))

    def _roll(self, e):
        if e in self.cur:
            self.done_sems.append((self.cur[e], self.cnt[e]))
        self.cur[e] = self._newsem("e" + e)
        self.cnt[e] = 0

    def _dsem(self, buf):
        if buf.dsem is None:
            if self.dsem_pool:
                buf.dsem, buf.dcnt = self.dsem_pool.pop()
            else:
                buf.dsem, buf.dcnt = self._newsem("d"), 0
            self.dbufs.append(buf)

    def full_barrier(self, recycle=True):
        deps = list(self.done_sems) + [(self.cur[e], self.cnt[e]) for e in self.eng if self.cnt[e] > 0]
        deps += list(self.dma_events.items())
        for e in self.eng:
            self._wait(e, deps)
        if recycle:
            for b in self.dbufs:
                self.dsem_pool.append((b.dsem, b.dcnt))
                b.dsem = None
            self.dbufs = []

    def _wait(self, e, deps):
        w = self.waited[e]
        best = {}
        for sem, val in deps:
            if e == "pe" and sem is self.cur["pe"]:
                continue
            if w.get(sem, 0) >= val:
                continue
            if best.get(sem, (None, 0))[1] < val:
                best[sem] = (sem, val)
        for sem, val in best.values():
            self.eng[e].wait_ge(sem, val)
            w[sem] = val
            self.ninstr += 1

    @staticmethod
    def _deps(reads, writes):
        deps = []
        for b in reads:
            if b.w is not None:
                deps.append(b.w)
        for b in writes:
            if b.w is not None:
                deps.append(b.w)
            for s, v in b.r.items():
                deps.append((s, v))
        return deps

    @staticmethod
    def _record(ev, reads, writes):
        for b in reads:
            if b.r.get(ev[0], 0) < ev[1]:
                b.r[ev[0]] = ev[1]
        for b in writes:
            b.w = ev
            b.r = {}

    def op(self, e, fn, reads=(), writes=()):
        self._wait(e, self._deps(reads, writes))
        if self.cnt[e] >= SEM_ROLL:
            self._roll(e)
        ins = fn(self.eng[e])
        self.cnt[e] += 1
        ev = (self.cur[e], self.cnt[e])
        ins.then_inc(ev[0], 1)
        self.ninstr += 1
        self._record(ev, reads, writes)
        return ev

    def dma(self, q, out, in_, reads=(), writes=(), **kw):
        prim = writes[0] if writes else reads[0]
        deps = []
        for b in reads:
            if b.w is not None:
                deps.append(b.w)
        for b in writes:
            if b.w is not None and not (b.dsem is not None and b.w[0] is b.dsem):
                deps.append(b.w)
            for s, v in b.r.items():
                deps.append((s, v))
        self._wait(q, deps)
        self._dsem(prim)
        ins = self.eng[q].dma_start(out=out, in_=in_, **kw)
        prim.dcnt += 16
        ev = (prim.dsem, prim.dcnt)
        ins.then_inc(ev[0], 16)
        self.dma_events[ev[0]] = ev[1]
        self.ninstr += 1
        self._record(ev, reads, writes)
        return ev

    def collective(self, kind, groups, in_ap, out_ap, reads, writes):
        self._wait("pool", self._deps(reads, writes))
        sem = self._newsem("cc")
        ins = self.eng["pool"].collective_compute(kind, ALU.bypass, replica_groups=groups, ins=[in_ap], outs=[out_ap])
        ins.then_inc(sem)
        ev = (sem, 1)
        self.dma_events[sem] = 1
        self._record(ev, reads, writes)
        return ev

    def wait_all(self, e, bufs):
        deps = []
        for b in bufs:
            if b.w is not None:
                deps.append(b.w)
            deps.extend(b.r.items())
        self._wait(e, deps)


class Ctx:
    def __init__(self):
        import contextlib

        self.nc = bass.Bass("TRN2", target_bir_lowering=False)
        self.stack = contextlib.ExitStack()
        self.P = Prog(self.nc, self.stack)
        self.n = 0
        self.root = self.stack
        self.override = {}
        self.fused = False
        self._banks = None

    def sb(self, shape, dt, name=None):
        self.n += 1
        t = self.stack.enter_context(self.nc.sbuf_tensor(f"{name or 't'}_{self.n}", list(shape), dt))
        return t

    def psum_banks(self):
        if self._banks is None:
            banks = []
            for i in range(8):
                t = self.root.enter_context(self.nc.psum_tensor(f"ps{i}", [128, 512], F32))
                banks.append((t, Buf(f"ps{i}")))
            self._banks = banks
        return self._banks

    def din(self, name, shape, dt=F32):
        if name in self.override:
            ap = self.override[name]
            assert list(ap.shape) == list(shape), (name, ap.shape, shape)
            return ap
        return self.nc.dram_tensor(name, list(shape), dt, kind="ExternalInput").ap()

    def dout(self, name, shape, dt=F32):
        if name in self.override:
            ap = self.override[name]
            assert list(ap.shape) == list(shape), (name, ap.shape, shape)
            return ap
        return self.nc.dram_tensor(name, list(shape), dt, kind="ExternalOutput").ap()

    def scratch(self, name, shape, dt):
        return self.nc.dram_tensor(name, list(shape), dt, kind="Internal").ap()

    def begin_phase(self, override):
        import contextlib

        self.fused = True
        self.override = override
        self.stack = contextlib.ExitStack()

    def finish(self, bufs):
        if self.fused:
            self.P.full_barrier()
            self.stack.close()
            self.stack = self.root
            self.override = {}
        else:
            self.P.wait_all("sp", bufs)
            self.stack.close()

    def close(self):
        self.stack.close()


def bf16_view(a):
    return np.ascontiguousarray(a).view(ml_dtypes.bfloat16) if a.dtype == np.uint16 else a


def build_k1(F_out, ntok=TPC, env=None):
    c = env or Ctx()
    nc, P = c.nc, c.P
    xT = c.din("xT", [D, ntok])
    W = c.din("W", [D, F_out])
    hT = c.dout("hT", [F_out, ntok], BF16)
    KT = D // 128
    xb = c.sb([128, KT, ntok], BF16, "xb")
    xb_buf = [Buf(f"xb{k}") for k in range(KT)]
    for k in range(KT):
        P.dma("pool", xb[:, k, :], xT[k * 128:(k + 1) * 128, :], writes=[xb_buf[k]], max_dma_last_dim=8192)
    banks = c.psum_banks()
    FB = 512
    nfb = (F_out + FB - 1) // FB
    wt = [(c.sb([128, KT, FB], BF16, "wt"), Buf(f"wt{i}")) for i in range(2)]
    NT = ntok // 512
    ot = [(c.sb([128, ntok], BF16, "ot"), [Buf(f"ot{i}_{t}") for t in range(NT)]) for i in range(2)]
    Wv = W.rearrange("(kt p) f -> p kt f", p=128)
    bi = 0
    oi = 0
    for fb in range(nfb):
        f0 = fb * FB
        fw = min(FB, F_out - f0)
        wtile, wbuf = wt[fb % 2]
        for k in range(KT):
            P.dma("pool", wtile[:, k, :fw], Wv[:, k, f0:f0 + fw], writes=[wbuf], max_dma_last_dim=8192)
        for fc in range((fw + 127) // 128):
            m = min(128, fw - fc * 128)
            otile, obufs = ot[oi % 2]
            oi += 1
            for t in range(NT):
                ps, pbuf = banks[bi % 8]
                bi += 1
                for k in range(KT):
                    P.op("pe", lambda e, k=k, ps=ps, t=t: e.matmul(
                        ps[:m, :], wtile[:, k, fc * 128:fc * 128 + m], xb[:, k, t * 512:(t + 1) * 512],
                        start=(k == 0), stop=(k == KT - 1)),
                        reads=[wbuf, xb_buf[k]], writes=[pbuf])
                if t % 2 == 0:
                    P.op("act", lambda e, ps=ps, t=t: e.copy(otile[:m, t * 512:(t + 1) * 512], ps[:m, :]),
                         reads=[pbuf], writes=[obufs[t]])
                else:
                    P.op("dve", lambda e, ps=ps, t=t: e.tensor_copy(otile[:m, t * 512:(t + 1) * 512], ps[:m, :]),
                         reads=[pbuf], writes=[obufs[t]])
            r0 = f0 + fc * 128
            P.dma("sp", hT[r0:r0 + m, :], otile[:m, :], reads=obufs)
    c.finish([b for _, bs in ot for b in bs])
    return nc


def _barrier(P, bufs):
    for e in ("pe", "act", "dve", "pool", "sp"):
        P.wait_all(e, bufs)


def _layer_norm(c, P, y, ybuf, g_t, b_t, gbuf, small, sbuf_small):
    stats, mv, sd = small
    nchunk = D // 512
    for k in range(nchunk):
        P.op("dve", lambda e: e.bn_stats(stats[:, k, :], y[:, k * 512:(k + 1) * 512]), reads=[ybuf], writes=[sbuf_small])
    P.op("dve", lambda e: e.bn_aggr(mv[:, :], stats[:, :, :]), reads=[sbuf_small], writes=[sbuf_small])
    P.op("dve", lambda e: e.tensor_scalar(out=sd[:, :], in0=mv[:, 1:2], scalar1=LN_EPS, scalar2=None, op0=ALU.add),
         reads=[sbuf_small], writes=[sbuf_small])
    P.op("act", lambda e: e.activation(out=sd[:, :], in_=sd[:, :], func=AF.Sqrt), reads=[sbuf_small], writes=[sbuf_small])
    P.op("dve", lambda e: e.reciprocal(sd[:, :], sd[:, :]), reads=[sbuf_small], writes=[sbuf_small])
    P.op("dve", lambda e: e.tensor_scalar(out=y, in0=y, scalar1=mv[:, 0:1], scalar2=sd[:, 0:1],
                                          op0=ALU.subtract, op1=ALU.mult), reads=[ybuf, sbuf_small], writes=[ybuf])
    P.op("dve", lambda e: e.tensor_tensor(out=y, in0=y, in1=g_t[:, :], op=ALU.mult), reads=[ybuf, gbuf], writes=[ybuf])
    P.op("dve", lambda e: e.tensor_tensor(out=y, in0=y, in1=b_t[:, :], op=ALU.add), reads=[ybuf, gbuf], writes=[ybuf])


def build_tail(moe, ntok=TPC, TG=1024, n_exp=None, FF=None, glu=False, env=None, tok_off=None, src_ntok=None, xT_out=False):
    c = env or Ctx()
    nc, P = c.nc, c.P
    E = (8 if moe else 1) if n_exp is None else n_exp
    if FF is None:
        FF = 7168 if moe else 5632
    KT = D // 128
    src_ntok = src_ntok or ntok
    import concourse.bass as _b

    def tsl(t0_):
        return slice(t0_, t0_ + TG) if tok_off is None else _b.ds(tok_off + t0_, TG)

    if xT_out:
        xTo = c.dout("xTo", [D, ntok], BF16)
        xts = c.sb([128, KT, 128], BF16, "xts")
        xtsb = Buf("xts")
    if glu:
        yT_d = c.din("yT", [1024, src_ntok])
        odT_d = c.din("odT", [1024, src_ntok], BF16)
        gluw = c.din("gluw", [1024 // 256, 128, 8, 256])
        glub_d = c.din("glub", [128, 8])
    else:
        oT = c.din("oT", [D, src_ntok], BF16)
    x = c.din("x", [ntok, D])
    Wout = c.din("Wout", [D // 256, 128, KT, 256])
    lng = [c.din(f"ln{i}g", [D]) for i in (1, 2)]
    lnb = [c.din(f"ln{i}b", [D]) for i in (1, 2)]
    W1 = c.din("W1", [E, FF // 256, 128, KT, 256])
    W3 = c.din("W3", [E, FF // 256, 128, KT, 256])
    W2 = c.din("W2", [E, FF, D])
    if moe:
        Wr = c.din("Wr", [128, KT * 8])
    xo = c.dout("xo", [ntok, D])
    NTT = TG // 128
    NH = TG // 512
    banks = c.psum_banks()
    bi = [0]

    def bank():
        b = banks[bi[0] % 8]
        bi[0] += 1
        return b

    yacc = c.sb([128, NTT, D], F32, "yacc")
    ybuf = [Buf(f"y{t}") for t in range(NTT)]
    x1Traw = c.sb([128, KT * TG], BF16, "x1T")
    x1T = x1Traw[:, :].rearrange("p (k t) -> p k t", k=KT)
    x1Tbuf = [Buf(f"x1T{t}") for t in range(NTT)]
    if glu:
        ystage = x1Traw[:, :].bitcast(F32).rearrange("p (k t) -> p k t", k=8)
        ysbuf = Buf("ystage")
        glub = c.sb([128, 8], F32, "glub")
        glubuf = Buf("glub")
        P.dma("sp", glub[:, :], glub_d, writes=[glubuf])
        obz, obg = Buf("obz"), Buf("obg")
        gsc1 = c.sb([128, TG], F32, "gsc1")
        gsc2 = c.sb([128, TG], F32, "gsc2")
        gscb, gscb2 = Buf("gsc1"), Buf("gsc2")
    R = c.sb([128, KT * TG], BF16, "R")
    ob = R[:, :].rearrange("p (k t) -> p k t", k=KT)
    obuf = Buf("ob")
    FBK = 256
    NFC = FBK // 128
    w2blk = [R[:, i * NFC * D:(i + 1) * NFC * D].rearrange("p (f d) -> p f d", f=NFC) for i in range(2)]
    w2buf = [Buf(f"w2b{i}") for i in range(2)]
    off = 2 * NFC * D
    gT = [R[:, off + i * NFC * TG: off + (i + 1) * NFC * TG].rearrange("p (f t) -> p f t", f=NFC) for i in range(2)]
    gTbuf = [[Buf(f"gT{i}_{j}") for j in range(NFC * NH)] for i in range(2)]
    off += 2 * NFC * TG
    stmp = [R[:, off + i * 512: off + (i + 1) * 512] for i in range(2)]
    stbuf = [Buf(f"st{i}") for i in range(2)]
    wb = [c.sb([128, KT, FBK], BF16, "wb") for _ in range(4)]
    wbuf = [Buf(f"wb{i}") for i in range(4)]
    lnt = [c.sb([128, D], F32, "lnt") for _ in range(2)]
    lnbuf = Buf("lnt")
    stats = c.sb([128, D // 512, 6], F32, "stats")
    mv = c.sb([128, 2], F32, "mv")
    sd = c.sb([128, 1], F32, "sd")
    smallbuf = Buf("small")
    ident = c.sb([128, 128], F32, "ident")
    identbuf = Buf("ident")
    pslock = Buf("pslock")
    P.op("pool", lambda e: e.memset(ident[:, :], 0.0), writes=[identbuf])
    P.op("pool", lambda e: e.affine_select(out=ident[:, :], in_=ident[:, :], pattern=[[-1, 128]], compare_op=ALU.not_equal,
                                           fill=1.0, base=0, channel_multiplier=1), reads=[identbuf], writes=[identbuf])
    if moe:
        xT32 = c.sb([128, KT, 128], F32, "xT32")
        xT32buf = Buf("xT32")
        wr = c.sb([128, KT, 8], F32, "wr")
        wrbuf = Buf("wr")
        P.dma("sp", wr[:, :, :].rearrange("p k e -> p (k e)"), Wr, writes=[wrbuf])
        gates = c.sb([128, NTT, 8], F32, "gates")
        gatebuf = [Buf(f"gate{t}") for t in range(NTT)]
        rt = c.sb([128, 64], F32, "rt")
        rtbuf = Buf("rt")
    allbufs = (ybuf + x1Tbuf + [obuf] + w2buf + [b for g in gTbuf for b in g] + stbuf + wbuf + [lnbuf, smallbuf]
               + [b for _, b in banks])
    if not glu:
        oTv = oT.rearrange("(k p) t -> p k t", p=128)
    wi = [0]

    for tg in range(ntok // TG):
        t0 = tg * TG
        _barrier(P, allbufs)
        if glu:
            P.dma("sp", ystage, yT_d.rearrange("(k p) t -> p k t", p=128)[:, :, tsl(t0)], writes=[ysbuf])
            for k in range(8):
                yk = ystage[:, k, :]
                P.op("act", lambda e: e.activation(out=gsc1[:, :], in_=yk, func=AF.Square), reads=[ysbuf], writes=[gscb])
                P.op("dve", lambda e: e.tensor_scalar(out=gsc1[:, :], in0=gsc1[:, :], scalar1=0.044715, scalar2=1.0, op0=ALU.mult, op1=ALU.add),
                     reads=[gscb], writes=[gscb])
                P.op("dve", lambda e: e.tensor_tensor(out=gsc1[:, :], in0=gsc1[:, :], in1=yk, op=ALU.mult), reads=[gscb, ysbuf], writes=[gscb])
                P.op("act", lambda e: e.activation(out=gsc1[:, :], in_=gsc1[:, :], func=AF.Tanh, scale=float(np.sqrt(2.0 / np.pi))),
                     reads=[gscb], writes=[gscb])
                P.op("act", lambda e: e.mul(gsc2[:, :], yk, 0.5), reads=[ysbuf], writes=[gscb2])
                P.op("dve", lambda e: e.scalar_tensor_tensor(out=ob[:, k, :], in0=gsc1[:, :], scalar=1.0, in1=gsc2[:, :], op0=ALU.add, op1=ALU.mult),
                     reads=[gscb, gscb2], writes=[obz])
            for fcb in range(1024 // FBK):
                wt_, wb_ = wb[wi[0] % 4], wbuf[wi[0] % 4]
                wi[0] += 1
                P.dma("pool", wt_[:, 0:8, :], gluw[fcb], writes=[wb_], max_dma_last_dim=8192)
                for fc in range(NFC):
                    f = fcb * NFC + fc
                    for h in range(NH):
                        ps, pb = bank()
                        for k in range(8):
                            P.op("pe", lambda e: e.matmul(ps[:, :], wt_[:, k, fc * 128:(fc + 1) * 128], ob[:, k, h * 512:(h + 1) * 512],
                                                          start=(k == 0), stop=(k == 7)), reads=[wb_, obz], writes=[pb])
                        P.op("act", lambda e: e.activation(out=ob[:, 8 + f, h * 512:(h + 1) * 512], in_=ps[:, :], func=AF.Sigmoid,
                                                           bias=glub[:, f:f + 1]), reads=[pb, glubuf], writes=[obg])
            P.op("dve", lambda e: e.tensor_tensor(out=ob[:, 0:8, :], in0=ob[:, 0:8, :], in1=ob[:, 8:16, :], op=ALU.mult),
                 reads=[obz, obg], writes=[obz, obuf])
            P.dma("sp", ob[:, 8:16, :], odT_d.rearrange("(k p) t -> p k t", p=128)[:, :, tsl(t0)], reads=[obz], writes=[obg, obuf])
            _barrier(P, allbufs + [ysbuf, obz, obg])
        else:
            P.dma("sp", ob, oTv[:, :, tsl(t0)], writes=[obuf])
        P.dma("sp", lnt[0][:, :], lng[0].partition_broadcast(128), writes=[lnbuf])
        P.dma("sp", lnt[1][:, :], lnb[0].partition_broadcast(128), writes=[lnbuf])
        for tt in range(NTT):
            P.dma("sp", yacc[:, tt, :], x[t0 + tt * 128:t0 + (tt + 1) * 128, :], writes=[ybuf[tt]])
        for cb in range(D // FBK):
            wt_, wb_ = wb[wi[0] % 4], wbuf[wi[0] % 4]
            wi[0] += 1
            P.dma("pool", wt_[:, :, :], Wout[cb], writes=[wb_], max_dma_last_dim=8192)
            for tt in range(NTT):
                ps, pb = bank()
                for k in range(KT):
                    P.op("pe", lambda e: e.matmul(ps[:, :FBK], ob[:, k, tt * 128:(tt + 1) * 128], wt_[:, k, :],
                                                  start=(k == 0), stop=(k == KT - 1)), reads=[obuf, wb_], writes=[pb])
                ysl = yacc[:, tt, cb * FBK:(cb + 1) * FBK]
                P.op("dve", lambda e: e.scalar_tensor_tensor(out=ysl, in0=ysl, scalar=ALPHA, in1=ps[:, :FBK],
                                                             op0=ALU.mult, op1=ALU.add), reads=[pb, ybuf[tt]], writes=[ybuf[tt]])
        for tt in range(NTT):
            y = yacc[:, tt, :]
            _layer_norm(c, P, y, ybuf[tt], lnt[0], lnt[1], lnbuf, (stats, mv, sd), smallbuf)
            for q in range(KT // 4):
                ps, pb = bank()
                for j in range(4):
                    k = q * 4 + j
                    P.op("pe", lambda e: e.transpose(ps[:, j * 128:(j + 1) * 128], y[:, k * 128:(k + 1) * 128], ident[:, :]),
                         reads=[ybuf[tt], identbuf], writes=[pb])
                P.op("act", lambda e: e.copy(x1T[:, q * 4:(q + 1) * 4, tt * 128:(tt + 1) * 128],
                                             ps[:, :].rearrange("p (j t) -> p j t", j=4)), reads=[pb], writes=[x1Tbuf[tt], pslock])
                if moe and not os.environ.get("DBG_NOXT32"):
                    P.op("dve", lambda e: e.tensor_copy(xT32[:, q * 4:(q + 1) * 4, :],
                                                        ps[:, :].rearrange("p (j t) -> p j t", j=4)), reads=[pb], writes=[xT32buf, pslock])
            if moe:
                ps, pb = bank()
                if os.environ.get("DBG_NOROUTER"):
                    P.op("dve", lambda e: e.tensor_copy(ps[:, :8], xT32[:, 0, 0:8]), reads=[xT32buf], writes=[pb])
                else:
                  for k in range(KT):
                    P.op("pe", lambda e: e.matmul(ps[:, :8], xT32[:, k, :], wr[:, k, :], start=(k == 0), stop=(k == KT - 1)),
                         reads=[xT32buf, wrbuf], writes=[pb])
                lg, m1, eq1, l2, m2, eq2, dd, w1, w2 = (rt[:, 0:8], rt[:, 8:9], rt[:, 16:24], rt[:, 24:32], rt[:, 9:10],
                                                        rt[:, 32:40], rt[:, 10:11], rt[:, 11:12], rt[:, 12:13])
                V = lambda fn, rd=(), wr_=(): P.op("dve", fn, reads=[rtbuf] + list(rd), writes=[rtbuf] + list(wr_))
                V(lambda e: e.tensor_copy(lg, ps[:, :8]), rd=[pb])
                V(lambda e: e.reduce_max(m1, lg, axis=AX.X))
                V(lambda e: e.tensor_scalar(out=eq1, in0=lg, scalar1=m1, scalar2=None, op0=ALU.is_equal))
                V(lambda e: e.scalar_tensor_tensor(out=l2, in0=eq1, scalar=-1e30, in1=lg, op0=ALU.mult, op1=ALU.add))
                V(lambda e: e.reduce_max(m2, l2, axis=AX.X))
                V(lambda e: e.tensor_scalar(out=eq2, in0=l2, scalar1=m2, scalar2=None, op0=ALU.is_equal))
                V(lambda e: e.tensor_tensor(out=dd, in0=m2, in1=m1, op=ALU.subtract))
                P.op("act", lambda e: e.activation(out=dd, in_=dd, func=AF.Exp), reads=[rtbuf], writes=[rtbuf])
                V(lambda e: e.tensor_scalar(out=w1, in0=dd, scalar1=1.0, scalar2=None, op0=ALU.add))
                V(lambda e: e.reciprocal(w1, w1))
                V(lambda e: e.tensor_tensor(out=w2, in0=dd, in1=w1, op=ALU.mult))
                V(lambda e: e.tensor_scalar(out=eq1, in0=eq1, scalar1=w1, scalar2=None, op0=ALU.mult))
                V(lambda e: e.scalar_tensor_tensor(out=gates[:, tt, :], in0=eq2, scalar=w2, in1=eq1, op0=ALU.mult, op1=ALU.add),
                  wr_=[gatebuf[tt]])
            P.op("act", lambda e: e.mul(y, y, ALPHA), reads=[ybuf[tt]], writes=[ybuf[tt]])
        _barrier(P, allbufs)
        blk = 0
        for ex in range(E):
            W2v = W2[ex].rearrange("(f p) d -> p f d", p=128)
            for fb in range(FF // FBK):
                w1t, w1b = wb[wi[0] % 4], wbuf[wi[0] % 4]
                w3t, w3b = wb[(wi[0] + 1) % 4], wbuf[(wi[0] + 1) % 4]
                wi[0] += 2
                P.dma("pool", w1t[:, :, :], W1[ex, fb], writes=[w1b], max_dma_last_dim=8192)
                P.dma("pool", w3t[:, :, :], W3[ex, fb], writes=[w3b], max_dma_last_dim=8192)
                w2t, w2b = w2blk[blk % 2], w2buf[blk % 2]
                gt, gb = gT[blk % 2], gTbuf[blk % 2]
                blk += 1
                P.dma("pool", w2t, W2v[:, fb * NFC:(fb + 1) * NFC, :], writes=[w2b], max_dma_last_dim=8192)
                for fc in range(NFC):
                    for h in range(NH):
                        ps1, pb1 = bank()
                        ps3, pb3 = bank()
                        rds = [x1Tbuf[h * 4 + j] for j in range(4)]
                        for k in range(KT):
                            P.op("pe", lambda e: e.matmul(ps1[:, :], w1t[:, k, fc * 128:(fc + 1) * 128], x1T[:, k, h * 512:(h + 1) * 512],
                                                          start=(k == 0), stop=(k == KT - 1)), reads=[w1b] + rds, writes=[pb1])
                        for k in range(KT):
                            P.op("pe", lambda e: e.matmul(ps3[:, :], w3t[:, k, fc * 128:(fc + 1) * 128], x1T[:, k, h * 512:(h + 1) * 512],
                                                          start=(k == 0), stop=(k == KT - 1)), reads=[w3b] + rds, writes=[pb3])
                        si = (fc * NH + h) % 2
                        P.op("act", lambda e: e.activation(out=stmp[si], in_=ps1[:, :], func=AF.Silu), reads=[pb1], writes=[stbuf[si]])
                        P.op("dve", lambda e: e.tensor_tensor(out=gt[:, fc, h * 512:(h + 1) * 512], in0=stmp[si], in1=ps3[:, :], op=ALU.mult),
                             reads=[stbuf[si], pb3], writes=[gb[fc * NH + h]])
                for tt in range(NTT):
                    for dg in range(D // 512):
                        ps, pb = bank()
                        for fc in range(NFC):
                            P.op("pe", lambda e: e.matmul(ps[:, :], gt[:, fc, tt * 128:(tt + 1) * 128], w2t[:, fc, dg * 512:(dg + 1) * 512],
                                                          start=(fc == 0), stop=(fc == NFC - 1)),
                                 reads=[gb[fc * NH + tt // 4], w2b], writes=[pb])
                        ysl = yacc[:, tt, dg * 512:(dg + 1) * 512]
                        if moe and not os.environ.get("DBG_NOGATE"):
                            P.op("dve", lambda e: e.scalar_tensor_tensor(out=ysl, in0=ps[:, :], scalar=gates[:, tt, ex:ex + 1], in1=ysl,
                                                                         op0=ALU.mult, op1=ALU.add),
                                 reads=[pb, ybuf[tt], gatebuf[tt]], writes=[ybuf[tt]])
                        else:
                            P.op("dve", lambda e: e.tensor_tensor(out=ysl, in0=ysl, in1=ps[:, :], op=ALU.add),
                                 reads=[pb, ybuf[tt]], writes=[ybuf[tt]])
        P.dma("sp", lnt[0][:, :], lng[1].partition_broadcast(128), writes=[lnbuf])
        P.dma("sp", lnt[1][:, :], lnb[1].partition_broadcast(128), writes=[lnbuf])
        for tt in range(NTT):
            y = yacc[:, tt, :]
            _layer_norm(c, P, y, ybuf[tt], lnt[0], lnt[1], lnbuf, (stats, mv, sd), smallbuf)
            P.dma("sp", xo[t0 + tt * 128:t0 + (tt + 1) * 128, :], y, reads=[ybuf[tt]])
            if xT_out:
                for q in range(KT // 4):
                    ps, pb = bank()
                    for j in range(4):
                        k = q * 4 + j
                        P.op("pe", lambda e: e.transpose(ps[:, j * 128:(j + 1) * 128], y[:, k * 128:(k + 1) * 128], ident[:, :]),
                             reads=[ybuf[tt], identbuf], writes=[pb])
                    P.op("act", lambda e: e.copy(xts[:, q * 4:(q + 1) * 4, :], ps[:, :].rearrange("p (j t) -> p j t", j=4)),
                         reads=[pb], writes=[xtsb])
                P.dma("sp", xTo.rearrange("(k p) t -> p k t", p=128)[:, :, t0 + tt * 128:t0 + (tt + 1) * 128], xts[:, :, :], reads=[xtsb])
    c.finish(ybuf + ([xtsb] if xT_out else []))
    return nc


def dw_mult(delta):
    d = np.asarray(delta)
    m = ((d >= 0) & (d <= 128)).astype(np.float32)
    m += ((d >= 0) & (d <= 512) & (d % 4 == 0))
    m += ((d >= 0) & (d <= 2048) & (d % 16 == 0))
    return m


QG = 512


def k2_consts():
    bpg = QG // 128
    s = np.arange(128)[:, None]
    t = np.arange(QG)[None, :]
    sbm = np.stack([((128 * r + s) < t) for r in range(bpg)]).astype(np.float32)
    dwm = np.stack([dw_mult(t - s - 128 * r) for r in range(-16, bpg)])
    tri = (np.arange(128)[:, None] >= np.arange(128)[None, :]).astype(np.float32)
    bf = ml_dtypes.bfloat16
    return {"sbm": np.ascontiguousarray(sbm.transpose(1, 0, 2)).astype(bf),
            "dwm": np.ascontiguousarray(dwm.transpose(1, 0, 2)).astype(bf),
            "tri": tri.astype(bf)}


def build_k2(seq=S, units=(0, 0, 1, 1), env=None):
    c = env or Ctx()
    nc, P = c.nc, c.P
    NU = len(units)
    NB = seq // 128
    NG = seq // QG
    BPG = QG // 128
    NBUF = 1024 // QG
    NSB = 4
    scale = 128 ** -0.5
    qT = c.din("q", [NU, 128, seq], BF16)
    kT = c.din("k", [NU, 128, seq], BF16)
    vv = c.din("v", [NU, 128, NB, 128], BF16)
    sbm_d = c.din("sbm", [128, BPG, QG], BF16)
    dwm_d = c.din("dwm", [128, 16 + BPG, QG], BF16)
    tri_d = c.din("tri", [128, 128], BF16)
    oT = c.dout("oT", [NU, 128, seq], BF16)
    banks = c.psum_banks()

    def subs(bk):
        out = []
        for t_, _ in bk:
            for h in range(512 // QG):
                out.append((t_[:, h * QG:(h + 1) * QG], Buf("pssub")))
        return out

    zb, cb, sb_ = subs(banks[0:2]), subs(banks[2:4]), subs(banks[4:6])
    ob = [(banks[6][0][:, 0:QG], banks[6][1]), (banks[7][0][:, 0:QG], banks[7][1])]
    sbm = c.sb([128, BPG, QG], BF16, "sbm")
    dwm = c.sb([128, 16 + BPG, QG], BF16, "dwm")
    tri = c.sb([128, 128], BF16, "tri")
    ones = c.sb([128, 128], BF16, "ones")
    cbuf = Buf("consts")
    P.dma("sp", sbm[:, :, :], sbm_d, writes=[cbuf])
    P.dma("sp", dwm[:, :, :], dwm_d, writes=[cbuf])
    P.dma("sp", tri[:, :], tri_d, writes=[cbuf])
    P.op("pool", lambda e: e.memset(ones[:, :], 1.0), writes=[cbuf])
    qs = c.sb([128, seq], BF16, "qs")
    kraw = c.sb([128, seq], BF16, "kraw")
    ks = c.sb([128, seq], BF16, "ks")
    nks = c.sb([128, seq], BF16, "nks")
    vs = c.sb([128, NB, 128], BF16, "vs")
    os_ = c.sb([128, seq], BF16, "os")
    qbuf, krbuf, kbuf, vbuf = Buf("q"), Buf("kraw"), Buf("k"), Buf("v")
    obufs = [Buf(f"o{g}") for g in range(NG)]
    mk = lambda dt, nm: [(c.sb([128, QG], dt, nm), Buf(f"{nm}{i}")) for i in range(NSB)]
    et, spt, spm, tmp, wt, wm = mk(F32, "et"), mk(BF16, "spt"), mk(BF16, "spm"), mk(F32, "tmp"), mk(BF16, "wt"), mk(BF16, "wm")
    Cs = [(c.sb([128, QG], F32, "Csb"), Buf(f"C{i}")) for i in range(2)]
    rec = c.sb([128, QG], F32, "rec")
    recbuf = Buf("rec")
    it = 0
    for u, kind in enumerate(units):
        P.dma("sp", qs[:, :], qT[u], writes=[qbuf])
        P.dma("sp", kraw[:, :], kT[u], writes=[krbuf])
        P.dma("sp", vs[:, :, :], vv[u], writes=[vbuf])
        P.op("act", lambda e: e.mul(ks[:, :], kraw[:, :], scale), reads=[krbuf], writes=[kbuf])
        if kind == 0:
            P.op("pool", lambda e: e.tensor_scalar(out=nks[:, :], in0=ks[:, :], scalar1=-1.0, scalar2=None, op0=ALU.mult),
                 reads=[kbuf], writes=[kbuf])
        work = []
        for g in range(NG):
            q0 = g * QG
            qsl = qs[:, q0:q0 + QG]
            o_ps, o_pb = ob[g % 2]
            Csb, Cbuf = Cs[g % 2]
            if kind == 0:
                jlist = list(range(BPG * g + BPG - 1, -1, -1))
                for n, j in enumerate(jlist):
                    it += 1
                    bp_ = it % NBUF
                    b_ = it % NSB
                    r = j - BPG * g
                    z_ps, z_pb = zb[bp_]
                    c_ps, c_pb = cb[bp_]
                    s_ps, s_pb = sb_[bp_]
                    e_t, e_b = et[b_]
                    sp_t, sp_b = spt[b_]
                    sm_t, sm_b = spm[b_] if r >= 0 else (sp_t, sp_b)
                    w_t, w_b = wt[b_]
                    t_t, t_b = tmp[b_]
                    wm_t, wm_b = wm[b_] if r >= 0 else (w_t, w_b)
                    last = (n == len(jlist) - 1)

                    def stA(j=j, r=r, qsl=qsl, z_ps=z_ps, z_pb=z_pb, e_t=e_t, e_b=e_b, sp_t=sp_t, sp_b=sp_b, sm_t=sm_t, sm_b=sm_b):
                        P.op("pe", lambda e: e.matmul(z_ps, ks[:, j * 128:(j + 1) * 128], qsl, start=True, stop=True), reads=[kbuf, qbuf], writes=[z_pb])
                        P.op("act", lambda e: e.activation(out=e_t[:, :], in_=z_ps, func=AF.Exp), reads=[z_pb], writes=[e_b])
                        P.op("act", lambda e: e.activation(out=sp_t[:, :], in_=e_t[:, :], func=AF.Ln, bias=1.0), reads=[e_b], writes=[sp_b])
                        if r >= 0:
                            P.op("pool", lambda e: e.tensor_tensor(out=sm_t[:, :], in0=sp_t[:, :], in1=sbm[:, r, :], op=ALU.mult),
                                 reads=[sp_b, cbuf], writes=[sm_b])

                    def stB(j=j, n=n, last=last, qsl=qsl, c_ps=c_ps, c_pb=c_pb, s_ps=s_ps, s_pb=s_pb, sm_t=sm_t, sm_b=sm_b, t_t=t_t, t_b=t_b,
                            Csb=Csb, Cbuf=Cbuf):
                        P.op("pe", lambda e: e.matmul(c_ps, tri[:, :], sm_t[:, :], start=True, stop=False), reads=[cbuf, sm_b], writes=[c_pb])
                        P.op("pe", lambda e: e.matmul(c_ps, nks[:, j * 128:(j + 1) * 128], qsl, start=False, stop=True),
                             reads=[kbuf, qbuf], writes=[c_pb])
                        if not last:
                            P.op("pe", lambda e: e.matmul(s_ps, ones[:, :], sm_t[:, :], start=True, stop=True), reads=[cbuf, sm_b], writes=[s_pb])
                        if n == 0:
                            if not last:
                                P.op("dve", lambda e: e.tensor_copy(Csb[:, :], s_ps), reads=[s_pb], writes=[Cbuf])
                        else:
                            P.op("dve", lambda e: e.tensor_tensor(out=t_t[:, :], in0=c_ps, in1=Csb[:, :], op=ALU.add),
                                 reads=[c_pb, Cbuf], writes=[t_b])
                            if not last:
                                P.op("dve", lambda e: e.tensor_tensor(out=Csb[:, :], in0=s_ps, in1=Csb[:, :], op=ALU.add),
                                     reads=[s_pb, Cbuf], writes=[Cbuf])

                    def stC(j=j, n=n, r=r, last=last, g=g, q0=q0, c_ps=c_ps, c_pb=c_pb, t_t=t_t, t_b=t_b, w_t=w_t, w_b=w_b, wm_t=wm_t, wm_b=wm_b,
                            o_ps=o_ps, o_pb=o_pb):
                        if n == 0:
                            P.op("act", lambda e: e.activation(out=w_t[:, :], in_=c_ps, func=AF.Exp, scale=-1.0), reads=[c_pb], writes=[w_b])
                        else:
                            P.op("act", lambda e: e.activation(out=w_t[:, :], in_=t_t[:, :], func=AF.Exp, scale=-1.0), reads=[t_b], writes=[w_b])
                        if r >= 0:
                            P.op("pool", lambda e: e.tensor_tensor(out=wm_t[:, :], in0=w_t[:, :], in1=sbm[:, r, :], op=ALU.mult),
                                 reads=[w_b, cbuf], writes=[wm_b])
                        P.op("pe", lambda e: e.matmul(o_ps, vs[:, j, :], wm_t[:, :], start=(n == 0), stop=last), reads=[vbuf, wm_b], writes=[o_pb])
                        if last:
                            P.op("act", lambda e: e.copy(os_[:, q0:q0 + QG], o_ps), reads=[o_pb], writes=[obufs[g]])

                    work.append((stA, stB, stC))
            else:
                jlist = list(range(max(0, BPG * g - 16), BPG * g + BPG))
                d_ps, d_pb = cb[g % NBUF]
                for n, j in enumerate(jlist):
                    it += 1
                    b_ = it % NSB
                    r = j - BPG * g
                    z_ps, z_pb = zb[it % NBUF]
                    w_t, w_b = wt[b_]
                    wm_t, wm_b = wm[b_]
                    first, last = (n == 0), (n == len(jlist) - 1)

                    def stA(j=j, r=r, qsl=qsl, z_ps=z_ps, z_pb=z_pb, w_t=w_t, w_b=w_b):
                        P.op("pe", lambda e: e.matmul(z_ps, ks[:, j * 128:(j + 1) * 128], qsl, start=True, stop=True),
                             reads=[kbuf, qbuf], writes=[z_pb])
                        P.op("act", lambda e: e.activation(out=w_t[:, :], in_=z_ps, func=AF.Exp), reads=[z_pb], writes=[w_b])

                    def stB(r=r, w_t=w_t, w_b=w_b, wm_t=wm_t, wm_b=wm_b):
                        P.op("dve", lambda e: e.tensor_tensor(out=wm_t[:, :], in0=w_t[:, :], in1=dwm[:, r + 16, :], op=ALU.mult),
                             reads=[w_b, cbuf], writes=[wm_b])

                    def stC(j=j, first=first, last=last, g=g, q0=q0, wm_t=wm_t, wm_b=wm_b, o_ps=o_ps, o_pb=o_pb, d_ps=d_ps, d_pb=d_pb):
                        P.op("pe", lambda e: e.matmul(o_ps, vs[:, j, :], wm_t[:, :], start=first, stop=last), reads=[vbuf, wm_b], writes=[o_pb])
                        P.op("pe", lambda e: e.matmul(d_ps, ones[:, :], wm_t[:, :], start=first, stop=last), reads=[cbuf, wm_b], writes=[d_pb])
                        if last:
                            P.op("dve", lambda e: e.reciprocal(rec[:, :], d_ps), reads=[d_pb], writes=[recbuf])
                            P.op("dve", lambda e: e.tensor_tensor(out=os_[:, q0:q0 + QG], in0=o_ps, in1=rec[:, :], op=ALU.mult),
                                 reads=[o_pb, recbuf], writes=[obufs[g]])

                    work.append((stA, stB, stC))
        N_ = len(work)
        for t in range(N_ + 2):
            if t < N_:
                work[t][0]()
            if 0 <= t - 1 < N_:
                work[t - 1][1]()
            if 0 <= t - 2 < N_:
                work[t - 2][2]()
        P.dma("sp", oT[u], os_[:, :], reads=obufs)
    c.finish(obufs)
    return nc


TWO_PI = 2.0 * np.pi


def s5_consts(L=64):
    return {"iota": np.tile(np.arange(L, dtype=np.float32), (128, 1))}


def build_s5(seq=S, npairs=8, L=64, env=None):
    c = env or Ctx()
    nc, P = c.nc, c.P
    NCH = seq // L
    NH = max(1, seq // 4096)
    HL = seq // NH
    NCHh = HL // L
    TPB = max(1, min(L, 512 // NCHh))
    uT = c.din("uT", [npairs, 32, seq], BF16)
    lam = c.din("lam", [npairs, 128, 3])
    Bm = c.din("Bm", [npairs, 128, 32])
    Cm = c.din("Cm", [npairs, 128, 32])
    Dk = c.din("Dk", [npairs, 32, 1])
    iota_d = c.din("iota", [128, L])
    yT = c.dout("yT", [npairs, 32, seq])
    banks = c.psum_banks()
    bi = [0]

    def bank():
        b = banks[bi[0] % 8]
        bi[0] += 1
        return b

    iota = c.sb([128, L], F32, "iota")
    iota1 = c.sb([128, L], F32, "iota1")
    ident = c.sb([128, 128], F32, "ident")
    cb = Buf("const")
    P.dma("sp", iota[:, :], iota_d, writes=[cb])
    P.op("pool", lambda e: e.memset(ident[:, :], 0.0), writes=[cb])
    P.op("pool", lambda e: e.affine_select(out=ident[:, :], in_=ident[:, :], pattern=[[-1, 128]], compare_op=ALU.not_equal,
                                           fill=1.0, base=0, channel_multiplier=1), reads=[cb], writes=[cb])
    P.op("dve", lambda e: e.tensor_scalar(out=iota1[:, :], in0=iota[:, :], scalar1=1.0, scalar2=None, op0=ALU.add), reads=[cb], writes=[cb])
    sm = c.sb([128, 64], F32, "sm")
    bm = c.sb([128, 32], F32, "bm")
    cm = c.sb([128, 32], F32, "cm")
    kbm = c.sb([128, 32], F32, "kbm")
    dk = c.sb([32, 1], F32, "dk")
    pre = Buf("pre")
    rr_f = c.sb([128, max(L, NCH)], F32, "rr_f")
    rr_i = c.sb([128, max(L, NCH)], I32, "rr_i")
    rr_g = c.sb([128, max(L, NCH)], F32, "rr_g")
    sint = c.sb([128, L], F32, "sint")
    cost = c.sb([128, L], F32, "cost")
    rpt = c.sb([128, L], F32, "rpt")
    G = [c.sb([128, L, 32], F32, "G") for _ in range(2)]
    Gb = Buf("G")
    Bt = [c.sb([32, L, 128], BF16, "Bt") for _ in range(2)]
    Btb = [Buf("Bt0"), Buf("Bt1")]
    Ct = [c.sb([128, L, 32], BF16, "Ct") for _ in range(4)]
    Ctb = Buf("Ct")
    pt = [c.sb([128, L, 16], F32, "pt") for _ in range(3)]
    for t_ in G:
        P.op("pool", lambda e: e.memset(t_[:, :, :], 0.0), writes=[Gb])
    for t_ in Ct:
        P.op("pool", lambda e: e.memset(t_[:, :, :], 0.0), writes=[Ctb])
    ub = c.sb([32, seq], BF16, "ub")
    ubuf = Buf("u")
    cc = [c.sb([128, HL], F32, "cc") for _ in range(2)]
    ccb = [Buf("cre"), Buf("cim")]
    at = c.sb([128, HL], F32, "at")
    atb = Buf("a")
    wb = [c.sb([128, seq], BF16, "wb") for _ in range(2)]
    wbb = [Buf("wre"), Buf("wim")]
    ysb = c.sb([32, HL], F32, "ysb")
    ybuf = Buf("y")
    ff = [c.sb([128, NCH], F32, "ff") for _ in range(2)]
    EE = [c.sb([128, NCH], F32, "EE") for _ in range(2)]
    gg = [c.sb([128, NCH], F32, "gg") for _ in range(2)]
    Rb = c.sb([128, NCH], F32, "Rb")
    Zb = [c.sb([128, NCH], BF16, "Zb") for _ in range(2)]
    chb = Buf("chunk")
    Zbb = Buf("Z")

    def V(fn, rd=(), wr=()):
        return P.op("dve", fn, reads=[pre] + list(rd), writes=[pre] + list(wr))

    def A(fn, rd=(), wr=()):
        return P.op("act", fn, reads=[pre] + list(rd), writes=[pre] + list(wr))

    def col(i):
        return sm[:, i:i + 1]

    def sincos(arg, n, s_out, c_out):
        for shift, out in ((0.0, s_out), (0.5 * np.pi, c_out)):
            f, i_, g = rr_f[:, :n], rr_i[:, :n], rr_g[:, :n]
            V(lambda e: e.tensor_scalar(out=g, in0=arg, scalar1=shift, scalar2=None, op0=ALU.add))
            V(lambda e: e.tensor_scalar(out=i_, in0=g, scalar1=1.0 / TWO_PI, scalar2=None, op0=ALU.mult))
            V(lambda e: e.tensor_copy(f, i_))
            V(lambda e: e.scalar_tensor_tensor(out=g, in0=f, scalar=-TWO_PI, in1=g, op0=ALU.mult, op1=ALU.add))
            V(lambda e: e.tensor_scalar(out=f, in0=g, scalar1=float(np.pi), scalar2=None, op0=ALU.is_gt))
            V(lambda e: e.scalar_tensor_tensor(out=g, in0=f, scalar=-TWO_PI, in1=g, op0=ALU.mult, op1=ALU.add))
            V(lambda e: e.tensor_scalar(out=f, in0=g, scalar1=-float(np.pi), scalar2=None, op0=ALU.is_lt))
            V(lambda e: e.scalar_tensor_tensor(out=g, in0=f, scalar=TWO_PI, in1=g, op0=ALU.mult, op1=ALU.add))
            A(lambda e: e.activation(out=out, in_=g, func=AF.Sin))

    for pr in range(npairs):
        P.dma("sp", sm[:, 0:3], lam[pr], writes=[pre])
        P.dma("sp", bm[:, :], Bm[pr], writes=[pre])
        P.dma("sp", cm[:, :], Cm[pr], writes=[pre])
        P.dma("sp", dk[:, :], Dk[pr], writes=[pre])
        P.dma("sp", ub[:, :], uT[pr], writes=[ubuf])
        lre, lim, ldt, dt_, rho, th, r_, sth, cth, nre, nim, l2, inv, kre, kim, t1, t2, phi, sph, cph, RL, nkim, sre, sim_, nsim = [
            col(i) for i in range(25)]
        A(lambda e: e.activation(out=dt_, in_=ldt, func=AF.Exp))
        V(lambda e: e.tensor_tensor(out=rho, in0=lre, in1=dt_, op=ALU.mult))
        V(lambda e: e.tensor_tensor(out=th, in0=lim, in1=dt_, op=ALU.mult))
        A(lambda e: e.activation(out=r_, in_=rho, func=AF.Exp))
        sincos(th, 1, sth, cth)
        V(lambda e: e.tensor_tensor(out=nre, in0=r_, in1=cth, op=ALU.mult))
        V(lambda e: e.tensor_scalar(out=nre, in0=nre, scalar1=-1.0, scalar2=None, op0=ALU.add))
        V(lambda e: e.tensor_tensor(out=nim, in0=r_, in1=sth, op=ALU.mult))
        V(lambda e: e.tensor_tensor(out=l2, in0=lre, in1=lre, op=ALU.mult))
        V(lambda e: e.scalar_tensor_tensor(out=l2, in0=lim, scalar=lim, in1=l2, op0=ALU.mult, op1=ALU.add))
        V(lambda e: e.reciprocal(inv, l2))
        V(lambda e: e.tensor_tensor(out=t1, in0=nre, in1=lre, op=ALU.mult))
        V(lambda e: e.scalar_tensor_tensor(out=t1, in0=nim, scalar=lim, in1=t1, op0=ALU.mult, op1=ALU.add))
        V(lambda e: e.tensor_tensor(out=kre, in0=t1, in1=inv, op=ALU.mult))
        V(lambda e: e.tensor_tensor(out=t1, in0=nim, in1=lre, op=ALU.mult))
        V(lambda e: e.tensor_tensor(out=t2, in0=nre, in1=lim, op=ALU.mult))
        V(lambda e: e.tensor_tensor(out=t1, in0=t1, in1=t2, op=ALU.subtract))
        V(lambda e: e.tensor_tensor(out=kim, in0=t1, in1=inv, op=ALU.mult))
        V(lambda e: e.tensor_scalar(out=nkim, in0=kim, scalar1=-1.0, scalar2=None, op0=ALU.mult))
        V(lambda e: e.tensor_scalar(out=kbm[:, 0:16], in0=bm[:, 0:16], scalar1=kre, scalar2=None, op0=ALU.mult))
        V(lambda e: e.scalar_tensor_tensor(out=kbm[:, 0:16], in0=bm[:, 16:32], scalar=nkim, in1=kbm[:, 0:16], op0=ALU.mult, op1=ALU.add))
        V(lambda e: e.tensor_scalar(out=kbm[:, 16:32], in0=bm[:, 16:32], scalar1=kre, scalar2=None, op0=ALU.mult))
        V(lambda e: e.scalar_tensor_tensor(out=kbm[:, 16:32], in0=bm[:, 0:16], scalar=kim, in1=kbm[:, 16:32], op0=ALU.mult, op1=ALU.add))
        V(lambda e: e.tensor_scalar(out=rpt[:, :], in0=iota[:, :], scalar1=th, scalar2=None, op0=ALU.mult), rd=[cb])
        sincos(rpt[:, :], L, sint[:, :], cost[:, :])
        A(lambda e: e.activation(out=rpt[:, :], in_=iota1[:, :], func=AF.Exp, scale=rho), rd=[cb])
        V(lambda e: e.tensor_scalar(out=phi, in0=th, scalar1=float(L), scalar2=None, op0=ALU.mult))
        sincos(phi, 1, sph, cph)
        V(lambda e: e.tensor_scalar(out=t1, in0=rho, scalar1=float(L), scalar2=None, op0=ALU.mult))
        A(lambda e: e.activation(out=RL, in_=t1, func=AF.Exp))
        for g2 in range(2):
            rows = slice(g2 * 64, g2 * 64 + 64)
            cols = slice(g2 * 16, g2 * 16 + 16)
            cs3 = cost[rows, :].unsqueeze(2).broadcast_to([64, L, 16])
            sn3 = sint[rows, :].unsqueeze(2).broadcast_to([64, L, 16])
            rp3 = rpt[rows, :].unsqueeze(2).broadcast_to([64, L, 16])
            kbre = kbm[rows, 0:16].unsqueeze(1).broadcast_to([64, L, 16])
            kbim = kbm[rows, 16:32].unsqueeze(1).broadcast_to([64, L, 16])
            cre3 = cm[rows, 0:16].unsqueeze(1).broadcast_to([64, L, 16])
            cim3 = cm[rows, 16:32].unsqueeze(1).broadcast_to([64, L, 16])
            p0, p1, p2 = [t_[rows, :, :] for t_ in pt]
            TT = lambda out, a, b, op, wr=(): V(lambda e: e.tensor_tensor(out=out, in0=a, in1=b, op=op), wr=wr)
            TT(p0, cs3, kbre, ALU.mult)
            TT(p1, sn3, kbim, ALU.mult)
            TT(G[0][rows, :, cols], p0, p1, ALU.add, wr=[Gb])
            TT(p0, cs3, kbim, ALU.mult)
            TT(p1, sn3, kbre, ALU.mult)
            TT(G[1][rows, :, cols], p0, p1, ALU.subtract, wr=[Gb])
            TT(p0, cs3, cre3, ALU.mult)
            TT(p1, sn3, cim3, ALU.mult)
            TT(p2, p0, p1, ALU.subtract)
            V(lambda e: e.tensor_copy(Ct[0][rows, :, cols], p2), wr=[Ctb])
            TT(Ct[2][rows, :, cols], p2, rp3, ALU.mult, wr=[Ctb])
            TT(p0, sn3, cre3, ALU.mult)
            TT(p1, cs3, cim3, ALU.mult)
            V(lambda e: e.scalar_tensor_tensor(out=p2, in0=p0, scalar=-1.0, in1=p1, op0=ALU.mult, op1=ALU.subtract))
            V(lambda e: e.tensor_copy(Ct[1][rows, :, cols], p2), wr=[Ctb])
            TT(Ct[3][rows, :, cols], p2, rp3, ALU.mult, wr=[Ctb])
        for ri in range(2):
            for q in range(L // 4):
                ps, pb = bank()
                for j in range(4):
                    P.op("pe", lambda e: e.transpose(ps[0:32, j * 128:(j + 1) * 128], G[ri][:, q * 4 + j, :], ident[:, :]),
                         reads=[Gb, cb, pre], writes=[pb])
                eng = "act" if (q % 2 == 0) else "dve"
                fn = (lambda e: e.copy(Bt[ri][:, q * 4:(q + 1) * 4, :], ps[0:32, :].rearrange("p (j m) -> p j m", j=4))) if eng == "act" else \
                     (lambda e: e.tensor_copy(Bt[ri][:, q * 4:(q + 1) * 4, :], ps[0:32, :].rearrange("p (j m) -> p j m", j=4)))
                P.op(eng, fn, reads=[pb], writes=[Btb[ri]])
        V(lambda e: e.memset(EE[0][:, 0:1], 1.0), wr=[chb])
        V(lambda e: e.memset(EE[1][:, 0:1], 0.0), wr=[chb])
        V(lambda e: e.tensor_copy(sre, cph))
        V(lambda e: e.tensor_copy(sim_, sph))
        n_ = 1
        while n_ < NCH:
            m_ = min(n_, NCH - n_)
            V(lambda e: e.tensor_scalar(out=nsim, in0=sim_, scalar1=-1.0, scalar2=None, op0=ALU.mult))
            V(lambda e: e.tensor_scalar(out=EE[0][:, n_:n_ + m_], in0=EE[0][:, 0:m_], scalar1=sre, scalar2=None, op0=ALU.mult), rd=[chb], wr=[chb])
            V(lambda e: e.scalar_tensor_tensor(out=EE[0][:, n_:n_ + m_], in0=EE[1][:, 0:m_], scalar=nsim, in1=EE[0][:, n_:n_ + m_],
                                               op0=ALU.mult, op1=ALU.add), rd=[chb], wr=[chb])
            V(lambda e: e.tensor_scalar(out=EE[1][:, n_:n_ + m_], in0=EE[0][:, 0:m_], scalar1=sim_, scalar2=None, op0=ALU.mult), rd=[chb], wr=[chb])
            V(lambda e: e.scalar_tensor_tensor(out=EE[1][:, n_:n_ + m_], in0=EE[1][:, 0:m_], scalar=sre, in1=EE[1][:, n_:n_ + m_],
                                               op0=ALU.mult, op1=ALU.add), rd=[chb], wr=[chb])
            n_ *= 2
            if n_ < NCH:
                V(lambda e: e.tensor_tensor(out=t1, in0=sre, in1=sre, op=ALU.mult))
                V(lambda e: e.scalar_tensor_tensor(out=t1, in0=sim_, scalar=nsim, in1=t1, op0=ALU.mult, op1=ALU.add))
                V(lambda e: e.tensor_tensor(out=t2, in0=sre, in1=sim_, op=ALU.mult))
                V(lambda e: e.tensor_scalar(out=sim_, in0=t2, scalar1=2.0, scalar2=None, op0=ALU.mult))
                V(lambda e: e.tensor_copy(sre, t1))
        a3 = at[:, :].rearrange("p (n t) -> p n t", t=L)
        V(lambda e: e.tensor_scalar(out=a3, in0=iota[:, :].unsqueeze(1).broadcast_to([128, NCHh, L]), scalar1=0.0, scalar2=r_,
                                    op0=ALU.mult, op1=ALU.add), rd=[cb], wr=[atb])
        V(lambda e: e.memset(at[:, 0::L], 0.0), rd=[atb], wr=[atb])
        V(lambda e: e.tensor_scalar(out=Rb[:, :], in0=EE[0][:, :], scalar1=0.0, scalar2=RL, op0=ALU.mult, op1=ALU.add), rd=[chb], wr=[chb])
        for hf in range(NH):
            off = hf * HL
            for tb in range(L // TPB):
                for ri in range(2):
                    ps, pb = bank()
                    for j in range(TPB):
                        tau = tb * TPB + j
                        P.op("pe", lambda e: e.matmul(ps[:, j * NCHh:(j + 1) * NCHh], Bt[ri][:, tau, :], ub[:, off + tau:off + HL:L],
                                                      start=True, stop=True), reads=[Btb[ri], ubuf], writes=[pb])
                    outv = cc[ri][:, :].rearrange("p (n t) -> p t n", t=L)[:, tb * TPB:(tb + 1) * TPB, :]
                    inv_ = ps[:, :TPB * NCHh].rearrange("p (t n) -> p t n", n=NCHh)
                    if ri == 0:
                        P.op("act", lambda e: e.copy(outv, inv_), reads=[pb], writes=[ccb[ri]])
                    else:
                        P.op("dve", lambda e: e.tensor_copy(outv, inv_), reads=[pb], writes=[ccb[ri]])
            for ri in range(2):
                P.op("dve", lambda e: e.tensor_tensor_scan(out=cc[ri][:, :], data0=at[:, :], data1=cc[ri][:, :], initial=0.0,
                                                           op0=ALU.mult, op1=ALU.add), reads=[atb, ccb[ri]], writes=[ccb[ri]])
                P.op("act", lambda e: e.copy(wb[ri][:, off:off + HL], cc[ri][:, :]), reads=[ccb[ri]], writes=[wbb[ri]])
                P.op("dve", lambda e: e.tensor_copy(ff[ri][:, hf * NCHh:(hf + 1) * NCHh], cc[ri][:, L - 1::L]),
                     reads=[ccb[ri]], writes=[chb])
        C2 = lambda fn: P.op("dve", fn, reads=[chb, pre], writes=[chb])
        C2(lambda e: e.tensor_tensor(out=gg[0][:, :], in0=EE[0][:, :], in1=ff[0][:, :], op=ALU.mult))
        C2(lambda e: e.tensor_tensor(out=rr_f[:, :NCH], in0=EE[1][:, :], in1=ff[1][:, :], op=ALU.mult))
        C2(lambda e: e.tensor_tensor(out=gg[0][:, :], in0=gg[0][:, :], in1=rr_f[:, :NCH], op=ALU.add))
        C2(lambda e: e.tensor_tensor(out=gg[1][:, :], in0=EE[0][:, :], in1=ff[1][:, :], op=ALU.mult))
        C2(lambda e: e.tensor_tensor(out=rr_f[:, :NCH], in0=EE[1][:, :], in1=ff[0][:, :], op=ALU.mult))
        C2(lambda e: e.tensor_tensor(out=gg[1][:, :], in0=gg[1][:, :], in1=rr_f[:, :NCH], op=ALU.subtract))
        for ri in range(2):
            C2(lambda e: e.tensor_tensor_scan(out=gg[ri][:, :], data0=Rb[:, :], data1=gg[ri][:, :], initial=0.0, op0=ALU.mult, op1=ALU.add))
        C2(lambda e: e.tensor_tensor(out=ff[0][:, :], in0=EE[0][:, :], in1=gg[0][:, :], op=ALU.mult))
        C2(lambda e: e.tensor_tensor(out=rr_f[:, :NCH], in0=EE[1][:, :], in1=gg[1][:, :], op=ALU.mult))
        C2(lambda e: e.tensor_tensor(out=ff[0][:, :], in0=ff[0][:, :], in1=rr_f[:, :NCH], op=ALU.subtract))
        C2(lambda e: e.tensor_tensor(out=ff[1][:, :], in0=EE[0][:, :], in1=gg[1][:, :], op=ALU.mult))
        C2(lambda e: e.tensor_tensor(out=rr_f[:, :NCH], in0=EE[1][:, :], in1=gg[0][:, :], op=ALU.mult))
        C2(lambda e: e.tensor_tensor(out=ff[1][:, :], in0=ff[1][:, :], in1=rr_f[:, :NCH], op=ALU.add))
        V(lambda e: e.tensor_scalar(out=t1, in0=sph, scalar1=-1.0, scalar2=None, op0=ALU.mult))
        ZW = lambda fn: P.op("dve", fn, reads=[chb, pre], writes=[Zbb, chb])
        ZW(lambda e: e.memset(Zb[0][:, 0:1], 0.0))
        ZW(lambda e: e.memset(Zb[1][:, 0:1], 0.0))
        if NCH > 1:
            ZW(lambda e: e.tensor_scalar(out=rr_f[:, :NCH - 1], in0=ff[0][:, :NCH - 1], scalar1=cph, scalar2=None, op0=ALU.mult))
            ZW(lambda e: e.scalar_tensor_tensor(out=Zb[0][:, 1:], in0=ff[1][:, :NCH - 1], scalar=t1, in1=rr_f[:, :NCH - 1], op0=ALU.mult, op1=ALU.add))
            ZW(lambda e: e.tensor_scalar(out=rr_f[:, :NCH - 1], in0=ff[0][:, :NCH - 1], scalar1=sph, scalar2=None, op0=ALU.mult))
            ZW(lambda e: e.scalar_tensor_tensor(out=Zb[1][:, 1:], in0=ff[1][:, :NCH - 1], scalar=cph, in1=rr_f[:, :NCH - 1], op0=ALU.mult, op1=ALU.add))
        for hf in range(NH):
            off = hf * HL
            for tb in range(L // TPB):
                ps, pb = bank()
                for j in range(TPB):
                    tau = tb * TPB + j
                    o_ = ps[0:32, j * NCHh:(j + 1) * NCHh]
                    P.op("pe", lambda e: e.matmul(o_, Ct[0][:, tau, :], wb[0][:, off + tau:off + HL:L], start=True, stop=False),
                         reads=[Ctb, wbb[0]], writes=[pb])
                    P.op("pe", lambda e: e.matmul(o_, Ct[1][:, tau, :], wb[1][:, off + tau:off + HL:L], start=False, stop=False),
                         reads=[Ctb, wbb[1]], writes=[pb])
                    P.op("pe", lambda e: e.matmul(o_, Ct[2][:, tau, :], Zb[0][:, hf * NCHh:(hf + 1) * NCHh], start=False, stop=False),
                         reads=[Ctb, Zbb], writes=[pb])
                    P.op("pe", lambda e: e.matmul(o_, Ct[3][:, tau, :], Zb[1][:, hf * NCHh:(hf + 1) * NCHh], start=False, stop=True),
                         reads=[Ctb, Zbb], writes=[pb])
                yv = ysb[:, :].rearrange("p (n t) -> p t n", t=L)[:, tb * TPB:(tb + 1) * TPB, :]
                uv = ub[:, off:off + HL].rearrange("p (n t) -> p t n", t=L)[:, tb * TPB:(tb + 1) * TPB, :]
                pv = ps[0:32, :TPB * NCHh].rearrange("p (t n) -> p t n", n=NCHh)
                P.op("dve", lambda e: e.scalar_tensor_tensor(out=yv, in0=uv, scalar=dk[:, 0:1], in1=pv, op0=ALU.mult, op1=ALU.add),
                     reads=[pb, ubuf, pre], writes=[ybuf])
            P.dma("sp", yT[pr, :, off:off + HL], ysb[:, :], reads=[ybuf])
    c.finish([ybuf])
    return nc


def gdn_consts():
    i = np.arange(64)[:, None]
    j = np.arange(64)[None, :]
    NEG = -30000.0
    mrow = np.ones((1, 512), np.float32)
    mrow[0, 0::64] = 0.0
    return {
        "gmask": np.stack([np.where(j < i, 0.0, NEG), np.where(j >= i, 0.0, NEG), (i <= j).astype(np.float32)], 1).astype(np.float32),
        "mrow": mrow,
    }


def build_gdn(seq=S, nheads=2, env=None, fm_out=False):
    c = env or Ctx()
    nc, P = c.nc, c.P
    NB = 8
    BT = NB * 64
    nbatch = seq // BT
    NCk = seq // 64
    qkv = c.din("qkv", [nheads, 3, 128, seq], BF16)
    gate = c.din("gate", [nheads, 64, NCk, 128], BF16)
    rows = c.din("rows", [nheads, 2, seq], BF16)
    cols = c.din("cols", [nheads, 64, 2, NCk], BF16)
    convw = c.din("convw", [nheads, 128, 12])
    hsc = c.din("hsc", [nheads, 2])
    normw = c.din("normw", [128])
    gmask_d = c.din("gmask", [64, 3, 64])
    mrow_d = c.din("mrow", [1, 512])
    if fm_out:
        oT = c.dout("oT", [nheads * 128, seq], BF16)
    else:
        oT = c.dout("o", [nheads, 64, NCk, 128], BF16)
    banks = c.psum_banks()
    bi = [0]

    def bank():
        b = banks[bi[0] % 8]
        bi[0] += 1
        return b

    cb = Buf("const")
    ident = c.sb([128, 128], F32, "ident")
    P.op("pool", lambda e: e.memset(ident[:, :], 0.0), writes=[cb])
    P.op("pool", lambda e: e.affine_select(out=ident[:, :], in_=ident[:, :], pattern=[[-1, 128]], compare_op=ALU.not_equal,
                                           fill=1.0, base=0, channel_multiplier=1), reads=[cb], writes=[cb])
    ones = c.sb([128, 128], F32, "ones")
    P.op("pool", lambda e: e.memset(ones[:, :], 1.0), writes=[cb])
    gmask = c.sb([64, 3, 64], F32, "gmask")
    mrow = c.sb([1, 512], F32, "mrow")
    nw = c.sb([64, 128], F32, "nw")
    P.dma("sp", gmask[:, :, :], gmask_d, writes=[cb])
    P.dma("sp", mrow[:, :], mrow_d, writes=[cb])
    P.dma("sp", nw[:, :], normw.partition_broadcast(64), writes=[cb])
    maskS = gmask[:, 0, :].unsqueeze(1).broadcast_to([64, NB, 64])
    maskU = gmask[:, 1, :].unsqueeze(1).broadcast_to([64, NB, 64])
    triI = gmask[:, 2, :]
    I3 = ident[0:64, 0:64].unsqueeze(1).broadcast_to([64, NB, 64])

    class H:
        pass

    hs = []
    for h in range(nheads):
        o = H()
        o.b = Buf(f"h{h}")
        o.cw = c.sb([128, 12], F32, "cw")
        o.sc = c.sb([128, 8], F32, "sc")
        o.xin = [c.sb([128, 3 + BT], BF16, "xin") for _ in range(3)]
        o.xb = [Buf(f"xin{h}_{i}") for i in range(3)]
        o.acc = [c.sb([128, BT], F32, "acc") for _ in range(3)]
        o.ab = [Buf(f"acc{h}_{i}") for i in range(3)]
        o.sq = c.sb([128, BT], F32, "sq")
        o.rs = c.sb([128, BT], F32, "rs")
        o.qT = c.sb([128, BT], BF16, "qT")
        o.kT = c.sb([128, BT], BF16, "kT")
        o.qdT = c.sb([128, BT], BF16, "qdT")
        o.qkb = Buf(f"qk{h}")
        o.kd_tm = c.sb([64, NB, 128], BF16, "kd_tm")
        o.kbg_tm = c.sb([64, NB, 128], BF16, "kbg_tm")
        o.vb_tm = c.sb([64, NB, 128], BF16, "vb_tm")
        o.tmb = Buf(f"tm{h}")
        o.gt = c.sb([64, NB, 128], BF16, "gt")
        o.sg = c.sb([64, NB, 128], F32, "sg")
        o.gtb = Buf(f"gt{h}")
        o.rowi = c.sb([1, BT], BF16, "rowi")
        o.rowf = c.sb([1, BT], F32, "rowf")
        o.gcrow = c.sb([1, BT], F32, "gcrow")
        o.rowb = Buf(f"row{h}")
        o.coli = c.sb([64, 2, NB], BF16, "coli")
        o.colf = c.sb([64, 16, NB], F32, "colf")
        o.colb = Buf(f"col{h}")
        o.gcbc = c.sb([128, BT], F32, "gcbc")
        o.egbc = c.sb([128, BT], F32, "egbc")
        o.gcb = Buf(f"gcbc{h}")
        o.dec = c.sb([64, NB, 64], F32, "dec")
        o.N = c.sb([64, NB, 64], F32, "N")
        o.NT = c.sb([64, NB, 64], F32, "NT")
        o.T = c.sb([64, NB, 64], F32, "T")
        o.TT = c.sb([64, NB, 64], F32, "TT")
        o.TTb = c.sb([64, NB, 64], BF16, "TTb")
        o.nb_ = Buf(f"N{h}")
        o.attT = c.sb([64, NB, 64], BF16, "attT")
        o.attb = Buf(f"att{h}")
        o.uval = c.sb([64, NB, 128], F32, "uval")
        o.ub = Buf(f"uval{h}")
        o.wkT = c.sb([128, NB, 64], BF16, "wkT")
        o.wkb = Buf(f"wk{h}")
        o.S = c.sb([128, 128], F32, "S")
        o.Sb = c.sb([128, 128], BF16, "Sb")
        o.Sbuf = Buf(f"S{h}")
        o.vnew = [c.sb([64, 128], BF16, "vnew") for _ in range(2)]
        o.vnb = [Buf(f"vn{h}_{i}") for i in range(2)]
        o.ss = c.sb([64, 4], F32, "ss")
        o.ssb = Buf(f"ss{h}")
        o.junk = c.sb([64, 128], F32, "junk")
        o.oout = c.sb([64, NB, 128], F32 if fm_out else BF16, "oout")
        o.ofm = c.sb([128, BT], BF16, "ofm")
        o.ofmb = Buf(f"ofm{h}")
        o.oob = Buf(f"oo{h}")
        hs.append(o)
        P.dma("sp", o.cw[:, :], convw[h], writes=[o.b])
        P.dma("sp", o.sc[:, 0:2], hsc[h].partition_broadcast(128), writes=[o.b])
        P.op("act", lambda e: e.activation(out=o.sc[:, 2:3], in_=o.sc[:, 0:1], func=AF.Exp), reads=[o.b], writes=[o.b])
        P.op("dve", lambda e: e.tensor_scalar(out=o.sc[:, 2:3], in0=o.sc[:, 2:3], scalar1=-1.0, scalar2=None, op0=ALU.mult), reads=[o.b], writes=[o.b])
        P.op("dve", lambda e: e.memset(o.S[:, :], 0.0), writes=[o.Sbuf])
        P.op("dve", lambda e: e.memset(o.Sb[:, :], 0.0), writes=[o.Sbuf])
    LNQ = -0.5 * float(np.log(128.0))

    def precompute(h, o, b):
        t0 = b * BT
        n0 = b * NB
        for i in range(3):
            if b == 0:
                P.op("dve", lambda e: e.memset(o.xin[i][:, 0:3], 0.0), writes=[o.xb[i]])
                P.dma("sp", o.xin[i][:, 3:], qkv[h, i, :, 0:BT], writes=[o.xb[i]])
            else:
                P.dma("sp", o.xin[i][:, :], qkv[h, i, :, t0 - 3:t0 + BT], writes=[o.xb[i]])
        P.dma("sp", o.gt[:, :, :], gate[h, :, n0:n0 + NB, :], writes=[o.gtb])
        P.dma("sp", o.rowi[:, :], rows[h, 0:1, t0:t0 + BT], writes=[o.rowb])
        for ci in range(2):
            P.dma("sp", o.coli[:, ci, :], cols[h, :, ci, n0:n0 + NB], writes=[o.colb], allow_slow_non_contiguous=True)
        for i in range(3):
            x, a = o.xin[i], o.acc[i]
            P.op("dve", lambda e: e.tensor_scalar(out=a[:, :], in0=x[:, 3:3 + BT], scalar1=o.cw[:, 4 * i + 3:4 * i + 4], scalar2=None, op0=ALU.mult),
                 reads=[o.xb[i], o.b], writes=[o.ab[i]])
            for tap in range(3):
                P.op("dve", lambda e: e.scalar_tensor_tensor(out=a[:, :], in0=x[:, tap:tap + BT], scalar=o.cw[:, 4 * i + tap:4 * i + tap + 1],
                                                             in1=a[:, :], op0=ALU.mult, op1=ALU.add), reads=[o.xb[i], o.ab[i], o.b], writes=[o.ab[i]])
            P.op("act", lambda e: e.activation(out=a[:, :], in_=a[:, :], func=AF.Silu), reads=[o.ab[i]], writes=[o.ab[i]])
        for i, dst, lnb in ((0, o.qT, LNQ), (1, o.kT, 0.0)):
            a = o.acc[i]
            P.op("act", lambda e: e.activation(out=o.sq[:, :], in_=a[:, :], func=AF.Square), reads=[o.ab[i]], writes=[o.b])
            ps, pb = bank()
            P.op("pe", lambda e: e.matmul(ps[:, :], ones[:, :], o.sq[:, :], start=True, stop=True), reads=[cb, o.b], writes=[pb])
            P.op("act", lambda e: e.activation(out=o.rs[:, :], in_=ps[:, :], func=AF.Ln, bias=RMS_EPS), reads=[pb], writes=[o.b])
            P.op("act", lambda e: e.activation(out=o.rs[:, :], in_=o.rs[:, :], func=AF.Exp, scale=-0.5, bias=lnb), reads=[o.b], writes=[o.b])
            P.op("dve", lambda e: e.tensor_tensor(out=a[:, :], in0=a[:, :], in1=o.rs[:, :], op=ALU.mult), reads=[o.ab[i], o.b], writes=[o.ab[i]])
            P.op("act", lambda e: e.copy(dst[:, :], a[:, :]), reads=[o.ab[i]], writes=[o.qkb])
        P.op("act", lambda e: e.activation(out=o.sg[:, :, :], in_=o.gt[:, :, :], func=AF.Silu), reads=[o.gtb], writes=[o.gtb])
        P.op("pool", lambda e: e.tensor_tensor(out=o.sg[:, :, :], in0=o.sg[:, :, :], in1=nw[:, :].unsqueeze(1).broadcast_to([64, NB, 128]), op=ALU.mult),
             reads=[o.gtb, cb], writes=[o.gtb])
        R_ = lambda eng, fn: P.op(eng, fn, reads=[o.rowb, o.b, cb], writes=[o.rowb])
        R_("act", lambda e: e.activation(out=o.rowf[:, :], in_=o.rowi[:, :], func=AF.Exp, bias=o.sc[0:1, 1:2]))
        R_("act", lambda e: e.activation(out=o.rowf[:, :], in_=o.rowf[:, :], func=AF.Ln, bias=1.0))
        R_("dve", lambda e: e.tensor_scalar(out=o.rowf[:, :], in0=o.rowf[:, :], scalar1=o.sc[0:1, 2:3], scalar2=None, op0=ALU.mult))
        R_("dve", lambda e: e.tensor_tensor_scan(out=o.gcrow[:, :], data0=mrow[:, :], data1=o.rowf[:, :], initial=0.0, op0=ALU.mult, op1=ALU.add))
        ps, pb = bank()
        P.op("pe", lambda e: e.matmul(ps[:, :], ones[0:1, :], o.gcrow[:, :], start=True, stop=True), reads=[cb, o.rowb], writes=[pb])
        P.op("dve", lambda e: e.tensor_copy(o.gcbc[:, :], ps[:, :]), reads=[pb], writes=[o.gcb])
        P.op("act", lambda e: e.activation(out=o.egbc[:, :], in_=o.gcbc[:, :], func=AF.Exp), reads=[o.gcb], writes=[o.gcb])
        C_ = lambda eng, fn, rd=(): P.op(eng, fn, reads=[o.colb, o.b, cb] + list(rd), writes=[o.colb])
        cf = lambda k: o.colf[:, k, :]
        C_("act", lambda e: e.activation(out=cf(0), in_=o.coli[:, 0, :], func=AF.Exp, bias=o.sc[0:64, 1:2]))
        C_("act", lambda e: e.activation(out=cf(0), in_=cf(0), func=AF.Ln, bias=1.0))
        C_("dve", lambda e: e.tensor_scalar(out=cf(0), in0=cf(0), scalar1=o.sc[0:64, 2:3], scalar2=None, op0=ALU.mult))
        C_("act", lambda e: e.activation(out=cf(1), in_=o.coli[:, 1, :], func=AF.Sigmoid))
        ps, pb = bank()
        P.op("pe", lambda e: e.matmul(ps[0:64, 0:NB], triI, cf(0), start=True, stop=True), reads=[cb, o.colb], writes=[pb])
        C_("dve", lambda e: e.tensor_copy(cf(2), ps[0:64, 0:NB]), rd=[pb])
        C_("act", lambda e: e.activation(out=cf(3), in_=cf(2), func=AF.Exp))
        C_("dve", lambda e: e.tensor_tensor(out=cf(4), in0=cf(3), in1=cf(1), op=ALU.mult))
        C_("dve", lambda e: e.tensor_copy(cf(5), o.gcbc[0:64, 63::64]), rd=[o.gcb])
        C_("dve", lambda e: e.tensor_tensor(out=cf(6), in0=cf(5), in1=cf(2), op=ALU.subtract))
        C_("act", lambda e: e.activation(out=cf(6), in_=cf(6), func=AF.Exp))
        C_("dve", lambda e: e.tensor_scalar(out=cf(7), in0=cf(1), scalar1=-1.0, scalar2=None, op0=ALU.mult))
        P.op("dve", lambda e: e.tensor_tensor(out=o.qdT[:, :], in0=o.acc[0][:, :], in1=o.egbc[:, :], op=ALU.mult),
             reads=[o.ab[0], o.gcb], writes=[o.qkb])
        for src, outs in ((1, ((o.kd_tm, 6), (o.kbg_tm, 4))), (2, ((o.vb_tm, 1),))):
            for q in range(NB // 4):
                ps, pb = bank()
                for j in range(4):
                    ch = q * 4 + j
                    P.op("pe", lambda e: e.transpose(ps[0:64, j * 128:(j + 1) * 128], o.acc[src][:, ch * 64:(ch + 1) * 64], ident[:, :]),
                         reads=[o.ab[src], cb], writes=[pb])
                pv = ps[0:64, :].rearrange("p (j d) -> p j d", j=4)
                for dst, k in outs:
                    P.op("dve", lambda e: e.tensor_tensor(out=dst[:, q * 4:(q + 1) * 4, :], in0=pv,
                                                          in1=o.colf[:, k, q * 4:(q + 1) * 4].unsqueeze(2).broadcast_to([64, 4, 128]), op=ALU.mult),
                         reads=[pb, o.colb], writes=[o.tmb])
        gcj = o.gcbc[0:64, :].rearrange("p (n j) -> p n j", j=64)
        gci = o.colf[:, 2, :].unsqueeze(2).broadcast_to([64, NB, 64])
        nbe = o.colf[:, 7, :].unsqueeze(2).broadcast_to([64, NB, 64])
        D_ = lambda eng, fn, rd=(), wr=(): P.op(eng, fn, reads=[o.nb_, o.colb, o.gcb, cb] + list(rd), writes=[o.nb_] + list(wr))
        D_("dve", lambda e: e.tensor_tensor(out=o.dec[:, :, :], in0=gci, in1=gcj, op=ALU.subtract))
        D_("dve", lambda e: e.tensor_tensor(out=o.dec[:, :, :], in0=o.dec[:, :, :], in1=maskS, op=ALU.add))
        D_("act", lambda e: e.activation(out=o.dec[:, :, :], in_=o.dec[:, :, :], func=AF.Exp))
        D_("dve", lambda e: e.tensor_tensor(out=o.dec[:, :, :], in0=o.dec[:, :, :], in1=nbe, op=ALU.mult))
        ps, pb = bank()
        for ch in range(NB):
            ksl = o.kT[:, ch * 64:(ch + 1) * 64]
            P.op("pe", lambda e: e.matmul(ps[0:64, ch * 64:(ch + 1) * 64], ksl, ksl, start=True, stop=True), reads=[o.qkb], writes=[pb])
        pv = ps[0:64, :].rearrange("p (n j) -> p n j", j=64)
        D_("dve", lambda e: e.tensor_tensor(out=o.N[:, :, :], in0=o.dec[:, :, :], in1=pv, op=ALU.mult), rd=[pb])
        ps, pb = bank()
        for ch in range(NB):
            P.op("pe", lambda e: e.transpose(ps[0:64, ch * 64:(ch + 1) * 64], o.N[:, ch, :], ident[0:64, 0:64]), reads=[o.nb_, cb], writes=[pb])
        pv = ps[0:64, :].rearrange("p (n j) -> p n j", j=64)
        D_("act", lambda e: e.copy(o.NT[:, :, :], pv), rd=[pb])
        D_("dve", lambda e: e.tensor_tensor(out=o.T[:, :, :], in0=o.N[:, :, :], in1=I3, op=ALU.add))
        D_("dve", lambda e: e.tensor_tensor(out=o.TT[:, :, :], in0=o.NT[:, :, :], in1=I3, op=ALU.add))
        for lvl in range(5):
            psa, pba = bank()
            psb, pbb = bank()
            for ch in range(NB):
                sl = slice(ch * 64, (ch + 1) * 64)
                P.op("pe", lambda e: e.matmul(psa[0:64, sl], o.NT[:, ch, :], o.N[:, ch, :], start=True, stop=True), reads=[o.nb_], writes=[pba])
                P.op("pe", lambda e: e.matmul(psb[0:64, sl], o.N[:, ch, :], o.NT[:, ch, :], start=True, stop=True), reads=[o.nb_], writes=[pbb])
            D_("act", lambda e: e.copy(o.N[:, :, :], psa[0:64, :].rearrange("p (n j) -> p n j", j=64)), rd=[pba])
            D_("dve", lambda e: e.tensor_copy(o.NT[:, :, :], psb[0:64, :].rearrange("p (n j) -> p n j", j=64)), rd=[pbb])
            psa, pba = bank()
            psb, pbb = bank()
            for ch in range(NB):
                sl = slice(ch * 64, (ch + 1) * 64)
                P.op("pe", lambda e: e.matmul(psa[0:64, sl], o.TT[:, ch, :], o.N[:, ch, :], start=True, stop=True), reads=[o.nb_], writes=[pba])
                P.op("pe", lambda e: e.matmul(psb[0:64, sl], o.N[:, ch, :], o.TT[:, ch, :], start=True, stop=True), reads=[o.nb_], writes=[pbb])
            D_("dve", lambda e: e.tensor_tensor(out=o.T[:, :, :], in0=o.T[:, :, :], in1=psa[0:64, :].rearrange("p (n j) -> p n j", j=64), op=ALU.add), rd=[pba])
            D_("dve", lambda e: e.tensor_tensor(out=o.TT[:, :, :], in0=o.TT[:, :, :], in1=psb[0:64, :].rearrange("p (n j) -> p n j", j=64), op=ALU.add), rd=[pbb])
        D_("act", lambda e: e.copy(o.TTb[:, :, :], o.TT[:, :, :]))
        for q in range(NB // 4):
            ps, pb = bank()
            for j in range(4):
                ch = q * 4 + j
                P.op("pe", lambda e: e.matmul(ps[0:64, j * 128:(j + 1) * 128], o.TTb[:, ch, :], o.vb_tm[:, ch, :], start=True, stop=True),
                     reads=[o.nb_, o.tmb], writes=[pb])
            P.op("act", lambda e: e.copy(o.uval[:, q * 4:(q + 1) * 4, :], ps[0:64, :].rearrange("p (j d) -> p j d", j=4)), reads=[pb], writes=[o.ub])
        ps, pb = bank()
        for ch in range(NB):
            P.op("pe", lambda e: e.matmul(ps[:, ch * 64:(ch + 1) * 64], o.kbg_tm[:, ch, :], o.TTb[:, ch, :], start=True, stop=True),
                 reads=[o.nb_, o.tmb], writes=[pb])
        P.op("act", lambda e: e.copy(o.wkT[:, :, :], ps[:, :].rearrange("p (n i) -> p n i", i=64)), reads=[pb], writes=[o.wkb])
        gcjc = o.colf[:, 2, :].unsqueeze(2).broadcast_to([64, NB, 64])
        D_("dve", lambda e: e.tensor_tensor(out=o.dec[:, :, :], in0=gcj, in1=gcjc, op=ALU.subtract))
        D_("dve", lambda e: e.tensor_tensor(out=o.dec[:, :, :], in0=o.dec[:, :, :], in1=maskU, op=ALU.add))
        D_("act", lambda e: e.activation(out=o.dec[:, :, :], in_=o.dec[:, :, :], func=AF.Exp))
        ps, pb = bank()
        for ch in range(NB):
            sl = slice(ch * 64, (ch + 1) * 64)
            P.op("pe", lambda e: e.matmul(ps[0:64, sl], o.kT[:, sl], o.qT[:, sl], start=True, stop=True), reads=[o.qkb], writes=[pb])
        D_("dve", lambda e: e.tensor_tensor(out=o.attT[:, :, :], in0=o.dec[:, :, :], in1=ps[0:64, :].rearrange("p (n i) -> p n i", i=64), op=ALU.mult),
           rd=[pb], wr=[o.attb])

    def chunk_step(h, o, b, ch):
        n = b * NB + ch
        sl = slice(ch * 64, (ch + 1) * 64)
        vn, vnb = o.vnew[n % 2], o.vnb[n % 2]
        ps1, pb1 = bank()
        P.op("pe", lambda e: e.matmul(ps1[0:64, 0:128], o.wkT[:, ch, :], o.Sb[:, :], start=True, stop=True), reads=[o.wkb, o.Sbuf], writes=[pb1])
        P.op("dve", lambda e: e.tensor_tensor(out=vn[:, :], in0=o.uval[:, ch, :], in1=ps1[0:64, 0:128], op=ALU.subtract), reads=[o.ub, pb1], writes=[vnb])
        ps2, pb2 = bank()
        P.op("pe", lambda e: e.matmul(ps2[0:64, 0:128], o.qdT[:, sl], o.Sb[:, :], start=True, stop=False), reads=[o.qkb, o.Sbuf], writes=[pb2])
        P.op("pe", lambda e: e.matmul(ps2[0:64, 0:128], o.attT[:, ch, :], vn[:, :], start=False, stop=True), reads=[o.attb, vnb], writes=[pb2])
        ps3, pb3 = bank()
        P.op("pe", lambda e: e.matmul(ps3[:, 0:128], o.kd_tm[:, ch, :], vn[:, :], start=True, stop=True), reads=[o.tmb, vnb], writes=[pb3])
        P.op("dve", lambda e: e.scalar_tensor_tensor(out=o.S[:, :], in0=o.S[:, :], scalar=o.egbc[:, ch * 64 + 63:ch * 64 + 64], in1=ps3[:, 0:128],
                                                     op0=ALU.mult, op1=ALU.add), reads=[pb3, o.gcb, o.Sbuf], writes=[o.Sbuf])
        P.op("act", lambda e: e.copy(o.Sb[:, :], o.S[:, :]), reads=[o.Sbuf], writes=[o.Sbuf])
        P.op("act", lambda e: e.activation(out=o.junk[:, :], in_=ps2[0:64, 0:128], func=AF.Square, accum_out=o.ss[:, 0:1]), reads=[pb2], writes=[o.ssb])
        P.op("act", lambda e: e.activation(out=o.ss[:, 1:2], in_=o.ss[:, 0:1], func=AF.Ln, scale=1.0 / 128.0, bias=RMS_EPS), reads=[o.ssb], writes=[o.ssb])
        P.op("act", lambda e: e.activation(out=o.ss[:, 2:3], in_=o.ss[:, 1:2], func=AF.Exp, scale=-0.5), reads=[o.ssb], writes=[o.ssb])
        P.op("dve", lambda e: e.scalar_tensor_tensor(out=o.oout[:, ch, :], in0=ps2[0:64, 0:128], scalar=o.ss[:, 2:3], in1=o.sg[:, ch, :],
                                                     op0=ALU.mult, op1=ALU.mult), reads=[pb2, o.ssb, o.gtb], writes=[o.oob])

    for b in range(nbatch):
        for h, o in enumerate(hs):
            precompute(h, o, b)
        for ch in range(NB):
            for h, o in enumerate(hs):
                chunk_step(h, o, b, ch)
        for h, o in enumerate(hs):
            if fm_out:
                ps, pb = bank()
                for ch in range(NB):
                    P.op("pe", lambda e: e.transpose(ps[:, ch * 64:(ch + 1) * 64], o.oout[:, ch, :], ident[0:64, 0:64]),
                         reads=[o.oob, cb], writes=[pb])
                P.op("act", lambda e: e.copy(o.ofm[:, :], ps[:, :]), reads=[pb], writes=[o.ofmb])
                P.dma("sp", oT[h * 128:(h + 1) * 128, b * BT:(b + 1) * BT], o.ofm[:, :], reads=[o.ofmb])
            else:
                P.dma("sp", oT[h, :, b * NB:(b + 1) * NB, :], o.oout[:, :, :], reads=[o.oob])
    c.finish([o.oob for o in hs] + [o.ofmb for o in hs])
    return nc


def phase_inproj(c, ntok, xsrc, src_bf16, W, segs, TB=2048):
    nc, P = c.nc, c.P
    KT = D // 128
    banks = c.psum_banks()
    bi = [0]

    def bank():
        b = banks[bi[0] % 8]
        bi[0] += 1
        return b

    xb = c.sb([128, KT, TB], BF16, "xb")
    xbuf = Buf("xb")
    wt = [(c.sb([128, KT, 512], BF16, "wt"), Buf(f"wt{i}")) for i in range(2)]
    NT = TB // 512
    ot = [(c.sb([128, TB], BF16, "ot"), [Buf(f"ot{i}_{t}") for t in range(NT)]) for i in range(2)]
    tt_ = [(c.sb([128, 512], BF16, "tt"), Buf(f"tt{i}")) for i in range(2)]
    wi = oi = ti = 0
    outbufs = []
    for blk in range(ntok // TB):
        woff = 0
        for k in range(KT):
            if src_bf16:
                P.dma("sp", xb[:, k, :], xsrc(blk, k), writes=[xbuf])
            else:
                P.dma("pool", xb[:, k, :], xsrc(blk, k), writes=[xbuf], max_dma_last_dim=8192)
        for kind, col0, ncols, out in segs:
            for f0 in range(col0, col0 + ncols, 512):
                fw = min(512, col0 + ncols - f0)
                wtile, wbuf = wt[wi % 2]
                wi += 1
                P.dma("pool", wtile[:, :, :fw], W[woff:woff + 128 * KT * fw].rearrange("(p k f) -> p k f", p=128, k=KT), writes=[wbuf],
                      max_dma_last_dim=8192)
                woff += 128 * KT * fw
                if kind == "fm":
                    for fc in range((fw + 127) // 128):
                        m = min(128, fw - fc * 128)
                        otile, obufs = ot[oi % 2]
                        oi += 1
                        for t in range(NT):
                            ps, pbuf = bank()
                            for k in range(KT):
                                P.op("pe", lambda e: e.matmul(ps[:m, :], wtile[:, k, fc * 128:fc * 128 + m], xb[:, k, t * 512:(t + 1) * 512],
                                                              start=(k == 0), stop=(k == KT - 1)), reads=[wbuf, xbuf], writes=[pbuf])
                            if t % 2 == 0:
                                P.op("act", lambda e: e.copy(otile[:m, t * 512:(t + 1) * 512], ps[:m, :]), reads=[pbuf], writes=[obufs[t]])
                            else:
                                P.op("dve", lambda e: e.tensor_copy(otile[:m, t * 512:(t + 1) * 512], ps[:m, :]), reads=[pbuf], writes=[obufs[t]])
                        r0 = f0 - col0 + fc * 128
                        P.dma("sp", out[r0:r0 + m, blk * TB:(blk + 1) * TB], otile[:m, :], reads=obufs)
                        outbufs.extend(obufs)
                else:
                    for t in range(TB // 128):
                        ps, pbuf = bank()
                        for k in range(KT):
                            P.op("pe", lambda e: e.matmul(ps[:, :fw], xb[:, k, t * 128:(t + 1) * 128], wtile[:, k, :fw],
                                                          start=(k == 0), stop=(k == KT - 1)), reads=[wbuf, xbuf], writes=[pbuf])
                        ttile, tbuf = tt_[ti % 2]
                        ti += 1
                        if t % 2 == 0:
                            P.op("act", lambda e: e.copy(ttile[:, :fw], ps[:, :fw]), reads=[pbuf], writes=[tbuf])
                        else:
                            P.op("dve", lambda e: e.tensor_copy(ttile[:, :fw], ps[:, :fw]), reads=[pbuf], writes=[tbuf])
                        c0 = f0 - col0
                        P.dma("sp", out[blk * TB + t * 128: blk * TB + (t + 1) * 128, c0:c0 + fw], ttile[:, :fw], reads=[tbuf])
                        outbufs.append(tbuf)
    c.finish(outbufs)


GROUPS = [[0, 1, 2, 3], [4, 5, 6, 7]]
L1COLS = 1028


def build_fused(stop_after=7):
    c = Ctx()
    nc, P = c.nc, c.P
    i_ = lambda n, sh, dt=F32: nc.dram_tensor(n, list(sh), dt, kind="ExternalInput").ap()

    def done(k):
        if stop_after == k:
            if k >= 3:
                dbg = Buf("dbg")
                P.dma("sp", out, x2tok, writes=[dbg])
                P.full_barrier()
            c.root.close()
            return True
        return False

    xTb = i_("xTb", [D, S])
    xtok = i_("xtok", [TPC, D])
    Win0 = i_("Win0", [D * 1536])
    sbm, dwm, tri = i_("sbm", [128, QG // 128, QG], BF16), i_("dwm", [128, 16 + QG // 128, QG], BF16), i_("tri", [128, 128], BF16)
    t0 = s5 = gd = t1 = Win1 = None
    if stop_after >= 3:
      t0 = {k: i_("t0_" + k, sh) for k, sh in (("Wout", [8, 128, 16, 256]), ("ln1g", [D]), ("ln1b", [D]), ("ln2g", [D]), ("ln2b", [D]),
                                             ("W1", [1, 22, 128, 16, 256]), ("W3", [1, 22, 128, 16, 256]), ("W2", [1, 5632, D]))}
    if stop_after >= 4:
      Win1 = i_("Win1", [D * (L1COLS + 256)])
      s5 = {k: i_("s5_" + k, sh) for k, sh in (("lam", [8, 128, 3]), ("Bm", [8, 128, 32]), ("Cm", [8, 128, 32]), ("Dk", [8, 32, 1]), ("iota", [128, 64]))}
    if stop_after >= 4:
      gd = {k: i_("gd_" + k, sh) for k, sh in (("convw", [2, 128, 12]), ("hsc", [2, 2]), ("normw", [128]), ("gmask", [64, 3, 64]), ("mrow", [1, 512]))}
    if stop_after >= 7:
      t1 = {k: i_("t1_" + k, sh) for k, sh in (("gluw", [4, 128, 8, 256]), ("glub", [128, 8]), ("Wout", [8, 128, 16, 256]), ("ln1g", [D]), ("ln1b", [D]),
                                             ("ln2g", [D]), ("ln2b", [D]), ("W1", [8, 28, 128, 16, 256]), ("W3", [8, 28, 128, 16, 256]), ("W2", [8, 7168, D]),
                                             ("Wr", [128, 128]))}
    out = nc.dram_tensor("out", [TPC, D], F32, kind="ExternalOutput").ap()
    hfm = c.scratch("hfm", [1024, S], BF16)
    vtok = c.scratch("vtok", [S, 512], BF16)
    ag1_in, ag1_out = c.scratch("ag1_in", [512, S], BF16), c.scratch("ag1_out", [2048, S], BF16)
    x2tok = c.scratch("x2tok", [TPC, D], F32)
    ag2_in, ag2_out = c.scratch("ag2_in", [D, TPC], BF16), c.scratch("ag2_out", [4 * D, TPC], BF16)
    h1fm = c.scratch("h1fm", [L1COLS, S], BF16)
    gate_tm = c.scratch("gate_tm", [S, 256], BF16)
    ag3y_in, ag3y_out = c.scratch("ag3y_in", [256, S], F32), c.scratch("ag3y_out", [1024, S], F32)
    ag3o_in, ag3o_out = c.scratch("ag3o_in", [256, S], BF16), c.scratch("ag3o_out", [1024, S], BF16)
    rank_off = (nc.sync.partition_id() % 4) * TPC
    dummy = Buf("cc")

    def gather(a_in, a_out, r0):
        for i in range(a_in.shape[0] // r0):
            P.collective("AllGather", GROUPS, a_in[i * r0:(i + 1) * r0, :], a_out[i * 4 * r0:(i + 1) * 4 * r0, :], reads=[dummy], writes=[dummy])

    def exchange(a_in, a_out, r0):
        gather(a_in, a_out, r0)
        P.full_barrier()

    c.begin_phase({})
    phase_inproj(c, S, lambda blk, k: xTb[k * 128:(k + 1) * 128, blk * 2048:(blk + 1) * 2048], False, Win0,
                 [("fm", 0, 1024, hfm), ("tm", 1024, 512, vtok)])
    hv = hfm.rearrange("(u two p) t -> two u p t", two=2, p=128)
    c.begin_phase({"q": hv[0], "k": hv[1], "v": vtok.rearrange("(n p) (u d) -> u p n d", p=128, u=4),
                   "sbm": sbm, "dwm": dwm, "tri": tri, "oT": ag1_in.rearrange("(u p) t -> u p t", p=128)})
    build_k2(env=c)
    if done(1):
        return nc
    exchange(ag1_in, ag1_out, 64)
    if done(2):
        return nc
    ov = dict(t0)
    ov.update({"oT": ag1_out, "x": xtok, "xo": x2tok, "xTo": ag2_in})
    c.begin_phase(ov)
    build_tail(False, env=c, tok_off=rank_off, src_ntok=S, xT_out=True)
    if done(3):
        return nc
    exchange(ag2_in, ag2_out, 256)
    c.begin_phase({})
    ag2v = ag2_out.rearrange("(i s j) t -> s i j t", i=8, s=4)
    phase_inproj(c, S, lambda blk, k: ag2v[blk, k // 2, (k % 2) * 128:(k % 2) * 128 + 128, :], True, Win1,
                 [("fm", 0, L1COLS, h1fm), ("tm", L1COLS, 256, gate_tm)])
    ov = dict(s5)
    ov.update({"uT": h1fm[0:256, :].rearrange("(r c) t -> r c t", c=32), "yT": ag3y_in.rearrange("(r c) t -> r c t", c=32)})
    c.begin_phase(ov)
    build_s5(env=c)
    if done(5):
        return nc
    gather(ag3y_in, ag3y_out, 32)
    ov = dict(gd)
    ov.update({"qkv": h1fm[256:1024, :].rearrange("(h i p) t -> h i p t", h=2, i=3),
               "gate": gate_tm.rearrange("(n p) (h d) -> h p n d", p=64, h=2),
               "rows": h1fm[1024:1028, :].rearrange("(h c) t -> h c t", c=2),
               "cols": h1fm[1024:1028, :].rearrange("(h c) (n p) -> h p c n", c=2, p=64),
               "oT": ag3o_in})
    c.begin_phase(ov)
    build_gdn(env=c, fm_out=True)
    if done(6):
        return nc
    exchange(ag3o_in, ag3o_out, 64)
    ov = dict(t1)
    ov.update({"yT": ag3y_out, "odT": ag3o_out, "x": x2tok, "xo": out})
    c.begin_phase(ov)
    build_tail(True, glu=True, env=c, tok_off=rank_off, src_ntok=S)
    c.root.close()
    return nc


_PROGS = {}


def _c(a):
    return np.ascontiguousarray(a)


def _tile_w(W, fb=256):
    K_, F_ = W.shape
    return _c(W.reshape(K_ // 128, 128, F_ // fb, fb).transpose(2, 1, 0, 3))


def _tile_flat(W, segs):
    parts = []
    for col0, ncols in segs:
        for f0 in range(col0, col0 + ncols, 512):
            fw = min(512, col0 + ncols - f0)
            parts.append(W[:, f0:f0 + fw].reshape(16, 128, fw).transpose(1, 0, 2).reshape(-1))
    return _c(np.concatenate(parts))


def kernel(**inp):
    f32 = np.float32
    g = lambda k: np.asarray(inp[k], dtype=f32)
    x0 = g("x").reshape(B, S, D)
    if "fused" not in _PROGS:
        _PROGS["fused"] = build_fused()
    nc = _PROGS["fused"]
    kc, sc, gc = k2_consts(), s5_consts(), gdn_consts()
    w_in0, w_in1 = g("even_w_in")[0], g("odd_w_in")[0]
    wout0, wout1 = g("even_w_out")[0], g("odd_w_out")[0]
    rank_rows = [np.concatenate([np.arange(kind * 1024 + h * 128, kind * 1024 + (h + 1) * 128)
                                 for kind, h in ((0, 2 * s_), (0, 2 * s_ + 1), (1, 2 * s_), (1, 2 * s_ + 1))]) for s_ in range(4)]
    perm0 = np.concatenate([rank_rows[s_][i * 64:(i + 1) * 64] for i in range(8) for s_ in range(4)])
    permy = np.concatenate([np.arange(s_ * 256 + i * 32, s_ * 256 + (i + 1) * 32) for i in range(8) for s_ in range(4)])
    permo = np.concatenate([np.arange(s_ * 256 + i * 64, s_ * 256 + (i + 1) * 64) for i in range(4) for s_ in range(4)])
    perm1 = np.concatenate([permy, 1024 + permo])
    shared = {"sbm": kc["sbm"], "dwm": kc["dwm"], "tri": kc["tri"],
              "t0_Wout": _tile_w(wout0[perm0]), "t0_ln1g": g("even_ln_mix_g")[0], "t0_ln1b": g("even_ln_mix_b")[0],
              "t0_ln2g": g("even_ln_ffn_g")[0], "t0_ln2b": g("even_ln_ffn_b")[0],
              "t0_W1": _tile_w(g("even_ffn_w1")[0])[None], "t0_W3": _tile_w(g("even_ffn_w3")[0])[None], "t0_W2": g("even_ffn_w2"),
              "s5_iota": sc["iota"], "gd_normw": g("odd_gdn_norm_w")[0], "gd_gmask": gc["gmask"], "gd_mrow": gc["mrow"],
              "t1_gluw": _tile_w(g("odd_glu_w")[0][permy][:, permy]), "t1_glub": _c(g("odd_glu_b")[0][permy].reshape(8, 128).T),
              "t1_Wout": _tile_w(wout1[perm1]), "t1_ln1g": g("odd_ln_mix_g")[0], "t1_ln1b": g("odd_ln_mix_b")[0],
              "t1_ln2g": g("odd_ln_ffn_g")[0], "t1_ln2b": g("odd_ln_ffn_b")[0],
              "t1_W1": np.stack([_tile_w(w) for w in g("odd_moe_w1")[0]]), "t1_W3": np.stack([_tile_w(w) for w in g("odd_moe_w3")[0]]),
              "t1_W2": g("odd_moe_w2")[0],
              "t1_Wr": _c(g("odd_router_w")[0].reshape(16, 128, 8).transpose(1, 0, 2).reshape(128, 128))}
    lre, lim, ldt = g("odd_ssm_lam_re")[0], g("odd_ssm_lam_im")[0], g("odd_ssm_log_dt")[0]
    bre, bim, cre, cim, dsk = g("odd_ssm_b_re")[0], g("odd_ssm_b_im")[0], g("odd_ssm_c_re")[0], g("odd_ssm_c_im")[0], g("odd_ssm_d")[0]
    cw = g("odd_gdn_conv_w")[0].reshape(4, 3, 8, 128)
    alog, dtb = g("odd_gdn_a_log")[0], g("odd_gdn_dt_bias")[0]
    xT = [_c(x0[b].T) for b in range(B)]
    in_maps = []
    for c in range(NCORES):
        b, r = c // 4, c % 4
        m = dict(shared)
        m["xTb"] = xT[b]
        m["xtok"] = _c(x0[b, r * TPC:(r + 1) * TPC])
        units = ((0, 2 * r), (0, 2 * r + 1), (1, 2 * r), (1, 2 * r + 1))
        cols = []
        for kind, h in units:
            cols += [np.arange(kind * 3072 + h * 128, kind * 3072 + (h + 1) * 128),
                     np.arange(kind * 3072 + 1024 + h * 128, kind * 3072 + 1024 + (h + 1) * 128)]
        for kind, h in units:
            cols.append(np.arange(kind * 3072 + 2048 + h * 128, kind * 3072 + 2048 + (h + 1) * 128))
        m["Win0"] = _tile_flat(w_in0[:, np.concatenate(cols)], [(0, 1024), (1024, 512)])
        hh = (2 * r, 2 * r + 1)
        cols = [np.arange(r * 256, (r + 1) * 256)]
        for h in hh:
            for i in range(3):
                cols.append(np.arange(1024 + i * 1024 + h * 128, 1024 + i * 1024 + (h + 1) * 128))
        for h in hh:
            cols.append(np.array([5128 + h, 5120 + h]))
        for h in hh:
            cols.append(np.arange(4096 + h * 128, 4096 + (h + 1) * 128))
        m["Win1"] = _tile_flat(w_in1[:, np.concatenate(cols)], [(0, L1COLS), (L1COLS, 256)])
        grp = np.arange(16 * r, 16 * r + 16)
        m["s5_lam"] = _c(np.stack([lre[grp], lim[grp], np.broadcast_to(ldt[grp][:, None], (16, 64))], -1).reshape(8, 128, 3))
        m["s5_Bm"] = _c(np.concatenate([bre[grp], bim[grp]], -1).reshape(8, 128, 32))
        m["s5_Cm"] = _c(np.concatenate([cre[grp].transpose(0, 2, 1), cim[grp].transpose(0, 2, 1)], -1).reshape(8, 128, 32))
        m["s5_Dk"] = _c(dsk[r * 256:(r + 1) * 256].reshape(8, 32, 1))
        m["gd_convw"] = _c(np.stack([cw[:, :, h, :].transpose(2, 1, 0).reshape(128, 12) for h in hh]))
        m["gd_hsc"] = _c(np.stack([alog[list(hh)], dtb[list(hh)]], 1))
        in_maps.append(m)
    res = run_bass_kernel_spmd(nc, in_maps, core_ids=list(range(NCORES)))
    outs = [np.asarray(res.results[c]["out"]) for c in range(NCORES)]
    return _c(np.concatenate(outs, axis=0).reshape(B, S, D).astype(f32, copy=False))
```

```python
import os
import numpy as np
import ml_dtypes
import concourse.bass as bass
import concourse.mybir as mybir
from concourse.bass_utils import run_bass_kernel_spmd

F32 = mybir.dt.float32
BF16 = mybir.dt.bfloat16
I32 = mybir.dt.int32
AF = mybir.ActivationFunctionType
ALU = mybir.AluOpType
AX = mybir.AxisListType

NCORES = 8
D = 2048
B = 2
S = 8192
NTOK = B * S
TPC = NTOK // NCORES
ALPHA = (2 * 2) ** 0.25
LN_EPS = 1e-5
RMS_EPS = 1e-6
SEM_ROLL = 30000


class Buf:
    __slots__ = ("name", "w", "r", "dsem", "dcnt")

    def __init__(self, name):
        self.name = name
        self.w = None
        self.r = {}
        self.dsem = None
        self.dcnt = 0


class Prog:
    def __init__(self, nc, stack):
        self.nc = nc
        self.stack = stack
        self.eng = {"pe": nc.tensor, "act": nc.scalar, "dve": nc.vector, "pool": nc.gpsimd, "sp": nc.sync}
        self.cur = {}
        self.cnt = {}
        self.waited = {e: {} for e in self.eng}
        self.nsem = 0
        self.done_sems = []
        self.dma_events = {}
        self.dbufs = []
        self.dsem_pool = []
        for e in self.eng:
            self._roll(e)
        self.ninstr = 0

    def _newsem(self, name):
        self.nsem += 1
        return self.stack.enter_context(self.nc.semaphore(f"{name}_{self.nsem}"))

    def _roll(self, e):
        if e in self.cur:
            self.done_sems.append((self.cur[e], self.cnt[e]))
        self.cur[e] = self._newsem("e" + e)
        self.cnt[e] = 0

    def _dsem(self, buf):
        if buf.dsem is None:
            if self.dsem_pool:
                buf.dsem, buf.dcnt = self.dsem_pool.pop()
            else:
                buf.dsem, buf.dcnt = self._newsem("d"), 0
            self.dbufs.append(buf)

    def full_barrier(self, recycle=True):
        deps = list(self.done_sems) + [(self.cur[e], self.cnt[e]) for e in self.eng if self.cnt[e] > 0]
        deps += list(self.dma_events.items())
        for e in self.eng:
            self._wait(e, deps)
        if recycle:
            for b in self.dbufs:
                self.dsem_pool.append((b.dsem, b.dcnt))
                b.dsem = None
            self.dbufs = []

    def _wait(self, e, deps):
        w = self.waited[e]
        best = {}
        for sem, val in deps:
            if e == "pe" and sem is self.cur["pe"]:
                continue
            if w.get(sem, 0) >= val:
                continue
            if best.get(sem, (None, 0))[1] < val:
                best[sem] = (sem, val)
        for sem, val in best.values():
            self.eng[e].wait_ge(sem, val)
            w[sem] = val
            self.ninstr += 1

    @staticmethod
    def _deps(reads, writes):
        deps = []
        for b in reads:
            if b.w is not None:
                deps.append(b.w)
        for b in writes:
            if b.w is not None:
                deps.append(b.w)
            for s, v in b.r.items():
                deps.append((s, v))
        return deps

    @staticmethod
    def _record(ev, reads, writes):
        for b in reads:
            if b.r.get(ev[0], 0) < ev[1]:
                b.r[ev[0]] = ev[1]
        for b in writes:
            b.w = ev
            b.r = {}

    def op(self, e, fn, reads=(), writes=()):
        self._wait(e, self._deps(reads, writes))
        if self.cnt[e] >= SEM_ROLL:
            self._roll(e)
        ins = fn(self.eng[e])
        self.cnt[e] += 1
        ev = (self.cur[e], self.cnt[e])
        ins.then_inc(ev[0], 1)
        self.ninstr += 1
        self._record(ev, reads, writes)
        return ev

    def dma(self, q, out, in_, reads=(), writes=(), **kw):
        prim = writes[0] if writes else reads[0]
        deps = []
        for b in reads:
            if b.w is not None:
                deps.append(b.w)
        for b in writes:
            if b.w is not None and not (b.dsem is not None and b.w[0] is b.dsem):
                deps.append(b.w)
            for s, v in b.r.items():
                deps.append((s, v))
        self._wait(q, deps)
        self._dsem(prim)
        ins = self.eng[q].dma_start(out=out, in_=in_, **kw)
        prim.dcnt += 16
        ev = (prim.dsem, prim.dcnt)
        ins.then_inc(ev[0], 16)
        self.dma_events[ev[0]] = ev[1]
        self.ninstr += 1
        self._record(ev, reads, writes)
        return ev

    def collective(self, kind, groups, in_ap, out_ap, reads, writes):
        self._wait("pool", self._deps(reads, writes))
        sem = self._newsem("cc")
        ins = self.eng["pool"].collective_compute(kind, ALU.bypass, replica_groups=groups, ins=[in_ap], outs=[out_ap])
        ins.then_inc(sem)
        ev = (sem, 1)
        self.dma_events[sem] = 1
        self._record(ev, reads, writes)
        return ev

    def wait_all(self, e, bufs):
        deps = []
        for b in bufs:
            if b.w is not None:
                deps.append(b.w)
            deps.extend(b.r.items())
        self._wait(e, deps)


class Ctx:
    def __init__(self):
        import contextlib

        self.nc = bass.Bass("TRN2", target_bir_lowering=False)
        self.stack = contextlib.ExitStack()
        self.P = Prog(self.nc, self.stack)
        self.n = 0
        self.root = self.stack
        self.override = {}
        self.fused = False
        self._banks = None

    def sb(self, shape, dt, name=None):
        self.n += 1
        t = self.stack.enter_context(self.nc.sbuf_tensor(f"{name or 't'}_{self.n}", list(shape), dt))
        return t

    def psum_banks(self):
        if self._banks is None:
            banks = []
            for i in range(8):
                t = self.root.enter_context(self.nc.psum_tensor(f"ps{i}", [128, 512], F32))
                banks.append((t, Buf(f"ps{i}")))
            self._banks = banks
        return self._banks

    def din(self, name, shape, dt=F32):
        if name in self.override:
            ap = self.override[name]
            assert list(ap.shape) == list(shape), (name, ap.shape, shape)
            return ap
        return self.nc.dram_tensor(name, list(shape), dt, kind="ExternalInput").ap()

    def dout(self, name, shape, dt=F32):
        if name in self.override:
            ap = self.override[name]
            assert list(ap.shape) == list(shape), (name, ap.shape, shape)
            return ap
        return self.nc.dram_tensor(name, list(shape), dt, kind="ExternalOutput").ap()

    def scratch(self, name, shape, dt):
        return self.nc.dram_tensor(name, list(shape), dt, kind="Internal").ap()

    def begin_phase(self, override):
        import contextlib

        self.fused = True
        self.override = override
        self.stack = contextlib.ExitStack()

    def finish(self, bufs):
        if self.fused:
            self.P.full_barrier()
            self.stack.close()
            self.stack = self.root
            self.override = {}
        else:
            self.P.wait_all("sp", bufs)
            self.stack.close()

    def close(self):
        self.stack.close()


def bf16_view(a):
    return np.ascontiguousarray(a).view(ml_dtypes.bfloat16) if a.dtype == np.uint16 else a


def build_k1(F_out, ntok=TPC, env=None):
    c = env or Ctx()
    nc, P = c.nc, c.P
    xT = c.din("xT", [D, ntok])
    W = c.din("W", [D, F_out])
    hT = c.dout("hT", [F_out, ntok], BF16)
    KT = D // 128
    xb = c.sb([128, KT, ntok], BF16, "xb")
    xb_buf = [Buf(f"xb{k}") for k in range(KT)]
    for k in range(KT):
        P.dma("pool", xb[:, k, :], xT[k * 128:(k + 1) * 128, :], writes=[xb_buf[k]], max_dma_last_dim=8192)
    banks = c.psum_banks()
    FB = 512
    nfb = (F_out + FB - 1) // FB
    wt = [(c.sb([128, KT, FB], BF16, "wt"), Buf(f"wt{i}")) for i in range(2)]
    NT = ntok // 512
    ot = [(c.sb([128, ntok], BF16, "ot"), [Buf(f"ot{i}_{t}") for t in range(NT)]) for i in range(2)]
    Wv = W.rearrange("(kt p) f -> p kt f", p=128)
    bi = 0
    oi = 0
    for fb in range(nfb):
        f0 = fb * FB
        fw = min(FB, F_out - f0)
        wtile, wbuf = wt[fb % 2]
        for k in range(KT):
            P.dma("pool", wtile[:, k, :fw], Wv[:, k, f0:f0 + fw], writes=[wbuf], max_dma_last_dim=8192)
        for fc in range((fw + 127) // 128):
            m = min(128, fw - fc * 128)
            otile, obufs = ot[oi % 2]
            oi += 1
            for t in range(NT):
                ps, pbuf = banks[bi % 8]
                bi += 1
                for k in range(KT):
                    P.op("pe", lambda e, k=k, ps=ps, t=t: e.matmul(
                        ps[:m, :], wtile[:, k, fc * 128:fc * 128 + m], xb[:, k, t * 512:(t + 1) * 512],
                        start=(k == 0), stop=(k == KT - 1)),
                        reads=[wbuf, xb_buf[k]], writes=[pbuf])
                if t % 2 == 0:
                    P.op("act", lambda e, ps=ps, t=t: e.copy(otile[:m, t * 512:(t + 1) * 512], ps[:m, :]),
                         reads=[pbuf], writes=[obufs[t]])
                else:
                    P.op("dve", lambda e, ps=ps, t=t: e.tensor_copy(otile[:m, t * 512:(t + 1) * 512], ps[:m, :]),
                         reads=[pbuf], writes=[obufs[t]])
            r0 = f0 + fc * 128
            P.dma("sp", hT[r0:r0 + m, :], otile[:m, :], reads=obufs)
    c.finish([b for _, bs in ot for b in bs])
    return nc


def _barrier(P, bufs):
    for e in ("pe", "act", "dve", "pool", "sp"):
        P.wait_all(e, bufs)


def _layer_norm(c, P, y, ybuf, g_t, b_t, gbuf, small, sbuf_small):
    stats, mv, sd = small
    nchunk = D // 512
    for k in range(nchunk):
        P.op("dve", lambda e: e.bn_stats(stats[:, k, :], y[:, k * 512:(k + 1) * 512]), reads=[ybuf], writes=[sbuf_small])
    P.op("dve", lambda e: e.bn_aggr(mv[:, :], stats[:, :, :]), reads=[sbuf_small], writes=[sbuf_small])
    P.op("dve", lambda e: e.tensor_scalar(out=sd[:, :], in0=mv[:, 1:2], scalar1=LN_EPS, scalar2=None, op0=ALU.add),
         reads=[sbuf_small], writes=[sbuf_small])
    P.op("act", lambda e: e.activation(out=sd[:, :], in_=sd[:, :], func=AF.Sqrt), reads=[sbuf_small], writes=[sbuf_small])
    P.op("dve", lambda e: e.reciprocal(sd[:, :], sd[:, :]), reads=[sbuf_small], writes=[sbuf_small])
    P.op("dve", lambda e: e.tensor_scalar(out=y, in0=y, scalar1=mv[:, 0:1], scalar2=sd[:, 0:1],
                                          op0=ALU.subtract, op1=ALU.mult), reads=[ybuf, sbuf_small], writes=[ybuf])
    P.op("dve", lambda e: e.tensor_tensor(out=y, in0=y, in1=g_t[:, :], op=ALU.mult), reads=[ybuf, gbuf], writes=[ybuf])
    P.op("dve", lambda e: e.tensor_tensor(out=y, in0=y, in1=b_t[:, :], op=ALU.add), reads=[ybuf, gbuf], writes=[ybuf])


def build_tail(moe, ntok=TPC, TG=1024, n_exp=None, FF=None, glu=False, env=None, tok_off=None, src_ntok=None, xT_out=False):
    c = env or Ctx()
    nc, P = c.nc, c.P
    E = (8 if moe else 1) if n_exp is None else n_exp
    if FF is None:
        FF = 7168 if moe else 5632
    KT = D // 128
    src_ntok = src_ntok or ntok
    import concourse.bass as _b

    def tsl(t0_):
        return slice(t0_, t0_ + TG) if tok_off is None else _b.ds(tok_off + t0_, TG)

    if xT_out:
        xTo = c.dout("xTo", [D, ntok], BF16)
        xts = c.sb([128, KT, 128], BF16, "xts")
        xtsb = Buf("xts")
    if glu:
        yT_d = c.din("yT", [1024, src_ntok])
        odT_d = c.din("odT", [1024, src_ntok], BF16)
        gluw = c.din("gluw", [1024 // 256, 128, 8, 256])
        glub_d = c.din("glub", [128, 8])
    else:
        oT = c.din("oT", [D, src_ntok], BF16)
    x = c.din("x", [ntok, D])
    Wout = c.din("Wout", [D // 256, 128, KT, 256])
    lng = [c.din(f"ln{i}g", [D]) for i in (1, 2)]
    lnb = [c.din(f"ln{i}b", [D]) for i in (1, 2)]
    W1 = c.din("W1", [E, FF // 256, 128, KT, 256])
    W3 = c.din("W3", [E, FF // 256, 128, KT, 256])
    W2 = c.din("W2", [E, FF, D])
    if moe:
        Wr = c.din("Wr", [128, KT * 8])
    xo = c.dout("xo", [ntok, D])
    NTT = TG // 128
    NH = TG // 512
    banks = c.psum_banks()
    bi = [0]

    def bank():
        b = banks[bi[0] % 8]
        bi[0] += 1
        return b

    yacc = c.sb([128, NTT, D], F32, "yacc")
    ybuf = [Buf(f"y{t}") for t in range(NTT)]
    x1Traw = c.sb([128, KT * TG], BF16, "x1T")
    x1T = x1Traw[:, :].rearrange("p (k t) -> p k t", k=KT)
    x1Tbuf = [Buf(f"x1T{t}") for t in range(NTT)]
    if glu:
        ystage = x1Traw[:, :].bitcast(F32).rearrange("p (k t) -> p k t", k=8)
        ysbuf = Buf("ystage")
        glub = c.sb([128, 8], F32, "glub")
        glubuf = Buf("glub")
        P.dma("sp", glub[:, :], glub_d, writes=[glubuf])
        obz, obg = Buf("obz"), Buf("obg")
        gsc1 = c.sb([128, TG], F32, "gsc1")
        gsc2 = c.sb([128, TG], F32, "gsc2")
        gscb, gscb2 = Buf("gsc1"), Buf("gsc2")
    R = c.sb([128, KT * TG], BF16, "R")
    ob = R[:, :].rearrange("p (k t) -> p k t", k=KT)
    obuf = Buf("ob")
    FBK = 256
    NFC = FBK // 128
    w2blk = [R[:, i * NFC * D:(i + 1) * NFC * D].rearrange("p (f d) -> p f d", f=NFC) for i in range(2)]
    w2buf = [Buf(f"w2b{i}") for i in range(2)]
    off = 2 * NFC * D
    gT = [R[:, off + i * NFC * TG: off + (i + 1) * NFC * TG].rearrange("p (f t) -> p f t", f=NFC) for i in range(2)]
    gTbuf = [[Buf(f"gT{i}_{j}") for j in range(NFC * NH)] for i in range(2)]
    off += 2 * NFC * TG
    stmp = [R[:, off + i * 512: off + (i + 1) * 512] for i in range(2)]
    stbuf = [Buf(f"st{i}") for i in range(2)]
    wb = [c.sb([128, KT, FBK], BF16, "wb") for _ in range(4)]
    wbuf = [Buf(f"wb{i}") for i in range(4)]
    lnt = [c.sb([128, D], F32, "lnt") for _ in range(2)]
    lnbuf = Buf("lnt")
    stats = c.sb([128, D // 512, 6], F32, "stats")
    mv = c.sb([128, 2], F32, "mv")
    sd = c.sb([128, 1], F32, "sd")
    smallbuf = Buf("small")
    ident = c.sb([128, 128], F32, "ident")
    identbuf = Buf("ident")
    pslock = Buf("pslock")
    P.op("pool", lambda e: e.memset(ident[:, :], 0.0), writes=[identbuf])
    P.op("pool", lambda e: e.affine_select(out=ident[:, :], in_=ident[:, :], pattern=[[-1, 128]], compare_op=ALU.not_equal,
                                           fill=1.0, base=0, channel_multiplier=1), reads=[identbuf], writes=[identbuf])
    if moe:
        xT32 = c.sb([128, KT, 128], F32, "xT32")
        xT32buf = Buf("xT32")
        wr = c.sb([128, KT, 8], F32, "wr")
        wrbuf = Buf("wr")
        P.dma("sp", wr[:, :, :].rearrange("p k e -> p (k e)"), Wr, writes=[wrbuf])
        gates = c.sb([128, NTT, 8], F32, "gates")
        gatebuf = [Buf(f"gate{t}") for t in range(NTT)]
        rt = c.sb([128, 64], F32, "rt")
        rtbuf = Buf("rt")
    allbufs = (ybuf + x1Tbuf + [obuf] + w2buf + [b for g in gTbuf for b in g] + stbuf + wbuf + [lnbuf, smallbuf]
               + [b for _, b in banks])
    if not glu:
        oTv = oT.rearrange("(k p) t -> p k t", p=128)
    wi = [0]

    for tg in range(ntok // TG):
        t0 = tg * TG
        _barrier(P, allbufs)
        if glu:
            P.dma("sp", ystage, yT_d.rearrange("(k p) t -> p k t", p=128)[:, :, tsl(t0)], writes=[ysbuf])
            for k in range(8):
                yk = ystage[:, k, :]
                P.op("act", lambda e: e.activation(out=gsc1[:, :], in_=yk, func=AF.Square), reads=[ysbuf], writes=[gscb])
                P.op("dve", lambda e: e.tensor_scalar(out=gsc1[:, :], in0=gsc1[:, :], scalar1=0.044715, scalar2=1.0, op0=ALU.mult, op1=ALU.add),
                     reads=[gscb], writes=[gscb])
                P.op("dve", lambda e: e.tensor_tensor(out=gsc1[:, :], in0=gsc1[:, :], in1=yk, op=ALU.mult), reads=[gscb, ysbuf], writes=[gscb])
                P.op("act", lambda e: e.activation(out=gsc1[:, :], in_=gsc1[:, :], func=AF.Tanh, scale=float(np.sqrt(2.0 / np.pi))),
                     reads=[gscb], writes=[gscb])
                P.op("act", lambda e: e.mul(gsc2[:, :], yk, 0.5), reads=[ysbuf], writes=[gscb2])
                P.op("dve", lambda e: e.scalar_tensor_tensor(out=ob[:, k, :], in0=gsc1[:, :], scalar=1.0, in1=gsc2[:, :], op0=ALU.add, op1=ALU.mult),
                     reads=[gscb, gscb2], writes=[obz])
            for fcb in range(1024 // FBK):
                wt_, wb_ = wb[wi[0] % 4], wbuf[wi[0] % 4]
                wi[0] += 1
                P.dma("pool", wt_[:, 0:8, :], gluw[fcb], writes=[wb_], max_dma_last_dim=8192)
                for fc in range(NFC):
                    f = fcb * NFC + fc
                    for h in range(NH):
                        ps, pb = bank()
                        for k in range(8):
                            P.op("pe", lambda e: e.matmul(ps[:, :], wt_[:, k, fc * 128:(fc + 1) * 128], ob[:, k, h * 512:(h + 1) * 512],
                                                          start=(k == 0), stop=(k == 7)), reads=[wb_, obz], writes=[pb])
                        P.op("act", lambda e: e.activation(out=ob[:, 8 + f, h * 512:(h + 1) * 512], in_=ps[:, :], func=AF.Sigmoid,
                                                           bias=glub[:, f:f + 1]), reads=[pb, glubuf], writes=[obg])
            P.op("dve", lambda e: e.tensor_tensor(out=ob[:, 0:8, :], in0=ob[:, 0:8, :], in1=ob[:, 8:16, :], op=ALU.mult),
                 reads=[obz, obg], writes=[obz, obuf])
            P.dma("sp", ob[:, 8:16, :], odT_d.rearrange("(k p) t -> p k t", p=128)[:, :, tsl(t0)], reads=[obz], writes=[obg, obuf])
            _barrier(P, allbufs + [ysbuf, obz, obg])
        else:
            P.dma("sp", ob, oTv[:, :, tsl(t0)], writes=[obuf])
        P.dma("sp", lnt[0][:, :], lng[0].partition_broadcast(128), writes=[lnbuf])
        P.dma("sp", lnt[1][:, :], lnb[0].partition_broadcast(128), writes=[lnbuf])
        for tt in range(NTT):
            P.dma("sp", yacc[:, tt, :], x[t0 + tt * 128:t0 + (tt + 1) * 128, :], writes=[ybuf[tt]])
        for cb in range(D // FBK):
            wt_, wb_ = wb[wi[0] % 4], wbuf[wi[0] % 4]
            wi[0] += 1
            P.dma("pool", wt_[:, :, :], Wout[cb], writes=[wb_], max_dma_last_dim=8192)
            for tt in range(NTT):
                ps, pb = bank()
                for k in range(KT):
                    P.op("pe", lambda e: e.matmul(ps[:, :FBK], ob[:, k, tt * 128:(tt + 1) * 128], wt_[:, k, :],
                                                  start=(k == 0), stop=(k == KT - 1)), reads=[obuf, wb_], writes=[pb])
                ysl = yacc[:, tt, cb * FBK:(cb + 1) * FBK]
                P.op("dve", lambda e: e.scalar_tensor_tensor(out=ysl, in0=ysl, scalar=ALPHA, in1=ps[:, :FBK],
                                                             op0=ALU.mult, op1=ALU.add), reads=[pb, ybuf[tt]], writes=[ybuf[tt]])
        for tt in range(NTT):
            y = yacc[:, tt, :]
            _layer_norm(c, P, y, ybuf[tt], lnt[0], lnt[1], lnbuf, (stats, mv, sd), smallbuf)
            for q in range(KT // 4):
                ps, pb = bank()
                for j in range(4):
                    k = q * 4 + j
                    P.op("pe", lambda e: e.transpose(ps[:, j * 128:(j + 1) * 128], y[:, k * 128:(k + 1) * 128], ident[:, :]),
                         reads=[ybuf[tt], identbuf], writes=[pb])
                P.op("act", lambda e: e.copy(x1T[:, q * 4:(q + 1) * 4, tt * 128:(tt + 1) * 128],
                                             ps[:, :].rearrange("p (j t) -> p j t", j=4)), reads=[pb], writes=[x1Tbuf[tt], pslock])
                if moe and not os.environ.get("DBG_NOXT32"):
                    P.op("dve", lambda e: e.tensor_copy(xT32[:, q * 4:(q + 1) * 4, :],
                                                        ps[:, :].rearrange("p (j t) -> p j t", j=4)), reads=[pb], writes=[xT32buf, pslock])
            if moe:
                ps, pb = bank()
                if os.environ.get("DBG_NOROUTER"):
                    P.op("dve", lambda e: e.tensor_copy(ps[:, :8], xT32[:, 0, 0:8]), reads=[xT32buf], writes=[pb])
                else:
                  for k in range(KT):
                    P.op("pe", lambda e: e.matmul(ps[:, :8], xT32[:, k, :], wr[:, k, :], start=(k == 0), stop=(k == KT - 1)),
                         reads=[xT32buf, wrbuf], writes=[pb])
                lg, m1, eq1, l2, m2, eq2, dd, w1, w2 = (rt[:, 0:8], rt[:, 8:9], rt[:, 16:24], rt[:, 24:32], rt[:, 9:10],
                                                        rt[:, 32:40], rt[:, 10:11], rt[:, 11:12], rt[:, 12:13])
                V = lambda fn, rd=(), wr_=(): P.op("dve", fn, reads=[rtbuf] + list(rd), writes=[rtbuf] + list(wr_))
                V(lambda e: e.tensor_copy(lg, ps[:, :8]), rd=[pb])
                V(lambda e: e.reduce_max(m1, lg, axis=AX.X))
                V(lambda e: e.tensor_scalar(out=eq1, in0=lg, scalar1=m1, scalar2=None, op0=ALU.is_equal))
                V(lambda e: e.scalar_tensor_tensor(out=l2, in0=eq1, scalar=-1e30, in1=lg, op0=ALU.mult, op1=ALU.add))
                V(lambda e: e.reduce_max(m2, l2, axis=AX.X))
                V(lambda e: e.tensor_scalar(out=eq2, in0=l2, scalar1=m2, scalar2=None, op0=ALU.is_equal))
                V(lambda e: e.tensor_tensor(out=dd, in0=m2, in1=m1, op=ALU.subtract))
                P.op("act", lambda e: e.activation(out=dd, in_=dd, func=AF.Exp), reads=[rtbuf], writes=[rtbuf])
                V(lambda e: e.tensor_scalar(out=w1, in0=dd, scalar1=1.0, scalar2=None, op0=ALU.add))
                V(lambda e: e.reciprocal(w1, w1))
                V(lambda e: e.tensor_tensor(out=w2, in0=dd, in1=w1, op=ALU.mult))
                V(lambda e: e.tensor_scalar(out=eq1, in0=eq1, scalar1=w1, scalar2=None, op0=ALU.mult))
                V(lambda e: e.scalar_tensor_tensor(out=gates[:, tt, :], in0=eq2, scalar=w2, in1=eq1, op0=ALU.mult, op1=ALU.add),
                  wr_=[gatebuf[tt]])
            P.op("act", lambda e: e.mul(y, y, ALPHA), reads=[ybuf[tt]], writes=[ybuf[tt]])
        _barrier(P, allbufs)
        blk = 0
        for ex in range(E):
            W2v = W2[ex].rearrange("(f p) d -> p f d", p=128)
            for fb in range(FF // FBK):
                w1t, w1b = wb[wi[0] % 4], wbuf[wi[0] % 4]
                w3t, w3b = wb[(wi[0] + 1) % 4], wbuf[(wi[0] + 1) % 4]
                wi[0] += 2
                P.dma("pool", w1t[:, :, :], W1[ex, fb], writes=[w1b], max_dma_last_dim=8192)
                P.dma("pool", w3t[:, :, :], W3[ex, fb], writes=[w3b], max_dma_last_dim=8192)
                w2t, w2b = w2blk[blk % 2], w2buf[blk % 2]
                gt, gb = gT[blk % 2], gTbuf[blk % 2]
                blk += 1
                P.dma("pool", w2t, W2v[:, fb * NFC:(fb + 1) * NFC, :], writes=[w2b], max_dma_last_dim=8192)
                for fc in range(NFC):
                    for h in range(NH):
                        ps1, pb1 = bank()
                        ps3, pb3 = bank()
                        rds = [x1Tbuf[h * 4 + j] for j in range(4)]
                        for k in range(KT):
                            P.op("pe", lambda e: e.matmul(ps1[:, :], w1t[:, k, fc * 128:(fc + 1) * 128], x1T[:, k, h * 512:(h + 1) * 512],
                                                          start=(k == 0), stop=(k == KT - 1)), reads=[w1b] + rds, writes=[pb1])
                        for k in range(KT):
                            P.op("pe", lambda e: e.matmul(ps3[:, :], w3t[:, k, fc * 128:(fc + 1) * 128], x1T[:, k, h * 512:(h + 1) * 512],
                                                          start=(k == 0), stop=(k == KT - 1)), reads=[w3b] + rds, writes=[pb3])
                        si = (fc * NH + h) % 2
                        P.op("act", lambda e: e.activation(out=stmp[si], in_=ps1[:, :], func=AF.Silu), reads=[pb1], writes=[stbuf[si]])
                        P.op("dve", lambda e: e.tensor_tensor(out=gt[:, fc, h * 512:(h + 1) * 512], in0=stmp[si], in1=ps3[:, :], op=ALU.mult),
                             reads=[stbuf[si], pb3], writes=[gb[fc * NH + h]])
                for tt in range(NTT):
                    for dg in range(D // 512):
                        ps, pb = bank()
                        for fc in range(NFC):
                            P.op("pe", lambda e: e.matmul(ps[:, :], gt[:, fc, tt * 128:(tt + 1) * 128], w2t[:, fc, dg * 512:(dg + 1) * 512],
                                                          start=(fc == 0), stop=(fc == NFC - 1)),
                                 reads=[gb[fc * NH + tt // 4], w2b], writes=[pb])
                        ysl = yacc[:, tt, dg * 512:(dg + 1) * 512]
                        if moe and not os.environ.get("DBG_NOGATE"):
                            P.op("dve", lambda e: e.scalar_tensor_tensor(out=ysl, in0=ps[:, :], scalar=gates[:, tt, ex:ex + 1], in1=ysl,
                                                                         op0=ALU.mult, op1=ALU.add),
                                 reads=[pb, ybuf[tt], gatebuf[tt]], writes=[ybuf[tt]])
                        else:
                            P.op("dve", lambda e: e.tensor_tensor(out=ysl, in0=ysl, in1=ps[:, :], op=ALU.add),
                                 reads=[pb, ybuf[tt]], writes=[ybuf[tt]])
        P.dma("sp", lnt[0][:, :], lng[1].partition_broadcast(128), writes=[lnbuf])
        P.dma("sp", lnt[1][:, :], lnb[1].partition_broadcast(128), writes=[lnbuf])
        for tt in range(NTT):
            y = yacc[:, tt, :]
            _layer_norm(c, P, y, ybuf[tt], lnt[0], lnt[1], lnbuf, (stats, mv, sd), smallbuf)
            P.dma("sp", xo[t0 + tt * 128:t0 + (tt + 1) * 128, :], y, reads=[ybuf[tt]])
            if xT_out:
                for q in range(KT // 4):
                    ps, pb = bank()
                    for j in range(4):
                        k = q * 4 + j
                        P.op("pe", lambda e: e.transpose(ps[:, j * 128:(j + 1) * 128], y[:, k * 128:(k + 1) * 128], ident[:, :]),
                             reads=[ybuf[tt], identbuf], writes=[pb])
                    P.op("act", lambda e: e.copy(xts[:, q * 4:(q + 1) * 4, :], ps[:, :].rearrange("p (j t) -> p j t", j=4)),
                         reads=[pb], writes=[xtsb])
                P.dma("sp", xTo.rearrange("(k p) t -> p k t", p=128)[:, :, t0 + tt * 128:t0 + (tt + 1) * 128], xts[:, :, :], reads=[xtsb])
    c.finish(ybuf + ([xtsb] if xT_out else []))
    return nc


def dw_mult(delta):
    d = np.asarray(delta)
    m = ((d >= 0) & (d <= 128)).astype(np.float32)
    m += ((d >= 0) & (d <= 512) & (d % 4 == 0))
    m += ((d >= 0) & (d <= 2048) & (d % 16 == 0))
    return m


QG = 512


def k2_consts():
    bpg = QG // 128
    s = np.arange(128)[:, None]
    t = np.arange(QG)[None, :]
    sbm = np.stack([((128 * r + s) < t) for r in range(bpg)]).astype(np.float32)
    dwm = np.stack([dw_mult(t - s - 128 * r) for r in range(-16, bpg)])
    tri = (np.arange(128)[:, None] >= np.arange(128)[None, :]).astype(np.float32)
    bf = ml_dtypes.bfloat16
    return {"sbm": np.ascontiguousarray(sbm.transpose(1, 0, 2)).astype(bf),
            "dwm": np.ascontiguousarray(dwm.transpose(1, 0, 2)).astype(bf),
            "tri": tri.astype(bf)}


def build_k2(seq=S, units=(0, 0, 1, 1), env=None):
    c = env or Ctx()
    nc, P = c.nc, c.P
    NU = len(units)
    NB = seq // 128
    NG = seq // QG
    BPG = QG // 128
    NBUF = 1024 // QG
    NSB = 6
    scale = 128 ** -0.5
    qT = c.din("q", [NU, 128, seq], BF16)
    kT = c.din("k", [NU, 128, seq], BF16)
    vv = c.din("v", [NU, 128, NB, 128], BF16)
    sbm_d = c.din("sbm", [128, BPG, QG], BF16)
    dwm_d = c.din("dwm", [128, 16 + BPG, QG], BF16)
    tri_d = c.din("tri", [128, 128], BF16)
    oT = c.dout("oT", [NU, 128, seq], BF16)
    banks = c.psum_banks()

    def subs(bk):
        out = []
        for t_, _ in bk:
            for h in range(512 // QG):
                out.append((t_[:, h * QG:(h + 1) * QG], Buf("pssub")))
        return out

    zb, cb, sb_ = subs(banks[0:2]), subs(banks[2:4]), subs(banks[4:6])
    ob = [(banks[6][0][:, 0:QG], banks[6][1]), (banks[7][0][:, 0:QG], banks[7][1])]
    sbm = c.sb([128, BPG, QG], BF16, "sbm")
    dwm = c.sb([128, 16 + BPG, QG], BF16, "dwm")
    tri = c.sb([128, 128], BF16, "tri")
    ones = c.sb([128, 128], BF16, "ones")
    cbuf = Buf("consts")
    P.dma("sp", sbm[:, :, :], sbm_d, writes=[cbuf])
    P.dma("sp", dwm[:, :, :], dwm_d, writes=[cbuf])
    P.dma("sp", tri[:, :], tri_d, writes=[cbuf])
    P.op("pool", lambda e: e.memset(ones[:, :], 1.0), writes=[cbuf])
    qs = c.sb([128, seq], BF16, "qs")
    kraw = c.sb([128, seq], BF16, "kraw")
    ks = c.sb([128, seq], BF16, "ks")
    nks = c.sb([128, seq], BF16, "nks")
    vs = c.sb([128, NB, 128], BF16, "vs")
    os_ = c.sb([128, seq], BF16, "os")
    qbuf, krbuf, kbuf, vbuf = Buf("q"), Buf("kraw"), Buf("k"), Buf("v")
    obufs = [Buf(f"o{g}") for g in range(NG)]
    mk = lambda dt, nm: [(c.sb([128, QG], dt, nm), Buf(f"{nm}{i}")) for i in range(NSB)]
    et, spt, spm, tmp, wt, wm = mk(F32, "et"), mk(BF16, "spt"), mk(BF16, "spm"), mk(F32, "tmp"), mk(BF16, "wt"), mk(BF16, "wm")
    Cs = [(c.sb([128, QG], F32, "Csb"), Buf(f"C{i}")) for i in range(2)]
    rec = c.sb([128, QG], F32, "rec")
    recbuf = Buf("rec")
    it = 0
    for u, kind in enumerate(units):
        P.dma("sp", qs[:, :], qT[u], writes=[qbuf])
        P.dma("sp", kraw[:, :], kT[u], writes=[krbuf])
        P.dma("sp", vs[:, :, :], vv[u], writes=[vbuf])
        P.op("act", lambda e: e.mul(ks[:, :], kraw[:, :], scale), reads=[krbuf], writes=[kbuf])
        if kind == 0:
            P.op("pool", lambda e: e.tensor_scalar(out=nks[:, :], in0=ks[:, :], scalar1=-1.0, scalar2=None, op0=ALU.mult),
                 reads=[kbuf], writes=[kbuf])
        work = []
        for g in range(NG):
            q0 = g * QG
            qsl = qs[:, q0:q0 + QG]
            o_ps, o_pb = ob[g % 2]
            Csb, Cbuf = Cs[g % 2]
            if kind == 0:
                jlist = list(range(BPG * g + BPG - 1, -1, -1))
                for n, j in enumerate(jlist):
                    it += 1
                    bp_ = it % NBUF
                    b_ = it % NSB
                    r = j - BPG * g
                    z_ps, z_pb = zb[bp_]
                    c_ps, c_pb = cb[bp_]
                    s_ps, s_pb = sb_[bp_]
                    e_t, e_b = et[b_]
                    sp_t, sp_b = spt[b_]
                    sm_t, sm_b = spm[b_] if r >= 0 else (sp_t, sp_b)
                    w_t, w_b = wt[b_]
                    t_t, t_b = tmp[b_]
                    wm_t, wm_b = wm[b_] if r >= 0 else (w_t, w_b)
                    last = (n == len(jlist) - 1)

                    def stA0(j=j, qsl=qsl, z_ps=z_ps, z_pb=z_pb, e_t=e_t, e_b=e_b):
                        P.op("pe", lambda e: e.matmul(z_ps, ks[:, j * 128:(j + 1) * 128], qsl, start=True, stop=True), reads=[kbuf, qbuf], writes=[z_pb])
                        P.op("act", lambda e: e.activation(out=e_t[:, :], in_=z_ps, func=AF.Exp), reads=[z_pb], writes=[e_b])

                    def stA(r=r, e_t=e_t, e_b=e_b, sp_t=sp_t, sp_b=sp_b, sm_t=sm_t, sm_b=sm_b):
                        P.op("act", lambda e: e.activation(out=sp_t[:, :], in_=e_t[:, :], func=AF.Ln, bias=1.0), reads=[e_b], writes=[sp_b])
                        if r >= 0:
                            P.op("pool", lambda e: e.tensor_tensor(out=sm_t[:, :], in0=sp_t[:, :], in1=sbm[:, r, :], op=ALU.mult),
                                 reads=[sp_b, cbuf], writes=[sm_b])

                    def stB(j=j, n=n, last=last, qsl=qsl, c_ps=c_ps, c_pb=c_pb, s_ps=s_ps, s_pb=s_pb, sm_t=sm_t, sm_b=sm_b, t_t=t_t, t_b=t_b,
                            Csb=Csb, Cbuf=Cbuf):
                        P.op("pe", lambda e: e.matmul(c_ps, tri[:, :], sm_t[:, :], start=True, stop=False), reads=[cbuf, sm_b], writes=[c_pb])
                        P.op("pe", lambda e: e.matmul(c_ps, nks[:, j * 128:(j + 1) * 128], qsl, start=False, stop=True),
                             reads=[kbuf, qbuf], writes=[c_pb])
                        if not last:
                            P.op("pe", lambda e: e.matmul(s_ps, ones[:, :], sm_t[:, :], start=True, stop=True), reads=[cbuf, sm_b], writes=[s_pb])
                        if n == 0:
                            if not last:
                                P.op("dve", lambda e: e.tensor_copy(Csb[:, :], s_ps), reads=[s_pb], writes=[Cbuf])
                        else:
                            P.op("dve", lambda e: e.tensor_tensor(out=t_t[:, :], in0=c_ps, in1=Csb[:, :], op=ALU.add),
                                 reads=[c_pb, Cbuf], writes=[t_b])
                            if not last:
                                P.op("dve", lambda e: e.tensor_tensor(out=Csb[:, :], in0=s_ps, in1=Csb[:, :], op=ALU.add),
                                     reads=[s_pb, Cbuf], writes=[Cbuf])

                    def stC(j=j, n=n, r=r, last=last, g=g, q0=q0, c_ps=c_ps, c_pb=c_pb, t_t=t_t, t_b=t_b, w_t=w_t, w_b=w_b, wm_t=wm_t, wm_b=wm_b,
                            o_ps=o_ps, o_pb=o_pb):
                        if n == 0:
                            P.op("act", lambda e: e.activation(out=w_t[:, :], in_=c_ps, func=AF.Exp, scale=-1.0), reads=[c_pb], writes=[w_b])
                        else:
                            P.op("act", lambda e: e.activation(out=w_t[:, :], in_=t_t[:, :], func=AF.Exp, scale=-1.0), reads=[t_b], writes=[w_b])
                        if r >= 0:
                            P.op("pool", lambda e: e.tensor_tensor(out=wm_t[:, :], in0=w_t[:, :], in1=sbm[:, r, :], op=ALU.mult),
                                 reads=[w_b, cbuf], writes=[wm_b])
                        P.op("pe", lambda e: e.matmul(o_ps, vs[:, j, :], wm_t[:, :], start=(n == 0), stop=last), reads=[vbuf, wm_b], writes=[o_pb])
                        if last:
                            P.op("act", lambda e: e.copy(os_[:, q0:q0 + QG], o_ps), reads=[o_pb], writes=[obufs[g]])

                    work.append((stA0, stA, stB, stC))
            else:
                jlist = list(range(max(0, BPG * g - 16), BPG * g + BPG))
                d_ps, d_pb = cb[g % NBUF]
                for n, j in enumerate(jlist):
                    it += 1
                    b_ = it % NSB
                    r = j - BPG * g
                    z_ps, z_pb = zb[it % NBUF]
                    w_t, w_b = wt[b_]
                    wm_t, wm_b = wm[b_]
                    first, last = (n == 0), (n == len(jlist) - 1)

                    def stA(j=j, r=r, qsl=qsl, z_ps=z_ps, z_pb=z_pb, w_t=w_t, w_b=w_b):
                        P.op("pe", lambda e: e.matmul(z_ps, ks[:, j * 128:(j + 1) * 128], qsl, start=True, stop=True),
                             reads=[kbuf, qbuf], writes=[z_pb])
                        P.op("act", lambda e: e.activation(out=w_t[:, :], in_=z_ps, func=AF.Exp), reads=[z_pb], writes=[w_b])

                    def stB(r=r, w_t=w_t, w_b=w_b, wm_t=wm_t, wm_b=wm_b):
                        P.op("dve", lambda e: e.tensor_tensor(out=wm_t[:, :], in0=w_t[:, :], in1=dwm[:, r + 16, :], op=ALU.mult),
                             reads=[w_b, cbuf], writes=[wm_b])

                    def stC(j=j, first=first, last=last, g=g, q0=q0, wm_t=wm_t, wm_b=wm_b, o_ps=o_ps, o_pb=o_pb, d_ps=d_ps, d_pb=d_pb):
                        P.op("pe", lambda e: e.matmul(o_ps, vs[:, j, :], wm_t[:, :], start=first, stop=last), reads=[vbuf, wm_b], writes=[o_pb])
                        P.op("pe", lambda e: e.matmul(d_ps, ones[:, :], wm_t[:, :], start=first, stop=last), reads=[cbuf, wm_b], writes=[d_pb])
                        if last:
                            P.op("dve", lambda e: e.reciprocal(rec[:, :], d_ps), reads=[d_pb], writes=[recbuf])
                            P.op("dve", lambda e: e.tensor_tensor(out=os_[:, q0:q0 + QG], in0=o_ps, in1=rec[:, :], op=ALU.mult),
                                 reads=[o_pb, recbuf], writes=[obufs[g]])

                    work.append((lambda: None, stA, stB, stC))
        N_ = len(work)
        for t in range(N_ + 3):
            for st in range(4):
                if 0 <= t - st < N_:
                    work[t - st][st]()
        P.dma("sp", oT[u], os_[:, :], reads=obufs)
    c.finish(obufs)
    return nc


TWO_PI = 2.0 * np.pi


def s5_consts(L=64):
    return {"iota": np.tile(np.arange(L, dtype=np.float32), (128, 1))}


def build_s5(seq=S, npairs=8, L=64, env=None):
    c = env or Ctx()
    nc, P = c.nc, c.P
    NCH = seq // L
    NH = max(1, seq // 4096)
    HL = seq // NH
    NCHh = HL // L
    TPB = max(1, min(L, 512 // NCHh))
    uT = c.din("uT", [npairs, 32, seq], BF16)
    lam = c.din("lam", [npairs, 128, 3])
    Bm = c.din("Bm", [npairs, 128, 32])
    Cm = c.din("Cm", [npairs, 128, 32])
    Dk = c.din("Dk", [npairs, 32, 1])
    iota_d = c.din("iota", [128, L])
    yT = c.dout("yT", [npairs, 32, seq])
    banks = c.psum_banks()
    bi = [0]

    def bank():
        b = banks[bi[0] % 8]
        bi[0] += 1
        return b

    iota = c.sb([128, L], F32, "iota")
    iota1 = c.sb([128, L], F32, "iota1")
    ident = c.sb([128, 128], F32, "ident")
    cb = Buf("const")
    P.dma("sp", iota[:, :], iota_d, writes=[cb])
    P.op("pool", lambda e: e.memset(ident[:, :], 0.0), writes=[cb])
    P.op("pool", lambda e: e.affine_select(out=ident[:, :], in_=ident[:, :], pattern=[[-1, 128]], compare_op=ALU.not_equal,
                                           fill=1.0, base=0, channel_multiplier=1), reads=[cb], writes=[cb])
    P.op("dve", lambda e: e.tensor_scalar(out=iota1[:, :], in0=iota[:, :], scalar1=1.0, scalar2=None, op0=ALU.add), reads=[cb], writes=[cb])
    sm = c.sb([128, 64], F32, "sm")
    bm = c.sb([128, 32], F32, "bm")
    cm = c.sb([128, 32], F32, "cm")
    kbm = c.sb([128, 32], F32, "kbm")
    dk = c.sb([32, 1], F32, "dk")
    pre = Buf("pre")
    rr_f = c.sb([128, max(L, NCH)], F32, "rr_f")
    rr_i = c.sb([128, max(L, NCH)], I32, "rr_i")
    rr_g = c.sb([128, max(L, NCH)], F32, "rr_g")
    sint = c.sb([128, L], F32, "sint")
    cost = c.sb([128, L], F32, "cost")
    rpt = c.sb([128, L], F32, "rpt")
    G = [c.sb([128, L, 32], F32, "G") for _ in range(2)]
    Gb = Buf("G")
    Bt = [c.sb([32, L, 128], BF16, "Bt") for _ in range(2)]
    Btb = [Buf("Bt0"), Buf("Bt1")]
    Ct = [c.sb([128, L, 32], BF16, "Ct") for _ in range(4)]
    Ctb = Buf("Ct")
    pt = [c.sb([128, L, 16], F32, "pt") for _ in range(3)]
    for t_ in G:
        P.op("pool", lambda e: e.memset(t_[:, :, :], 0.0), writes=[Gb])
    for t_ in Ct:
        P.op("pool", lambda e: e.memset(t_[:, :, :], 0.0), writes=[Ctb])
    ub = c.sb([32, seq], BF16, "ub")
    ubuf = Buf("u")
    cc = [c.sb([128, HL], F32, "cc") for _ in range(2)]
    ccb = [Buf("cre"), Buf("cim")]
    at = c.sb([128, HL], F32, "at")
    atb = Buf("a")
    wb = [c.sb([128, seq], BF16, "wb") for _ in range(2)]
    wbb = [Buf("wre"), Buf("wim")]
    ysb = c.sb([32, HL], F32, "ysb")
    ybuf = Buf("y")
    ff = [c.sb([128, NCH], F32, "ff") for _ in range(2)]
    EE = [c.sb([128, NCH], F32, "EE") for _ in range(2)]
    gg = [c.sb([128, NCH], F32, "gg") for _ in range(2)]
    Rb = c.sb([128, NCH], F32, "Rb")
    Zb = [c.sb([128, NCH], BF16, "Zb") for _ in range(2)]
    chb = Buf("chunk")
    Zbb = Buf("Z")

    def V(fn, rd=(), wr=()):
        return P.op("dve", fn, reads=[pre] + list(rd), writes=[pre] + list(wr))

    def A(fn, rd=(), wr=()):
        return P.op("act", fn, reads=[pre] + list(rd), writes=[pre] + list(wr))

    def col(i):
        return sm[:, i:i + 1]

    def sincos(arg, n, s_out, c_out):
        for shift, out in ((0.0, s_out), (0.5 * np.pi, c_out)):
            f, i_, g = rr_f[:, :n], rr_i[:, :n], rr_g[:, :n]
            V(lambda e: e.tensor_scalar(out=g, in0=arg, scalar1=shift, scalar2=None, op0=ALU.add))
            V(lambda e: e.tensor_scalar(out=i_, in0=g, scalar1=1.0 / TWO_PI, scalar2=None, op0=ALU.mult))
            V(lambda e: e.tensor_copy(f, i_))
            V(lambda e: e.scalar_tensor_tensor(out=g, in0=f, scalar=-TWO_PI, in1=g, op0=ALU.mult, op1=ALU.add))
            V(lambda e: e.tensor_scalar(out=f, in0=g, scalar1=float(np.pi), scalar2=None, op0=ALU.is_gt))
            V(lambda e: e.scalar_tensor_tensor(out=g, in0=f, scalar=-TWO_PI, in1=g, op0=ALU.mult, op1=ALU.add))
            V(lambda e: e.tensor_scalar(out=f, in0=g, scalar1=-float(np.pi), scalar2=None, op0=ALU.is_lt))
            V(lambda e: e.scalar_tensor_tensor(out=g, in0=f, scalar=TWO_PI, in1=g, op0=ALU.mult, op1=ALU.add))
            A(lambda e: e.activation(out=out, in_=g, func=AF.Sin))

    for pr in range(npairs):
        P.dma("sp", sm[:, 0:3], lam[pr], writes=[pre])
        P.dma("sp", bm[:, :], Bm[pr], writes=[pre])
        P.dma("sp", cm[:, :], Cm[pr], writes=[pre])
        P.dma("sp", dk[:, :], Dk[pr], writes=[pre])
        P.dma("sp", ub[:, :], uT[pr], writes=[ubuf])
        lre, lim, ldt, dt_, rho, th, r_, sth, cth, nre, nim, l2, inv, kre, kim, t1, t2, phi, sph, cph, RL, nkim, sre, sim_, nsim = [
            col(i) for i in range(25)]
        A(lambda e: e.activation(out=dt_, in_=ldt, func=AF.Exp))
        V(lambda e: e.tensor_tensor(out=rho, in0=lre, in1=dt_, op=ALU.mult))
        V(lambda e: e.tensor_tensor(out=th, in0=lim, in1=dt_, op=ALU.mult))
        A(lambda e: e.activation(out=r_, in_=rho, func=AF.Exp))
        sincos(th, 1, sth, cth)
        V(lambda e: e.tensor_tensor(out=nre, in0=r_, in1=cth, op=ALU.mult))
        V(lambda e: e.tensor_scalar(out=nre, in0=nre, scalar1=-1.0, scalar2=None, op0=ALU.add))
        V(lambda e: e.tensor_tensor(out=nim, in0=r_, in1=sth, op=ALU.mult))
        V(lambda e: e.tensor_tensor(out=l2, in0=lre, in1=lre, op=ALU.mult))
        V(lambda e: e.scalar_tensor_tensor(out=l2, in0=lim, scalar=lim, in1=l2, op0=ALU.mult, op1=ALU.add))
        V(lambda e: e.reciprocal(inv, l2))
        V(lambda e: e.tensor_tensor(out=t1, in0=nre, in1=lre, op=ALU.mult))
        V(lambda e: e.scalar_tensor_tensor(out=t1, in0=nim, scalar=lim, in1=t1, op0=ALU.mult, op1=ALU.add))
        V(lambda e: e.tensor_tensor(out=kre, in0=t1, in1=inv, op=ALU.mult))
        V(lambda e: e.tensor_tensor(out=t1, in0=nim, in1=lre, op=ALU.mult))
        V(lambda e: e.tensor_tensor(out=t2, in0=nre, in1=lim, op=ALU.mult))
        V(lambda e: e.tensor_tensor(out=t1, in0=t1, in1=t2, op=ALU.subtract))
        V(lambda e: e.tensor_tensor(out=kim, in0=t1, in1=inv, op=ALU.mult))
        V(lambda e: e.tensor_scalar(out=nkim, in0=kim, scalar1=-1.0, scalar2=None, op0=ALU.mult))
        V(lambda e: e.tensor_scalar(out=kbm[:, 0:16], in0=bm[:, 0:16], scalar1=kre, scalar2=None, op0=ALU.mult))
        V(lambda e: e.scalar_tensor_tensor(out=kbm[:, 0:16], in0=bm[:, 16:32], scalar=nkim, in1=kbm[:, 0:16], op0=ALU.mult, op1=ALU.add))
        V(lambda e: e.tensor_scalar(out=kbm[:, 16:32], in0=bm[:, 16:32], scalar1=kre, scalar2=None, op0=ALU.mult))
        V(lambda e: e.scalar_tensor_tensor(out=kbm[:, 16:32], in0=bm[:, 0:16], scalar=kim, in1=kbm[:, 16:32], op0=ALU.mult, op1=ALU.add))
        V(lambda e: e.tensor_scalar(out=rpt[:, :], in0=iota[:, :], scalar1=th, scalar2=None, op0=ALU.mult), rd=[cb])
        sincos(rpt[:, :], L, sint[:, :], cost[:, :])
        A(lambda e: e.activation(out=rpt[:, :], in_=iota1[:, :], func=AF.Exp, scale=rho), rd=[cb])
        V(lambda e: e.tensor_scalar(out=phi, in0=th, scalar1=float(L), scalar2=None, op0=ALU.mult))
        sincos(phi, 1, sph, cph)
        V(lambda e: e.tensor_scalar(out=t1, in0=rho, scalar1=float(L), scalar2=None, op0=ALU.mult))
        A(lambda e: e.activation(out=RL, in_=t1, func=AF.Exp))
        for g2 in range(2):
            rows = slice(g2 * 64, g2 * 64 + 64)
            cols = slice(g2 * 16, g2 * 16 + 16)
            cs3 = cost[rows, :].unsqueeze(2).broadcast_to([64, L, 16])
            sn3 = sint[rows, :].unsqueeze(2).broadcast_to([64, L, 16])
            rp3 = rpt[rows, :].unsqueeze(2).broadcast_to([64, L, 16])
            kbre = kbm[rows, 0:16].unsqueeze(1).broadcast_to([64, L, 16])
            kbim = kbm[rows, 16:32].unsqueeze(1).broadcast_to([64, L, 16])
            cre3 = cm[rows, 0:16].unsqueeze(1).broadcast_to([64, L, 16])
            cim3 = cm[rows, 16:32].unsqueeze(1).broadcast_to([64, L, 16])
            p0, p1, p2 = [t_[rows, :, :] for t_ in pt]
            TT = lambda out, a, b, op, wr=(): V(lambda e: e.tensor_tensor(out=out, in0=a, in1=b, op=op), wr=wr)
            TT(p0, cs3, kbre, ALU.mult)
            TT(p1, sn3, kbim, ALU.mult)
            TT(G[0][rows, :, cols], p0, p1, ALU.add, wr=[Gb])
            TT(p0, cs3, kbim, ALU.mult)
            TT(p1, sn3, kbre, ALU.mult)
            TT(G[1][rows, :, cols], p0, p1, ALU.subtract, wr=[Gb])
            TT(p0, cs3, cre3, ALU.mult)
            TT(p1, sn3, cim3, ALU.mult)
            TT(p2, p0, p1, ALU.subtract)
            V(lambda e: e.tensor_copy(Ct[0][rows, :, cols], p2), wr=[Ctb])
            TT(Ct[2][rows, :, cols], p2, rp3, ALU.mult, wr=[Ctb])
            TT(p0, sn3, cre3, ALU.mult)
            TT(p1, cs3, cim3, ALU.mult)
            V(lambda e: e.scalar_tensor_tensor(out=p2, in0=p0, scalar=-1.0, in1=p1, op0=ALU.mult, op1=ALU.subtract))
            V(lambda e: e.tensor_copy(Ct[1][rows, :, cols], p2), wr=[Ctb])
            TT(Ct[3][rows, :, cols], p2, rp3, ALU.mult, wr=[Ctb])
        for ri in range(2):
            for q in range(L // 4):
                ps, pb = bank()
                for j in range(4):
                    P.op("pe", lambda e: e.transpose(ps[0:32, j * 128:(j + 1) * 128], G[ri][:, q * 4 + j, :], ident[:, :]),
                         reads=[Gb, cb, pre], writes=[pb])
                eng = "act" if (q % 2 == 0) else "dve"
                fn = (lambda e: e.copy(Bt[ri][:, q * 4:(q + 1) * 4, :], ps[0:32, :].rearrange("p (j m) -> p j m", j=4))) if eng == "act" else \
                     (lambda e: e.tensor_copy(Bt[ri][:, q * 4:(q + 1) * 4, :], ps[0:32, :].rearrange("p (j m) -> p j m", j=4)))
                P.op(eng, fn, reads=[pb], writes=[Btb[ri]])
        V(lambda e: e.memset(EE[0][:, 0:1], 1.0), wr=[chb])
        V(lambda e: e.memset(EE[1][:, 0:1], 0.0), wr=[chb])
        V(lambda e: e.tensor_copy(sre, cph))
        V(lambda e: e.tensor_copy(sim_, sph))
        n_ = 1
        while n_ < NCH:
            m_ = min(n_, NCH - n_)
            V(lambda e: e.tensor_scalar(out=nsim, in0=sim_, scalar1=-1.0, scalar2=None, op0=ALU.mult))
            V(lambda e: e.tensor_scalar(out=EE[0][:, n_:n_ + m_], in0=EE[0][:, 0:m_], scalar1=sre, scalar2=None, op0=ALU.mult), rd=[chb], wr=[chb])
            V(lambda e: e.scalar_tensor_tensor(out=EE[0][:, n_:n_ + m_], in0=EE[1][:, 0:m_], scalar=nsim, in1=EE[0][:, n_:n_ + m_],
                                               op0=ALU.mult, op1=ALU.add), rd=[chb], wr=[chb])
            V(lambda e: e.tensor_scalar(out=EE[1][:, n_:n_ + m_], in0=EE[0][:, 0:m_], scalar1=sim_, scalar2=None, op0=ALU.mult), rd=[chb], wr=[chb])
            V(lambda e: e.scalar_tensor_tensor(out=EE[1][:, n_:n_ + m_], in0=EE[1][:, 0:m_], scalar=sre, in1=EE[1][:, n_:n_ + m_],
                                               op0=ALU.mult, op1=ALU.add), rd=[chb], wr=[chb])
            n_ *= 2
            if n_ < NCH:
                V(lambda e: e.tensor_tensor(out=t1, in0=sre, in1=sre, op=ALU.mult))
                V(lambda e: e.scalar_tensor_tensor(out=t1, in0=sim_, scalar=nsim, in1=t1, op0=ALU.mult, op1=ALU.add))
                V(lambda e: e.tensor_tensor(out=t2, in0=sre, in1=sim_, op=ALU.mult))
                V(lambda e: e.tensor_scalar(out=sim_, in0=t2, scalar1=2.0, scalar2=None, op0=ALU.mult))
                V(lambda e: e.tensor_copy(sre, t1))
        a3 = at[:, :].rearrange("p (n t) -> p n t", t=L)
        V(lambda e: e.tensor_scalar(out=a3, in0=iota[:, :].unsqueeze(1).broadcast_to([128, NCHh, L]), scalar1=0.0, scalar2=r_,
                                    op0=ALU.mult, op1=ALU.add), rd=[cb], wr=[atb])
        V(lambda e: e.memset(at[:, 0::L], 0.0), rd=[atb], wr=[atb])
        V(lambda e: e.tensor_scalar(out=Rb[:, :], in0=EE[0][:, :], scalar1=0.0, scalar2=RL, op0=ALU.mult, op1=ALU.add), rd=[chb], wr=[chb])
        for hf in range(NH):
            off = hf * HL
            for tb in range(L // TPB):
                for ri in range(2):
                    ps, pb = bank()
                    for j in range(TPB):
                        tau = tb * TPB + j
                        P.op("pe", lambda e: e.matmul(ps[:, j * NCHh:(j + 1) * NCHh], Bt[ri][:, tau, :], ub[:, off + tau:off + HL:L],
                                                      start=True, stop=True), reads=[Btb[ri], ubuf], writes=[pb])
                    outv = cc[ri][:, :].rearrange("p (n t) -> p t n", t=L)[:, tb * TPB:(tb + 1) * TPB, :]
                    inv_ = ps[:, :TPB * NCHh].rearrange("p (t n) -> p t n", n=NCHh)
                    if ri == 0:
                        P.op("act", lambda e: e.copy(outv, inv_), reads=[pb], writes=[ccb[ri]])
                    else:
                        P.op("dve", lambda e: e.tensor_copy(outv, inv_), reads=[pb], writes=[ccb[ri]])
            for ri in range(2):
                P.op("dve", lambda e: e.tensor_tensor_scan(out=cc[ri][:, :], data0=at[:, :], data1=cc[ri][:, :], initial=0.0,
                                                           op0=ALU.mult, op1=ALU.add), reads=[atb, ccb[ri]], writes=[ccb[ri]])
                P.op("act", lambda e: e.copy(wb[ri][:, off:off + HL], cc[ri][:, :]), reads=[ccb[ri]], writes=[wbb[ri]])
                P.op("dve", lambda e: e.tensor_copy(ff[ri][:, hf * NCHh:(hf + 1) * NCHh], cc[ri][:, L - 1::L]),
                     reads=[ccb[ri]], writes=[chb])
        C2 = lambda fn: P.op("dve", fn, reads=[chb, pre], writes=[chb])
        C2(lambda e: e.tensor_tensor(out=gg[0][:, :], in0=EE[0][:, :], in1=ff[0][:, :], op=ALU.mult))
        C2(lambda e: e.tensor_tensor(out=rr_f[:, :NCH], in0=EE[1][:, :], in1=ff[1][:, :], op=ALU.mult))
        C2(lambda e: e.tensor_tensor(out=gg[0][:, :], in0=gg[0][:, :], in1=rr_f[:, :NCH], op=ALU.add))
        C2(lambda e: e.tensor_tensor(out=gg[1][:, :], in0=EE[0][:, :], in1=ff[1][:, :], op=ALU.mult))
        C2(lambda e: e.tensor_tensor(out=rr_f[:, :NCH], in0=EE[1][:, :], in1=ff[0][:, :], op=ALU.mult))
        C2(lambda e: e.tensor_tensor(out=gg[1][:, :], in0=gg[1][:, :], in1=rr_f[:, :NCH], op=ALU.subtract))
        for ri in range(2):
            C2(lambda e: e.tensor_tensor_scan(out=gg[ri][:, :], data0=Rb[:, :], data1=gg[ri][:, :], initial=0.0, op0=ALU.mult, op1=ALU.add))
        C2(lambda e: e.tensor_tensor(out=ff[0][:, :], in0=EE[0][:, :], in1=gg[0][:, :], op=ALU.mult))
        C2(lambda e: e.tensor_tensor(out=rr_f[:, :NCH], in0=EE[1][:, :], in1=gg[1][:, :], op=ALU.mult))
        C2(lambda e: e.tensor_tensor(out=ff[0][:, :], in0=ff[0][:, :], in1=rr_f[:, :NCH], op=ALU.subtract))
        C2(lambda e: e.tensor_tensor(out=ff[1][:, :], in0=EE[0][:, :], in1=gg[1][:, :], op=ALU.mult))
        C2(lambda e: e.tensor_tensor(out=rr_f[:, :NCH], in0=EE[1][:, :], in1=gg[0][:, :], op=ALU.mult))
        C2(lambda e: e.tensor_tensor(out=ff[1][:, :], in0=ff[1][:, :], in1=rr_f[:, :NCH], op=ALU.add))
        V(lambda e: e.tensor_scalar(out=t1, in0=sph, scalar1=-1.0, scalar2=None, op0=ALU.mult))
        ZW = lambda fn: P.op("dve", fn, reads=[chb, pre], writes=[Zbb, chb])
        ZW(lambda e: e.memset(Zb[0][:, 0:1], 0.0))
        ZW(lambda e: e.memset(Zb[1][:, 0:1], 0.0))
        if NCH > 1:
            ZW(lambda e: e.tensor_scalar(out=rr_f[:, :NCH - 1], in0=ff[0][:, :NCH - 1], scalar1=cph, scalar2=None, op0=ALU.mult))
            ZW(lambda e: e.scalar_tensor_tensor(out=Zb[0][:, 1:], in0=ff[1][:, :NCH - 1], scalar=t1, in1=rr_f[:, :NCH - 1], op0=ALU.mult, op1=ALU.add))
            ZW(lambda e: e.tensor_scalar(out=rr_f[:, :NCH - 1], in0=ff[0][:, :NCH - 1], scalar1=sph, scalar2=None, op0=ALU.mult))
            ZW(lambda e: e.scalar_tensor_tensor(out=Zb[1][:, 1:], in0=ff[1][:, :NCH - 1], scalar=cph, in1=rr_f[:, :NCH - 1], op0=ALU.mult, op1=ALU.add))
        for hf in range(NH):
            off = hf * HL
            for tb in range(L // TPB):
                ps, pb = bank()
                for j in range(TPB):
                    tau = tb * TPB + j
                    o_ = ps[0:32, j * NCHh:(j + 1) * NCHh]
                    P.op("pe", lambda e: e.matmul(o_, Ct[0][:, tau, :], wb[0][:, off + tau:off + HL:L], start=True, stop=False),
                         reads=[Ctb, wbb[0]], writes=[pb])
                    P.op("pe", lambda e: e.matmul(o_, Ct[1][:, tau, :], wb[1][:, off + tau:off + HL:L], start=False, stop=False),
                         reads=[Ctb, wbb[1]], writes=[pb])
                    P.op("pe", lambda e: e.matmul(o_, Ct[2][:, tau, :], Zb[0][:, hf * NCHh:(hf + 1) * NCHh], start=False, stop=False),
                         reads=[Ctb, Zbb], writes=[pb])
                    P.op("pe", lambda e: e.matmul(o_, Ct[3][:, tau, :], Zb[1][:, hf * NCHh:(hf + 1) * NCHh], start=False, stop=True),
                         reads=[Ctb, Zbb], writes=[pb])
                yv = ysb[:, :].rearrange("p (n t) -> p t n", t=L)[:, tb * TPB:(tb + 1) * TPB, :]
                uv = ub[:, off:off + HL].rearrange("p (n t) -> p t n", t=L)[:, tb * TPB:(tb + 1) * TPB, :]
                pv = ps[0:32, :TPB * NCHh].rearrange("p (t n) -> p t n", n=NCHh)
                P.op("dve", lambda e: e.scalar_tensor_tensor(out=yv, in0=uv, scalar=dk[:, 0:1], in1=pv, op0=ALU.mult, op1=ALU.add),
                     reads=[pb, ubuf, pre], writes=[ybuf])
            P.dma("sp", yT[pr, :, off:off + HL], ysb[:, :], reads=[ybuf])
    c.finish([ybuf])
    return nc


def gdn_consts():
    i = np.arange(64)[:, None]
    j = np.arange(64)[None, :]
    NEG = -30000.0
    mrow = np.ones((1, 512), np.float32)
    mrow[0, 0::64] = 0.0
    return {
        "gmask": np.stack([np.where(j < i, 0.0, NEG), np.where(j >= i, 0.0, NEG), (i <= j).astype(np.float32)], 1).astype(np.float32),
        "mrow": mrow,
    }


def build_gdn(seq=S, nheads=2, env=None, fm_out=False):
    c = env or Ctx()
    nc, P = c.nc, c.P
    NB = 8
    BT = NB * 64
    nbatch = seq // BT
    NCk = seq // 64
    qkv = c.din("qkv", [nheads, 3, 128, seq], BF16)
    gate = c.din("gate", [nheads, 64, NCk, 128], BF16)
    rows = c.din("rows", [nheads, 2, seq], BF16)
    cols = c.din("cols", [nheads, 64, 2, NCk], BF16)
    convw = c.din("convw", [nheads, 128, 12])
    hsc = c.din("hsc", [nheads, 2])
    normw = c.din("normw", [128])
    gmask_d = c.din("gmask", [64, 3, 64])
    mrow_d = c.din("mrow", [1, 512])
    if fm_out:
        oT = c.dout("oT", [nheads * 128, seq], BF16)
    else:
        oT = c.dout("o", [nheads, 64, NCk, 128], BF16)
    banks = c.psum_banks()
    bi = [0]

    def bank():
        b = banks[bi[0] % 8]
        bi[0] += 1
        return b

    cb = Buf("const")
    ident = c.sb([128, 128], F32, "ident")
    P.op("pool", lambda e: e.memset(ident[:, :], 0.0), writes=[cb])
    P.op("pool", lambda e: e.affine_select(out=ident[:, :], in_=ident[:, :], pattern=[[-1, 128]], compare_op=ALU.not_equal,
                                           fill=1.0, base=0, channel_multiplier=1), reads=[cb], writes=[cb])
    ones = c.sb([128, 128], F32, "ones")
    P.op("pool", lambda e: e.memset(ones[:, :], 1.0), writes=[cb])
    gmask = c.sb([64, 3, 64], F32, "gmask")
    mrow = c.sb([1, 512], F32, "mrow")
    nw = c.sb([64, 128], F32, "nw")
    P.dma("sp", gmask[:, :, :], gmask_d, writes=[cb])
    P.dma("sp", mrow[:, :], mrow_d, writes=[cb])
    P.dma("sp", nw[:, :], normw.partition_broadcast(64), writes=[cb])
    maskS = gmask[:, 0, :].unsqueeze(1).broadcast_to([64, NB, 64])
    maskU = gmask[:, 1, :].unsqueeze(1).broadcast_to([64, NB, 64])
    triI = gmask[:, 2, :]
    I3 = ident[0:64, 0:64].unsqueeze(1).broadcast_to([64, NB, 64])

    class H:
        pass

    hs = []
    for h in range(nheads):
        o = H()
        o.b = Buf(f"h{h}")
        o.cw = c.sb([128, 12], F32, "cw")
        o.sc = c.sb([128, 8], F32, "sc")
        o.xin = [c.sb([128, 3 + BT], BF16, "xin") for _ in range(3)]
        o.xb = [Buf(f"xin{h}_{i}") for i in range(3)]
        o.acc = [c.sb([128, BT], F32, "acc") for _ in range(3)]
        o.ab = [Buf(f"acc{h}_{i}") for i in range(3)]
        o.sq = c.sb([128, BT], F32, "sq")
        o.rs = c.sb([128, BT], F32, "rs")
        o.qT = c.sb([128, BT], BF16, "qT")
        o.kT = c.sb([128, BT], BF16, "kT")
        o.qdT = c.sb([128, BT], BF16, "qdT")
        o.qkb = Buf(f"qk{h}")
        o.kd_tm = c.sb([64, NB, 128], BF16, "kd_tm")
        o.kbg_tm = c.sb([64, NB, 128], BF16, "kbg_tm")
        o.vb_tm = c.sb([64, NB, 128], BF16, "vb_tm")
        o.tmb = Buf(f"tm{h}")
        o.gt = c.sb([64, NB, 128], BF16, "gt")
        o.sg = c.sb([64, NB, 128], F32, "sg")
        o.gtb = Buf(f"gt{h}")
        o.rowi = c.sb([1, BT], BF16, "rowi")
        o.rowf = c.sb([1, BT], F32, "rowf")
        o.gcrow = c.sb([1, BT], F32, "gcrow")
        o.rowb = Buf(f"row{h}")
        o.coli = c.sb([64, 2, NB], BF16, "coli")
        o.colf = c.sb([64, 16, NB], F32, "colf")
        o.colb = Buf(f"col{h}")
        o.gcbc = c.sb([128, BT], F32, "gcbc")
        o.egbc = c.sb([128, BT], F32, "egbc")
        o.gcb = Buf(f"gcbc{h}")
        o.dec = c.sb([64, NB, 64], F32, "dec")
        o.N = c.sb([64, NB, 64], F32, "N")
        o.NT = c.sb([64, NB, 64], F32, "NT")
        o.T = c.sb([64, NB, 64], F32, "T")
        o.TT = c.sb([64, NB, 64], F32, "TT")
        o.TTb = c.sb([64, NB, 64], BF16, "TTb")
        o.nb_ = Buf(f"N{h}")
        o.attT = c.sb([64, NB, 64], BF16, "attT")
        o.attb = Buf(f"att{h}")
        o.uval = c.sb([64, NB, 128], F32, "uval")
        o.ub = Buf(f"uval{h}")
        o.wkT = c.sb([128, NB, 64], BF16, "wkT")
        o.wkb = Buf(f"wk{h}")
        o.S = c.sb([128, 128], F32, "S")
        o.Sb = c.sb([128, 128], BF16, "Sb")
        o.Sbuf = Buf(f"S{h}")
        o.vnew = [c.sb([64, 128], BF16, "vnew") for _ in range(2)]
        o.vnb = [Buf(f"vn{h}_{i}") for i in range(2)]
        o.ss = c.sb([64, 4], F32, "ss")
        o.ss4 = [c.sb([64, 4], F32, "ss4") for _ in range(2)]
        o.ssb = Buf(f"ss{h}")
        o.junk = c.sb([64, 128], F32, "junk")
        o.oout = c.sb([64, NB, 128], F32 if fm_out else BF16, "oout")
        o.ofm = c.sb([128, BT], BF16, "ofm")
        o.ofmb = Buf(f"ofm{h}")
        o.oob = Buf(f"oo{h}")
        hs.append(o)
        P.dma("sp", o.cw[:, :], convw[h], writes=[o.b])
        P.dma("sp", o.sc[:, 0:2], hsc[h].partition_broadcast(128), writes=[o.b])
        P.op("act", lambda e: e.activation(out=o.sc[:, 2:3], in_=o.sc[:, 0:1], func=AF.Exp), reads=[o.b], writes=[o.b])
        P.op("dve", lambda e: e.tensor_scalar(out=o.sc[:, 2:3], in0=o.sc[:, 2:3], scalar1=-1.0, scalar2=None, op0=ALU.mult), reads=[o.b], writes=[o.b])
        P.op("dve", lambda e: e.memset(o.S[:, :], 0.0), writes=[o.Sbuf])
        P.op("dve", lambda e: e.memset(o.Sb[:, :], 0.0), writes=[o.Sbuf])
    LNQ = -0.5 * float(np.log(128.0))

    def precompute(h, o, b):
        t0 = b * BT
        n0 = b * NB
        for i in range(3):
            if b == 0:
                P.op("dve", lambda e: e.memset(o.xin[i][:, 0:3], 0.0), writes=[o.xb[i]])
                P.dma("sp", o.xin[i][:, 3:], qkv[h, i, :, 0:BT], writes=[o.xb[i]])
            else:
                P.dma("sp", o.xin[i][:, :], qkv[h, i, :, t0 - 3:t0 + BT], writes=[o.xb[i]])
        P.dma("sp", o.gt[:, :, :], gate[h, :, n0:n0 + NB, :], writes=[o.gtb])
        P.dma("sp", o.rowi[:, :], rows[h, 0:1, t0:t0 + BT], writes=[o.rowb])
        for ci in range(2):
            P.dma("sp", o.coli[:, ci, :], cols[h, :, ci, n0:n0 + NB], writes=[o.colb], allow_slow_non_contiguous=True)
        yield
        for i in range(3):
            x, a = o.xin[i], o.acc[i]
            P.op("dve", lambda e: e.tensor_scalar(out=a[:, :], in0=x[:, 3:3 + BT], scalar1=o.cw[:, 4 * i + 3:4 * i + 4], scalar2=None, op0=ALU.mult),
                 reads=[o.xb[i], o.b], writes=[o.ab[i]])
            for tap in range(3):
                P.op("dve", lambda e: e.scalar_tensor_tensor(out=a[:, :], in0=x[:, tap:tap + BT], scalar=o.cw[:, 4 * i + tap:4 * i + tap + 1],
                                                             in1=a[:, :], op0=ALU.mult, op1=ALU.add), reads=[o.xb[i], o.ab[i], o.b], writes=[o.ab[i]])
            P.op("act", lambda e: e.activation(out=a[:, :], in_=a[:, :], func=AF.Silu), reads=[o.ab[i]], writes=[o.ab[i]])
            yield
        yield
        for i, dst, lnb in ((0, o.qT, LNQ), (1, o.kT, 0.0)):
            a = o.acc[i]
            P.op("act", lambda e: e.activation(out=o.sq[:, :], in_=a[:, :], func=AF.Square), reads=[o.ab[i]], writes=[o.b])
            ps, pb = bank()
            P.op("pe", lambda e: e.matmul(ps[:, :], ones[:, :], o.sq[:, :], start=True, stop=True), reads=[cb, o.b], writes=[pb])
            P.op("act", lambda e: e.activation(out=o.rs[:, :], in_=ps[:, :], func=AF.Ln, bias=RMS_EPS), reads=[pb], writes=[o.b])
            P.op("act", lambda e: e.activation(out=o.rs[:, :], in_=o.rs[:, :], func=AF.Exp, scale=-0.5, bias=lnb), reads=[o.b], writes=[o.b])
            P.op("dve", lambda e: e.tensor_tensor(out=a[:, :], in0=a[:, :], in1=o.rs[:, :], op=ALU.mult), reads=[o.ab[i], o.b], writes=[o.ab[i]])
            P.op("act", lambda e: e.copy(dst[:, :], a[:, :]), reads=[o.ab[i]], writes=[o.qkb])
            yield
        yield
        P.op("act", lambda e: e.activation(out=o.sg[:, :, :], in_=o.gt[:, :, :], func=AF.Silu), reads=[o.gtb], writes=[o.gtb])
        P.op("pool", lambda e: e.tensor_tensor(out=o.sg[:, :, :], in0=o.sg[:, :, :], in1=nw[:, :].unsqueeze(1).broadcast_to([64, NB, 128]), op=ALU.mult),
             reads=[o.gtb, cb], writes=[o.gtb])
        yield
        R_ = lambda eng, fn: P.op(eng, fn, reads=[o.rowb, o.b, cb], writes=[o.rowb])
        R_("act", lambda e: e.activation(out=o.rowf[:, :], in_=o.rowi[:, :], func=AF.Exp, bias=o.sc[0:1, 1:2]))
        R_("act", lambda e: e.activation(out=o.rowf[:, :], in_=o.rowf[:, :], func=AF.Ln, bias=1.0))
        R_("dve", lambda e: e.tensor_scalar(out=o.rowf[:, :], in0=o.rowf[:, :], scalar1=o.sc[0:1, 2:3], scalar2=None, op0=ALU.mult))
        R_("dve", lambda e: e.tensor_tensor_scan(out=o.gcrow[:, :], data0=mrow[:, :], data1=o.rowf[:, :], initial=0.0, op0=ALU.mult, op1=ALU.add))
        ps, pb = bank()
        P.op("pe", lambda e: e.matmul(ps[:, :], ones[0:1, :], o.gcrow[:, :], start=True, stop=True), reads=[cb, o.rowb], writes=[pb])
        P.op("dve", lambda e: e.tensor_copy(o.gcbc[:, :], ps[:, :]), reads=[pb], writes=[o.gcb])
        P.op("act", lambda e: e.activation(out=o.egbc[:, :], in_=o.gcbc[:, :], func=AF.Exp), reads=[o.gcb], writes=[o.gcb])
        yield
        C_ = lambda eng, fn, rd=(): P.op(eng, fn, reads=[o.colb, o.b, cb] + list(rd), writes=[o.colb])
        cf = lambda k: o.colf[:, k, :]
        C_("act", lambda e: e.activation(out=cf(0), in_=o.coli[:, 0, :], func=AF.Exp, bias=o.sc[0:64, 1:2]))
        C_("act", lambda e: e.activation(out=cf(0), in_=cf(0), func=AF.Ln, bias=1.0))
        C_("dve", lambda e: e.tensor_scalar(out=cf(0), in0=cf(0), scalar1=o.sc[0:64, 2:3], scalar2=None, op0=ALU.mult))
        C_("act", lambda e: e.activation(out=cf(1), in_=o.coli[:, 1, :], func=AF.Sigmoid))
        ps, pb = bank()
        P.op("pe", lambda e: e.matmul(ps[0:64, 0:NB], triI, cf(0), start=True, stop=True), reads=[cb, o.colb], writes=[pb])
        C_("dve", lambda e: e.tensor_copy(cf(2), ps[0:64, 0:NB]), rd=[pb])
        C_("act", lambda e: e.activation(out=cf(3), in_=cf(2), func=AF.Exp))
        C_("dve", lambda e: e.tensor_tensor(out=cf(4), in0=cf(3), in1=cf(1), op=ALU.mult))
        C_("dve", lambda e: e.tensor_copy(cf(5), o.gcbc[0:64, 63::64]), rd=[o.gcb])
        C_("dve", lambda e: e.tensor_tensor(out=cf(6), in0=cf(5), in1=cf(2), op=ALU.subtract))
        C_("act", lambda e: e.activation(out=cf(6), in_=cf(6), func=AF.Exp))
        C_("dve", lambda e: e.tensor_scalar(out=cf(7), in0=cf(1), scalar1=-1.0, scalar2=None, op0=ALU.mult))
        yield
        P.op("dve", lambda e: e.tensor_tensor(out=o.qdT[:, :], in0=o.acc[0][:, :], in1=o.egbc[:, :], op=ALU.mult),
             reads=[o.ab[0], o.gcb], writes=[o.qkb])
        yield
        for src, outs in ((1, ((o.kd_tm, 6), (o.kbg_tm, 4))), (2, ((o.vb_tm, 1),))):
            for q in range(NB // 4):
                ps, pb = bank()
                for j in range(4):
                    ch = q * 4 + j
                    P.op("pe", lambda e: e.transpose(ps[0:64, j * 128:(j + 1) * 128], o.acc[src][:, ch * 64:(ch + 1) * 64], ident[:, :]),
                         reads=[o.ab[src], cb], writes=[pb])
                pv = ps[0:64, :].rearrange("p (j d) -> p j d", j=4)
                for dst, k in outs:
                    P.op("dve", lambda e: e.tensor_tensor(out=dst[:, q * 4:(q + 1) * 4, :], in0=pv,
                                                          in1=o.colf[:, k, q * 4:(q + 1) * 4].unsqueeze(2).broadcast_to([64, 4, 128]), op=ALU.mult),
                         reads=[pb, o.colb], writes=[o.tmb])
        yield
        gcj = o.gcbc[0:64, :].rearrange("p (n j) -> p n j", j=64)
        gci = o.colf[:, 2, :].unsqueeze(2).broadcast_to([64, NB, 64])
        nbe = o.colf[:, 7, :].unsqueeze(2).broadcast_to([64, NB, 64])
        D_ = lambda eng, fn, rd=(), wr=(): P.op(eng, fn, reads=[o.nb_, o.colb, o.gcb, cb] + list(rd), writes=[o.nb_] + list(wr))
        D_("dve", lambda e: e.tensor_tensor(out=o.dec[:, :, :], in0=gci, in1=gcj, op=ALU.subtract))
        D_("dve", lambda e: e.tensor_tensor(out=o.dec[:, :, :], in0=o.dec[:, :, :], in1=maskS, op=ALU.add))
        D_("act", lambda e: e.activation(out=o.dec[:, :, :], in_=o.dec[:, :, :], func=AF.Exp))
        D_("dve", lambda e: e.tensor_tensor(out=o.dec[:, :, :], in0=o.dec[:, :, :], in1=nbe, op=ALU.mult))
        ps, pb = bank()
        for ch in range(NB):
            ksl = o.kT[:, ch * 64:(ch + 1) * 64]
            P.op("pe", lambda e: e.matmul(ps[0:64, ch * 64:(ch + 1) * 64], ksl, ksl, start=True, stop=True), reads=[o.qkb], writes=[pb])
        pv = ps[0:64, :].rearrange("p (n j) -> p n j", j=64)
        D_("dve", lambda e: e.tensor_tensor(out=o.N[:, :, :], in0=o.dec[:, :, :], in1=pv, op=ALU.mult), rd=[pb])
        ps, pb = bank()
        for ch in range(NB):
            P.op("pe", lambda e: e.transpose(ps[0:64, ch * 64:(ch + 1) * 64], o.N[:, ch, :], ident[0:64, 0:64]), reads=[o.nb_, cb], writes=[pb])
        pv = ps[0:64, :].rearrange("p (n j) -> p n j", j=64)
        D_("act", lambda e: e.copy(o.NT[:, :, :], pv), rd=[pb])
        D_("dve", lambda e: e.tensor_tensor(out=o.T[:, :, :], in0=o.N[:, :, :], in1=I3, op=ALU.add))
        D_("dve", lambda e: e.tensor_tensor(out=o.TT[:, :, :], in0=o.NT[:, :, :], in1=I3, op=ALU.add))
        for lvl in range(5):
            psa, pba = bank()
            psb, pbb = bank()
            for ch in range(NB):
                sl = slice(ch * 64, (ch + 1) * 64)
                P.op("pe", lambda e: e.matmul(psa[0:64, sl], o.NT[:, ch, :], o.N[:, ch, :], start=True, stop=True), reads=[o.nb_], writes=[pba])
                P.op("pe", lambda e: e.matmul(psb[0:64, sl], o.N[:, ch, :], o.NT[:, ch, :], start=True, stop=True), reads=[o.nb_], writes=[pbb])
            D_("act", lambda e: e.copy(o.N[:, :, :], psa[0:64, :].rearrange("p (n j) -> p n j", j=64)), rd=[pba])
            D_("dve", lambda e: e.tensor_copy(o.NT[:, :, :], psb[0:64, :].rearrange("p (n j) -> p n j", j=64)), rd=[pbb])
            yield
            psa, pba = bank()
            psb, pbb = bank()
            for ch in range(NB):
                sl = slice(ch * 64, (ch + 1) * 64)
                P.op("pe", lambda e: e.matmul(psa[0:64, sl], o.TT[:, ch, :], o.N[:, ch, :], start=True, stop=True), reads=[o.nb_], writes=[pba])
                P.op("pe", lambda e: e.matmul(psb[0:64, sl], o.N[:, ch, :], o.TT[:, ch, :], start=True, stop=True), reads=[o.nb_], writes=[pbb])
            D_("dve", lambda e: e.tensor_tensor(out=o.T[:, :, :], in0=o.T[:, :, :], in1=psa[0:64, :].rearrange("p (n j) -> p n j", j=64), op=ALU.add), rd=[pba])
            D_("dve", lambda e: e.tensor_tensor(out=o.TT[:, :, :], in0=o.TT[:, :, :], in1=psb[0:64, :].rearrange("p (n j) -> p n j", j=64), op=ALU.add), rd=[pbb])
            yield
        D_("act", lambda e: e.copy(o.TTb[:, :, :], o.TT[:, :, :]))
        yield
        for q in range(NB // 4):
            ps, pb = bank()
            for j in range(4):
                ch = q * 4 + j
                P.op("pe", lambda e: e.matmul(ps[0:64, j * 128:(j + 1) * 128], o.TTb[:, ch, :], o.vb_tm[:, ch, :], start=True, stop=True),
                     reads=[o.nb_, o.tmb], writes=[pb])
            P.op("act", lambda e: e.copy(o.uval[:, q * 4:(q + 1) * 4, :], ps[0:64, :].rearrange("p (j d) -> p j d", j=4)), reads=[pb], writes=[o.ub])
        ps, pb = bank()
        for ch in range(NB):
            P.op("pe", lambda e: e.matmul(ps[:, ch * 64:(ch + 1) * 64], o.kbg_tm[:, ch, :], o.TTb[:, ch, :], start=True, stop=True),
                 reads=[o.nb_, o.tmb], writes=[pb])
        P.op("act", lambda e: e.copy(o.wkT[:, :, :], ps[:, :].rearrange("p (n i) -> p n i", i=64)), reads=[pb], writes=[o.wkb])
        yield
        gcjc = o.colf[:, 2, :].unsqueeze(2).broadcast_to([64, NB, 64])
        D_("dve", lambda e: e.tensor_tensor(out=o.dec[:, :, :], in0=gcj, in1=gcjc, op=ALU.subtract))
        D_("dve", lambda e: e.tensor_tensor(out=o.dec[:, :, :], in0=o.dec[:, :, :], in1=maskU, op=ALU.add))
        D_("act", lambda e: e.activation(out=o.dec[:, :, :], in_=o.dec[:, :, :], func=AF.Exp))
        ps, pb = bank()
        for ch in range(NB):
            sl = slice(ch * 64, (ch + 1) * 64)
            P.op("pe", lambda e: e.matmul(ps[0:64, sl], o.kT[:, sl], o.qT[:, sl], start=True, stop=True), reads=[o.qkb], writes=[pb])
        D_("dve", lambda e: e.tensor_tensor(out=o.attT[:, :, :], in0=o.dec[:, :, :], in1=ps[0:64, :].rearrange("p (n i) -> p n i", i=64), op=ALU.mult),
           rd=[pb], wr=[o.attb])

    def chunk_crit(h, o, b, ch):
        n = b * NB + ch
        sl = slice(ch * 64, (ch + 1) * 64)
        vn, vnb = o.vnew[n % 2], o.vnb[n % 2]
        ps1, pb1 = banks[4 * h + 0]
        ps3, pb3 = banks[4 * h + 1]
        ps2, pb2 = banks[4 * h + 2 + (n % 2)]
        P.op("pe", lambda e: e.matmul(ps1[0:64, 0:128], o.wkT[:, ch, :], o.Sb[:, :], start=True, stop=True), reads=[o.wkb, o.Sbuf], writes=[pb1])
        P.op("dve", lambda e: e.tensor_tensor(out=vn[:, :], in0=o.uval[:, ch, :], in1=ps1[0:64, 0:128], op=ALU.subtract), reads=[o.ub, pb1], writes=[vnb])
        P.op("pe", lambda e: e.matmul(ps3[:, 0:128], o.kd_tm[:, ch, :], vn[:, :], start=True, stop=True), reads=[o.tmb, vnb], writes=[pb3])
        P.op("pe", lambda e: e.matmul(ps2[0:64, 0:128], o.qdT[:, sl], o.Sb[:, :], start=True, stop=False), reads=[o.qkb, o.Sbuf], writes=[pb2])
        P.op("pe", lambda e: e.matmul(ps2[0:64, 0:128], o.attT[:, ch, :], vn[:, :], start=False, stop=True), reads=[o.attb, vnb], writes=[pb2])
        P.op("dve", lambda e: e.scalar_tensor_tensor(out=o.S[:, :], in0=o.S[:, :], scalar=o.egbc[:, ch * 64 + 63:ch * 64 + 64], in1=ps3[:, 0:128],
                                                     op0=ALU.mult, op1=ALU.add), reads=[pb3, o.gcb, o.Sbuf], writes=[o.Sbuf])
        P.op("act", lambda e: e.copy(o.Sb[:, :], o.S[:, :]), reads=[o.Sbuf], writes=[o.Sbuf])

    def chunk_out(h, o, b, ch):
        n = b * NB + ch
        ps2, pb2 = banks[4 * h + 2 + (n % 2)]
        ss = o.ss4[n % 2]
        P.op("act", lambda e: e.activation(out=o.junk[:, :], in_=ps2[0:64, 0:128], func=AF.Square, accum_out=ss[:, 0:1]), reads=[pb2], writes=[o.ssb])
        P.op("act", lambda e: e.activation(out=ss[:, 1:2], in_=ss[:, 0:1], func=AF.Ln, scale=1.0 / 128.0, bias=RMS_EPS), reads=[o.ssb], writes=[o.ssb])
        P.op("act", lambda e: e.activation(out=ss[:, 2:3], in_=ss[:, 1:2], func=AF.Exp, scale=-0.5), reads=[o.ssb], writes=[o.ssb])
        P.op("dve", lambda e: e.scalar_tensor_tensor(out=o.oout[:, ch, :], in0=ps2[0:64, 0:128], scalar=ss[:, 2:3], in1=o.sg[:, ch, :],
                                                     op0=ALU.mult, op1=ALU.mult), reads=[pb2, o.ssb, o.gtb], writes=[o.oob])

    for b in range(nbatch):
        alive = [precompute(h, o, b) for h, o in enumerate(hs)]
        while alive:
            for g_ in list(alive):
                try:
                    next(g_)
                except StopIteration:
                    alive.remove(g_)
        for ch in range(NB + 1):
            if ch < NB:
                for h, o in enumerate(hs):
                    chunk_crit(h, o, b, ch)
            if ch >= 1:
                for h, o in enumerate(hs):
                    chunk_out(h, o, b, ch - 1)
        for h, o in enumerate(hs):
            if fm_out:
                ps, pb = bank()
                for ch in range(NB):
                    P.op("pe", lambda e: e.transpose(ps[:, ch * 64:(ch + 1) * 64], o.oout[:, ch, :], ident[0:64, 0:64]),
                         reads=[o.oob, cb], writes=[pb])
                P.op("act", lambda e: e.copy(o.ofm[:, :], ps[:, :]), reads=[pb], writes=[o.ofmb])
                P.dma("sp", oT[h * 128:(h + 1) * 128, b * BT:(b + 1) * BT], o.ofm[:, :], reads=[o.ofmb])
            else:
                P.dma("sp", oT[h, :, b * NB:(b + 1) * NB, :], o.oout[:, :, :], reads=[o.oob])
    c.finish([o.oob for o in hs] + [o.ofmb for o in hs])
    return nc


def phase_inproj(c, ntok, xsrc, src_bf16, W, segs, TB=2048):
    nc, P = c.nc, c.P
    KT = D // 128
    banks = c.psum_banks()
    bi = [0]

    def bank():
        b = banks[bi[0] % 8]
        bi[0] += 1
        return b

    xb = c.sb([128, KT, TB], BF16, "xb")
    xbuf = Buf("xb")
    wt = [(c.sb([128, KT, 512], BF16, "wt"), Buf(f"wt{i}")) for i in range(2)]
    NT = TB // 512
    ot = [(c.sb([128, TB], BF16, "ot"), [Buf(f"ot{i}_{t}") for t in range(NT)]) for i in range(2)]
    tt_ = [(c.sb([128, 512], BF16, "tt"), Buf(f"tt{i}")) for i in range(2)]
    wi = oi = ti = 0
    outbufs = []
    for blk in range(ntok // TB):
        woff = 0
        for k in range(KT):
            if src_bf16:
                P.dma("sp", xb[:, k, :], xsrc(blk, k), writes=[xbuf])
            else:
                P.dma("pool", xb[:, k, :], xsrc(blk, k), writes=[xbuf], max_dma_last_dim=8192)
        for kind, col0, ncols, out in segs:
            for f0 in range(col0, col0 + ncols, 512):
                fw = min(512, col0 + ncols - f0)
                wtile, wbuf = wt[wi % 2]
                wi += 1
                P.dma("pool", wtile[:, :, :fw], W[woff:woff + 128 * KT * fw].rearrange("(p k f) -> p k f", p=128, k=KT), writes=[wbuf],
                      max_dma_last_dim=8192)
                woff += 128 * KT * fw
                if kind == "fm":
                    for fc in range((fw + 127) // 128):
                        m = min(128, fw - fc * 128)
                        otile, obufs = ot[oi % 2]
                        oi += 1
                        for t in range(NT):
                            ps, pbuf = bank()
                            for k in range(KT):
                                P.op("pe", lambda e: e.matmul(ps[:m, :], wtile[:, k, fc * 128:fc * 128 + m], xb[:, k, t * 512:(t + 1) * 512],
                                                              start=(k == 0), stop=(k == KT - 1)), reads=[wbuf, xbuf], writes=[pbuf])
                            if t % 2 == 0:
                                P.op("act", lambda e: e.copy(otile[:m, t * 512:(t + 1) * 512], ps[:m, :]), reads=[pbuf], writes=[obufs[t]])
                            else:
                                P.op("dve", lambda e: e.tensor_copy(otile[:m, t * 512:(t + 1) * 512], ps[:m, :]), reads=[pbuf], writes=[obufs[t]])
                        r0 = f0 - col0 + fc * 128
                        P.dma("sp", out[r0:r0 + m, blk * TB:(blk + 1) * TB], otile[:m, :], reads=obufs)
                        outbufs.extend(obufs)
                else:
                    for t in range(TB // 128):
                        ps, pbuf = bank()
                        for k in range(KT):
                            P.op("pe", lambda e: e.matmul(ps[:, :fw], xb[:, k, t * 128:(t + 1) * 128], wtile[:, k, :fw],
                                                          start=(k == 0), stop=(k == KT - 1)), reads=[wbuf, xbuf], writes=[pbuf])
                        ttile, tbuf = tt_[ti % 2]
                        ti += 1
                        if t % 2 == 0:
                            P.op("act", lambda e: e.copy(ttile[:, :fw], ps[:, :fw]), reads=[pbuf], writes=[tbuf])
                        else:
                            P.op("dve", lambda e: e.tensor_copy(ttile[:, :fw], ps[:, :fw]), reads=[pbuf], writes=[tbuf])
                        c0 = f0 - col0
                        P.dma("sp", out[blk * TB + t * 128: blk * TB + (t + 1) * 128, c0:c0 + fw], ttile[:, :fw], reads=[tbuf])
                        outbufs.append(tbuf)
    c.finish(outbufs)


GROUPS = [[0, 1, 2, 3], [4, 5, 6, 7]]
L1COLS = 1028


def build_fused(stop_after=7):
    c = Ctx()
    nc, P = c.nc, c.P
    i_ = lambda n, sh, dt=F32: nc.dram_tensor(n, list(sh), dt, kind="ExternalInput").ap()

    def done(k):
        if stop_after == k:
            if k >= 3:
                dbg = Buf("dbg")
                P.dma("sp", out, x2tok, writes=[dbg])
                P.full_barrier()
            c.root.close()
            return True
        return False

    xTb = i_("xTb", [D, S])
    xtok = i_("xtok", [TPC, D])
    Win0 = i_("Win0", [D * 1536])
    sbm, dwm, tri = i_("sbm", [128, QG // 128, QG], BF16), i_("dwm", [128, 16 + QG // 128, QG], BF16), i_("tri", [128, 128], BF16)
    t0 = s5 = gd = t1 = Win1 = None
    if stop_after >= 3:
      t0 = {k: i_("t0_" + k, sh) for k, sh in (("Wout", [8, 128, 16, 256]), ("ln1g", [D]), ("ln1b", [D]), ("ln2g", [D]), ("ln2b", [D]),
                                             ("W1", [1, 22, 128, 16, 256]), ("W3", [1, 22, 128, 16, 256]), ("W2", [1, 5632, D]))}
    if stop_after >= 4:
      Win1 = i_("Win1", [D * (L1COLS + 256)])
      s5 = {k: i_("s5_" + k, sh) for k, sh in (("lam", [8, 128, 3]), ("Bm", [8, 128, 32]), ("Cm", [8, 128, 32]), ("Dk", [8, 32, 1]), ("iota", [128, 64]))}
    if stop_after >= 4:
      gd = {k: i_("gd_" + k, sh) for k, sh in (("convw", [2, 128, 12]), ("hsc", [2, 2]), ("normw", [128]), ("gmask", [64, 3, 64]), ("mrow", [1, 512]))}
    if stop_after >= 7:
      t1 = {k: i_("t1_" + k, sh) for k, sh in (("gluw", [4, 128, 8, 256]), ("glub", [128, 8]), ("Wout", [8, 128, 16, 256]), ("ln1g", [D]), ("ln1b", [D]),
                                             ("ln2g", [D]), ("ln2b", [D]), ("W1", [8, 28, 128, 16, 256]), ("W3", [8, 28, 128, 16, 256]), ("W2", [8, 7168, D]),
                                             ("Wr", [128, 128]))}
    out = nc.dram_tensor("out", [TPC, D], F32, kind="ExternalOutput").ap()
    hfm = c.scratch("hfm", [1024, S], BF16)
    vtok = c.scratch("vtok", [S, 512], BF16)
    ag1_in, ag1_out = c.scratch("ag1_in", [512, S], BF16), c.scratch("ag1_out", [2048, S], BF16)
    x2tok = c.scratch("x2tok", [TPC, D], F32)
    ag2_in, ag2_out = c.scratch("ag2_in", [D, TPC], BF16), c.scratch("ag2_out", [4 * D, TPC], BF16)
    h1fm = c.scratch("h1fm", [L1COLS, S], BF16)
    gate_tm = c.scratch("gate_tm", [S, 256], BF16)
    ag3y_in, ag3y_out = c.scratch("ag3y_in", [256, S], F32), c.scratch("ag3y_out", [1024, S], F32)
    ag3o_in, ag3o_out = c.scratch("ag3o_in", [256, S], BF16), c.scratch("ag3o_out", [1024, S], BF16)
    rank_off = (nc.sync.partition_id() % 4) * TPC
    dummy = Buf("cc")

    def gather(a_in, a_out, r0):
        for i in range(a_in.shape[0] // r0):
            P.collective("AllGather", GROUPS, a_in[i * r0:(i + 1) * r0, :], a_out[i * 4 * r0:(i + 1) * 4 * r0, :], reads=[dummy], writes=[dummy])

    def exchange(a_in, a_out, r0):
        gather(a_in, a_out, r0)
        P.full_barrier()

    c.begin_phase({})
    phase_inproj(c, S, lambda blk, k: xTb[k * 128:(k + 1) * 128, blk * 2048:(blk + 1) * 2048], False, Win0,
                 [("fm", 0, 1024, hfm), ("tm", 1024, 512, vtok)])
    hv = hfm.rearrange("(u two p) t -> two u p t", two=2, p=128)
    c.begin_phase({"q": hv[0], "k": hv[1], "v": vtok.rearrange("(n p) (u d) -> u p n d", p=128, u=4),
                   "sbm": sbm, "dwm": dwm, "tri": tri, "oT": ag1_in.rearrange("(u p) t -> u p t", p=128)})
    build_k2(env=c)
    if done(1):
        return nc
    exchange(ag1_in, ag1_out, 64)
    if done(2):
        return nc
    ov = dict(t0)
    ov.update({"oT": ag1_out, "x": xtok, "xo": x2tok, "xTo": ag2_in})
    c.begin_phase(ov)
    build_tail(False, env=c, tok_off=rank_off, src_ntok=S, xT_out=True)
    if done(3):
        return nc
    exchange(ag2_in, ag2_out, 256)
    c.begin_phase({})
    ag2v = ag2_out.rearrange("(i s j) t -> s i j t", i=8, s=4)
    phase_inproj(c, S, lambda blk, k: ag2v[blk, k // 2, (k % 2) * 128:(k % 2) * 128 + 128, :], True, Win1,
                 [("fm", 0, L1COLS, h1fm), ("tm", L1COLS, 256, gate_tm)])
    ov = dict(s5)
    ov.update({"uT": h1fm[0:256, :].rearrange("(r c) t -> r c t", c=32), "yT": ag3y_in.rearrange("(r c) t -> r c t", c=32)})
    c.begin_phase(ov)
    build_s5(env=c)
    if done(5):
        return nc
    gather(ag3y_in, ag3y_out, 32)
    ov = dict(gd)
    ov.update({"qkv": h1fm[256:1024, :].rearrange("(h i p) t -> h i p t", h=2, i=3),
               "gate": gate_tm.rearrange("(n p) (h d) -> h p n d", p=64, h=2),
               "rows": h1fm[1024:1028, :].rearrange("(h c) t -> h c t", c=2),
               "cols": h1fm[1024:1028, :].rearrange("(h c) (n p) -> h p c n", c=2, p=64),
               "oT": ag3o_in})
    c.begin_phase(ov)
    build_gdn(env=c, fm_out=True)
    if done(6):
        return nc
    exchange(ag3o_in, ag3o_out, 64)
    ov = dict(t1)
    ov.update({"yT": ag3y_out, "odT": ag3o_out, "x": x2tok, "xo": out})
    c.begin_phase(ov)
    build_tail(True, glu=True, env=c, tok_off=rank_off, src_ntok=S)
    c.root.close()
    return nc


_PROGS = {}


def _c(a):
    return np.ascontiguousarray(a)


def _tile_w(W, fb=256):
    K_, F_ = W.shape
    return _c(W.reshape(K_ // 128, 128, F_ // fb, fb).transpose(2, 1, 0, 3))


def _tile_flat(W, segs):
    parts = []
    for col0, ncols in segs:
        for f0 in range(col0, col0 + ncols, 512):
            fw = min(512, col0 + ncols - f0)
            parts.append(W[:, f0:f0 + fw].reshape(16, 128, fw).transpose(1, 0, 2).reshape(-1))
    return _c(np.concatenate(parts))


def kernel(**inp):
    f32 = np.float32
    g = lambda k: np.asarray(inp[k], dtype=f32)
    x0 = g("x").reshape(B, S, D)
    if "fused" not in _PROGS:
        _PROGS["fused"] = build_fused()
    nc = _PROGS["fused"]
    kc, sc, gc = k2_consts(), s5_consts(), gdn_consts()
    w_in0, w_in1 = g("even_w_in")[0], g("odd_w_in")[0]
    wout0, wout1 = g("even_w_out")[0], g("odd_w_out")[0]
    rank_rows = [np.concatenate([np.arange(kind * 1024 + h * 128, kind * 1024 + (h + 1) * 128)
                                 for kind, h in ((0, 2 * s_), (0, 2 * s_ + 1), (1, 2 * s_), (1, 2 * s_ + 1))]) for s_ in range(4)]
    perm0 = np.concatenate([rank_rows[s_][i * 64:(i + 1) * 64] for i in range(8) for s_ in range(4)])
    permy = np.concatenate([np.arange(s_ * 256 + i * 32, s_ * 256 + (i + 1) * 32) for i in range(8) for s_ in range(4)])
    permo = np.concatenate([np.arange(s_ * 256 + i * 64, s_ * 256 + (i + 1) * 64) for i in range(4) for s_ in range(4)])
    perm1 = np.concatenate([permy, 1024 + permo])
    shared = {"sbm": kc["sbm"], "dwm": kc["dwm"], "tri": kc["tri"],
              "t0_Wout": _tile_w(wout0[perm0]), "t0_ln1g": g("even_ln_mix_g")[0], "t0_ln1b": g("even_ln_mix_b")[0],
              "t0_ln2g": g("even_ln_ffn_g")[0], "t0_ln2b": g("even_ln_ffn_b")[0],
              "t0_W1": _tile_w(g("even_ffn_w1")[0])[None], "t0_W3": _tile_w(g("even_ffn_w3")[0])[None], "t0_W2": g("even_ffn_w2"),
              "s5_iota": sc["iota"], "gd_normw": g("odd_gdn_norm_w")[0], "gd_gmask": gc["gmask"], "gd_mrow": gc["mrow"],
              "t1_gluw": _tile_w(g("odd_glu_w")[0][permy][:, permy]), "t1_glub": _c(g("odd_glu_b")[0][permy].reshape(8, 128).T),
              "t1_Wout": _tile_w(wout1[perm1]), "t1_ln1g": g("odd_ln_mix_g")[0], "t1_ln1b": g("odd_ln_mix_b")[0],
              "t1_ln2g": g("odd_ln_ffn_g")[0], "t1_ln2b": g("odd_ln_ffn_b")[0],
              "t1_W1": np.stack([_tile_w(w) for w in g("odd_moe_w1")[0]]), "t1_W3": np.stack([_tile_w(w) for w in g("odd_moe_w3")[0]]),
              "t1_W2": g("odd_moe_w2")[0],
              "t1_Wr": _c(g("odd_router_w")[0].reshape(16, 128, 8).transpose(1, 0, 2).reshape(128, 128))}
    lre, lim, ldt = g("odd_ssm_lam_re")[0], g("odd_ssm_lam_im")[0], g("odd_ssm_log_dt")[0]
    bre, bim, cre, cim, dsk = g("odd_ssm_b_re")[0], g("odd_ssm_b_im")[0], g("odd_ssm_c_re")[0], g("odd_ssm_c_im")[0], g("odd_ssm_d")[0]
    cw = g("odd_gdn_conv_w")[0].reshape(4, 3, 8, 128)
    alog, dtb = g("odd_gdn_a_log")[0], g("odd_gdn_dt_bias")[0]
    xT = [_c(x0[b].T) for b in range(B)]
    in_maps = []
    for c in range(NCORES):
        b, r = c // 4, c % 4
        m = dict(shared)
        m["xTb"] = xT[b]
        m["xtok"] = _c(x0[b, r * TPC:(r + 1) * TPC])
        units = ((0, 2 * r), (0, 2 * r + 1), (1, 2 * r), (1, 2 * r + 1))
        cols = []
        for kind, h in units:
            cols += [np.arange(kind * 3072 + h * 128, kind * 3072 + (h + 1) * 128),
                     np.arange(kind * 3072 + 1024 + h * 128, kind * 3072 + 1024 + (h + 1) * 128)]
        for kind, h in units:
            cols.append(np.arange(kind * 3072 + 2048 + h * 128, kind * 3072 + 2048 + (h + 1) * 128))
        m["Win0"] = _tile_flat(w_in0[:, np.concatenate(cols)], [(0, 1024), (1024, 512)])
        hh = (2 * r, 2 * r + 1)
        cols = [np.arange(r * 256, (r + 1) * 256)]
        for h in hh:
            for i in range(3):
                cols.append(np.arange(1024 + i * 1024 + h * 128, 1024 + i * 1024 + (h + 1) * 128))
        for h in hh:
            cols.append(np.array([5128 + h, 5120 + h]))
        for h in hh:
            cols.append(np.arange(4096 + h * 128, 4096 + (h + 1) * 128))
        m["Win1"] = _tile_flat(w_in1[:, np.concatenate(cols)], [(0, L1COLS), (L1COLS, 256)])
        grp = np.arange(16 * r, 16 * r + 16)
        m["s5_lam"] = _c(np.stack([lre[grp], lim[grp], np.broadcast_to(ldt[grp][:, None], (16, 64))], -1).reshape(8, 128, 3))
        m["s5_Bm"] = _c(np.concatenate([bre[grp], bim[grp]], -1).reshape(8, 128, 32))
        m["s5_Cm"] = _c(np.concatenate([cre[grp].transpose(0, 2, 1), cim[grp].transpose(0, 2, 1)], -1).reshape(8, 128, 32))
        m["s5_Dk"] = _c(dsk[r * 256:(r + 1) * 256].reshape(8, 32, 1))
        m["gd_convw"] = _c(np.stack([cw[:, :, h, :].transpose(2, 1, 0).reshape(128, 12) for h in hh]))
        m["gd_hsc"] = _c(np.stack([alog[list(hh)], dtb[list(hh)]], 1))
        in_maps.append(m)
    res = run_bass_kernel_spmd(nc, in_maps, core_ids=list(range(NCORES)))
    outs = [np.asarray(res.results[c]["out"]) for c in range(NCORES)]
    return _c(np.concatenate(outs, axis=0).reshape(B, S, D).astype(f32, copy=False))
```

```python
import os
import numpy as np
import ml_dtypes
import concourse.bass as bass
import concourse.mybir as mybir
from concourse.bass_utils import run_bass_kernel_spmd

F32 = mybir.dt.float32
BF16 = mybir.dt.bfloat16
I32 = mybir.dt.int32
AF = mybir.ActivationFunctionType
ALU = mybir.AluOpType
AX = mybir.AxisListType

NCORES = 8
D = 2048
B = 2
S = 8192
NTOK = B * S
TPC = NTOK // NCORES
ALPHA = (2 * 2) ** 0.25
LN_EPS = 1e-5
RMS_EPS = 1e-6
SEM_ROLL = 30000


class Buf:
    __slots__ = ("name", "w", "r", "dsem", "dcnt")

    def __init__(self, name):
        self.name = name
        self.w = None
        self.r = {}
        self.dsem = None
        self.dcnt = 0


class Prog:
    def __init__(self, nc, stack):
        self.nc = nc
        self.stack = stack
        self.eng = {"pe": nc.tensor, "act": nc.scalar, "dve": nc.vector, "pool": nc.gpsimd, "sp": nc.sync}
        self.cur = {}
        self.cnt = {}
        self.waited = {e: {} for e in self.eng}
        self.nsem = 0
        self.done_sems = []
        self.dma_events = {}
        self.dbufs = []
        self.dsem_pool = []
        for e in self.eng:
            self._roll(e)
        self.ninstr = 0

    def _newsem(self, name):
        self.nsem += 1
        return self.stack.enter_context(self.nc.semaphore(f"{name}_{self.nsem}"))

    def _roll(self, e):
        if e in self.cur:
            self.done_sems.append((self.cur[e], self.cnt[e]))
        self.cur[e] = self._newsem("e" + e)
        self.cnt[e] = 0

    def _dsem(self, buf):
        if buf.dsem is None:
            if self.dsem_pool:
                buf.dsem, buf.dcnt = self.dsem_pool.pop()
            else:
                buf.dsem, buf.dcnt = self._newsem("d"), 0
            self.dbufs.append(buf)

    def full_barrier(self, recycle=True):
        deps = list(self.done_sems) + [(self.cur[e], self.cnt[e]) for e in self.eng if self.cnt[e] > 0]
        deps += list(self.dma_events.items())
        for e in self.eng:
            self._wait(e, deps)
        if recycle:
            for b in self.dbufs:
                self.dsem_pool.append((b.dsem, b.dcnt))
                b.dsem = None
            self.dbufs = []

    def _wait(self, e, deps):
        w = self.waited[e]
        best = {}
        for sem, val in deps:
            if e == "pe" and sem is self.cur["pe"]:
                continue
            if w.get(sem, 0) >= val:
                continue
            if best.get(sem, (None, 0))[1] < val:
                best[sem] = (sem, val)
        for sem, val in best.values():
            self.eng[e].wait_ge(sem, val)
            w[sem] = val
            self.ninstr += 1

    @staticmethod
    def _deps(reads, writes):
        deps = []
        for b in reads:
            if b.w is not None:
                deps.append(b.w)
        for b in writes:
            if b.w is not None:
                deps.append(b.w)
            for s, v in b.r.items():
                deps.append((s, v))
        return deps

    @staticmethod
    def _record(ev, reads, writes):
        for b in reads:
            if b.r.get(ev[0], 0) < ev[1]:
                b.r[ev[0]] = ev[1]
        for b in writes:
            b.w = ev
            b.r = {}

    def op(self, e, fn, reads=(), writes=()):
        self._wait(e, self._deps(reads, writes))
        if self.cnt[e] >= SEM_ROLL:
            self._roll(e)
        ins = fn(self.eng[e])
        self.cnt[e] += 1
        ev = (self.cur[e], self.cnt[e])
        ins.then_inc(ev[0], 1)
        self.ninstr += 1
        self._record(ev, reads, writes)
        return ev

    def dma(self, q, out, in_, reads=(), writes=(), **kw):
        prim = writes[0] if writes else reads[0]
        deps = []
        for b in reads:
            if b.w is not None:
                deps.append(b.w)
        for b in writes:
            if b.w is not None and not (b.dsem is not None and b.w[0] is b.dsem):
                deps.append(b.w)
            for s, v in b.r.items():
                deps.append((s, v))
        self._wait(q, deps)
        self._dsem(prim)
        ins = self.eng[q].dma_start(out=out, in_=in_, **kw)
        prim.dcnt += 16
        ev = (prim.dsem, prim.dcnt)
        ins.then_inc(ev[0], 16)
        self.dma_events[ev[0]] = ev[1]
        self.ninstr += 1
        self._record(ev, reads, writes)
        return ev

    def collective(self, kind, groups, in_ap, out_ap, reads, writes):
        self._wait("pool", self._deps(reads, writes))
        sem = self._newsem("cc")
        ins = self.eng["pool"].collective_compute(kind, ALU.bypass, replica_groups=groups, ins=[in_ap], outs=[out_ap])
        ins.then_inc(sem)
        ev = (sem, 1)
        self.dma_events[sem] = 1
        self._record(ev, reads, writes)
        return ev

    def wait_all(self, e, bufs):
        deps = []
        for b in bufs:
            if b.w is not None:
                deps.append(b.w)
            deps.extend(b.r.items())
        self._wait(e, deps)


class Ctx:
    def __init__(self):
        import contextlib

        self.nc = bass.Bass("TRN2", target_bir_lowering=False)
        self.stack = contextlib.ExitStack()
        self.P = Prog(self.nc, self.stack)
        self.n = 0
        self.root = self.stack
        self.override = {}
        self.fused = False
        self._banks = None

    def sb(self, shape, dt, name=None):
        self.n += 1
        t = self.stack.enter_context(self.nc.sbuf_tensor(f"{name or 't'}_{self.n}", list(shape), dt))
        return t

    def psum_banks(self):
        if self._banks is None:
            banks = []
            for i in range(8):
                t = self.root.enter_context(self.nc.psum_tensor(f"ps{i}", [128, 512], F32))
                banks.append((t, Buf(f"ps{i}")))
            self._banks = banks
        return self._banks

    def din(self, name, shape, dt=F32):
        if name in self.override:
            ap = self.override[name]
            assert list(ap.shape) == list(shape), (name, ap.shape, shape)
            return ap
        return self.nc.dram_tensor(name, list(shape), dt, kind="ExternalInput").ap()

    def dout(self, name, shape, dt=F32):
        if name in self.override:
            ap = self.override[name]
            assert list(ap.shape) == list(shape), (name, ap.shape, shape)
            return ap
        return self.nc.dram_tensor(name, list(shape), dt, kind="ExternalOutput").ap()

    def scratch(self, name, shape, dt):
        return self.nc.dram_tensor(name, list(shape), dt, kind="Internal").ap()

    def begin_phase(self, override):
        import contextlib

        self.fused = True
        self.override = override
        self.stack = contextlib.ExitStack()

    def finish(self, bufs):
        if self.fused:
            self.P.full_barrier()
            self.stack.close()
            self.stack = self.root
            self.override = {}
        else:
            self.P.wait_all("sp", bufs)
            self.stack.close()

    def close(self):
        self.stack.close()


def bf16_view(a):
    return np.ascontiguousarray(a).view(ml_dtypes.bfloat16) if a.dtype == np.uint16 else a


def build_k1(F_out, ntok=TPC, env=None):
    c = env or Ctx()
    nc, P = c.nc, c.P
    xT = c.din("xT", [D, ntok])
    W = c.din("W", [D, F_out])
    hT = c.dout("hT", [F_out, ntok], BF16)
    KT = D // 128
    xb = c.sb([128, KT, ntok], BF16, "xb")
    xb_buf = [Buf(f"xb{k}") for k in range(KT)]
    for k in range(KT):
        P.dma("pool", xb[:, k, :], xT[k * 128:(k + 1) * 128, :], writes=[xb_buf[k]], max_dma_last_dim=8192)
    banks = c.psum_banks()
    FB = 512
    nfb = (F_out + FB - 1) // FB
    wt = [(c.sb([128, KT, FB], BF16, "wt"), Buf(f"wt{i}")) for i in range(2)]
    NT = ntok // 512
    ot = [(c.sb([128, ntok], BF16, "ot"), [Buf(f"ot{i}_{t}") for t in range(NT)]) for i in range(2)]
    Wv = W.rearrange("(kt p) f -> p kt f", p=128)
    bi = 0
    oi = 0
    for fb in range(nfb):
        f0 = fb * FB
        fw = min(FB, F_out - f0)
        wtile, wbuf = wt[fb % 2]
        for k in range(KT):
            P.dma("pool", wtile[:, k, :fw], Wv[:, k, f0:f0 + fw], writes=[wbuf], max_dma_last_dim=8192)
        for fc in range((fw + 127) // 128):
            m = min(128, fw - fc * 128)
            otile, obufs = ot[oi % 2]
            oi += 1
            for t in range(NT):
                ps, pbuf = banks[bi % 8]
                bi += 1
                for k in range(KT):
                    P.op("pe", lambda e, k=k, ps=ps, t=t: e.matmul(
                        ps[:m, :], wtile[:, k, fc * 128:fc * 128 + m], xb[:, k, t * 512:(t + 1) * 512],
                        start=(k == 0), stop=(k == KT - 1)),
                        reads=[wbuf, xb_buf[k]], writes=[pbuf])
                if t % 2 == 0:
                    P.op("act", lambda e, ps=ps, t=t: e.copy(otile[:m, t * 512:(t + 1) * 512], ps[:m, :]),
                         reads=[pbuf], writes=[obufs[t]])
                else:
                    P.op("dve", lambda e, ps=ps, t=t: e.tensor_copy(otile[:m, t * 512:(t + 1) * 512], ps[:m, :]),
                         reads=[pbuf], writes=[obufs[t]])
            r0 = f0 + fc * 128
            P.dma("sp", hT[r0:r0 + m, :], otile[:m, :], reads=obufs)
    c.finish([b for _, bs in ot for b in bs])
    return nc


def _barrier(P, bufs):
    for e in ("pe", "act", "dve", "pool", "sp"):
        P.wait_all(e, bufs)


def _layer_norm(c, P, y, ybuf, g_t, b_t, gbuf, small, sbuf_small):
    stats, mv, sd = small
    nchunk = D // 512
    for k in range(nchunk):
        P.op("dve", lambda e: e.bn_stats(stats[:, k, :], y[:, k * 512:(k + 1) * 512]), reads=[ybuf], writes=[sbuf_small])
    P.op("dve", lambda e: e.bn_aggr(mv[:, :], stats[:, :, :]), reads=[sbuf_small], writes=[sbuf_small])
    P.op("dve", lambda e: e.tensor_scalar(out=sd[:, :], in0=mv[:, 1:2], scalar1=LN_EPS, scalar2=None, op0=ALU.add),
         reads=[sbuf_small], writes=[sbuf_small])
    P.op("act", lambda e: e.activation(out=sd[:, :], in_=sd[:, :], func=AF.Sqrt), reads=[sbuf_small], writes=[sbuf_small])
    P.op("dve", lambda e: e.reciprocal(sd[:, :], sd[:, :]), reads=[sbuf_small], writes=[sbuf_small])
    P.op("dve", lambda e: e.tensor_scalar(out=y, in0=y, scalar1=mv[:, 0:1], scalar2=sd[:, 0:1],
                                          op0=ALU.subtract, op1=ALU.mult), reads=[ybuf, sbuf_small], writes=[ybuf])
    P.op("dve", lambda e: e.tensor_tensor(out=y, in0=y, in1=g_t[:, :], op=ALU.mult), reads=[ybuf, gbuf], writes=[ybuf])
    P.op("dve", lambda e: e.tensor_tensor(out=y, in0=y, in1=b_t[:, :], op=ALU.add), reads=[ybuf, gbuf], writes=[ybuf])


def build_tail(moe, ntok=TPC, TG=1024, n_exp=None, FF=None, glu=False, env=None, tok_off=None, src_ntok=None, xT_out=False):
    c = env or Ctx()
    nc, P = c.nc, c.P
    E = (8 if moe else 1) if n_exp is None else n_exp
    if FF is None:
        FF = 7168 if moe else 5632
    KT = D // 128
    src_ntok = src_ntok or ntok
    import concourse.bass as _b

    def tsl(t0_):
        return slice(t0_, t0_ + TG) if tok_off is None else _b.ds(tok_off + t0_, TG)

    if xT_out:
        xTo = c.dout("xTo", [D, ntok], BF16)
        xts = c.sb([128, KT, 128], BF16, "xts")
        xtsb = Buf("xts")
    if glu:
        yT_d = c.din("yT", [1024, src_ntok])
        odT_d = c.din("odT", [1024, src_ntok], BF16)
        gluw = c.din("gluw", [1024 // 256, 128, 8, 256])
        glub_d = c.din("glub", [128, 8])
    else:
        oT = c.din("oT", [D, src_ntok], BF16)
    x = c.din("x", [ntok, D])
    Wout = c.din("Wout", [D // 256, 128, KT, 256])
    lng = [c.din(f"ln{i}g", [D]) for i in (1, 2)]
    lnb = [c.din(f"ln{i}b", [D]) for i in (1, 2)]
    W1 = c.din("W1", [E, FF // 256, 128, KT, 256])
    W3 = c.din("W3", [E, FF // 256, 128, KT, 256])
    W2 = c.din("W2", [E, FF, D])
    if moe:
        Wr = c.din("Wr", [128, KT * 8])
    xo = c.dout("xo", [ntok, D])
    NTT = TG // 128
    NH = TG // 512
    banks = c.psum_banks()
    bi = [0]

    def bank():
        b = banks[bi[0] % 8]
        bi[0] += 1
        return b

    yacc = c.sb([128, NTT, D], F32, "yacc")
    ybuf = [Buf(f"y{t}") for t in range(NTT)]
    x1Traw = c.sb([128, KT * TG], BF16, "x1T")
    x1T = x1Traw[:, :].rearrange("p (k t) -> p k t", k=KT)
    x1Tbuf = [Buf(f"x1T{t}") for t in range(NTT)]
    if glu:
        ystage = x1Traw[:, :].bitcast(F32).rearrange("p (k t) -> p k t", k=8)
        ysbuf = Buf("ystage")
        glub = c.sb([128, 8], F32, "glub")
        glubuf = Buf("glub")
        P.dma("sp", glub[:, :], glub_d, writes=[glubuf])
        obz, obg = Buf("obz"), Buf("obg")
        gsc1 = c.sb([128, TG], F32, "gsc1")
        gsc2 = c.sb([128, TG], F32, "gsc2")
        gscb, gscb2 = Buf("gsc1"), Buf("gsc2")
    R = c.sb([128, KT * TG], BF16, "R")
    ob = R[:, :].rearrange("p (k t) -> p k t", k=KT)
    obuf = Buf("ob")
    FBK = 256
    NFC = FBK // 128
    w2blk = [R[:, i * NFC * D:(i + 1) * NFC * D].rearrange("p (f d) -> p f d", f=NFC) for i in range(2)]
    w2buf = [Buf(f"w2b{i}") for i in range(2)]
    off = 2 * NFC * D
    gT = [R[:, off + i * NFC * TG: off + (i + 1) * NFC * TG].rearrange("p (f t) -> p f t", f=NFC) for i in range(2)]
    gTbuf = [[Buf(f"gT{i}_{j}") for j in range(NFC * NH)] for i in range(2)]
    off += 2 * NFC * TG
    stmp = [R[:, off + i * 512: off + (i + 1) * 512] for i in range(2)]
    stbuf = [Buf(f"st{i}") for i in range(2)]
    wb = [c.sb([128, KT, FBK], BF16, "wb") for _ in range(4)]
    wbuf = [Buf(f"wb{i}") for i in range(4)]
    lnt = [c.sb([128, D], F32, "lnt") for _ in range(2)]
    lnbuf = Buf("lnt")
    stats = c.sb([128, D // 512, 6], F32, "stats")
    mv = c.sb([128, 2], F32, "mv")
    sd = c.sb([128, 1], F32, "sd")
    smallbuf = Buf("small")
    ident = c.sb([128, 128], F32, "ident")
    identbuf = Buf("ident")
    pslock = Buf("pslock")
    P.op("pool", lambda e: e.memset(ident[:, :], 0.0), writes=[identbuf])
    P.op("pool", lambda e: e.affine_select(out=ident[:, :], in_=ident[:, :], pattern=[[-1, 128]], compare_op=ALU.not_equal,
                                           fill=1.0, base=0, channel_multiplier=1), reads=[identbuf], writes=[identbuf])
    if moe:
        xT32 = c.sb([128, KT, 128], F32, "xT32")
        xT32buf = Buf("xT32")
        wr = c.sb([128, KT, 8], F32, "wr")
        wrbuf = Buf("wr")
        P.dma("sp", wr[:, :, :].rearrange("p k e -> p (k e)"), Wr, writes=[wrbuf])
        gates = c.sb([128, NTT, 8], F32, "gates")
        gatebuf = [Buf(f"gate{t}") for t in range(NTT)]
        rt = c.sb([128, 64], F32, "rt")
        rtbuf = Buf("rt")
    allbufs = (ybuf + x1Tbuf + [obuf] + w2buf + [b for g in gTbuf for b in g] + stbuf + wbuf + [lnbuf, smallbuf]
               + [b for _, b in banks])
    if not glu:
        oTv = oT.rearrange("(k p) t -> p k t", p=128)
    wi = [0]

    for tg in range(ntok // TG):
        t0 = tg * TG
        _barrier(P, allbufs)
        if glu:
            P.dma("sp", ystage, yT_d.rearrange("(k p) t -> p k t", p=128)[:, :, tsl(t0)], writes=[ysbuf])
            for k in range(8):
                yk = ystage[:, k, :]
                P.op("act", lambda e: e.activation(out=gsc1[:, :], in_=yk, func=AF.Square), reads=[ysbuf], writes=[gscb])
                P.op("dve", lambda e: e.tensor_scalar(out=gsc1[:, :], in0=gsc1[:, :], scalar1=0.044715, scalar2=1.0, op0=ALU.mult, op1=ALU.add),
                     reads=[gscb], writes=[gscb])
                P.op("dve", lambda e: e.tensor_tensor(out=gsc1[:, :], in0=gsc1[:, :], in1=yk, op=ALU.mult), reads=[gscb, ysbuf], writes=[gscb])
                P.op("act", lambda e: e.activation(out=gsc1[:, :], in_=gsc1[:, :], func=AF.Tanh, scale=float(np.sqrt(2.0 / np.pi))),
                     reads=[gscb], writes=[gscb])
                P.op("act", lambda e: e.mul(gsc2[:, :], yk, 0.5), reads=[ysbuf], writes=[gscb2])
                P.op("dve", lambda e: e.scalar_tensor_tensor(out=ob[:, k, :], in0=gsc1[:, :], scalar=1.0, in1=gsc2[:, :], op0=ALU.add, op1=ALU.mult),
                     reads=[gscb, gscb2], writes=[obz])
            for fcb in range(1024 // FBK):
                wt_, wb_ = wb[wi[0] % 4], wbuf[wi[0] % 4]
                wi[0] += 1
                P.dma("pool", wt_[:, 0:8, :], gluw[fcb], writes=[wb_], max_dma_last_dim=8192)
                for fc in range(NFC):
                    f = fcb * NFC + fc
                    for h in range(NH):
                        ps, pb = bank()
                        for k in range(8):
                            P.op("pe", lambda e: e.matmul(ps[:, :], wt_[:, k, fc * 128:(fc + 1) * 128], ob[:, k, h * 512:(h + 1) * 512],
                                                          start=(k == 0), stop=(k == 7)), reads=[wb_, obz], writes=[pb])
                        P.op("act", lambda e: e.activation(out=ob[:, 8 + f, h * 512:(h + 1) * 512], in_=ps[:, :], func=AF.Sigmoid,
                                                           bias=glub[:, f:f + 1]), reads=[pb, glubuf], writes=[obg])
            P.op("dve", lambda e: e.tensor_tensor(out=ob[:, 0:8, :], in0=ob[:, 0:8, :], in1=ob[:, 8:16, :], op=ALU.mult),
                 reads=[obz, obg], writes=[obz, obuf])
            P.dma("sp", ob[:, 8:16, :], odT_d.rearrange("(k p) t -> p k t", p=128)[:, :, tsl(t0)], reads=[obz], writes=[obg, obuf])
            _barrier(P, allbufs + [ysbuf, obz, obg])
        else:
            P.dma("sp", ob, oTv[:, :, tsl(t0)], writes=[obuf])
        P.dma("sp", lnt[0][:, :], lng[0].partition_broadcast(128), writes=[lnbuf])
        P.dma("sp", lnt[1][:, :], lnb[0].partition_broadcast(128), writes=[lnbuf])
        for tt in range(NTT):
            P.dma("sp", yacc[:, tt, :], x[t0 + tt * 128:t0 + (tt + 1) * 128, :], writes=[ybuf[tt]])
        for cb in range(D // FBK):
            wt_, wb_ = wb[wi[0] % 4], wbuf[wi[0] % 4]
            wi[0] += 1
            P.dma("pool", wt_[:, :, :], Wout[cb], writes=[wb_], max_dma_last_dim=8192)
            for tt in range(NTT):
                ps, pb = bank()
                for k in range(KT):
                    P.op("pe", lambda e: e.matmul(ps[:, :FBK], ob[:, k, tt * 128:(tt + 1) * 128], wt_[:, k, :],
                                                  start=(k == 0), stop=(k == KT - 1)), reads=[obuf, wb_], writes=[pb])
                ysl = yacc[:, tt, cb * FBK:(cb + 1) * FBK]
                P.op("dve", lambda e: e.scalar_tensor_tensor(out=ysl, in0=ysl, scalar=ALPHA, in1=ps[:, :FBK],
                                                             op0=ALU.mult, op1=ALU.add), reads=[pb, ybuf[tt]], writes=[ybuf[tt]])
        for tt in range(NTT):
            y = yacc[:, tt, :]
            _layer_norm(c, P, y, ybuf[tt], lnt[0], lnt[1], lnbuf, (stats, mv, sd), smallbuf)
            for q in range(KT // 4):
                ps, pb = bank()
                for j in range(4):
                    k = q * 4 + j
                    P.op("pe", lambda e: e.transpose(ps[:, j * 128:(j + 1) * 128], y[:, k * 128:(k + 1) * 128], ident[:, :]),
                         reads=[ybuf[tt], identbuf], writes=[pb])
                P.op("act", lambda e: e.copy(x1T[:, q * 4:(q + 1) * 4, tt * 128:(tt + 1) * 128],
                                             ps[:, :].rearrange("p (j t) -> p j t", j=4)), reads=[pb], writes=[x1Tbuf[tt], pslock])
                if moe and not os.environ.get("DBG_NOXT32"):
                    P.op("dve", lambda e: e.tensor_copy(xT32[:, q * 4:(q + 1) * 4, :],
                                                        ps[:, :].rearrange("p (j t) -> p j t", j=4)), reads=[pb], writes=[xT32buf, pslock])
            if moe:
                ps, pb = bank()
                if os.environ.get("DBG_NOROUTER"):
                    P.op("dve", lambda e: e.tensor_copy(ps[:, :8], xT32[:, 0, 0:8]), reads=[xT32buf], writes=[pb])
                else:
                  for k in range(KT):
                    P.op("pe", lambda e: e.matmul(ps[:, :8], xT32[:, k, :], wr[:, k, :], start=(k == 0), stop=(k == KT - 1)),
                         reads=[xT32buf, wrbuf], writes=[pb])
                lg, m1, eq1, l2, m2, eq2, dd, w1, w2 = (rt[:, 0:8], rt[:, 8:9], rt[:, 16:24], rt[:, 24:32], rt[:, 9:10],
                                                        rt[:, 32:40], rt[:, 10:11], rt[:, 11:12], rt[:, 12:13])
                V = lambda fn, rd=(), wr_=(): P.op("dve", fn, reads=[rtbuf] + list(rd), writes=[rtbuf] + list(wr_))
                V(lambda e: e.tensor_copy(lg, ps[:, :8]), rd=[pb])
                V(lambda e: e.reduce_max(m1, lg, axis=AX.X))
                V(lambda e: e.tensor_scalar(out=eq1, in0=lg, scalar1=m1, scalar2=None, op0=ALU.is_equal))
                V(lambda e: e.scalar_tensor_tensor(out=l2, in0=eq1, scalar=-1e30, in1=lg, op0=ALU.mult, op1=ALU.add))
                V(lambda e: e.reduce_max(m2, l2, axis=AX.X))
                V(lambda e: e.tensor_scalar(out=eq2, in0=l2, scalar1=m2, scalar2=None, op0=ALU.is_equal))
                V(lambda e: e.tensor_tensor(out=dd, in0=m2, in1=m1, op=ALU.subtract))
                P.op("act", lambda e: e.activation(out=dd, in_=dd, func=AF.Exp), reads=[rtbuf], writes=[rtbuf])
                V(lambda e: e.tensor_scalar(out=w1, in0=dd, scalar1=1.0, scalar2=None, op0=ALU.add))
                V(lambda e: e.reciprocal(w1, w1))
                V(lambda e: e.tensor_tensor(out=w2, in0=dd, in1=w1, op=ALU.mult))
                V(lambda e: e.tensor_scalar(out=eq1, in0=eq1, scalar1=w1, scalar2=None, op0=ALU.mult))
                V(lambda e: e.scalar_tensor_tensor(out=gates[:, tt, :], in0=eq2, scalar=w2, in1=eq1, op0=ALU.mult, op1=ALU.add),
                  wr_=[gatebuf[tt]])
            P.op("act", lambda e: e.mul(y, y, ALPHA), reads=[ybuf[tt]], writes=[ybuf[tt]])
        _barrier(P, allbufs)
        blk = 0
        for ex in range(E):
            W2v = W2[ex].rearrange("(f p) d -> p f d", p=128)
            for fb in range(FF // FBK):
                w1t, w1b = wb[wi[0] % 4], wbuf[wi[0] % 4]
                w3t, w3b = wb[(wi[0] + 1) % 4], wbuf[(wi[0] + 1) % 4]
                wi[0] += 2
                P.dma("pool", w1t[:, :, :], W1[ex, fb], writes=[w1b], max_dma_last_dim=8192)
                P.dma("pool", w3t[:, :, :], W3[ex, fb], writes=[w3b], max_dma_last_dim=8192)
                w2t, w2b = w2blk[blk % 2], w2buf[blk % 2]
                gt, gb = gT[blk % 2], gTbuf[blk % 2]
                blk += 1
                P.dma("pool", w2t, W2v[:, fb * NFC:(fb + 1) * NFC, :], writes=[w2b], max_dma_last_dim=8192)
                for fc in range(NFC):
                    for h in range(NH):
                        ps1, pb1 = bank()
                        ps3, pb3 = bank()
                        rds = [x1Tbuf[h * 4 + j] for j in range(4)]
                        for k in range(KT):
                            P.op("pe", lambda e: e.matmul(ps1[:, :], w1t[:, k, fc * 128:(fc + 1) * 128], x1T[:, k, h * 512:(h + 1) * 512],
                                                          start=(k == 0), stop=(k == KT - 1)), reads=[w1b] + rds, writes=[pb1])
                        for k in range(KT):
                            P.op("pe", lambda e: e.matmul(ps3[:, :], w3t[:, k, fc * 128:(fc + 1) * 128], x1T[:, k, h * 512:(h + 1) * 512],
                                                          start=(k == 0), stop=(k == KT - 1)), reads=[w3b] + rds, writes=[pb3])
                        si = (fc * NH + h) % 2
                        P.op("act", lambda e: e.activation(out=stmp[si], in_=ps1[:, :], func=AF.Silu), reads=[pb1], writes=[stbuf[si]])
                        P.op("dve", lambda e: e.tensor_tensor(out=gt[:, fc, h * 512:(h + 1) * 512], in0=stmp[si], in1=ps3[:, :], op=ALU.mult),
                             reads=[stbuf[si], pb3], writes=[gb[fc * NH + h]])
                for tt in range(NTT):
                    for dg in range(D // 512):
                        ps, pb = bank()
                        for fc in range(NFC):
                            P.op("pe", lambda e: e.matmul(ps[:, :], gt[:, fc, tt * 128:(tt + 1) * 128], w2t[:, fc, dg * 512:(dg + 1) * 512],
                                                          start=(fc == 0), stop=(fc == NFC - 1)),
                                 reads=[gb[fc * NH + tt // 4], w2b], writes=[pb])
                        ysl = yacc[:, tt, dg * 512:(dg + 1) * 512]
                        if moe and not os.environ.get("DBG_NOGATE"):
                            P.op("dve", lambda e: e.scalar_tensor_tensor(out=ysl, in0=ps[:, :], scalar=gates[:, tt, ex:ex + 1], in1=ysl,
                                                                         op0=ALU.mult, op1=ALU.add),
                                 reads=[pb, ybuf[tt], gatebuf[tt]], writes=[ybuf[tt]])
                        else:
                            P.op("dve", lambda e: e.tensor_tensor(out=ysl, in0=ysl, in1=ps[:, :], op=ALU.add),
                                 reads=[pb, ybuf[tt]], writes=[ybuf[tt]])
        P.dma("sp", lnt[0][:, :], lng[1].partition_broadcast(128), writes=[lnbuf])
        P.dma("sp", lnt[1][:, :], lnb[1].partition_broadcast(128), writes=[lnbuf])
        for tt in range(NTT):
            y = yacc[:, tt, :]
            _layer_norm(c, P, y, ybuf[tt], lnt[0], lnt[1], lnbuf, (stats, mv, sd), smallbuf)
            P.dma("sp", xo[t0 + tt * 128:t0 + (tt + 1) * 128, :], y, reads=[ybuf[tt]])
            if xT_out:
                for q in range(KT // 4):
                    ps, pb = bank()
                    for j in range(4):
                        k = q * 4 + j
                        P.op("pe", lambda e: e.transpose(ps[:, j * 128:(j + 1) * 128], y[:, k * 128:(k + 1) * 128], ident[:, :]),
                             reads=[ybuf[tt], identbuf], writes=[pb])
                    P.op("act", lambda e: e.copy(xts[:, q * 4:(q + 1) * 4, :], ps[:, :].rearrange("p (j t) -> p j t", j=4)),
                         reads=[pb], writes=[xtsb])
                P.dma("sp", xTo.rearrange("(k p) t -> p k t", p=128)[:, :, t0 + tt * 128:t0 + (tt + 1) * 128], xts[:, :, :], reads=[xtsb])
    c.finish(ybuf + ([xtsb] if xT_out else []))
    return nc


def dw_mult(delta):
    d = np.asarray(delta)
    m = ((d >= 0) & (d <= 128)).astype(np.float32)
    m += ((d >= 0) & (d <= 512) & (d % 4 == 0))
    m += ((d >= 0) & (d <= 2048) & (d % 16 == 0))
    return m


QG = 512


def k2_consts():
    bpg = QG // 128
    s = np.arange(128)[:, None]
    t = np.arange(QG)[None, :]
    sbm = np.stack([((128 * r + s) < t) for r in range(bpg)]).astype(np.float32)
    dwm = np.stack([dw_mult(t - s - 128 * r) for r in range(-16, bpg)])
    tri = (np.arange(128)[:, None] >= np.arange(128)[None, :]).astype(np.float32)
    bf = ml_dtypes.bfloat16
    return {"sbm": np.ascontiguousarray(sbm.transpose(1, 0, 2)).astype(bf),
            "dwm": np.ascontiguousarray(dwm.transpose(1, 0, 2)).astype(bf),
            "tri": tri.astype(bf)}


def build_k2(seq=S, units=(0, 0, 1, 1), env=None, on_unit_done=None):
    c = env or Ctx()
    nc, P = c.nc, c.P
    NU = len(units)
    NB = seq // 128
    NG = seq // QG
    BPG = QG // 128
    NBUF = 1024 // QG
    NSB = 6
    scale = 128 ** -0.5
    qT = c.din("q", [NU, 128, seq], BF16)
    kT = c.din("k", [NU, 128, seq], BF16)
    vv = c.din("v", [NU, 128, NB, 128], BF16)
    sbm_d = c.din("sbm", [128, BPG, QG], BF16)
    dwm_d = c.din("dwm", [128, 16 + BPG, QG], BF16)
    tri_d = c.din("tri", [128, 128], BF16)
    oT = c.dout("oT", [NU, 128, seq], BF16)
    banks = c.psum_banks()

    def subs(bk):
        out = []
        for t_, _ in bk:
            for h in range(512 // QG):
                out.append((t_[:, h * QG:(h + 1) * QG], Buf("pssub")))
        return out

    zb, cb, sb_ = subs(banks[0:2]), subs(banks[2:4]), subs(banks[4:6])
    ob = [(banks[6][0][:, 0:QG], banks[6][1]), (banks[7][0][:, 0:QG], banks[7][1])]
    sbm = c.sb([128, BPG, QG], BF16, "sbm")
    dwm = c.sb([128, 16 + BPG, QG], BF16, "dwm")
    tri = c.sb([128, 128], BF16, "tri")
    ones = c.sb([128, 128], BF16, "ones")
    cbuf = Buf("consts")
    P.dma("sp", sbm[:, :, :], sbm_d, writes=[cbuf])
    P.dma("sp", dwm[:, :, :], dwm_d, writes=[cbuf])
    P.dma("sp", tri[:, :], tri_d, writes=[cbuf])
    P.op("pool", lambda e: e.memset(ones[:, :], 1.0), writes=[cbuf])
    qs = c.sb([128, seq], BF16, "qs")
    kraw = c.sb([128, seq], BF16, "kraw")
    ks = c.sb([128, seq], BF16, "ks")
    nks = c.sb([128, seq], BF16, "nks")
    vs = c.sb([128, NB, 128], BF16, "vs")
    os_ = c.sb([128, seq], BF16, "os")
    qbuf, krbuf, kbuf, vbuf = Buf("q"), Buf("kraw"), Buf("k"), Buf("v")
    obufs = [Buf(f"o{g}") for g in range(NG)]
    mk = lambda dt, nm: [(c.sb([128, QG], dt, nm), Buf(f"{nm}{i}")) for i in range(NSB)]
    et, spt, spm, tmp, wt, wm = mk(F32, "et"), mk(BF16, "spt"), mk(BF16, "spm"), mk(F32, "tmp"), mk(BF16, "wt"), mk(BF16, "wm")
    Cs = [(c.sb([128, QG], F32, "Csb"), Buf(f"C{i}")) for i in range(2)]
    rec = c.sb([128, QG], F32, "rec")
    recbuf = Buf("rec")
    it = 0
    for u, kind in enumerate(units):
        P.dma("sp", qs[:, :], qT[u], writes=[qbuf])
        P.dma("sp", kraw[:, :], kT[u], writes=[krbuf])
        P.dma("sp", vs[:, :, :], vv[u], writes=[vbuf])
        P.op("act", lambda e: e.mul(ks[:, :], kraw[:, :], scale), reads=[krbuf], writes=[kbuf])
        if kind == 0:
            P.op("pool", lambda e: e.tensor_scalar(out=nks[:, :], in0=ks[:, :], scalar1=-1.0, scalar2=None, op0=ALU.mult),
                 reads=[kbuf], writes=[kbuf])
        work = []
        for g in range(NG):
            q0 = g * QG
            qsl = qs[:, q0:q0 + QG]
            o_ps, o_pb = ob[g % 2]
            Csb, Cbuf = Cs[g % 2]
            if kind == 0:
                jlist = list(range(BPG * g + BPG - 1, -1, -1))
                for n, j in enumerate(jlist):
                    it += 1
                    bp_ = it % NBUF
                    b_ = it % NSB
                    r = j - BPG * g
                    z_ps, z_pb = zb[bp_]
                    c_ps, c_pb = cb[bp_]
                    s_ps, s_pb = sb_[bp_]
                    e_t, e_b = et[b_]
                    sp_t, sp_b = spt[b_]
                    sm_t, sm_b = spm[b_] if r >= 0 else (sp_t, sp_b)
                    w_t, w_b = wt[b_]
                    t_t, t_b = tmp[b_]
                    wm_t, wm_b = wm[b_] if r >= 0 else (w_t, w_b)
                    last = (n == len(jlist) - 1)

                    def stA0(j=j, qsl=qsl, z_ps=z_ps, z_pb=z_pb, e_t=e_t, e_b=e_b):
                        P.op("pe", lambda e: e.matmul(z_ps, ks[:, j * 128:(j + 1) * 128], qsl, start=True, stop=True), reads=[kbuf, qbuf], writes=[z_pb])
                        P.op("act", lambda e: e.activation(out=e_t[:, :], in_=z_ps, func=AF.Exp), reads=[z_pb], writes=[e_b])

                    def stA(r=r, e_t=e_t, e_b=e_b, sp_t=sp_t, sp_b=sp_b, sm_t=sm_t, sm_b=sm_b):
                        P.op("act", lambda e: e.activation(out=sp_t[:, :], in_=e_t[:, :], func=AF.Ln, bias=1.0), reads=[e_b], writes=[sp_b])
                        if r >= 0:
                            P.op("pool", lambda e: e.tensor_tensor(out=sm_t[:, :], in0=sp_t[:, :], in1=sbm[:, r, :], op=ALU.mult),
                                 reads=[sp_b, cbuf], writes=[sm_b])

                    def stB(j=j, n=n, last=last, qsl=qsl, c_ps=c_ps, c_pb=c_pb, s_ps=s_ps, s_pb=s_pb, sm_t=sm_t, sm_b=sm_b, t_t=t_t, t_b=t_b,
                            Csb=Csb, Cbuf=Cbuf):
                        P.op("pe", lambda e: e.matmul(c_ps, tri[:, :], sm_t[:, :], start=True, stop=False), reads=[cbuf, sm_b], writes=[c_pb])
                        P.op("pe", lambda e: e.matmul(c_ps, nks[:, j * 128:(j + 1) * 128], qsl, start=False, stop=True),
                             reads=[kbuf, qbuf], writes=[c_pb])
                        if not last:
                            P.op("pe", lambda e: e.matmul(s_ps, ones[:, :], sm_t[:, :], start=True, stop=True), reads=[cbuf, sm_b], writes=[s_pb])
                        if n == 0:
                            if not last:
                                P.op("dve", lambda e: e.tensor_copy(Csb[:, :], s_ps), reads=[s_pb], writes=[Cbuf])
                        else:
                            P.op("dve", lambda e: e.tensor_tensor(out=t_t[:, :], in0=c_ps, in1=Csb[:, :], op=ALU.add),
                                 reads=[c_pb, Cbuf], writes=[t_b])
                            if not last:
                                P.op("dve", lambda e: e.tensor_tensor(out=Csb[:, :], in0=s_ps, in1=Csb[:, :], op=ALU.add),
                                     reads=[s_pb, Cbuf], writes=[Cbuf])

                    def stC(j=j, n=n, r=r, last=last, g=g, q0=q0, c_ps=c_ps, c_pb=c_pb, t_t=t_t, t_b=t_b, w_t=w_t, w_b=w_b, wm_t=wm_t, wm_b=wm_b,
                            o_ps=o_ps, o_pb=o_pb):
                        if n == 0:
                            P.op("act", lambda e: e.activation(out=w_t[:, :], in_=c_ps, func=AF.Exp, scale=-1.0), reads=[c_pb], writes=[w_b])
                        else:
                            P.op("act", lambda e: e.activation(out=w_t[:, :], in_=t_t[:, :], func=AF.Exp, scale=-1.0), reads=[t_b], writes=[w_b])
                        if r >= 0:
                            P.op("pool", lambda e: e.tensor_tensor(out=wm_t[:, :], in0=w_t[:, :], in1=sbm[:, r, :], op=ALU.mult),
                                 reads=[w_b, cbuf], writes=[wm_b])
                        P.op("pe", lambda e: e.matmul(o_ps, vs[:, j, :], wm_t[:, :], start=(n == 0), stop=last), reads=[vbuf, wm_b], writes=[o_pb])
                        if last:
                            P.op("act", lambda e: e.copy(os_[:, q0:q0 + QG], o_ps), reads=[o_pb], writes=[obufs[g]])

                    work.append((stA0, stA, stB, stC))
            else:
                jlist = list(range(max(0, BPG * g - 16), BPG * g + BPG))
                d_ps, d_pb = cb[g % NBUF]
                for n, j in enumerate(jlist):
                    it += 1
                    b_ = it % NSB
                    r = j - BPG * g
                    z_ps, z_pb = zb[it % NBUF]
                    w_t, w_b = wt[b_]
                    wm_t, wm_b = wm[b_]
                    first, last = (n == 0), (n == len(jlist) - 1)

                    def stA(j=j, r=r, qsl=qsl, z_ps=z_ps, z_pb=z_pb, w_t=w_t, w_b=w_b):
                        P.op("pe", lambda e: e.matmul(z_ps, ks[:, j * 128:(j + 1) * 128], qsl, start=True, stop=True),
                             reads=[kbuf, qbuf], writes=[z_pb])
                        P.op("act", lambda e: e.activation(out=w_t[:, :], in_=z_ps, func=AF.Exp), reads=[z_pb], writes=[w_b])

                    def stB(r=r, w_t=w_t, w_b=w_b, wm_t=wm_t, wm_b=wm_b):
                        P.op("dve", lambda e: e.tensor_tensor(out=wm_t[:, :], in0=w_t[:, :], in1=dwm[:, r + 16, :], op=ALU.mult),
                             reads=[w_b, cbuf], writes=[wm_b])

                    def stC(j=j, first=first, last=last, g=g, q0=q0, wm_t=wm_t, wm_b=wm_b, o_ps=o_ps, o_pb=o_pb, d_ps=d_ps, d_pb=d_pb):
                        P.op("pe", lambda e: e.matmul(o_ps, vs[:, j, :], wm_t[:, :], start=first, stop=last), reads=[vbuf, wm_b], writes=[o_pb])
                        P.op("pe", lambda e: e.matmul(d_ps, ones[:, :], wm_t[:, :], start=first, stop=last), reads=[cbuf, wm_b], writes=[d_pb])
                        if last:
                            P.op("dve", lambda e: e.reciprocal(rec[:, :], d_ps), reads=[d_pb], writes=[recbuf])
                            P.op("dve", lambda e: e.tensor_tensor(out=os_[:, q0:q0 + QG], in0=o_ps, in1=rec[:, :], op=ALU.mult),
                                 reads=[o_pb, recbuf], writes=[obufs[g]])

                    work.append((lambda: None, stA, stB, stC))
        N_ = len(work)
        for t in range(N_ + 3):
            for st in range(4):
                if 0 <= t - st < N_:
                    work[t - st][st]()
        odram = Buf(f"odram{u}")
        P.dma("sp", oT[u], os_[:, :], reads=obufs, writes=[odram])
        if on_unit_done is not None:
            on_unit_done(u, odram)
    c.finish(obufs)
    return nc


TWO_PI = 2.0 * np.pi


def s5_consts(L=64):
    return {"iota": np.tile(np.arange(L, dtype=np.float32), (128, 1))}


def build_s5(seq=S, npairs=8, L=64, env=None):
    c = env or Ctx()
    nc, P = c.nc, c.P
    NCH = seq // L
    NH = max(1, seq // 4096)
    HL = seq // NH
    NCHh = HL // L
    TPB = max(1, min(L, 512 // NCHh))
    uT = c.din("uT", [npairs, 32, seq], BF16)
    lam = c.din("lam", [npairs, 128, 3])
    Bm = c.din("Bm", [npairs, 128, 32])
    Cm = c.din("Cm", [npairs, 128, 32])
    Dk = c.din("Dk", [npairs, 32, 1])
    iota_d = c.din("iota", [128, L])
    yT = c.dout("yT", [npairs, 32, seq])
    banks = c.psum_banks()
    bi = [0]

    def bank():
        b = banks[bi[0] % 8]
        bi[0] += 1
        return b

    iota = c.sb([128, L], F32, "iota")
    iota1 = c.sb([128, L], F32, "iota1")
    ident = c.sb([128, 128], F32, "ident")
    cb = Buf("const")
    P.dma("sp", iota[:, :], iota_d, writes=[cb])
    P.op("pool", lambda e: e.memset(ident[:, :], 0.0), writes=[cb])
    P.op("pool", lambda e: e.affine_select(out=ident[:, :], in_=ident[:, :], pattern=[[-1, 128]], compare_op=ALU.not_equal,
                                           fill=1.0, base=0, channel_multiplier=1), reads=[cb], writes=[cb])
    P.op("dve", lambda e: e.tensor_scalar(out=iota1[:, :], in0=iota[:, :], scalar1=1.0, scalar2=None, op0=ALU.add), reads=[cb], writes=[cb])
    sm = c.sb([128, 64], F32, "sm")
    bm = c.sb([128, 32], F32, "bm")
    cm = c.sb([128, 32], F32, "cm")
    kbm = c.sb([128, 32], F32, "kbm")
    dk = c.sb([32, 1], F32, "dk")
    pre = Buf("pre")
    rr_f = c.sb([128, max(L, NCH)], F32, "rr_f")
    rr_i = c.sb([128, max(L, NCH)], I32, "rr_i")
    rr_g = c.sb([128, max(L, NCH)], F32, "rr_g")
    sint = c.sb([128, L], F32, "sint")
    cost = c.sb([128, L], F32, "cost")
    rpt = c.sb([128, L], F32, "rpt")
    G = [c.sb([128, L, 32], F32, "G") for _ in range(2)]
    Gb = Buf("G")
    Bt = [c.sb([32, L, 128], BF16, "Bt") for _ in range(2)]
    Btb = [Buf("Bt0"), Buf("Bt1")]
    Ct = [c.sb([128, L, 32], BF16, "Ct") for _ in range(4)]
    Ctb = Buf("Ct")
    pt = [c.sb([128, L, 16], F32, "pt") for _ in range(3)]
    for t_ in G:
        P.op("pool", lambda e: e.memset(t_[:, :, :], 0.0), writes=[Gb])
    for t_ in Ct:
        P.op("pool", lambda e: e.memset(t_[:, :, :], 0.0), writes=[Ctb])
    ub = c.sb([32, seq], BF16, "ub")
    ubuf = Buf("u")
    cc = [c.sb([128, HL], F32, "cc") for _ in range(2)]
    ccb = [Buf("cre"), Buf("cim")]
    at = c.sb([128, HL], F32, "at")
    atb = Buf("a")
    wb = [c.sb([128, seq], BF16, "wb") for _ in range(2)]
    wbb = [Buf("wre"), Buf("wim")]
    ysb = c.sb([32, HL], F32, "ysb")
    ybuf = Buf("y")
    ff = [c.sb([128, NCH], F32, "ff") for _ in range(2)]
    EE = [c.sb([128, NCH], F32, "EE") for _ in range(2)]
    gg = [c.sb([128, NCH], F32, "gg") for _ in range(2)]
    Rb = c.sb([128, NCH], F32, "Rb")
    Zb = [c.sb([128, NCH], BF16, "Zb") for _ in range(2)]
    chb = Buf("chunk")
    Zbb = Buf("Z")

    def V(fn, rd=(), wr=()):
        return P.op("dve", fn, reads=[pre] + list(rd), writes=[pre] + list(wr))

    def A(fn, rd=(), wr=()):
        return P.op("act", fn, reads=[pre] + list(rd), writes=[pre] + list(wr))

    def col(i):
        return sm[:, i:i + 1]

    def sincos(arg, n, s_out, c_out):
        for shift, out in ((0.0, s_out), (0.5 * np.pi, c_out)):
            f, i_, g = rr_f[:, :n], rr_i[:, :n], rr_g[:, :n]
            V(lambda e: e.tensor_scalar(out=g, in0=arg, scalar1=shift, scalar2=None, op0=ALU.add))
            V(lambda e: e.tensor_scalar(out=i_, in0=g, scalar1=1.0 / TWO_PI, scalar2=None, op0=ALU.mult))
            V(lambda e: e.tensor_copy(f, i_))
            V(lambda e: e.scalar_tensor_tensor(out=g, in0=f, scalar=-TWO_PI, in1=g, op0=ALU.mult, op1=ALU.add))
            V(lambda e: e.tensor_scalar(out=f, in0=g, scalar1=float(np.pi), scalar2=None, op0=ALU.is_gt))
            V(lambda e: e.scalar_tensor_tensor(out=g, in0=f, scalar=-TWO_PI, in1=g, op0=ALU.mult, op1=ALU.add))
            V(lambda e: e.tensor_scalar(out=f, in0=g, scalar1=-float(np.pi), scalar2=None, op0=ALU.is_lt))
            V(lambda e: e.scalar_tensor_tensor(out=g, in0=f, scalar=TWO_PI, in1=g, op0=ALU.mult, op1=ALU.add))
            A(lambda e: e.activation(out=out, in_=g, func=AF.Sin))

    for pr in range(npairs):
        P.dma("sp", sm[:, 0:3], lam[pr], writes=[pre])
        P.dma("sp", bm[:, :], Bm[pr], writes=[pre])
        P.dma("sp", cm[:, :], Cm[pr], writes=[pre])
        P.dma("sp", dk[:, :], Dk[pr], writes=[pre])
        P.dma("sp", ub[:, :], uT[pr], writes=[ubuf])
        lre, lim, ldt, dt_, rho, th, r_, sth, cth, nre, nim, l2, inv, kre, kim, t1, t2, phi, sph, cph, RL, nkim, sre, sim_, nsim = [
            col(i) for i in range(25)]
        A(lambda e: e.activation(out=dt_, in_=ldt, func=AF.Exp))
        V(lambda e: e.tensor_tensor(out=rho, in0=lre, in1=dt_, op=ALU.mult))
        V(lambda e: e.tensor_tensor(out=th, in0=lim, in1=dt_, op=ALU.mult))
        A(lambda e: e.activation(out=r_, in_=rho, func=AF.Exp))
        sincos(th, 1, sth, cth)
        V(lambda e: e.tensor_tensor(out=nre, in0=r_, in1=cth, op=ALU.mult))
        V(lambda e: e.tensor_scalar(out=nre, in0=nre, scalar1=-1.0, scalar2=None, op0=ALU.add))
        V(lambda e: e.tensor_tensor(out=nim, in0=r_, in1=sth, op=ALU.mult))
        V(lambda e: e.tensor_tensor(out=l2, in0=lre, in1=lre, op=ALU.mult))
        V(lambda e: e.scalar_tensor_tensor(out=l2, in0=lim, scalar=lim, in1=l2, op0=ALU.mult, op1=ALU.add))
        V(lambda e: e.reciprocal(inv, l2))
        V(lambda e: e.tensor_tensor(out=t1, in0=nre, in1=lre, op=ALU.mult))
        V(lambda e: e.scalar_tensor_tensor(out=t1, in0=nim, scalar=lim, in1=t1, op0=ALU.mult, op1=ALU.add))
        V(lambda e: e.tensor_tensor(out=kre, in0=t1, in1=inv, op=ALU.mult))
        V(lambda e: e.tensor_tensor(out=t1, in0=nim, in1=lre, op=ALU.mult))
        V(lambda e: e.tensor_tensor(out=t2, in0=nre, in1=lim, op=ALU.mult))
        V(lambda e: e.tensor_tensor(out=t1, in0=t1, in1=t2, op=ALU.subtract))
        V(lambda e: e.tensor_tensor(out=kim, in0=t1, in1=inv, op=ALU.mult))
        V(lambda e: e.tensor_scalar(out=nkim, in0=kim, scalar1=-1.0, scalar2=None, op0=ALU.mult))
        V(lambda e: e.tensor_scalar(out=kbm[:, 0:16], in0=bm[:, 0:16], scalar1=kre, scalar2=None, op0=ALU.mult))
        V(lambda e: e.scalar_tensor_tensor(out=kbm[:, 0:16], in0=bm[:, 16:32], scalar=nkim, in1=kbm[:, 0:16], op0=ALU.mult, op1=ALU.add))
        V(lambda e: e.tensor_scalar(out=kbm[:, 16:32], in0=bm[:, 16:32], scalar1=kre, scalar2=None, op0=ALU.mult))
        V(lambda e: e.scalar_tensor_tensor(out=kbm[:, 16:32], in0=bm[:, 0:16], scalar=kim, in1=kbm[:, 16:32], op0=ALU.mult, op1=ALU.add))
        V(lambda e: e.tensor_scalar(out=rpt[:, :], in0=iota[:, :], scalar1=th, scalar2=None, op0=ALU.mult), rd=[cb])
        sincos(rpt[:, :], L, sint[:, :], cost[:, :])
        A(lambda e: e.activation(out=rpt[:, :], in_=iota1[:, :], func=AF.Exp, scale=rho), rd=[cb])
        V(lambda e: e.tensor_scalar(out=phi, in0=th, scalar1=float(L), scalar2=None, op0=ALU.mult))
        sincos(phi, 1, sph, cph)
        V(lambda e: e.tensor_scalar(out=t1, in0=rho, scalar1=float(L), scalar2=None, op0=ALU.mult))
        A(lambda e: e.activation(out=RL, in_=t1, func=AF.Exp))
        for g2 in range(2):
            rows = slice(g2 * 64, g2 * 64 + 64)
            cols = slice(g2 * 16, g2 * 16 + 16)
            cs3 = cost[rows, :].unsqueeze(2).broadcast_to([64, L, 16])
            sn3 = sint[rows, :].unsqueeze(2).broadcast_to([64, L, 16])
            rp3 = rpt[rows, :].unsqueeze(2).broadcast_to([64, L, 16])
            kbre = kbm[rows, 0:16].unsqueeze(1).broadcast_to([64, L, 16])
            kbim = kbm[rows, 16:32].unsqueeze(1).broadcast_to([64, L, 16])
            cre3 = cm[rows, 0:16].unsqueeze(1).broadcast_to([64, L, 16])
            cim3 = cm[rows, 16:32].unsqueeze(1).broadcast_to([64, L, 16])
            p0, p1, p2 = [t_[rows, :, :] for t_ in pt]
            TT = lambda out, a, b, op, wr=(): V(lambda e: e.tensor_tensor(out=out, in0=a, in1=b, op=op), wr=wr)
            TT(p0, cs3, kbre, ALU.mult)
            TT(p1, sn3, kbim, ALU.mult)
            TT(G[0][rows, :, cols], p0, p1, ALU.add, wr=[Gb])
            TT(p0, cs3, kbim, ALU.mult)
            TT(p1, sn3, kbre, ALU.mult)
            TT(G[1][rows, :, cols], p0, p1, ALU.subtract, wr=[Gb])
            TT(p0, cs3, cre3, ALU.mult)
            TT(p1, sn3, cim3, ALU.mult)
            TT(p2, p0, p1, ALU.subtract)
            V(lambda e: e.tensor_copy(Ct[0][rows, :, cols], p2), wr=[Ctb])
            TT(Ct[2][rows, :, cols], p2, rp3, ALU.mult, wr=[Ctb])
            TT(p0, sn3, cre3, ALU.mult)
            TT(p1, cs3, cim3, ALU.mult)
            V(lambda e: e.scalar_tensor_tensor(out=p2, in0=p0, scalar=-1.0, in1=p1, op0=ALU.mult, op1=ALU.subtract))
            V(lambda e: e.tensor_copy(Ct[1][rows, :, cols], p2), wr=[Ctb])
            TT(Ct[3][rows, :, cols], p2, rp3, ALU.mult, wr=[Ctb])
        for ri in range(2):
            for q in range(L // 4):
                ps, pb = bank()
                for j in range(4):
                    P.op("pe", lambda e: e.transpose(ps[0:32, j * 128:(j + 1) * 128], G[ri][:, q * 4 + j, :], ident[:, :]),
                         reads=[Gb, cb, pre], writes=[pb])
                eng = "act" if (q % 2 == 0) else "dve"
                fn = (lambda e: e.copy(Bt[ri][:, q * 4:(q + 1) * 4, :], ps[0:32, :].rearrange("p (j m) -> p j m", j=4))) if eng == "act" else \
                     (lambda e: e.tensor_copy(Bt[ri][:, q * 4:(q + 1) * 4, :], ps[0:32, :].rearrange("p (j m) -> p j m", j=4)))
                P.op(eng, fn, reads=[pb], writes=[Btb[ri]])
        V(lambda e: e.memset(EE[0][:, 0:1], 1.0), wr=[chb])
        V(lambda e: e.memset(EE[1][:, 0:1], 0.0), wr=[chb])
        V(lambda e: e.tensor_copy(sre, cph))
        V(lambda e: e.tensor_copy(sim_, sph))
        n_ = 1
        while n_ < NCH:
            m_ = min(n_, NCH - n_)
            V(lambda e: e.tensor_scalar(out=nsim, in0=sim_, scalar1=-1.0, scalar2=None, op0=ALU.mult))
            V(lambda e: e.tensor_scalar(out=EE[0][:, n_:n_ + m_], in0=EE[0][:, 0:m_], scalar1=sre, scalar2=None, op0=ALU.mult), rd=[chb], wr=[chb])
            V(lambda e: e.scalar_tensor_tensor(out=EE[0][:, n_:n_ + m_], in0=EE[1][:, 0:m_], scalar=nsim, in1=EE[0][:, n_:n_ + m_],
                                               op0=ALU.mult, op1=ALU.add), rd=[chb], wr=[chb])
            V(lambda e: e.tensor_scalar(out=EE[1][:, n_:n_ + m_], in0=EE[0][:, 0:m_], scalar1=sim_, scalar2=None, op0=ALU.mult), rd=[chb], wr=[chb])
            V(lambda e: e.scalar_tensor_tensor(out=EE[1][:, n_:n_ + m_], in0=EE[1][:, 0:m_], scalar=sre, in1=EE[1][:, n_:n_ + m_],
                                               op0=ALU.mult, op1=ALU.add), rd=[chb], wr=[chb])
            n_ *= 2
            if n_ < NCH:
                V(lambda e: e.tensor_tensor(out=t1, in0=sre, in1=sre, op=ALU.mult))
                V(lambda e: e.scalar_tensor_tensor(out=t1, in0=sim_, scalar=nsim, in1=t1, op0=ALU.mult, op1=ALU.add))
                V(lambda e: e.tensor_tensor(out=t2, in0=sre, in1=sim_, op=ALU.mult))
                V(lambda e: e.tensor_scalar(out=sim_, in0=t2, scalar1=2.0, scalar2=None, op0=ALU.mult))
                V(lambda e: e.tensor_copy(sre, t1))
        a3 = at[:, :].rearrange("p (n t) -> p n t", t=L)
        V(lambda e: e.tensor_scalar(out=a3, in0=iota[:, :].unsqueeze(1).broadcast_to([128, NCHh, L]), scalar1=0.0, scalar2=r_,
                                    op0=ALU.mult, op1=ALU.add), rd=[cb], wr=[atb])
        V(lambda e: e.memset(at[:, 0::L], 0.0), rd=[atb], wr=[atb])
        V(lambda e: e.tensor_scalar(out=Rb[:, :], in0=EE[0][:, :], scalar1=0.0, scalar2=RL, op0=ALU.mult, op1=ALU.add), rd=[chb], wr=[chb])
        for hf in range(NH):
            off = hf * HL
            for tb in range(L // TPB):
                for ri in range(2):
                    ps, pb = bank()
                    for j in range(TPB):
                        tau = tb * TPB + j
                        P.op("pe", lambda e: e.matmul(ps[:, j * NCHh:(j + 1) * NCHh], Bt[ri][:, tau, :], ub[:, off + tau:off + HL:L],
                                                      start=True, stop=True), reads=[Btb[ri], ubuf], writes=[pb])
                    outv = cc[ri][:, :].rearrange("p (n t) -> p t n", t=L)[:, tb * TPB:(tb + 1) * TPB, :]
                    inv_ = ps[:, :TPB * NCHh].rearrange("p (t n) -> p t n", n=NCHh)
                    if ri == 0:
                        P.op("act", lambda e: e.copy(outv, inv_), reads=[pb], writes=[ccb[ri]])
                    else:
                        P.op("dve", lambda e: e.tensor_copy(outv, inv_), reads=[pb], writes=[ccb[ri]])
            for ri in range(2):
                P.op("dve", lambda e: e.tensor_tensor_scan(out=cc[ri][:, :], data0=at[:, :], data1=cc[ri][:, :], initial=0.0,
                                                           op0=ALU.mult, op1=ALU.add), reads=[atb, ccb[ri]], writes=[ccb[ri]])
                P.op("act", lambda e: e.copy(wb[ri][:, off:off + HL], cc[ri][:, :]), reads=[ccb[ri]], writes=[wbb[ri]])
                P.op("dve", lambda e: e.tensor_copy(ff[ri][:, hf * NCHh:(hf + 1) * NCHh], cc[ri][:, L - 1::L]),
                     reads=[ccb[ri]], writes=[chb])
        C2 = lambda fn: P.op("dve", fn, reads=[chb, pre], writes=[chb])
        C2(lambda e: e.tensor_tensor(out=gg[0][:, :], in0=EE[0][:, :], in1=ff[0][:, :], op=ALU.mult))
        C2(lambda e: e.tensor_tensor(out=rr_f[:, :NCH], in0=EE[1][:, :], in1=ff[1][:, :], op=ALU.mult))
        C2(lambda e: e.tensor_tensor(out=gg[0][:, :], in0=gg[0][:, :], in1=rr_f[:, :NCH], op=ALU.add))
        C2(lambda e: e.tensor_tensor(out=gg[1][:, :], in0=EE[0][:, :], in1=ff[1][:, :], op=ALU.mult))
        C2(lambda e: e.tensor_tensor(out=rr_f[:, :NCH], in0=EE[1][:, :], in1=ff[0][:, :], op=ALU.mult))
        C2(lambda e: e.tensor_tensor(out=gg[1][:, :], in0=gg[1][:, :], in1=rr_f[:, :NCH], op=ALU.subtract))
        for ri in range(2):
            C2(lambda e: e.tensor_tensor_scan(out=gg[ri][:, :], data0=Rb[:, :], data1=gg[ri][:, :], initial=0.0, op0=ALU.mult, op1=ALU.add))
        C2(lambda e: e.tensor_tensor(out=ff[0][:, :], in0=EE[0][:, :], in1=gg[0][:, :], op=ALU.mult))
        C2(lambda e: e.tensor_tensor(out=rr_f[:, :NCH], in0=EE[1][:, :], in1=gg[1][:, :], op=ALU.mult))
        C2(lambda e: e.tensor_tensor(out=ff[0][:, :], in0=ff[0][:, :], in1=rr_f[:, :NCH], op=ALU.subtract))
        C2(lambda e: e.tensor_tensor(out=ff[1][:, :], in0=EE[0][:, :], in1=gg[1][:, :], op=ALU.mult))
        C2(lambda e: e.tensor_tensor(out=rr_f[:, :NCH], in0=EE[1][:, :], in1=gg[0][:, :], op=ALU.mult))
        C2(lambda e: e.tensor_tensor(out=ff[1][:, :], in0=ff[1][:, :], in1=rr_f[:, :NCH], op=ALU.add))
        V(lambda e: e.tensor_scalar(out=t1, in0=sph, scalar1=-1.0, scalar2=None, op0=ALU.mult))
        ZW = lambda fn: P.op("dve", fn, reads=[chb, pre], writes=[Zbb, chb])
        ZW(lambda e: e.memset(Zb[0][:, 0:1], 0.0))
        ZW(lambda e: e.memset(Zb[1][:, 0:1], 0.0))
        if NCH > 1:
            ZW(lambda e: e.tensor_scalar(out=rr_f[:, :NCH - 1], in0=ff[0][:, :NCH - 1], scalar1=cph, scalar2=None, op0=ALU.mult))
            ZW(lambda e: e.scalar_tensor_tensor(out=Zb[0][:, 1:], in0=ff[1][:, :NCH - 1], scalar=t1, in1=rr_f[:, :NCH - 1], op0=ALU.mult, op1=ALU.add))
            ZW(lambda e: e.tensor_scalar(out=rr_f[:, :NCH - 1], in0=ff[0][:, :NCH - 1], scalar1=sph, scalar2=None, op0=ALU.mult))
            ZW(lambda e: e.scalar_tensor_tensor(out=Zb[1][:, 1:], in0=ff[1][:, :NCH - 1], scalar=cph, in1=rr_f[:, :NCH - 1], op0=ALU.mult, op1=ALU.add))
        for hf in range(NH):
            off = hf * HL
            for tb in range(L // TPB):
                ps, pb = bank()
                for j in range(TPB):
                    tau = tb * TPB + j
                    o_ = ps[0:32, j * NCHh:(j + 1) * NCHh]
                    P.op("pe", lambda e: e.matmul(o_, Ct[0][:, tau, :], wb[0][:, off + tau:off + HL:L], start=True, stop=False),
                         reads=[Ctb, wbb[0]], writes=[pb])
                    P.op("pe", lambda e: e.matmul(o_, Ct[1][:, tau, :], wb[1][:, off + tau:off + HL:L], start=False, stop=False),
                         reads=[Ctb, wbb[1]], writes=[pb])
                    P.op("pe", lambda e: e.matmul(o_, Ct[2][:, tau, :], Zb[0][:, hf * NCHh:(hf + 1) * NCHh], start=False, stop=False),
                         reads=[Ctb, Zbb], writes=[pb])
                    P.op("pe", lambda e: e.matmul(o_, Ct[3][:, tau, :], Zb[1][:, hf * NCHh:(hf + 1) * NCHh], start=False, stop=True),
                         reads=[Ctb, Zbb], writes=[pb])
                yv = ysb[:, :].rearrange("p (n t) -> p t n", t=L)[:, tb * TPB:(tb + 1) * TPB, :]
                uv = ub[:, off:off + HL].rearrange("p (n t) -> p t n", t=L)[:, tb * TPB:(tb + 1) * TPB, :]
                pv = ps[0:32, :TPB * NCHh].rearrange("p (t n) -> p t n", n=NCHh)
                P.op("dve", lambda e: e.scalar_tensor_tensor(out=yv, in0=uv, scalar=dk[:, 0:1], in1=pv, op0=ALU.mult, op1=ALU.add),
                     reads=[pb, ubuf, pre], writes=[ybuf])
            P.dma("sp", yT[pr, :, off:off + HL], ysb[:, :], reads=[ybuf])
    c.finish([ybuf])
    return nc


def gdn_consts():
    i = np.arange(64)[:, None]
    j = np.arange(64)[None, :]
    NEG = -30000.0
    mrow = np.ones((1, 512), np.float32)
    mrow[0, 0::64] = 0.0
    return {
        "gmask": np.stack([np.where(j < i, 0.0, NEG), np.where(j >= i, 0.0, NEG), (i <= j).astype(np.float32)], 1).astype(np.float32),
        "mrow": mrow,
    }


def build_gdn(seq=S, nheads=2, env=None, fm_out=False):
    c = env or Ctx()
    nc, P = c.nc, c.P
    NB = 8
    BT = NB * 64
    nbatch = seq // BT
    NCk = seq // 64
    qkv = c.din("qkv", [nheads, 3, 128, seq], BF16)
    gate = c.din("gate", [nheads, 64, NCk, 128], BF16)
    rows = c.din("rows", [nheads, 2, seq], BF16)
    cols = c.din("cols", [nheads, 64, 2, NCk], BF16)
    convw = c.din("convw", [nheads, 128, 12])
    hsc = c.din("hsc", [nheads, 2])
    normw = c.din("normw", [128])
    gmask_d = c.din("gmask", [64, 3, 64])
    mrow_d = c.din("mrow", [1, 512])
    if fm_out:
        oT = c.dout("oT", [nheads * 128, seq], BF16)
    else:
        oT = c.dout("o", [nheads, 64, NCk, 128], BF16)
    banks = c.psum_banks()
    bi = [0]

    def bank():
        b = banks[bi[0] % 8]
        bi[0] += 1
        return b

    cb = Buf("const")
    ident = c.sb([128, 128], F32, "ident")
    P.op("pool", lambda e: e.memset(ident[:, :], 0.0), writes=[cb])
    P.op("pool", lambda e: e.affine_select(out=ident[:, :], in_=ident[:, :], pattern=[[-1, 128]], compare_op=ALU.not_equal,
                                           fill=1.0, base=0, channel_multiplier=1), reads=[cb], writes=[cb])
    ones = c.sb([128, 128], F32, "ones")
    P.op("pool", lambda e: e.memset(ones[:, :], 1.0), writes=[cb])
    gmask = c.sb([64, 3, 64], F32, "gmask")
    mrow = c.sb([1, 512], F32, "mrow")
    nw = c.sb([64, 128], F32, "nw")
    P.dma("sp", gmask[:, :, :], gmask_d, writes=[cb])
    P.dma("sp", mrow[:, :], mrow_d, writes=[cb])
    P.dma("sp", nw[:, :], normw.partition_broadcast(64), writes=[cb])
    maskS = gmask[:, 0, :].unsqueeze(1).broadcast_to([64, NB, 64])
    maskU = gmask[:, 1, :].unsqueeze(1).broadcast_to([64, NB, 64])
    triI = gmask[:, 2, :]
    I3 = ident[0:64, 0:64].unsqueeze(1).broadcast_to([64, NB, 64])

    class H:
        pass

    hs = []
    for h in range(nheads):
        o = H()
        o.b = Buf(f"h{h}")
        o.cw = c.sb([128, 12], F32, "cw")
        o.sc = c.sb([128, 8], F32, "sc")
        o.xin = [c.sb([128, 3 + BT], BF16, "xin") for _ in range(3)]
        o.xb = [Buf(f"xin{h}_{i}") for i in range(3)]
        o.acc = [c.sb([128, BT], F32, "acc") for _ in range(3)]
        o.ab = [Buf(f"acc{h}_{i}") for i in range(3)]
        o.sq = c.sb([128, BT], F32, "sq")
        o.rs = c.sb([128, BT], F32, "rs")
        o.qT = c.sb([128, BT], BF16, "qT")
        o.kT = c.sb([128, BT], BF16, "kT")
        o.qdT = c.sb([128, BT], BF16, "qdT")
        o.qkb = Buf(f"qk{h}")
        o.kd_tm = c.sb([64, NB, 128], BF16, "kd_tm")
        o.kbg_tm = c.sb([64, NB, 128], BF16, "kbg_tm")
        o.vb_tm = c.sb([64, NB, 128], BF16, "vb_tm")
        o.tmb = Buf(f"tm{h}")
        o.gt = c.sb([64, NB, 128], BF16, "gt")
        o.sg = c.sb([64, NB, 128], F32, "sg")
        o.gtb = Buf(f"gt{h}")
        o.rowi = c.sb([1, BT], BF16, "rowi")
        o.rowf = c.sb([1, BT], F32, "rowf")
        o.gcrow = c.sb([1, BT], F32, "gcrow")
        o.rowb = Buf(f"row{h}")
        o.coli = c.sb([64, 2, NB], BF16, "coli")
        o.colf = c.sb([64, 16, NB], F32, "colf")
        o.colb = Buf(f"col{h}")
        o.gcbc = c.sb([128, BT], F32, "gcbc")
        o.egbc = c.sb([128, BT], F32, "egbc")
        o.gcb = Buf(f"gcbc{h}")
        o.dec = c.sb([64, NB, 64], F32, "dec")
        o.N = c.sb([64, NB, 64], F32, "N")
        o.NT = c.sb([64, NB, 64], F32, "NT")
        o.T = c.sb([64, NB, 64], F32, "T")
        o.TT = c.sb([64, NB, 64], F32, "TT")
        o.TTb = c.sb([64, NB, 64], BF16, "TTb")
        o.nb_ = Buf(f"N{h}")
        o.attT = c.sb([64, NB, 64], BF16, "attT")
        o.attb = Buf(f"att{h}")
        o.uval = c.sb([64, NB, 128], F32, "uval")
        o.ub = Buf(f"uval{h}")
        o.wkT = c.sb([128, NB, 64], BF16, "wkT")
        o.wkb = Buf(f"wk{h}")
        o.S = c.sb([128, 128], F32, "S")
        o.Sb = c.sb([128, 128], BF16, "Sb")
        o.Sbuf = Buf(f"S{h}")
        o.vnew = [c.sb([64, 128], BF16, "vnew") for _ in range(2)]
        o.vnb = [Buf(f"vn{h}_{i}") for i in range(2)]
        o.ss = c.sb([64, 4], F32, "ss")
        o.ss4 = [c.sb([64, 4], F32, "ss4") for _ in range(2)]
        o.ssb = Buf(f"ss{h}")
        o.junk = c.sb([64, 128], F32, "junk")
        o.oout = c.sb([64, NB, 128], F32 if fm_out else BF16, "oout")
        o.ofm = c.sb([128, BT], BF16, "ofm")
        o.ofmb = Buf(f"ofm{h}")
        o.oob = Buf(f"oo{h}")
        hs.append(o)
        P.dma("sp", o.cw[:, :], convw[h], writes=[o.b])
        P.dma("sp", o.sc[:, 0:2], hsc[h].partition_broadcast(128), writes=[o.b])
        P.op("act", lambda e: e.activation(out=o.sc[:, 2:3], in_=o.sc[:, 0:1], func=AF.Exp), reads=[o.b], writes=[o.b])
        P.op("dve", lambda e: e.tensor_scalar(out=o.sc[:, 2:3], in0=o.sc[:, 2:3], scalar1=-1.0, scalar2=None, op0=ALU.mult), reads=[o.b], writes=[o.b])
        P.op("dve", lambda e: e.memset(o.S[:, :], 0.0), writes=[o.Sbuf])
        P.op("dve", lambda e: e.memset(o.Sb[:, :], 0.0), writes=[o.Sbuf])
    LNQ = -0.5 * float(np.log(128.0))

    def precompute(h, o, b):
        t0 = b * BT
        n0 = b * NB
        for i in range(3):
            if b == 0:
                P.op("dve", lambda e: e.memset(o.xin[i][:, 0:3], 0.0), writes=[o.xb[i]])
                P.dma("sp", o.xin[i][:, 3:], qkv[h, i, :, 0:BT], writes=[o.xb[i]])
            else:
                P.dma("sp", o.xin[i][:, :], qkv[h, i, :, t0 - 3:t0 + BT], writes=[o.xb[i]])
        P.dma("sp", o.gt[:, :, :], gate[h, :, n0:n0 + NB, :], writes=[o.gtb])
        P.dma("sp", o.rowi[:, :], rows[h, 0:1, t0:t0 + BT], writes=[o.rowb])
        for ci in range(2):
            P.dma("sp", o.coli[:, ci, :], cols[h, :, ci, n0:n0 + NB], writes=[o.colb], allow_slow_non_contiguous=True)
        yield
        for i in range(3):
            x, a = o.xin[i], o.acc[i]
            P.op("dve", lambda e: e.tensor_scalar(out=a[:, :], in0=x[:, 3:3 + BT], scalar1=o.cw[:, 4 * i + 3:4 * i + 4], scalar2=None, op0=ALU.mult),
                 reads=[o.xb[i], o.b], writes=[o.ab[i]])
            for tap in range(3):
                P.op("dve", lambda e: e.scalar_tensor_tensor(out=a[:, :], in0=x[:, tap:tap + BT], scalar=o.cw[:, 4 * i + tap:4 * i + tap + 1],
                                                             in1=a[:, :], op0=ALU.mult, op1=ALU.add), reads=[o.xb[i], o.ab[i], o.b], writes=[o.ab[i]])
            P.op("act", lambda e: e.activation(out=a[:, :], in_=a[:, :], func=AF.Silu), reads=[o.ab[i]], writes=[o.ab[i]])
            yield
        yield
        for i, dst, lnb in ((0, o.qT, LNQ), (1, o.kT, 0.0)):
            a = o.acc[i]
            P.op("act", lambda e: e.activation(out=o.sq[:, :], in_=a[:, :], func=AF.Square), reads=[o.ab[i]], writes=[o.b])
            ps, pb = bank()
            P.op("pe", lambda e: e.matmul(ps[:, :], ones[:, :], o.sq[:, :], start=True, stop=True), reads=[cb, o.b], writes=[pb])
            P.op("act", lambda e: e.activation(out=o.rs[:, :], in_=ps[:, :], func=AF.Ln, bias=RMS_EPS), reads=[pb], writes=[o.b])
            P.op("act", lambda e: e.activation(out=o.rs[:, :], in_=o.rs[:, :], func=AF.Exp, scale=-0.5, bias=lnb), reads=[o.b], writes=[o.b])
            P.op("dve", lambda e: e.tensor_tensor(out=a[:, :], in0=a[:, :], in1=o.rs[:, :], op=ALU.mult), reads=[o.ab[i], o.b], writes=[o.ab[i]])
            P.op("act", lambda e: e.copy(dst[:, :], a[:, :]), reads=[o.ab[i]], writes=[o.qkb])
            yield
        yield
        P.op("act", lambda e: e.activation(out=o.sg[:, :, :], in_=o.gt[:, :, :], func=AF.Silu), reads=[o.gtb], writes=[o.gtb])
        P.op("pool", lambda e: e.tensor_tensor(out=o.sg[:, :, :], in0=o.sg[:, :, :], in1=nw[:, :].unsqueeze(1).broadcast_to([64, NB, 128]), op=ALU.mult),
             reads=[o.gtb, cb], writes=[o.gtb])
        yield
        R_ = lambda eng, fn: P.op(eng, fn, reads=[o.rowb, o.b, cb], writes=[o.rowb])
        R_("act", lambda e: e.activation(out=o.rowf[:, :], in_=o.rowi[:, :], func=AF.Exp, bias=o.sc[0:1, 1:2]))
        R_("act", lambda e: e.activation(out=o.rowf[:, :], in_=o.rowf[:, :], func=AF.Ln, bias=1.0))
        R_("dve", lambda e: e.tensor_scalar(out=o.rowf[:, :], in0=o.rowf[:, :], scalar1=o.sc[0:1, 2:3], scalar2=None, op0=ALU.mult))
        R_("dve", lambda e: e.tensor_tensor_scan(out=o.gcrow[:, :], data0=mrow[:, :], data1=o.rowf[:, :], initial=0.0, op0=ALU.mult, op1=ALU.add))
        ps, pb = bank()
        P.op("pe", lambda e: e.matmul(ps[:, :], ones[0:1, :], o.gcrow[:, :], start=True, stop=True), reads=[cb, o.rowb], writes=[pb])
        P.op("dve", lambda e: e.tensor_copy(o.gcbc[:, :], ps[:, :]), reads=[pb], writes=[o.gcb])
        P.op("act", lambda e: e.activation(out=o.egbc[:, :], in_=o.gcbc[:, :], func=AF.Exp), reads=[o.gcb], writes=[o.gcb])
        yield
        C_ = lambda eng, fn, rd=(): P.op(eng, fn, reads=[o.colb, o.b, cb] + list(rd), writes=[o.colb])
        cf = lambda k: o.colf[:, k, :]
        C_("act", lambda e: e.activation(out=cf(0), in_=o.coli[:, 0, :], func=AF.Exp, bias=o.sc[0:64, 1:2]))
        C_("act", lambda e: e.activation(out=cf(0), in_=cf(0), func=AF.Ln, bias=1.0))
        C_("dve", lambda e: e.tensor_scalar(out=cf(0), in0=cf(0), scalar1=o.sc[0:64, 2:3], scalar2=None, op0=ALU.mult))
        C_("act", lambda e: e.activation(out=cf(1), in_=o.coli[:, 1, :], func=AF.Sigmoid))
        ps, pb = bank()
        P.op("pe", lambda e: e.matmul(ps[0:64, 0:NB], triI, cf(0), start=True, stop=True), reads=[cb, o.colb], writes=[pb])
        C_("dve", lambda e: e.tensor_copy(cf(2), ps[0:64, 0:NB]), rd=[pb])
        C_("act", lambda e: e.activation(out=cf(3), in_=cf(2), func=AF.Exp))
        C_("dve", lambda e: e.tensor_tensor(out=cf(4), in0=cf(3), in1=cf(1), op=ALU.mult))
        C_("dve", lambda e: e.tensor_copy(cf(5), o.gcbc[0:64, 63::64]), rd=[o.gcb])
        C_("dve", lambda e: e.tensor_tensor(out=cf(6), in0=cf(5), in1=cf(2), op=ALU.subtract))
        C_("act", lambda e: e.activation(out=cf(6), in_=cf(6), func=AF.Exp))
        C_("dve", lambda e: e.tensor_scalar(out=cf(7), in0=cf(1), scalar1=-1.0, scalar2=None, op0=ALU.mult))
        yield
        P.op("dve", lambda e: e.tensor_tensor(out=o.qdT[:, :], in0=o.acc[0][:, :], in1=o.egbc[:, :], op=ALU.mult),
             reads=[o.ab[0], o.gcb], writes=[o.qkb])
        yield
        for src, outs in ((1, ((o.kd_tm, 6), (o.kbg_tm, 4))), (2, ((o.vb_tm, 1),))):
            for q in range(NB // 4):
                ps, pb = bank()
                for j in range(4):
                    ch = q * 4 + j
                    P.op("pe", lambda e: e.transpose(ps[0:64, j * 128:(j + 1) * 128], o.acc[src][:, ch * 64:(ch + 1) * 64], ident[:, :]),
                         reads=[o.ab[src], cb], writes=[pb])
                pv = ps[0:64, :].rearrange("p (j d) -> p j d", j=4)
                for dst, k in outs:
                    P.op("dve", lambda e: e.tensor_tensor(out=dst[:, q * 4:(q + 1) * 4, :], in0=pv,
                                                          in1=o.colf[:, k, q * 4:(q + 1) * 4].unsqueeze(2).broadcast_to([64, 4, 128]), op=ALU.mult),
                         reads=[pb, o.colb], writes=[o.tmb])
        yield
        gcj = o.gcbc[0:64, :].rearrange("p (n j) -> p n j", j=64)
        gci = o.colf[:, 2, :].unsqueeze(2).broadcast_to([64, NB, 64])
        nbe = o.colf[:, 7, :].unsqueeze(2).broadcast_to([64, NB, 64])
        D_ = lambda eng, fn, rd=(), wr=(): P.op(eng, fn, reads=[o.nb_, o.colb, o.gcb, cb] + list(rd), writes=[o.nb_] + list(wr))
        D_("dve", lambda e: e.tensor_tensor(out=o.dec[:, :, :], in0=gci, in1=gcj, op=ALU.subtract))
        D_("dve", lambda e: e.tensor_tensor(out=o.dec[:, :, :], in0=o.dec[:, :, :], in1=maskS, op=ALU.add))
        D_("act", lambda e: e.activation(out=o.dec[:, :, :], in_=o.dec[:, :, :], func=AF.Exp))
        D_("dve", lambda e: e.tensor_tensor(out=o.dec[:, :, :], in0=o.dec[:, :, :], in1=nbe, op=ALU.mult))
        ps, pb = bank()
        for ch in range(NB):
            ksl = o.kT[:, ch * 64:(ch + 1) * 64]
            P.op("pe", lambda e: e.matmul(ps[0:64, ch * 64:(ch + 1) * 64], ksl, ksl, start=True, stop=True), reads=[o.qkb], writes=[pb])
        pv = ps[0:64, :].rearrange("p (n j) -> p n j", j=64)
        D_("dve", lambda e: e.tensor_tensor(out=o.N[:, :, :], in0=o.dec[:, :, :], in1=pv, op=ALU.mult), rd=[pb])
        ps, pb = bank()
        for ch in range(NB):
            P.op("pe", lambda e: e.transpose(ps[0:64, ch * 64:(ch + 1) * 64], o.N[:, ch, :], ident[0:64, 0:64]), reads=[o.nb_, cb], writes=[pb])
        pv = ps[0:64, :].rearrange("p (n j) -> p n j", j=64)
        D_("act", lambda e: e.copy(o.NT[:, :, :], pv), rd=[pb])
        D_("dve", lambda e: e.tensor_tensor(out=o.T[:, :, :], in0=o.N[:, :, :], in1=I3, op=ALU.add))
        D_("dve", lambda e: e.tensor_tensor(out=o.TT[:, :, :], in0=o.NT[:, :, :], in1=I3, op=ALU.add))
        for lvl in range(5):
            psa, pba = bank()
            psb, pbb = bank()
            for ch in range(NB):
                sl = slice(ch * 64, (ch + 1) * 64)
                P.op("pe", lambda e: e.matmul(psa[0:64, sl], o.NT[:, ch, :], o.N[:, ch, :], start=True, stop=True), reads=[o.nb_], writes=[pba])
                P.op("pe", lambda e: e.matmul(psb[0:64, sl], o.N[:, ch, :], o.NT[:, ch, :], start=True, stop=True), reads=[o.nb_], writes=[pbb])
            D_("act", lambda e: e.copy(o.N[:, :, :], psa[0:64, :].rearrange("p (n j) -> p n j", j=64)), rd=[pba])
            D_("dve", lambda e: e.tensor_copy(o.NT[:, :, :], psb[0:64, :].rearrange("p (n j) -> p n j", j=64)), rd=[pbb])
            yield
            psa, pba = bank()
            psb, pbb = bank()
            for ch in range(NB):
                sl = slice(ch * 64, (ch + 1) * 64)
                P.op("pe", lambda e: e.matmul(psa[0:64, sl], o.TT[:, ch, :], o.N[:, ch, :], start=True, stop=True), reads=[o.nb_], writes=[pba])
                P.op("pe", lambda e: e.matmul(psb[0:64, sl], o.N[:, ch, :], o.TT[:, ch, :], start=True, stop=True), reads=[o.nb_], writes=[pbb])
            D_("dve", lambda e: e.tensor_tensor(out=o.T[:, :, :], in0=o.T[:, :, :], in1=psa[0:64, :].rearrange("p (n j) -> p n j", j=64), op=ALU.add), rd=[pba])
            D_("dve", lambda e: e.tensor_tensor(out=o.TT[:, :, :], in0=o.TT[:, :, :], in1=psb[0:64, :].rearrange("p (n j) -> p n j", j=64), op=ALU.add), rd=[pbb])
            yield
        D_("act", lambda e: e.copy(o.TTb[:, :, :], o.TT[:, :, :]))
        yield
        for q in range(NB // 4):
            ps, pb = bank()
            for j in range(4):
                ch = q * 4 + j
                P.op("pe", lambda e: e.matmul(ps[0:64, j * 128:(j + 1) * 128], o.TTb[:, ch, :], o.vb_tm[:, ch, :], start=True, stop=True),
                     reads=[o.nb_, o.tmb], writes=[pb])
            P.op("act", lambda e: e.copy(o.uval[:, q * 4:(q + 1) * 4, :], ps[0:64, :].rearrange("p (j d) -> p j d", j=4)), reads=[pb], writes=[o.ub])
        ps, pb = bank()
        for ch in range(NB):
            P.op("pe", lambda e: e.matmul(ps[:, ch * 64:(ch + 1) * 64], o.kbg_tm[:, ch, :], o.TTb[:, ch, :], start=True, stop=True),
                 reads=[o.nb_, o.tmb], writes=[pb])
        P.op("act", lambda e: e.copy(o.wkT[:, :, :], ps[:, :].rearrange("p (n i) -> p n i", i=64)), reads=[pb], writes=[o.wkb])
        yield
        gcjc = o.colf[:, 2, :].unsqueeze(2).broadcast_to([64, NB, 64])
        D_("dve", lambda e: e.tensor_tensor(out=o.dec[:, :, :], in0=gcj, in1=gcjc, op=ALU.subtract))
        D_("dve", lambda e: e.tensor_tensor(out=o.dec[:, :, :], in0=o.dec[:, :, :], in1=maskU, op=ALU.add))
        D_("act", lambda e: e.activation(out=o.dec[:, :, :], in_=o.dec[:, :, :], func=AF.Exp))
        ps, pb = bank()
        for ch in range(NB):
            sl = slice(ch * 64, (ch + 1) * 64)
            P.op("pe", lambda e: e.matmul(ps[0:64, sl], o.kT[:, sl], o.qT[:, sl], start=True, stop=True), reads=[o.qkb], writes=[pb])
        D_("dve", lambda e: e.tensor_tensor(out=o.attT[:, :, :], in0=o.dec[:, :, :], in1=ps[0:64, :].rearrange("p (n i) -> p n i", i=64), op=ALU.mult),
           rd=[pb], wr=[o.attb])

    def chunk_crit(h, o, b, ch):
        n = b * NB + ch
        sl = slice(ch * 64, (ch + 1) * 64)
        vn, vnb = o.vnew[n % 2], o.vnb[n % 2]
        ps1, pb1 = banks[4 * h + 0]
        ps3, pb3 = banks[4 * h + 1]
        ps2, pb2 = banks[4 * h + 2 + (n % 2)]
        P.op("pe", lambda e: e.matmul(ps1[0:64, 0:128], o.wkT[:, ch, :], o.Sb[:, :], start=True, stop=True), reads=[o.wkb, o.Sbuf], writes=[pb1])
        P.op("dve", lambda e: e.tensor_tensor(out=vn[:, :], in0=o.uval[:, ch, :], in1=ps1[0:64, 0:128], op=ALU.subtract), reads=[o.ub, pb1], writes=[vnb])
        P.op("pe", lambda e: e.matmul(ps3[:, 0:128], o.kd_tm[:, ch, :], vn[:, :], start=True, stop=True), reads=[o.tmb, vnb], writes=[pb3])
        P.op("pe", lambda e: e.matmul(ps2[0:64, 0:128], o.qdT[:, sl], o.Sb[:, :], start=True, stop=False), reads=[o.qkb, o.Sbuf], writes=[pb2])
        P.op("pe", lambda e: e.matmul(ps2[0:64, 0:128], o.attT[:, ch, :], vn[:, :], start=False, stop=True), reads=[o.attb, vnb], writes=[pb2])
        P.op("dve", lambda e: e.scalar_tensor_tensor(out=o.S[:, :], in0=o.S[:, :], scalar=o.egbc[:, ch * 64 + 63:ch * 64 + 64], in1=ps3[:, 0:128],
                                                     op0=ALU.mult, op1=ALU.add), reads=[pb3, o.gcb, o.Sbuf], writes=[o.Sbuf])
        P.op("act", lambda e: e.copy(o.Sb[:, :], o.S[:, :]), reads=[o.Sbuf], writes=[o.Sbuf])

    def chunk_out(h, o, b, ch):
        n = b * NB + ch
        ps2, pb2 = banks[4 * h + 2 + (n % 2)]
        ss = o.ss4[n % 2]
        P.op("act", lambda e: e.activation(out=o.junk[:, :], in_=ps2[0:64, 0:128], func=AF.Square, accum_out=ss[:, 0:1]), reads=[pb2], writes=[o.ssb])
        P.op("act", lambda e: e.activation(out=ss[:, 1:2], in_=ss[:, 0:1], func=AF.Ln, scale=1.0 / 128.0, bias=RMS_EPS), reads=[o.ssb], writes=[o.ssb])
        P.op("act", lambda e: e.activation(out=ss[:, 2:3], in_=ss[:, 1:2], func=AF.Exp, scale=-0.5), reads=[o.ssb], writes=[o.ssb])
        P.op("dve", lambda e: e.scalar_tensor_tensor(out=o.oout[:, ch, :], in0=ps2[0:64, 0:128], scalar=ss[:, 2:3], in1=o.sg[:, ch, :],
                                                     op0=ALU.mult, op1=ALU.mult), reads=[pb2, o.ssb, o.gtb], writes=[o.oob])

    for b in range(nbatch):
        alive = [precompute(h, o, b) for h, o in enumerate(hs)]
        while alive:
            for g_ in list(alive):
                try:
                    next(g_)
                except StopIteration:
                    alive.remove(g_)
        for ch in range(NB + 1):
            if ch < NB:
                for h, o in enumerate(hs):
                    chunk_crit(h, o, b, ch)
            if ch >= 1:
                for h, o in enumerate(hs):
                    chunk_out(h, o, b, ch - 1)
        for h, o in enumerate(hs):
            if fm_out:
                ps, pb = bank()
                for ch in range(NB):
                    P.op("pe", lambda e: e.transpose(ps[:, ch * 64:(ch + 1) * 64], o.oout[:, ch, :], ident[0:64, 0:64]),
                         reads=[o.oob, cb], writes=[pb])
                P.op("act", lambda e: e.copy(o.ofm[:, :], ps[:, :]), reads=[pb], writes=[o.ofmb])
                P.dma("sp", oT[h * 128:(h + 1) * 128, b * BT:(b + 1) * BT], o.ofm[:, :], reads=[o.ofmb])
            else:
                P.dma("sp", oT[h, :, b * NB:(b + 1) * NB, :], o.oout[:, :, :], reads=[o.oob])
    c.finish([o.oob for o in hs] + [o.ofmb for o in hs])
    return nc


def phase_inproj(c, ntok, xsrc, src_bf16, W, segs, TB=2048):
    nc, P = c.nc, c.P
    KT = D // 128
    banks = c.psum_banks()
    bi = [0]

    def bank():
        b = banks[bi[0] % 8]
        bi[0] += 1
        return b

    xb = c.sb([128, KT, TB], BF16, "xb")
    xbuf = Buf("xb")
    wt = [(c.sb([128, KT, 512], BF16, "wt"), Buf(f"wt{i}")) for i in range(2)]
    NT = TB // 512
    ot = [(c.sb([128, TB], BF16, "ot"), [Buf(f"ot{i}_{t}") for t in range(NT)]) for i in range(2)]
    tt_ = [(c.sb([128, 512], BF16, "tt"), Buf(f"tt{i}")) for i in range(2)]
    wi = oi = ti = 0
    outbufs = []
    for blk in range(ntok // TB):
        woff = 0
        for k in range(KT):
            if src_bf16:
                P.dma("sp", xb[:, k, :], xsrc(blk, k), writes=[xbuf])
            else:
                P.dma("pool", xb[:, k, :], xsrc(blk, k), writes=[xbuf], max_dma_last_dim=8192)
        for kind, col0, ncols, out in segs:
            for f0 in range(col0, col0 + ncols, 512):
                fw = min(512, col0 + ncols - f0)
                wtile, wbuf = wt[wi % 2]
                wi += 1
                P.dma("pool", wtile[:, :, :fw], W[woff:woff + 128 * KT * fw].rearrange("(p k f) -> p k f", p=128, k=KT), writes=[wbuf],
                      max_dma_last_dim=8192)
                woff += 128 * KT * fw
                if kind == "fm":
                    for fc in range((fw + 127) // 128):
                        m = min(128, fw - fc * 128)
                        otile, obufs = ot[oi % 2]
                        oi += 1
                        for t in range(NT):
                            ps, pbuf = bank()
                            for k in range(KT):
                                P.op("pe", lambda e: e.matmul(ps[:m, :], wtile[:, k, fc * 128:fc * 128 + m], xb[:, k, t * 512:(t + 1) * 512],
                                                              start=(k == 0), stop=(k == KT - 1)), reads=[wbuf, xbuf], writes=[pbuf])
                            if t % 2 == 0:
                                P.op("act", lambda e: e.copy(otile[:m, t * 512:(t + 1) * 512], ps[:m, :]), reads=[pbuf], writes=[obufs[t]])
                            else:
                                P.op("dve", lambda e: e.tensor_copy(otile[:m, t * 512:(t + 1) * 512], ps[:m, :]), reads=[pbuf], writes=[obufs[t]])
                        r0 = f0 - col0 + fc * 128
                        P.dma("sp", out[r0:r0 + m, blk * TB:(blk + 1) * TB], otile[:m, :], reads=obufs)
                        outbufs.extend(obufs)
                else:
                    for t in range(TB // 128):
                        ps, pbuf = bank()
                        for k in range(KT):
                            P.op("pe", lambda e: e.matmul(ps[:, :fw], xb[:, k, t * 128:(t + 1) * 128], wtile[:, k, :fw],
                                                          start=(k == 0), stop=(k == KT - 1)), reads=[wbuf, xbuf], writes=[pbuf])
                        ttile, tbuf = tt_[ti % 2]
                        ti += 1
                        if t % 2 == 0:
                            P.op("act", lambda e: e.copy(ttile[:, :fw], ps[:, :fw]), reads=[pbuf], writes=[tbuf])
                        else:
                            P.op("dve", lambda e: e.tensor_copy(ttile[:, :fw], ps[:, :fw]), reads=[pbuf], writes=[tbuf])
                        c0 = f0 - col0
                        P.dma("sp", out[blk * TB + t * 128: blk * TB + (t + 1) * 128, c0:c0 + fw], ttile[:, :fw], reads=[tbuf])
                        outbufs.append(tbuf)
    c.finish(outbufs)


GROUPS = [[0, 1, 2, 3], [4, 5, 6, 7]]
L1COLS = 1028


def build_fused(stop_after=7):
    c = Ctx()
    nc, P = c.nc, c.P
    i_ = lambda n, sh, dt=F32: nc.dram_tensor(n, list(sh), dt, kind="ExternalInput").ap()

    def done(k):
        if stop_after == k:
            if k >= 3:
                dbg = Buf("dbg")
                P.dma("sp", out, x2tok, writes=[dbg])
                P.full_barrier()
            c.root.close()
            return True
        return False

    xTb = i_("xTb", [D, S])
    xtok = i_("xtok", [TPC, D])
    Win0 = i_("Win0", [D * 1536])
    sbm, dwm, tri = i_("sbm", [128, QG // 128, QG], BF16), i_("dwm", [128, 16 + QG // 128, QG], BF16), i_("tri", [128, 128], BF16)
    t0 = s5 = gd = t1 = Win1 = None
    if stop_after >= 3:
      t0 = {k: i_("t0_" + k, sh) for k, sh in (("Wout", [8, 128, 16, 256]), ("ln1g", [D]), ("ln1b", [D]), ("ln2g", [D]), ("ln2b", [D]),
                                             ("W1", [1, 22, 128, 16, 256]), ("W3", [1, 22, 128, 16, 256]), ("W2", [1, 5632, D]))}
    if stop_after >= 4:
      Win1 = i_("Win1", [D * (L1COLS + 256)])
      s5 = {k: i_("s5_" + k, sh) for k, sh in (("lam", [8, 128, 3]), ("Bm", [8, 128, 32]), ("Cm", [8, 128, 32]), ("Dk", [8, 32, 1]), ("iota", [128, 64]))}
    if stop_after >= 4:
      gd = {k: i_("gd_" + k, sh) for k, sh in (("convw", [2, 128, 12]), ("hsc", [2, 2]), ("normw", [128]), ("gmask", [64, 3, 64]), ("mrow", [1, 512]))}
    if stop_after >= 7:
      t1 = {k: i_("t1_" + k, sh) for k, sh in (("gluw", [4, 128, 8, 256]), ("glub", [128, 8]), ("Wout", [8, 128, 16, 256]), ("ln1g", [D]), ("ln1b", [D]),
                                             ("ln2g", [D]), ("ln2b", [D]), ("W1", [8, 28, 128, 16, 256]), ("W3", [8, 28, 128, 16, 256]), ("W2", [8, 7168, D]),
                                             ("Wr", [128, 128]))}
    out = nc.dram_tensor("out", [TPC, D], F32, kind="ExternalOutput").ap()
    hfm = c.scratch("hfm", [1024, S], BF16)
    vtok = c.scratch("vtok", [S, 512], BF16)
    ag1_in, ag1_out = c.scratch("ag1_in", [512, S], BF16), c.scratch("ag1_out", [2048, S], BF16)
    x2tok = c.scratch("x2tok", [TPC, D], F32)
    ag2_in, ag2_out = c.scratch("ag2_in", [D, TPC], BF16), c.scratch("ag2_out", [4 * D, TPC], BF16)
    h1fm = c.scratch("h1fm", [L1COLS, S], BF16)
    gate_tm = c.scratch("gate_tm", [S, 256], BF16)
    ag3y_in, ag3y_out = c.scratch("ag3y_in", [256, S], F32), c.scratch("ag3y_out", [1024, S], F32)
    ag3o_in, ag3o_out = c.scratch("ag3o_in", [256, S], BF16), c.scratch("ag3o_out", [1024, S], BF16)
    rank_off = (nc.sync.partition_id() % 4) * TPC
    dummy = Buf("cc")

    def gather(a_in, a_out, r0):
        for i in range(a_in.shape[0] // r0):
            P.collective("AllGather", GROUPS, a_in[i * r0:(i + 1) * r0, :], a_out[i * 4 * r0:(i + 1) * 4 * r0, :], reads=[dummy], writes=[dummy])

    def exchange(a_in, a_out, r0):
        gather(a_in, a_out, r0)
        P.full_barrier()

    c.begin_phase({})
    phase_inproj(c, S, lambda blk, k: xTb[k * 128:(k + 1) * 128, blk * 2048:(blk + 1) * 2048], False, Win0,
                 [("fm", 0, 1024, hfm), ("tm", 1024, 512, vtok)])
    hv = hfm.rearrange("(u two p) t -> two u p t", two=2, p=128)
    c.begin_phase({"q": hv[0], "k": hv[1], "v": vtok.rearrange("(n p) (u d) -> u p n d", p=128, u=4),
                   "sbm": sbm, "dwm": dwm, "tri": tri, "oT": ag1_in.rearrange("(u p) t -> u p t", p=128)})
    def gather_unit(u, odram):
        for i in (2 * u, 2 * u + 1):
            P.collective("AllGather", GROUPS, ag1_in[i * 64:(i + 1) * 64, :], ag1_out[i * 256:(i + 1) * 256, :], reads=[odram], writes=[dummy])

    build_k2(env=c, on_unit_done=gather_unit)
    if done(1):
        return nc
    P.full_barrier()
    if done(2):
        return nc
    ov = dict(t0)
    ov.update({"oT": ag1_out, "x": xtok, "xo": x2tok, "xTo": ag2_in})
    c.begin_phase(ov)
    build_tail(False, env=c, tok_off=rank_off, src_ntok=S, xT_out=True)
    if done(3):
        return nc
    exchange(ag2_in, ag2_out, 256)
    c.begin_phase({})
    ag2v = ag2_out.rearrange("(i s j) t -> s i j t", i=8, s=4)
    phase_inproj(c, S, lambda blk, k: ag2v[blk, k // 2, (k % 2) * 128:(k % 2) * 128 + 128, :], True, Win1,
                 [("fm", 0, L1COLS, h1fm), ("tm", L1COLS, 256, gate_tm)])
    ov = dict(s5)
    ov.update({"uT": h1fm[0:256, :].rearrange("(r c) t -> r c t", c=32), "yT": ag3y_in.rearrange("(r c) t -> r c t", c=32)})
    c.begin_phase(ov)
    build_s5(env=c)
    if done(5):
        return nc
    gather(ag3y_in, ag3y_out, 32)
    ov = dict(gd)
    ov.update({"qkv": h1fm[256:1024, :].rearrange("(h i p) t -> h i p t", h=2, i=3),
               "gate": gate_tm.rearrange("(n p) (h d) -> h p n d", p=64, h=2),
               "rows": h1fm[1024:1028, :].rearrange("(h c) t -> h c t", c=2),
               "cols": h1fm[1024:1028, :].rearrange("(h c) (n p) -> h p c n", c=2, p=64),
               "oT": ag3o_in})
    c.begin_phase(ov)
    build_gdn(env=c, fm_out=True)
    if done(6):
        return nc
    exchange(ag3o_in, ag3o_out, 64)
    ov = dict(t1)
    ov.update({"yT": ag3y_out, "odT": ag3o_out, "x": x2tok, "xo": out})
    c.begin_phase(ov)
    build_tail(True, glu=True, env=c, tok_off=rank_off, src_ntok=S)
    c.root.close()
    return nc


_PROGS = {}


def _c(a):
    return np.ascontiguousarray(a)


def _tile_w(W, fb=256):
    K_, F_ = W.shape
    return _c(W.reshape(K_ // 128, 128, F_ // fb, fb).transpose(2, 1, 0, 3))


def _tile_flat(W, segs):
    parts = []
    for col0, ncols in segs:
        for f0 in range(col0, col0 + ncols, 512):
            fw = min(512, col0 + ncols - f0)
            parts.append(W[:, f0:f0 + fw].reshape(16, 128, fw).transpose(1, 0, 2).reshape(-1))
    return _c(np.concatenate(parts))


def kernel(**inp):
    f32 = np.float32
    g = lambda k: np.asarray(inp[k], dtype=f32)
    x0 = g("x").reshape(B, S, D)
    if "fused" not in _PROGS:
        _PROGS["fused"] = build_fused()
    nc = _PROGS["fused"]
    kc, sc, gc = k2_consts(), s5_consts(), gdn_consts()
    w_in0, w_in1 = g("even_w_in")[0], g("odd_w_in")[0]
    wout0, wout1 = g("even_w_out")[0], g("odd_w_out")[0]
    rank_rows = [np.concatenate([np.arange(kind * 1024 + h * 128, kind * 1024 + (h + 1) * 128)
                                 for kind, h in ((0, 2 * s_), (0, 2 * s_ + 1), (1, 2 * s_), (1, 2 * s_ + 1))]) for s_ in range(4)]
    perm0 = np.concatenate([rank_rows[s_][i * 64:(i + 1) * 64] for i in range(8) for s_ in range(4)])
    permy = np.concatenate([np.arange(s_ * 256 + i * 32, s_ * 256 + (i + 1) * 32) for i in range(8) for s_ in range(4)])
    permo = np.concatenate([np.arange(s_ * 256 + i * 64, s_ * 256 + (i + 1) * 64) for i in range(4) for s_ in range(4)])
    perm1 = np.concatenate([permy, 1024 + permo])
    shared = {"sbm": kc["sbm"], "dwm": kc["dwm"], "tri": kc["tri"],
              "t0_Wout": _tile_w(wout0[perm0]), "t0_ln1g": g("even_ln_mix_g")[0], "t0_ln1b": g("even_ln_mix_b")[0],
              "t0_ln2g": g("even_ln_ffn_g")[0], "t0_ln2b": g("even_ln_ffn_b")[0],
              "t0_W1": _tile_w(g("even_ffn_w1")[0])[None], "t0_W3": _tile_w(g("even_ffn_w3")[0])[None], "t0_W2": g("even_ffn_w2"),
              "s5_iota": sc["iota"], "gd_normw": g("odd_gdn_norm_w")[0], "gd_gmask": gc["gmask"], "gd_mrow": gc["mrow"],
              "t1_gluw": _tile_w(g("odd_glu_w")[0][permy][:, permy]), "t1_glub": _c(g("odd_glu_b")[0][permy].reshape(8, 128).T),
              "t1_Wout": _tile_w(wout1[perm1]), "t1_ln1g": g("odd_ln_mix_g")[0], "t1_ln1b": g("odd_ln_mix_b")[0],
              "t1_ln2g": g("odd_ln_ffn_g")[0], "t1_ln2b": g("odd_ln_ffn_b")[0],
              "t1_W1": np.stack([_tile_w(w) for w in g("odd_moe_w1")[0]]), "t1_W3": np.stack([_tile_w(w) for w in g("odd_moe_w3")[0]]),
              "t1_W2": g("odd_moe_w2")[0],
              "t1_Wr": _c(g("odd_router_w")[0].reshape(16, 128, 8).transpose(1, 0, 2).reshape(128, 128))}
    lre, lim, ldt = g("odd_ssm_lam_re")[0], g("odd_ssm_lam_im")[0], g("odd_ssm_log_dt")[0]
    bre, bim, cre, cim, dsk = g("odd_ssm_b_re")[0], g("odd_ssm_b_im")[0], g("odd_ssm_c_re")[0], g("odd_ssm_c_im")[0], g("odd_ssm_d")[0]
    cw = g("odd_gdn_conv_w")[0].reshape(4, 3, 8, 128)
    alog, dtb = g("odd_gdn_a_log")[0], g("odd_gdn_dt_bias")[0]
    xT = [_c(x0[b].T) for b in range(B)]
    in_maps = []
    for c in range(NCORES):
        b, r = c // 4, c % 4
        m = dict(shared)
        m["xTb"] = xT[b]
        m["xtok"] = _c(x0[b, r * TPC:(r + 1) * TPC])
        units = ((0, 2 * r), (0, 2 * r + 1), (1, 2 * r), (1, 2 * r + 1))
        cols = []
        for kind, h in units:
            cols += [np.arange(kind * 3072 + h * 128, kind * 3072 + (h + 1) * 128),
                     np.arange(kind * 3072 + 1024 + h * 128, kind * 3072 + 1024 + (h + 1) * 128)]
        for kind, h in units:
            cols.append(np.arange(kind * 3072 + 2048 + h * 128, kind * 3072 + 2048 + (h + 1) * 128))
        m["Win0"] = _tile_flat(w_in0[:, np.concatenate(cols)], [(0, 1024), (1024, 512)])
        hh = (2 * r, 2 * r + 1)
        cols = [np.arange(r * 256, (r + 1) * 256)]
        for h in hh:
            for i in range(3):
                cols.append(np.arange(1024 + i * 1024 + h * 128, 1024 + i * 1024 + (h + 1) * 128))
        for h in hh:
            cols.append(np.array([5128 + h, 5120 + h]))
        for h in hh:
            cols.append(np.arange(4096 + h * 128, 4096 + (h + 1) * 128))
        m["Win1"] = _tile_flat(w_in1[:, np.concatenate(cols)], [(0, L1COLS), (L1COLS, 256)])
        grp = np.arange(16 * r, 16 * r + 16)
        m["s5_lam"] = _c(np.stack([lre[grp], lim[grp], np.broadcast_to(ldt[grp][:, None], (16, 64))], -1).reshape(8, 128, 3))
        m["s5_Bm"] = _c(np.concatenate([bre[grp], bim[grp]], -1).reshape(8, 128, 32))
        m["s5_Cm"] = _c(np.concatenate([cre[grp].transpose(0, 2, 1), cim[grp].transpose(0, 2, 1)], -1).reshape(8, 128, 32))
        m["s5_Dk"] = _c(dsk[r * 256:(r + 1) * 256].reshape(8, 32, 1))
        m["gd_convw"] = _c(np.stack([cw[:, :, h, :].transpose(2, 1, 0).reshape(128, 12) for h in hh]))
        m["gd_hsc"] = _c(np.stack([alog[list(hh)], dtb[list(hh)]], 1))
        in_maps.append(m)
    res = run_bass_kernel_spmd(nc, in_maps, core_ids=list(range(NCORES)))
    outs = [np.asarray(res.results[c]["out"]) for c in range(NCORES)]
    return _c(np.concatenate(outs, axis=0).reshape(B, S, D).astype(f32, copy=False))
```
